# Optimizing a Trainium2 kernel written in Bass

```python
import jax
import jax.numpy as jnp
from jax import lax
import numpy as np

D_MODEL = 1024
BATCH = 2
SEQ = 8192
DEPTH = 1
DEC_BATCH = 32
DEC_SEQ = 4
PAST_LEN = 8192
PAGE_SIZE = 128

R_HEADS = 8
R_HEAD = 64
R_WIDTH = R_HEADS * R_HEAD
LORA_W = 64
LORA_A = 64
LORA_G = 128
R_COLS = 3 * R_WIDTH + LORA_W + LORA_A + LORA_G
GN_EPS = 64e-5

N_HEADS = 8
N_KV = 2
HPG = N_HEADS // N_KV
HEAD_DIM = 64
A_WIDTH = N_HEADS * HEAD_DIM
KV_WIDTH = N_KV * HEAD_DIM
A_COLS = A_WIDTH + 6 * KV_WIDTH + 3 * N_HEADS
ROT_DIM = HEAD_DIM // 4
ROPE_THETA = 500000.0
CMP_STRIDE = 16
CMP_LEN = 2 * CMP_STRIDE
CMP_HIDDEN = 256
SEL_BLOCK = 64
SEL_TOP = 16
WINDOW = 512
Q_BLOCK = 128

IN_COLS = R_COLS + A_COLS + 2 * D_MODEL

N_EXPERTS = 32
TOP_K = 4
D_FF = 1024
SWIGLU_LIMIT = 7.0
SWIGLU_ALPHA = 1.702
EXPERT_BLOCK = 128

DN_ALPHA = (2 * DEPTH) ** 0.25
DN_BETA = (8 * DEPTH) ** -0.25
LN_EPS = 1e-5
NEG = -1e30

kernel_name = 'rwkv7_nsa_gated_moe_decoder_step'


def layer_norm(x, g, b):
    xf = x.astype(jnp.float32)
    mu = xf.mean(-1, keepdims=True)
    var = jnp.square(xf - mu).mean(-1, keepdims=True)
    return ((xf - mu) * lax.rsqrt(var + LN_EPS) * g + b).astype(x.dtype)


def rope(x, pos):
    half = ROT_DIM // 2
    inv = ROPE_THETA ** (-jnp.arange(half, dtype=jnp.float32) * 2.0 / ROT_DIM)
    ang = pos.astype(jnp.float32)[:, None] * inv
    cos, sin = jnp.cos(ang)[:, None, :], jnp.sin(ang)[:, None, :]
    xf = x.astype(jnp.float32)
    x1, x2 = xf[..., :half], xf[..., half:ROT_DIM]
    out = jnp.concatenate([x1 * cos - x2 * sin, x2 * cos + x1 * sin, xf[..., ROT_DIM:]], axis=-1)
    return out.astype(x.dtype)


def project(x, w_in, pos):
    B, T, _ = x.shape
    p = x @ w_in
    pr = p[..., :R_COLS]
    pa = p[..., R_COLS:R_COLS + A_COLS]
    mg = p[..., R_COLS + A_COLS:]
    q = rope(pa[..., :A_WIDTH].reshape(B, T, N_HEADS, HEAD_DIM), pos)
    kvs = [pa[..., A_WIDTH + i * KV_WIDTH:A_WIDTH + (i + 1) * KV_WIDTH].reshape(B, T, N_KV, HEAD_DIM)
           for i in range(6)]
    kvs = [rope(z, pos) if i % 2 == 0 else z for i, z in enumerate(kvs)]
    gates = jax.nn.sigmoid(pa[..., A_WIDTH + 6 * KV_WIDTH:]).reshape(B, T, N_KV, HPG, 3)
    return pr, q, kvs, gates, mg


def wkv_scan(s0, r, w, k, v, a, b):
    def step(s, inp):
        r_t, w_t, k_t, v_t, a_t, b_t = inp
        sa = jnp.einsum('bhij,bhj->bhi', s, a_t)
        s = s * w_t[:, :, None, :] + sa[..., None] * b_t[:, :, None, :] + v_t[..., None] * k_t[:, :, None, :]
        return s, jnp.einsum('bhij,bhj->bhi', s, r_t)
    xs = tuple(jnp.swapaxes(z, 0, 1) for z in (r, w, k, v, a, b))
    s, ys = lax.scan(step, s0, xs)
    return jnp.swapaxes(ys, 0, 1), s


def rwkv_mixer(pr, shift_prev, wkv0, mu, w0, w_w2, a0, w_a2, g_w2, k_k, k_a, r_k, gn_g, gn_b):
    B, T, _ = pr.shape
    prev = jnp.concatenate([shift_prev[:, None, :].astype(pr.dtype), pr[:, :-1]], axis=1)
    xm = (pr + (prev - pr) * mu).astype(jnp.float32)
    o1, o2, o3 = R_WIDTH, 2 * R_WIDTH, 3 * R_WIDTH
    o4 = o3 + LORA_W
    o5 = o4 + LORA_A
    r, k, v = xm[..., :o1], xm[..., o1:o2], xm[..., o2:o3]
    xw, xa, xg = xm[..., o3:o4], xm[..., o4:o5], xm[..., o5:]
    w_log = -jax.nn.softplus(-(w0 + jnp.tanh(xw) @ w_w2)) - 0.5
    a = jax.nn.sigmoid(a0 + xa @ w_a2)
    g = jax.nn.sigmoid(xg) @ g_w2
    heads = lambda z: z.reshape(B, T, R_HEADS, R_HEAD)
    kk = heads(k * k_k)
    kk = kk / jnp.maximum(jnp.linalg.norm(kk, axis=-1, keepdims=True), 1e-12)
    k_h = heads(k * (1.0 + (a - 1.0) * k_a))
    r_h, v_h, a_h = heads(r), heads(v), heads(a)
    decay = jnp.exp(-jnp.exp(heads(w_log)))
    y, wkv = wkv_scan(wkv0.astype(jnp.float32), r_h, decay, k_h, v_h, -kk, kk * a_h)
    mean = y.mean(-1, keepdims=True)
    var = jnp.square(y - mean).mean(-1, keepdims=True)
    yn = ((y - mean) * lax.rsqrt(var + GN_EPS)).reshape(B, T, R_WIDTH) * gn_g + gn_b
    bonus = (jnp.sum(r_h * k_h * r_k, axis=-1, keepdims=True) * v_h).reshape(B, T, R_WIDTH)
    out = ((yn + bonus) * g).astype(pr.dtype)
    return out, wkv.astype(pr.dtype), pr[:, -1]


def compress(kv, pe, w1, b1, w2, b2):
    B, L = kv.shape[:2]
    n_chunk = L // CMP_STRIDE
    ch = kv[:, :n_chunk * CMP_STRIDE].reshape(B, n_chunk, CMP_STRIDE, N_KV, HEAD_DIM)
    blk = jnp.concatenate([ch[:, :-1], ch[:, 1:]], axis=2) + pe[:, None, :]
    blk = jnp.transpose(blk, (0, 3, 1, 2, 4)).reshape(B, N_KV, n_chunk - 1, CMP_LEN * HEAD_DIM)
    return jax.nn.gelu(blk @ w1 + b1) @ w2 + b2


def nsa_attend(q, q_pos, kc, vc, kb, vb, kw, vw, w_pos, gates):
    f32 = jnp.float32
    B, Tq = q.shape[:2]
    NC, NS = kc.shape[2], kb.shape[2]
    qf = q.astype(f32) * (HEAD_DIM ** -0.5)
    c_start = jnp.arange(NC) * CMP_STRIDE
    c_ok = (c_start + CMP_LEN - 1)[None, :] <= q_pos[:, None]
    s_c = jnp.einsum('bqghd,bgnd->bqghn', qf, kc.astype(f32))
    s_c = jnp.where(c_ok[None, :, None, None], s_c, NEG)
    p_c = jax.nn.softmax(s_c, axis=-1) * c_ok.any(-1)[None, :, None, None, None]
    o_c = jnp.einsum('bqghn,bgnd->bqghd', p_c, vc.astype(f32))
    s_start = jnp.arange(NS) * SEL_BLOCK
    cover = ((c_start[:, None] < s_start[None, :] + SEL_BLOCK)
             & (c_start[:, None] + CMP_LEN > s_start[None, :])).astype(f32)
    imp = jnp.einsum('bqghn,ns->bqgs', p_c, cover)
    blk = jnp.arange(NS)
    cur = (q_pos // SEL_BLOCK)[:, None]
    forced = (blk[None] == 0) | (blk[None] == cur) | (blk[None] == cur - 1)
    causal = s_start[None] <= q_pos[:, None]
    score = jnp.where(forced[None, :, None], 1e6, imp)
    score = jnp.where(causal[None, :, None], score, NEG)
    top_s, sel = lax.top_k(score, min(SEL_TOP, NS))
    valid = top_s > 0.5 * NEG
    bi = jnp.arange(B)[:, None, None, None]
    gi = jnp.arange(N_KV)[None, None, :, None]
    kg = kb[bi, gi, sel].astype(f32)
    vg = vb[bi, gi, sel].astype(f32)
    s_s = jnp.einsum('bqghd,bqgnsd->bqghns', qf, kg)
    k_pos = sel[..., None] * SEL_BLOCK + jnp.arange(SEL_BLOCK)
    s_ok = (k_pos <= q_pos[None, :, None, None, None]) & valid[..., None]
    s_s = jnp.where(s_ok[:, :, :, None], s_s, NEG)
    p_s = jax.nn.softmax(s_s.reshape(B, Tq, N_KV, HPG, -1), axis=-1).reshape(s_s.shape)
    o_s = jnp.einsum('bqghns,bqgnsd->bqghd', p_s, vg)
    w_ok = ((w_pos[None] <= q_pos[:, None]) & (w_pos[None] >= q_pos[:, None] - WINDOW)
            & (w_pos[None] >= 0))
    s_w = jnp.einsum('bqghd,bkgd->bqghk', qf, kw.astype(f32))
    s_w = jnp.where(w_ok[None, :, None, None], s_w, NEG)
    o_w = jnp.einsum('bqghk,bkgd->bqghd', jax.nn.softmax(s_w, axis=-1), vw.astype(f32))
    g = gates.astype(f32)
    return g[..., 0:1] * o_c + g[..., 1:2] * o_s + g[..., 2:3] * o_w


def nsa_prompt(q, kvs, gates, pe, w1, b1, w2, b2):
    kc_raw, vc_raw, ks, vs, kw, vw = kvs
    B, T = q.shape[:2]
    kc = compress(kc_raw, pe[0], w1[0], b1[0], w2[0], b2[0])
    vc = compress(vc_raw, pe[1], w1[1], b1[1], w2[1], b2[1])
    ns = T // SEL_BLOCK
    to_blocks = lambda z: jnp.transpose(z.reshape(B, ns, SEL_BLOCK, N_KV, HEAD_DIM), (0, 3, 1, 2, 4))
    kb, vb = to_blocks(ks), to_blocks(vs)
    pad = jnp.zeros((B, WINDOW, N_KV, HEAD_DIM), kw.dtype)
    kw_p = jnp.concatenate([pad, kw], axis=1)
    vw_p = jnp.concatenate([pad, vw], axis=1)
    qg = q.reshape(B, T, N_KV, HPG, HEAD_DIM)
    span = WINDOW + Q_BLOCK

    def query_block(i):
        s = i * Q_BLOCK
        sl = lambda z, n: lax.dynamic_slice_in_dim(z, s, n, axis=1)
        return nsa_attend(sl(qg, Q_BLOCK), s + jnp.arange(Q_BLOCK), kc, vc, kb, vb,
                          sl(kw_p, span), sl(vw_p, span), s - WINDOW + jnp.arange(span),
                          sl(gates, Q_BLOCK))

    o = lax.map(query_block, jnp.arange(T // Q_BLOCK))
    return jnp.moveaxis(o, 0, 1).reshape(B, T, A_WIDTH).astype(q.dtype)


def nsa_sample(q, kvs, gates, cmp_cache, sel_cache, win_cache, page_table, pe, w1, b1, w2, b2):
    kc_new, vc_new, ks_new, vs_new, kw_new, vw_new = kvs
    DB, DS = q.shape[:2]
    past = page_table.shape[1] * PAGE_SIZE
    total = past + DS

    def full_rows(cache, k_new, v_new):
        rows = cache[page_table].reshape(DB, past, 2, N_KV, HEAD_DIM)
        return (jnp.concatenate([rows[:, :, 0], k_new], axis=1),
                jnp.concatenate([rows[:, :, 1], v_new], axis=1))

    kc_all, vc_all = full_rows(cmp_cache, kc_new, vc_new)
    ks_all, vs_all = full_rows(sel_cache, ks_new, vs_new)
    kc = compress(kc_all, pe[0], w1[0], b1[0], w2[0], b2[0])
    vc = compress(vc_all, pe[1], w1[1], b1[1], w2[1], b2[1])
    ns = -(-total // SEL_BLOCK)
    padn = ns * SEL_BLOCK - total
    to_blocks = lambda z: jnp.transpose(
        jnp.pad(z, ((0, 0), (0, padn), (0, 0), (0, 0))).reshape(DB, ns, SEL_BLOCK, N_KV, HEAD_DIM),
        (0, 3, 1, 2, 4))
    wb = win_cache.shape[1]
    kw_all = jnp.concatenate([win_cache[:, :, 0], kw_new], axis=1)
    vw_all = jnp.concatenate([win_cache[:, :, 1], vw_new], axis=1)
    o = nsa_attend(q.reshape(DB, DS, N_KV, HPG, HEAD_DIM), past + jnp.arange(DS), kc, vc,
                   to_blocks(ks_all), to_blocks(vs_all), kw_all, vw_all,
                   past - wb + jnp.arange(wb + DS), gates)
    new_win = jnp.stack([kw_all[:, DS:], vw_all[:, DS:]], axis=2)
    return o.reshape(DB, DS, A_WIDTH).astype(q.dtype), new_win


def moe(x, router_w, router_b, mlp1_w, mlp1_b, mlp2_w, mlp2_b):
    T, D = x.shape
    logits = (x @ router_w + router_b).astype(jnp.float32)
    top_v, top_e = lax.top_k(logits, TOP_K)
    gate = jax.nn.softmax(top_v, axis=-1)
    n_assign = T * TOP_K
    e_flat = top_e.reshape(-1)
    order = jnp.argsort(e_flat)
    e_sorted = e_flat[order]
    tok_sorted = (order // TOP_K).astype(jnp.int32)
    gate_sorted = gate.reshape(-1)[order]
    counts = jnp.bincount(e_flat, length=N_EXPERTS)
    padded = (counts + EXPERT_BLOCK - 1) // EXPERT_BLOCK * EXPERT_BLOCK
    start = jnp.cumsum(counts) - counts
    pend = jnp.cumsum(padded)
    pstart = pend - padded
    dest = pstart[e_sorted] + jnp.arange(n_assign) - start[e_sorted]
    n_blocks = -(-n_assign // EXPERT_BLOCK) + N_EXPERTS
    n_rows = n_blocks * EXPERT_BLOCK
    row_tok = jnp.full((n_rows,), T, jnp.int32).at[dest].set(tok_sorted)
    row_gate = jnp.zeros((n_rows,), jnp.float32).at[dest].set(gate_sorted)
    block_e = jnp.minimum(jnp.searchsorted(pend, jnp.arange(n_blocks) * EXPERT_BLOCK, side='right'),
                          N_EXPERTS - 1)
    xb = jnp.concatenate([x, jnp.zeros((1, D), x.dtype)])[row_tok].reshape(n_blocks, EXPERT_BLOCK, D)

    def expert_block(args):
        xe, e = args
        h = xe @ mlp1_w[e] + mlp1_b[e]
        glu = jnp.minimum(h[:, :D_FF], SWIGLU_LIMIT)
        lin = jnp.clip(h[:, D_FF:], -SWIGLU_LIMIT, SWIGLU_LIMIT)
        return (glu * jax.nn.sigmoid(SWIGLU_ALPHA * glu) * (lin + 1.0)) @ mlp2_w[e] + mlp2_b[e]

    yb = lax.map(expert_block, (xb, block_e)).reshape(n_rows, D)
    y = jnp.zeros((T + 1, D), jnp.float32).at[row_tok].add(yb.astype(jnp.float32) * row_gate[:, None])
    return y[:T].astype(x.dtype)


def merge_and_ffn(x, y_r, y_a, mg, w_pa, w_pb, w_o, ln1_g, ln1_b, router_w, router_b,
                  mlp1_w, mlp1_b, mlp2_w, mlp2_b, ln2_g, ln2_b):
    m = (jax.nn.sigmoid(mg[..., :D_MODEL]) * (y_r @ w_pa)
         + jax.nn.sigmoid(mg[..., D_MODEL:]) * (y_a @ w_pb))
    h = layer_norm(DN_ALPHA * x + m @ w_o, ln1_g, ln1_b)
    B, T, D = h.shape
    f = moe(h.reshape(B * T, D), router_w, router_b, mlp1_w, mlp1_b, mlp2_w, mlp2_b).reshape(B, T, D)
    return layer_norm(DN_ALPHA * h + f, ln2_g, ln2_b)


def setup_inputs(seed: int = 0) -> dict:
    key = jax.random.key(seed)
    keys = iter(jax.random.split(key, 48))

    def nrm(shape, scale):
        return jax.random.normal(next(keys), shape, jnp.float32) * scale

    L = DEPTH
    n_pages = PAST_LEN // PAGE_SIZE
    used = DEC_BATCH * n_pages
    n_pool = used + max(1, used // 4)
    win_buf = min(WINDOW, PAST_LEN)
    page_table = jax.random.permutation(next(keys), n_pool)[:used].reshape(DEC_BATCH, n_pages).astype(jnp.int32)
    kv_shape = (L, n_pool, PAGE_SIZE, 2, N_KV, HEAD_DIM)
    return {
        'x_prompt': nrm((BATCH, SEQ, D_MODEL), 1.0),
        'x_sample': nrm((DEC_BATCH, DEC_SEQ, D_MODEL), 1.0),
        'cache_cmp_kv': nrm(kv_shape, 1.0),
        'cache_sel_kv': nrm(kv_shape, 1.0),
        'cache_win_kv': nrm((L, DEC_BATCH, win_buf, 2, N_KV, HEAD_DIM), 1.0),
        'state_wkv': nrm((L, DEC_BATCH, R_HEADS, R_HEAD, R_HEAD), 0.5),
        'state_shift': nrm((L, DEC_BATCH, R_COLS), 1.0),
        'page_table': page_table,
        'w_in': nrm((L, D_MODEL, IN_COLS), D_MODEL ** -0.5),
        'mu_shift': jax.random.uniform(next(keys), (L, R_COLS), jnp.float32),
        'w0': nrm((L, R_WIDTH), 0.5) - 1.0,
        'w_w2': nrm((L, LORA_W, R_WIDTH), LORA_W ** -0.5),
        'a0': nrm((L, R_WIDTH), 0.5),
        'w_a2': nrm((L, LORA_A, R_WIDTH), LORA_A ** -0.5),
        'g_w2': nrm((L, LORA_G, R_WIDTH), LORA_G ** -0.5),
        'k_k': 0.85 + nrm((L, R_WIDTH), 0.05),
        'k_a': 1.0 + nrm((L, R_WIDTH), 0.05),
        'r_k': nrm((L, R_HEADS, R_HEAD), 0.1),
        'gn_g': 1.0 + nrm((L, R_WIDTH), 0.05),
        'gn_b': nrm((L, R_WIDTH), 0.01),
        'cmp_pe': nrm((L, 2, CMP_LEN, HEAD_DIM), 0.1),
        'cmp_w1': nrm((L, 2, CMP_LEN * HEAD_DIM, CMP_HIDDEN), (CMP_LEN * HEAD_DIM) ** -0.5),
        'cmp_b1': nrm((L, 2, CMP_HIDDEN), 0.01),
        'cmp_w2': nrm((L, 2, CMP_HIDDEN, HEAD_DIM), CMP_HIDDEN ** -0.5),
        'cmp_b2': nrm((L, 2, HEAD_DIM), 0.01),
        'w_pa': nrm((L, R_WIDTH, D_MODEL), R_WIDTH ** -0.5),
        'w_pb': nrm((L, A_WIDTH, D_MODEL), A_WIDTH ** -0.5),
        'w_o': nrm((L, D_MODEL, D_MODEL), DN_BETA * D_MODEL ** -0.5),
        'ln1_g': 1.0 + nrm((L, D_MODEL), 0.05),
        'ln1_b': nrm((L, D_MODEL), 0.01),
        'router_w': nrm((L, D_MODEL, N_EXPERTS), D_MODEL ** -0.5),
        'router_b': nrm((L, N_EXPERTS), 0.01),
        'mlp1_w': nrm((L, N_EXPERTS, D_MODEL, 2 * D_FF), D_MODEL ** -0.5),
        'mlp1_b': nrm((L, N_EXPERTS, 2 * D_FF), 0.01),
        'mlp2_w': nrm((L, N_EXPERTS, D_FF, D_MODEL), DN_BETA * D_FF ** -0.5),
        'mlp2_b': nrm((L, N_EXPERTS, D_MODEL), 0.01),
        'ln2_g': 1.0 + nrm((L, D_MODEL), 0.05),
        'ln2_b': nrm((L, D_MODEL), 0.01),
    }


def reference(x_prompt, x_sample, cache_cmp_kv, cache_sel_kv, cache_win_kv, state_wkv, state_shift,
              page_table, w_in, mu_shift, w0, w_w2, a0, w_a2, g_w2, k_k, k_a, r_k, gn_g, gn_b,
              cmp_pe, cmp_w1, cmp_b1, cmp_w2, cmp_b2, w_pa, w_pb, w_o, ln1_g, ln1_b,
              router_w, router_b, mlp1_w, mlp1_b, mlp2_w, mlp2_b, ln2_g, ln2_b):
    B, T, _ = x_prompt.shape
    DB, DS, _ = x_sample.shape
    past = page_table.shape[1] * PAGE_SIZE
    pos_p = jnp.arange(T)
    pos_s = past + jnp.arange(DS)
    wb_p = min(WINDOW, T)
    hp, hs = x_prompt, x_sample
    cmp_p, sel_p, win_p, wkv_p, shift_p = [], [], [], [], []
    cmp_s, sel_s, win_s, wkv_s, shift_s = [], [], [], [], []
    for l in range(DEPTH):
        rwkv_w = (mu_shift[l], w0[l], w_w2[l], a0[l], w_a2[l], g_w2[l], k_k[l], k_a[l], r_k[l],
                  gn_g[l], gn_b[l])
        cmp_w = (cmp_pe[l], cmp_w1[l], cmp_b1[l], cmp_w2[l], cmp_b2[l])
        out_w = (w_pa[l], w_pb[l], w_o[l], ln1_g[l], ln1_b[l], router_w[l], router_b[l],
                 mlp1_w[l], mlp1_b[l], mlp2_w[l], mlp2_b[l], ln2_g[l], ln2_b[l])
        pr, q, kvs, gates, mg = project(hp, w_in[l], pos_p)
        y_r, wkv, shift = rwkv_mixer(pr, jnp.zeros((B, R_COLS), pr.dtype),
                                     jnp.zeros((B, R_HEADS, R_HEAD, R_HEAD), jnp.float32), *rwkv_w)
        y_a = nsa_prompt(q, kvs, gates, *cmp_w)
        cmp_p.append(jnp.stack([kvs[0], kvs[1]], axis=2))
        sel_p.append(jnp.stack([kvs[2], kvs[3]], axis=2))
        win_p.append(jnp.stack([kvs[4][:, T - wb_p:], kvs[5][:, T - wb_p:]], axis=2))
        wkv_p.append(wkv)
        shift_p.append(shift)
        hp = merge_and_ffn(hp, y_r, y_a, mg, *out_w)
        pr, q, kvs, gates, mg = project(hs, w_in[l], pos_s)
        y_r, wkv, shift = rwkv_mixer(pr, state_shift[l], state_wkv[l], *rwkv_w)
        y_a, new_win = nsa_sample(q, kvs, gates, cache_cmp_kv[l], cache_sel_kv[l], cache_win_kv[l],
                                  page_table, *cmp_w)
        cmp_s.append(jnp.stack([kvs[0], kvs[1]], axis=2))
        sel_s.append(jnp.stack([kvs[2], kvs[3]], axis=2))
        win_s.append(new_win)
        wkv_s.append(wkv)
        shift_s.append(shift)
        hs = merge_and_ffn(hs, y_r, y_a, mg, *out_w)
    cmp_kv_prompt = jnp.stack(cmp_p)
    sel_kv_prompt = jnp.stack(sel_p)
    win_kv_prompt = jnp.stack(win_p)
    wkv_prompt = jnp.stack(wkv_p)
    shift_prompt = jnp.stack(shift_p)
    cmp_kv_sample = jnp.stack(cmp_s)
    sel_kv_sample = jnp.stack(sel_s)
    win_kv_sample = jnp.stack(win_s)
    wkv_sample = jnp.stack(wkv_s)
    shift_sample = jnp.stack(shift_s)
    return (hp, hs, cmp_kv_prompt, sel_kv_prompt, win_kv_prompt, wkv_prompt, shift_prompt,
            cmp_kv_sample, sel_kv_sample, win_kv_sample, wkv_sample, shift_sample)
```

```python
import numpy as np
from contextlib import ExitStack
import concourse.bass as bass
import concourse.mybir as mybir
from concourse.bass_utils import run_bass_kernel_spmd

F32 = mybir.dt.float32
BF16 = mybir.dt.bfloat16
I32 = mybir.dt.int32
ALU = mybir.AluOpType
AF = mybir.ActivationFunctionType
AX = mybir.AxisListType

D = 1024
SEQ = 8192
NT = SEQ // 128
R_COLS = 1792
KV0 = 1792 + 512
NKV = 768
NA = R_COLS + NKV
IN_COLS = 5144
DS = 4
SB = 4
NS = SB * DS
PAST = 8192


class R:
    __slots__ = ("name", "w", "rs", "excl")

    def __init__(self, name=""):
        self.name = name
        self.w = None
        self.rs = []
        self.excl = False


class Prog:
    NDMA = 24

    def __init__(self, nc, stack):
        self.nc = nc
        self.stack = stack
        self.eng = {"pe": nc.tensor, "dve": nc.vector, "act": nc.scalar,
                    "pool": nc.gpsimd, "sp": nc.sync}
        self.sems = {}
        self.cnt = {}
        self.cur = {}
        self.dead = set()
        self.epoch = 0
        for k in self.eng:
            key = k + "#0"
            self.sems[key] = stack.enter_context(nc.semaphore("prog_" + k + "_0"))
            self.cnt[key] = 0
            self.cur[k] = key
        self.dq = {}
        for q in ("sp", "pool", "act"):
            pool = []
            for i in range(self.NDMA):
                key = "d_%s_%d" % (q, i)
                self.sems[key] = stack.enter_context(nc.semaphore(key))
                self.cnt[key] = 0
                pool.append(key)
            self.dq[q] = [pool, 0]
        self.waited = {k: {} for k in self.eng}
        self.n_inst = 0
        self.n_wait = 0

    def _need(self, e, deps):
        best = {}
        for d in deps:
            if d is None:
                continue
            k, v = d
            if k in self.dead:
                continue
            if v > best.get(k, 0):
                best[k] = v
        for k, v in best.items():
            if self.waited[e].get(k, 0) >= v:
                continue
            self.eng[e].wait_ge(self.sems[k], v)
            self.waited[e][k] = v
            self.n_wait += 1

    def _deps(self, reads, writes):
        deps = []
        for r in reads:
            deps.append(r.w)
            if r.excl:
                deps.extend(r.rs)
        for w in writes:
            deps.append(w.w)
            deps.extend(w.rs)
        return deps

    def _commit(self, tok, reads, writes):
        for r in reads:
            if r.excl:
                r.w = tok
                r.rs = []
                continue
            r.rs.append(tok)
            if len(r.rs) > 48:
                best = {}
                for k, v in r.rs:
                    if v > best.get(k, 0):
                        best[k] = v
                r.rs = list(best.items())
        for w in writes:
            w.w = tok
            w.rs = []

    def op(self, e, fn, reads=(), writes=()):
        self._need(e, self._deps(reads, writes))
        ins = fn(self.eng[e])
        key = self.cur[e]
        self.cnt[key] += 1
        ins.then_inc(self.sems[key], 1)
        tok = (key, self.cnt[key])
        self._commit(tok, reads, writes)
        self.n_inst += 1
        return tok

    def dma(self, q, out, in_, reads=(), writes=(), **kw):
        pool, idx = self.dq[q]
        key = pool[idx % len(pool)]
        self.dq[q][1] = idx + 1
        deps = self._deps(reads, writes)
        if self.cnt[key] > 0:
            deps.append((key, self.cnt[key]))
        self._need(q, deps)
        ins = self.eng[q].dma_start(out=out, in_=in_, **kw)
        self.cnt[key] += 16
        ins.then_inc(self.sems[key], 16)
        tok = (key, self.cnt[key])
        self._commit(tok, reads, writes)
        self.n_inst += 1
        return tok

    def finish(self, regions):
        self._need("sp", [r.w for r in regions])

    def barrier(self):
        allc = [(k, v) for k, v in self.cnt.items() if v > 0 and k not in self.dead]
        for e in self.eng:
            self._need(e, allc)
        for e in self.eng:
            key = self.cur[e]
            if self.cnt[key] > 12000:
                self.dead.add(key)
                self.epoch += 1
                nk = "%s#%d" % (e, self.epoch)
                self.sems[nk] = self.stack.enter_context(self.nc.semaphore("prog_%s_%d" % (e, self.epoch)))
                self.cnt[nk] = 0
                self.cur[e] = nk


def PT(t, name):
    x = T(t, name)
    x.r.excl = True
    return x


class T:
    def __init__(self, t, name):
        self.t = t
        self.r = R(name)

    def __getitem__(self, k):
        return self.t[k]


def phase_A(nc, P, Dr, outs):
    x_full, x_s, w_in = Dr["x_full"], Dr["x_s"], Dr["w_in"]
    p_full, p_samp = Dr["p_full"], Dr["p_samp"]
    with ExitStack() as st:
        def sb(name, shape, dt=F32):
            return T(st.enter_context(nc.sbuf_tensor("a_" + name, list(shape), dt)), name)

        def ps(name, shape, dt=F32):
            return PT(st.enter_context(nc.psum_tensor("a_" + name, list(shape), dt)), name)

        ident = sb("ident", [128, 128], BF16)
        P.op("pool", lambda e: e.memset(ident[:], 0.0), writes=[ident.r])
        P.op("pool", lambda e: e.affine_select(ident[:], ident[:], [[-1, 128]], ALU.not_equal, 1.0,
                                               base=0, channel_multiplier=1),
             reads=[ident.r], writes=[ident.r])
        ropeT = sb("ropeT", [128, NT, 16])
        P.dma("sp", ropeT[:], Dr["rope_p"].rearrange("(n p) d -> p n d", p=128), writes=[ropeT.r])
        ropeS = sb("ropeS", [NS, 16])
        P.dma("sp", ropeS[:], Dr["rope_s"][:, :], writes=[ropeS.r])

        wA = sb("wA", [128, 8, NA], BF16)
        for kc in range(8):
            P.dma("pool", wA[:, kc, 0:R_COLS], w_in[kc * 128:(kc + 1) * 128, 0:R_COLS], writes=[wA.r])
            P.dma("pool", wA[:, kc, R_COLS:NA], w_in[kc * 128:(kc + 1) * 128, KV0:KV0 + NKV], writes=[wA.r])

        xb = [sb("xb%d" % i, [128, D], BF16) for i in range(2)]
        xT = [sb("xT%d" % i, [128, 8, 128], BF16) for i in range(2)]
        pt = [sb("ptile%d" % i, [128, NA]) for i in range(2)]
        rtmp = [sb("rtmp%d" % i, [128, 4, 6, 8]) for i in range(2)]
        ptr = [ps("ptr%d" % i, [128, 8, 128], BF16) for i in range(2)]
        pmm = [ps("pmm%d" % i, [128, 512]) for i in range(5)]
        pmm_i = [0]

        def rope_apply(tile_t, c0, cs_ap, tmp, n):
            v = tile_t.t[0:n, c0:c0 + 768].rearrange("p (a k g d) -> p a k g d", a=3, k=2, g=2)
            x1 = v[:, :, 0, :, 0:8]
            x2 = v[:, :, 0, :, 8:16]
            cos = cs_ap[:, 0:8].unsqueeze(1).unsqueeze(1).to_broadcast([n, 3, 2, 8])
            sin = cs_ap[:, 8:16].unsqueeze(1).unsqueeze(1).to_broadcast([n, 3, 2, 8])
            t = tmp.t[0:n]
            a1, a2, a3, a4 = [t[:, i, :, :].rearrange("p (a g) d -> p a g d", a=3) for i in range(4)]
            rd = [tile_t.r, tmp.r]
            P.op("dve", lambda e: e.tensor_tensor(a1, x1, cos, ALU.mult), reads=rd, writes=[tmp.r])
            P.op("dve", lambda e: e.tensor_tensor(a2, x2, sin, ALU.mult), reads=rd, writes=[tmp.r])
            P.op("dve", lambda e: e.tensor_tensor(a3, x2, cos, ALU.mult), reads=rd, writes=[tmp.r])
            P.op("dve", lambda e: e.tensor_tensor(a4, x1, sin, ALU.mult), reads=rd, writes=[tmp.r])
            P.op("dve", lambda e: e.tensor_tensor(x1, a1, a2, ALU.subtract), reads=rd, writes=[tile_t.r])
            P.op("dve", lambda e: e.tensor_tensor(x2, a3, a4, ALU.add), reads=rd, writes=[tile_t.r])

        r_pfull = R("p_full")
        r_out = R("outsA")
        outs.append(r_out)
        for ti in range(NT):
            b = ti % 2
            t0 = ti * 128
            P.dma("pool", xb[b][:], x_full[t0:t0 + 128, :], writes=[xb[b].r])
            for kc in range(8):
                P.op("pe", lambda e: e.transpose(ptr[b][:, kc, :], xb[b][:, kc * 128:(kc + 1) * 128], ident[:]),
                     reads=[xb[b].r, ident.r], writes=[ptr[b].r])
            P.op("act", lambda e: e.copy(xT[b][:], ptr[b][:]), reads=[ptr[b].r], writes=[xT[b].r])
            for nch in range(5):
                pm = pmm[pmm_i[0] % 5]
                pmm_i[0] += 1
                for kc in range(8):
                    P.op("pe", lambda e: e.matmul(pm[:], xT[b][:, kc, :], wA[:, kc, nch * 512:(nch + 1) * 512],
                                                  start=(kc == 0), stop=(kc == 7)),
                         reads=[xT[b].r, wA.r], writes=[pm.r])
                dst = pt[b][:, nch * 512:(nch + 1) * 512]
                if nch % 2 == 0:
                    P.op("dve", lambda e: e.tensor_copy(dst, pm[:]), reads=[pm.r], writes=[pt[b].r])
                else:
                    P.op("act", lambda e: e.copy(dst, pm[:]), reads=[pm.r], writes=[pt[b].r])
            rope_apply(pt[b], R_COLS, ropeT[:, ti, :], rtmp[b], 128)
            P.dma("sp", p_full[t0:t0 + 128, :], pt[b][:], reads=[pt[b].r], writes=[r_pfull])
            P.dma("sp", Dr["o_cmp_p"][t0:t0 + 128, :], pt[b][:, R_COLS:R_COLS + 256], reads=[pt[b].r], writes=[r_out])
            P.dma("sp", Dr["o_sel_p"][t0:t0 + 128, :], pt[b][:, R_COLS + 256:R_COLS + 512], reads=[pt[b].r], writes=[r_out])
            if ti >= NT - 4:
                w0 = (ti - (NT - 4)) * 128
                P.dma("sp", Dr["o_win_p"][w0:w0 + 128, :], pt[b][:, R_COLS + 512:R_COLS + 768], reads=[pt[b].r], writes=[r_out])
            if ti == NT - 1:
                P.dma("sp", Dr["o_shift_p"][0:1, :], pt[b][127:128, 0:R_COLS], reads=[pt[b].r], writes=[r_out])

        xsb = sb("xsb", [NS, D], BF16)
        xsT = sb("xsT", [128, 8, NS], BF16)
        psT = ptr[0]
        P.dma("pool", xsb[:], x_s[:, :], writes=[xsb.r])
        for kc in range(8):
            P.op("pe", lambda e: e.transpose(psT[:, kc, 0:NS], xsb[:, kc * 128:(kc + 1) * 128], ident[0:NS, 0:NS]),
                 reads=[xsb.r, ident.r], writes=[psT.r])
        P.op("act", lambda e: e.copy(xsT[:], psT[:, :, 0:NS]), reads=[psT.r], writes=[xsT.r])
        psamp = sb("psamp", [NS, IN_COLS])
        wS = [sb("wS%d" % i, [128, 8, 512], BF16) for i in range(2)]
        ncht = (IN_COLS + 511) // 512
        for nch in range(ncht):
            c0 = nch * 512
            cw = min(512, IN_COLS - c0)
            wb = wS[nch % 2]
            for kc in range(8):
                P.dma("pool", wb[:, kc, 0:cw], w_in[kc * 128:(kc + 1) * 128, c0:c0 + cw], writes=[wb.r])
            pm = pmm[pmm_i[0] % 5]
            pmm_i[0] += 1
            for kc in range(8):
                P.op("pe", lambda e: e.matmul(pm[0:NS, 0:cw], xsT[:, kc, :], wb[:, kc, 0:cw],
                                              start=(kc == 0), stop=(kc == 7)),
                     reads=[xsT.r, wb.r], writes=[pm.r])
            P.op("dve", lambda e: e.tensor_copy(psamp[:, c0:c0 + cw], pm[0:NS, 0:cw]), reads=[pm.r], writes=[psamp.r])
        rope_apply(psamp, KV0, ropeS[:, :], rtmp[0], NS)
        r_psamp = R("p_samp")
        P.dma("sp", p_samp[:, :], psamp[:], reads=[psamp.r], writes=[r_psamp])
        P.dma("sp", Dr["o_cmp_s"][:, :], psamp[:, KV0:KV0 + 256], reads=[psamp.r], writes=[r_out])
        P.dma("sp", Dr["o_sel_s"][:, :], psamp[:, KV0 + 256:KV0 + 512], reads=[psamp.r], writes=[r_out])
        for bb in range(SB):
            P.dma("sp", Dr["o_win_s"][bb, 508:512, :], psamp[bb * DS:(bb + 1) * DS, KV0 + 512:KV0 + 768],
                  reads=[psamp.r], writes=[r_out])
            P.dma("sp", Dr["o_win_s"][bb, 0:508, :], Dr["cache_win"][bb, 4:512, :], writes=[r_out])
            P.dma("sp", Dr["o_shift_s"][bb:bb + 1, :], psamp[bb * DS + DS - 1:bb * DS + DS, 0:R_COLS],
                  reads=[psamp.r], writes=[r_out])
        Dr["r_pfull"] = r_pfull
        Dr["r_psamp"] = r_psamp
        P.barrier()

VEC_OFF = {"mu": (0, 1792), "w0": (1792, 512), "a0": (2304, 512), "kk": (2816, 512), "ka": (3328, 512),
           "gng": (3840, 512), "gnb": (4352, 512), "rk": (4864, 512)}
NVEC = 5376
NMASK = 128 + 128 + 512 + 128 + 128


def phase_B(nc, P, Dr, outs):
    with ExitStack() as st:
        def sb(name, shape, dt=F32):
            return T(st.enter_context(nc.sbuf_tensor("b_" + name, list(shape), dt)), name)

        def ps(name, shape, dt=F32):
            return PT(st.enter_context(nc.psum_tensor("b_" + name, list(shape), dt)), name)

        vecs = sb("vecs", [128, NVEC])
        P.dma("sp", vecs[:], Dr["vecs"][:, :], writes=[vecs.r])
        V = lambda k: vecs[:, VEC_OFF[k][0]:VEC_OFF[k][0] + VEC_OFF[k][1]]
        wlora = sb("wlora", [128, 512])
        P.dma("sp", wlora[0:64, :], Dr["w_w2"][:, :], writes=[wlora.r])
        P.dma("sp", wlora[64:128, :], Dr["w_a2"][:, :], writes=[wlora.r])
        gw2 = sb("gw2", [128, 512])
        P.dma("sp", gw2[:], Dr["g_w2"][:, :], writes=[gw2.r])
        masks = sb("masks", [128, NMASK])
        P.dma("sp", masks[:], Dr["masks"][:, :], writes=[masks.r])
        Lblk = masks[:, 0:128]
        Oblk = masks[:, 128:256]
        maskMA2 = masks[:, 256:768]
        maskNT = masks[:, 768:896]
        identF = masks[:, 896:1024]
        tmask = sb("tmask", [128, 1])
        P.dma("sp", tmask[:], Dr["tmask"][:, :], writes=[tmask.r])
        ones = sb("onesc", [128, 1])
        P.op("pool", lambda e: e.memset(ones[:], 1.0), writes=[ones.r])

        pr = sb("pr", [128, R_COLS])
        prev = sb("prev", [128, R_COLS])
        xm = sb("xm", [128, R_COLS])
        la = sb("la", [128, 256])
        laT = sb("laT", [128, 256])
        tA = sb("tA", [128, 512])
        tB = sb("tB", [128, 512])
        lw = sb("lw", [128, 512])
        aicl = sb("aicl", [128, 512])
        g_sb = sb("g_sb", [128, 512])
        kk = sb("kk", [128, 512])
        kkn = sb("kkn", [128, 512])
        kh = sb("kh", [128, 512])
        a_s = sb("a_s", [128, 512])
        b_s = sb("b_s", [128, 512])
        ss = sb("ss", [128, 8])
        rinv = sb("rinv", [128, 8])
        cum_sb = sb("cum_sb", [128, 512])
        e_sb = sb("e_sb", [128, 512])
        einv = sb("einv", [128, 512])
        ea = sb("ea", [128, 512])
        ec = sb("ec", [128, 512])
        at = sb("at", [128, 512])
        rt = sb("rt", [128, 512])
        bt = sb("bt", [128, 512])
        kt = sb("kt", [128, 512])
        bh = sb("bh", [128, 512])
        kh2 = sb("kh2", [128, 512])
        wc = sb("wc", [128, 8])
        FM_ar = sb("FM_ar", [128, 4, 256])
        FM_b = sb("FM_b", [128, 4, 128])
        FM_k = sb("FM_k", [128, 4, 128])
        MM = [sb("MM%d" % h, [128, 512]) for h in range(8)]
        Tm = [sb("Tm%d" % h, [128, 128]) for h in range(8)]
        XX = [[sb("XX%d_%d" % (p, i), [128, 256]) for i in range(2)] for p in range(2)]
        PMb = [sb("PM%d" % p, [128, 128]) for p in range(2)]
        ZT_sb = sb("ZT_sb", [128, 512])
        UT_sb = sb("UT_sb", [128, 512])
        y_sb = sb("y_sb", [128, 512])
        yc = sb("yc", [128, 512])
        st1 = sb("st1", [128, 8])
        st2 = sb("st2", [128, 8])
        yo = sb("yo", [128, 512], BF16)
        ST = sb("ST", [128, 256])
        Sio = sb("Sio", [64, 512])

        pg = [ps("pg%d" % i, [128, 512]) for i in range(2)]
        pi = ps("pi", [128, 512])
        pv = [ps("pv%d" % i, [128, 512]) for i in range(2)]
        ZT_ps = ps("ZT_ps", [128, 512])
        UT_ps = ps("UT_ps", [128, 512])
        SN_ps = ps("SN_ps", [128, 512])

        def TT(eng, out, in0, in1, op, rd, wr):
            P.op(eng, lambda e: e.tensor_tensor(out, in0, in1, op), reads=rd, writes=wr)

        def ACT(out, in_, func, rd, wr, **kw):
            P.op("act", lambda e: e.activation(out, in_, func, **kw), reads=rd, writes=wr)

        def MMUL(out, lhsT, rhs, rd, wr, start=True, stop=True):
            P.op("pe", lambda e: e.matmul(out, lhsT, rhs, start=start, stop=stop), reads=rd, writes=wr)

        def TR(out, in_, idn, rd, wr):
            P.op("pe", lambda e: e.transpose(out, in_, idn), reads=rd + [masks.r], writes=wr)

        def load_state(src_ap):
            P.dma("sp", Sio[:].rearrange("i (h j) -> i h j", h=8), src_ap.rearrange("h i j -> i h j"), writes=[Sio.r])
            for hp in range(4):
                TR(pg[0][:, hp * 64:(hp + 1) * 64], Sio[:, hp * 128:(hp + 1) * 128], identF[0:64, 0:64], [Sio.r], [pg[0].r])
            P.op("dve", lambda e: e.tensor_copy(ST[:], pg[0][:, 0:256]), reads=[pg[0].r], writes=[ST.r])

        def store_state(dst_ap, rout):
            for hp in range(4):
                TR(pg[0][0:64, hp * 128:(hp + 1) * 128], ST[:, hp * 64:(hp + 1) * 64], identF, [ST.r], [pg[0].r])
            P.op("dve", lambda e: e.tensor_copy(Sio[:], pg[0][0:64, :]), reads=[pg[0].r], writes=[Sio.r])
            P.dma("sp", dst_ap.rearrange("h i j -> i h j"), Sio[:].rearrange("i (h j) -> i h j", h=8), reads=[Sio.r], writes=[rout])

        def rwkv_tile(load_fn, sample, pre_chunk, post_chunk, y_store):
            load_fn(pr, prev)
            TT("pool", prev[:], prev[:], pr[:], ALU.subtract, [prev.r, pr.r], [prev.r])
            TT("pool", prev[:], prev[:], V("mu"), ALU.mult, [prev.r, vecs.r], [prev.r])
            TT("dve", xm[:], prev[:], pr[:], ALU.add, [prev.r, pr.r], [xm.r])
            r_ = xm[:, 0:512]
            k_ = xm[:, 512:1024]
            v_ = xm[:, 1024:1536]
            ACT(la[:, 0:64], xm[:, 1536:1600], AF.Tanh, [xm.r], [la.r])
            ACT(la[:, 64:128], xm[:, 1600:1664], AF.Copy, [xm.r], [la.r])
            ACT(la[:, 128:256], xm[:, 1664:1792], AF.Sigmoid, [xm.r], [la.r])
            TR(pg[0][:, 0:128], la[:, 0:128], identF, [la.r], [pg[0].r])
            TR(pg[0][:, 128:256], la[:, 128:256], identF, [la.r], [pg[0].r])
            ACT(laT[:], pg[0][:, 0:256], AF.Copy, [pg[0].r], [laT.r])
            MMUL(pg[1][:], laT[0:64, 0:128], wlora[0:64, :], [laT.r, wlora.r], [pg[1].r])
            TT("dve", tA[:], pg[1][:], V("w0"), ALU.add, [pg[1].r, vecs.r], [tA.r])
            ACT(tA[:], tA[:], AF.Sigmoid, [tA.r], [tA.r])
            if sample:
                P.op("dve", lambda e: e.tensor_scalar(lw[:], tA[:], -0.6065306597126334, tmask[:, 0:1], ALU.mult, ALU.mult),
                     reads=[tA.r, tmask.r], writes=[lw.r])
            else:
                P.op("dve", lambda e: e.tensor_scalar(lw[:], tA[:], -0.6065306597126334, None, ALU.mult),
                     reads=[tA.r], writes=[lw.r])
            MMUL(pg[0][:], laT[64:128, 0:128], wlora[64:128, :], [laT.r, wlora.r], [pg[0].r])
            TT("dve", aicl[:], pg[0][:], V("a0"), ALU.add, [pg[0].r, vecs.r], [aicl.r])
            ACT(aicl[:], aicl[:], AF.Sigmoid, [aicl.r], [aicl.r])
            MMUL(pg[1][:], laT[:, 128:256], gw2[:], [laT.r, gw2.r], [pg[1].r])
            ACT(g_sb[:], pg[1][:], AF.Copy, [pg[1].r], [g_sb.r])
            TT("pool", kk[:], k_, V("kk"), ALU.mult, [xm.r, vecs.r], [kk.r])
            TT("pool", kkn[:], kk[:], kk[:], ALU.mult, [kk.r], [kkn.r])
            P.op("dve", lambda e: e.tensor_reduce(ss[:], kkn[:].rearrange("p (h j) -> p h j", h=8), AX.X, ALU.add),
                 reads=[kkn.r], writes=[ss.r])
            P.op("dve", lambda e: e.tensor_scalar(ss[:], ss[:], 1e-24, None, ALU.max), reads=[ss.r], writes=[ss.r])
            ACT(rinv[:], ss[:], AF.Sqrt, [ss.r], [rinv.r])
            P.op("dve", lambda e: e.reciprocal(rinv[:], rinv[:]), reads=[rinv.r], writes=[rinv.r])
            TT("dve", kkn[:].rearrange("p (h j) -> p h j", h=8), kk[:].rearrange("p (h j) -> p h j", h=8),
               rinv[:].unsqueeze(2).to_broadcast([128, 8, 64]), ALU.mult, [kk.r, rinv.r], [kkn.r])
            P.op("dve", lambda e: e.scalar_tensor_tensor(kh[:], aicl[:], -1.0, V("ka"), ALU.add, ALU.mult),
                 reads=[aicl.r, vecs.r], writes=[kh.r])
            P.op("dve", lambda e: e.scalar_tensor_tensor(kh[:], kh[:], 1.0, k_, ALU.add, ALU.mult),
                 reads=[kh.r, xm.r], writes=[kh.r])
            P.op("pool", lambda e: e.tensor_scalar(a_s[:], kkn[:], -1.0, None, ALU.mult), reads=[kkn.r], writes=[a_s.r])
            TT("pool", b_s[:], kkn[:], aicl[:], ALU.mult, [kkn.r, aicl.r], [b_s.r])
            if sample:
                P.op("dve", lambda e: e.tensor_scalar(kh[:], kh[:], tmask[:, 0:1], None, ALU.mult), reads=[kh.r, tmask.r], writes=[kh.r])
                P.op("dve", lambda e: e.tensor_scalar(b_s[:], b_s[:], tmask[:, 0:1], None, ALU.mult), reads=[b_s.r, tmask.r], writes=[b_s.r])
            MMUL(pg[0][:], Lblk, lw[:], [masks.r, lw.r], [pg[0].r])
            MMUL(pg[1][:], Oblk, lw[:], [masks.r, lw.r], [pg[1].r])
            ACT(cum_sb[:], pg[0][:], AF.Copy, [pg[0].r], [cum_sb.r])
            ACT(e_sb[:], pg[0][:], AF.Exp, [pg[0].r], [e_sb.r])
            ACT(einv[:], pg[0][:], AF.Exp, [pg[0].r], [einv.r], scale=-1.0)
            TT("dve", tA[:], pg[0][:], lw[:], ALU.subtract, [pg[0].r, lw.r], [tA.r])
            ACT(ea[:], tA[:], AF.Exp, [tA.r], [ea.r])
            TT("dve", tB[:], pg[1][:], cum_sb[:], ALU.subtract, [pg[1].r, cum_sb.r], [tB.r])
            ACT(ec[:], tB[:], AF.Exp, [tB.r], [ec.r])
            TT("pool", at[:], a_s[:], ea[:], ALU.mult, [a_s.r, ea.r], [at.r])
            TT("dve", rt[:], r_, e_sb[:], ALU.mult, [xm.r, e_sb.r], [rt.r])
            TT("pool", bt[:], b_s[:], einv[:], ALU.mult, [b_s.r, einv.r], [bt.r])
            TT("dve", kt[:], kh[:], einv[:], ALU.mult, [kh.r, einv.r], [kt.r])
            TT("pool", bh[:], b_s[:], ec[:], ALU.mult, [b_s.r, ec.r], [bh.r])
            TT("dve", kh2[:], kh[:], ec[:], ALU.mult, [kh.r, ec.r], [kh2.r])
            for c2 in range(2):
                rows = slice(c2 * 64, c2 * 64 + 64)
                for hp in range(4):
                    MMUL(SN_ps[:, 256 + c2 * 4 + hp:256 + c2 * 4 + hp + 1], lw[rows, hp * 128:(hp + 1) * 128], ones[rows, 0:1],
                         [lw.r, ones.r], [SN_ps.r])
            ACT(wc[:], SN_ps[:, 256:264], AF.Exp, [SN_ps.r], [wc.r])
            for qi, q in enumerate((at, rt)):
                for hp in range(4):
                    TR(pg[qi][:, hp * 128:(hp + 1) * 128], q[:, hp * 128:(hp + 1) * 128], identF, [q.r], [pg[qi].r])
                P.op("act" if qi == 0 else "dve",
                     (lambda e: e.copy(FM_ar[:, :, 0:128], pg[0][:].rearrange("p (h t) -> p h t", h=4))) if qi == 0 else
                     (lambda e: e.tensor_copy(FM_ar[:, :, 128:256], pg[1][:].rearrange("p (h t) -> p h t", h=4))),
                     reads=[pg[qi].r], writes=[FM_ar.r])
            for qi, (q, dst) in enumerate(((bt, FM_b), (kt, FM_k))):
                for hp in range(4):
                    TR(pg[qi][:, hp * 128:(hp + 1) * 128], q[:, hp * 128:(hp + 1) * 128], identF, [q.r], [pg[qi].r])
                if qi == 0:
                    P.op("act", lambda e: e.copy(dst[:].rearrange("p h t -> p (h t)"), pg[0][:]), reads=[pg[0].r], writes=[dst.r])
                else:
                    P.op("dve", lambda e: e.tensor_copy(dst[:].rearrange("p h t -> p (h t)"), pg[1][:]), reads=[pg[1].r], writes=[dst.r])
            for h in range(8):
                hp, h2 = h // 2, h % 2
                rows = slice(h2 * 64, h2 * 64 + 64)
                par = h % 2
                MMUL(pi[:, 0:256], FM_b[rows, hp, :], FM_ar[rows, hp, :], [FM_b.r, FM_ar.r], [pi.r])
                MMUL(pi[:, 256:512], FM_k[rows, hp, :], FM_ar[rows, hp, :], [FM_k.r, FM_ar.r], [pi.r])
                MMUL(pv[par][:, 384:512], FM_ar[rows, hp, 0:128], FM_b[rows, hp, :], [FM_ar.r, FM_b.r], [pv[par].r])
                TT("dve", MM[h][:], pi[:], maskMA2, ALU.mult, [pi.r, masks.r], [MM[h].r])
                X = XX[par][0]
                TT("dve", X[:, 128:256], pv[par][:, 384:512], maskNT, ALU.mult, [pv[par].r, masks.r], [X.r])
                P.op("pool", lambda e: e.tensor_copy(X[:, 0:128], MM[h][:, 0:128]), reads=[MM[h].r], writes=[X.r])
                cur = Tm[h] if False else PMb[par]
                TT("pool", PMb[par][:], MM[h][:, 0:128], identF, ALU.add, [MM[h].r, masks.r], [PMb[par].r])
                for k in range(1, 6):
                    Xo = XX[par][(k - 1) % 2]
                    Xn = XX[par][k % 2]
                    MMUL(pv[par][:, 128:256], Xo[:, 0:128], Xo[:, 128:256], [Xo.r], [pv[par].r])
                    if k < 5:
                        MMUL(pv[par][:, 0:128], Xo[:, 128:256], Xo[:, 0:128], [Xo.r], [pv[par].r])
                        P.op("act", lambda e: e.copy(Xn[:], pv[par][:, 0:256]), reads=[pv[par].r], writes=[Xn.r])
                    else:
                        P.op("act", lambda e: e.copy(Xn[:, 128:256], pv[par][:, 128:256]), reads=[pv[par].r], writes=[Xn.r])
                    MMUL(pv[par][:, 256:384], Xn[:, 128:256], PMb[par][:], [Xn.r, PMb[par].r], [pv[par].r])
                    dstP = Tm[h] if k == 5 else PMb[par]
                    TT("dve", dstP[:], pv[par][:, 256:384], PMb[par][:], ALU.add, [pv[par].r, PMb[par].r], [dstP.r])
            for c2 in range(2):
                cs = slice(c2 * 64, c2 * 64 + 64)
                cc = slice(c2 * 64, c2 * 64 + 64)
                if pre_chunk is not None:
                    pre_chunk(c2)
                for h in range(8):
                    hp, h2 = h // 2, h % 2
                    rows = slice(h2 * 64, h2 * 64 + 64)
                    hc = slice(h * 64, h * 64 + 64)
                    Sh = ST[rows, hp * 64:(hp + 1) * 64]
                    MMUL(ZT_ps[cs, hc], FM_ar[rows, hp, cc], Sh, [FM_ar.r, ST.r], [ZT_ps.r], start=True, stop=False)
                    MMUL(ZT_ps[cs, hc], MM[h][cs, 256 + c2 * 64:256 + c2 * 64 + 64], xm[cs, 1024 + h * 64:1024 + h * 64 + 64],
                         [MM[h].r, xm.r], [ZT_ps.r], start=False, stop=True)
                P.op("act", lambda e: e.copy(ZT_sb[cs, :], ZT_ps[cs, :]), reads=[ZT_ps.r], writes=[ZT_sb.r])
                for h in range(8):
                    hc = slice(h * 64, h * 64 + 64)
                    MMUL(UT_ps[cs, hc], Tm[h][cs, cc], ZT_sb[cs, hc], [Tm[h].r, ZT_sb.r], [UT_ps.r])
                P.op("dve", lambda e: e.tensor_copy(UT_sb[cs, :], UT_ps[cs, :]), reads=[UT_ps.r], writes=[UT_sb.r])
                yps = pg[c2]
                for h in range(8):
                    hp, h2 = h // 2, h % 2
                    rows = slice(h2 * 64, h2 * 64 + 64)
                    hc = slice(h * 64, h * 64 + 64)
                    Sh = ST[rows, hp * 64:(hp + 1) * 64]
                    vh = xm[cs, 1024 + h * 64:1024 + h * 64 + 64]
                    MMUL(yps[cs, hc], FM_ar[rows, hp, 128 + c2 * 64:128 + c2 * 64 + 64], Sh, [FM_ar.r, ST.r], [yps.r], start=True, stop=False)
                    MMUL(yps[cs, hc], MM[h][cs, 128 + c2 * 64:128 + c2 * 64 + 64], UT_sb[cs, hc], [MM[h].r, UT_sb.r], [yps.r], start=False, stop=False)
                    MMUL(yps[cs, hc], MM[h][cs, 384 + c2 * 64:384 + c2 * 64 + 64], vh, [MM[h].r, xm.r], [yps.r], start=False, stop=True)
                    MMUL(SN_ps[rows, hp * 64:(hp + 1) * 64], bh[cs, hc], UT_sb[cs, hc], [bh.r, UT_sb.r], [SN_ps.r], start=True, stop=False)
                    MMUL(SN_ps[rows, hp * 64:(hp + 1) * 64], kh2[cs, hc], vh, [kh2.r, xm.r], [SN_ps.r], start=False, stop=True)
                P.op("act", lambda e: e.copy(y_sb[cs, :], yps[cs, :]), reads=[yps.r], writes=[y_sb.r])
                TT("dve", ST[:].rearrange("p (h i) -> p h i", h=4), ST[:].rearrange("p (h i) -> p h i", h=4),
                   wc[:, c2 * 4:c2 * 4 + 4].unsqueeze(2).to_broadcast([128, 4, 64]), ALU.mult, [ST.r, wc.r], [ST.r])
                TT("dve", ST[:], ST[:], SN_ps[:, 0:256], ALU.add, [ST.r, SN_ps.r], [ST.r])
                if post_chunk is not None:
                    post_chunk(c2)
            y3 = y_sb[:].rearrange("p (h j) -> p h j", h=8)
            yc3 = yc[:].rearrange("p (h j) -> p h j", h=8)
            P.op("dve", lambda e: e.tensor_reduce(st1[:], y3, AX.X, ALU.add), reads=[y_sb.r], writes=[st1.r])
            P.op("dve", lambda e: e.tensor_scalar(st1[:], st1[:], 1.0 / 64, None, ALU.mult), reads=[st1.r], writes=[st1.r])
            TT("dve", yc3, y3, st1[:].unsqueeze(2).to_broadcast([128, 8, 64]), ALU.subtract, [y_sb.r, st1.r], [yc.r])
            TT("pool", tA[:], yc[:], yc[:], ALU.mult, [yc.r], [tA.r])
            P.op("dve", lambda e: e.tensor_reduce(st2[:], tA[:].rearrange("p (h j) -> p h j", h=8), AX.X, ALU.add), reads=[tA.r], writes=[st2.r])
            P.op("dve", lambda e: e.tensor_scalar(st2[:], st2[:], 1.0 / 64, 64e-5, ALU.mult, ALU.add), reads=[st2.r], writes=[st2.r])
            ACT(st2[:], st2[:], AF.Sqrt, [st2.r], [st2.r])
            P.op("dve", lambda e: e.reciprocal(st2[:], st2[:]), reads=[st2.r], writes=[st2.r])
            TT("dve", yc3, yc3, st2[:].unsqueeze(2).to_broadcast([128, 8, 64]), ALU.mult, [yc.r, st2.r], [yc.r])
            TT("pool", yc[:], yc[:], V("gng"), ALU.mult, [yc.r, vecs.r], [yc.r])
            TT("pool", yc[:], yc[:], V("gnb"), ALU.add, [yc.r, vecs.r], [yc.r])
            TT("pool", tB[:], r_, kh[:], ALU.mult, [xm.r, kh.r], [tB.r])
            TT("pool", tB[:], tB[:], V("rk"), ALU.mult, [tB.r, vecs.r], [tB.r])
            P.op("dve", lambda e: e.tensor_reduce(st1[:], tB[:].rearrange("p (h j) -> p h j", h=8), AX.X, ALU.add), reads=[tB.r], writes=[st1.r])
            TT("dve", tB[:].rearrange("p (h j) -> p h j", h=8), v_.rearrange("p (h j) -> p h j", h=8),
               st1[:].unsqueeze(2).to_broadcast([128, 8, 64]), ALU.mult, [xm.r, st1.r], [tB.r])
            TT("dve", yc[:], yc[:], tB[:], ALU.add, [yc.r, tB.r], [yc.r])
            TT("dve", yo[:], yc[:], g_sb[:], ALU.mult, [yc.r, g_sb.r], [yo.r])
            y_store(yo)

        P.op("dve", lambda e: e.memset(ST[:], 0.0), writes=[ST.r])
        r_yr = R("y_r")
        r_out = R("outB")
        outs.append(r_out)
        p_full = Dr["p_full"]
        for ti in range(NT):
            t0 = ti * 128

            def load_fn(pr_t, prev_t, t0=t0, ti=ti):
                P.dma("sp", pr_t[:], p_full[t0:t0 + 128, 0:R_COLS], reads=[Dr["r_pfull"]], writes=[pr_t.r])
                if ti == 0:
                    P.op("dve", lambda e: e.memset(prev_t[0:1, :], 0.0), writes=[prev_t.r])
                    P.dma("sp", prev_t[1:128, :], p_full[0:127, 0:R_COLS], reads=[Dr["r_pfull"]], writes=[prev_t.r])
                else:
                    P.dma("sp", prev_t[:], p_full[t0 - 1:t0 + 127, 0:R_COLS], reads=[Dr["r_pfull"]], writes=[prev_t.r])

            def y_store(yo_t, t0=t0):
                P.dma("pool", Dr["y_r"][t0:t0 + 128, :], yo_t[:], reads=[yo_t.r], writes=[r_yr])

            post = None
            if ti == NT - 1:
                def post(c2):
                    if c2 == 1:
                        store_state(Dr["o_wkv_p"], r_out)
            rwkv_tile(load_fn, False, None, post, y_store)

        p_samp = Dr["p_samp"]
        for tp in range(SB // 2):
            def load_fn(pr_t, prev_t, tp=tp):
                P.op("dve", lambda e: e.memset(pr_t[:], 0.0), writes=[pr_t.r])
                P.op("pool", lambda e: e.memset(prev_t[:], 0.0), writes=[prev_t.r])
                for c2 in range(2):
                    bb = tp * 2 + c2
                    P.dma("sp", pr_t[c2 * 64:c2 * 64 + DS, :], p_samp[bb * DS:(bb + 1) * DS, 0:R_COLS],
                          reads=[Dr["r_psamp"]], writes=[pr_t.r])
                    P.dma("sp", prev_t[c2 * 64:c2 * 64 + 1, :], Dr["state_shift"][bb:bb + 1, :], writes=[prev_t.r])
                    P.dma("sp", prev_t[c2 * 64 + 1:c2 * 64 + DS, :], p_samp[bb * DS:(bb + 1) * DS - 1, 0:R_COLS],
                          reads=[Dr["r_psamp"]], writes=[prev_t.r])

            def pre(c2, tp=tp):
                load_state(Dr["state_wkv"][tp * 2 + c2])

            def post(c2, tp=tp):
                store_state(Dr["o_wkv_s"][tp * 2 + c2], r_out)

            def y_store(yo_t, tp=tp):
                for c2 in range(2):
                    bb = tp * 2 + c2
                    P.dma("pool", Dr["y_r_s"][bb * DS:(bb + 1) * DS, :], yo_t[c2 * 64:c2 * 64 + DS, :], reads=[yo_t.r], writes=[r_yr])

            rwkv_tile(load_fn, True, pre, post, y_store)
        Dr["r_yr"] = r_yr
        P.barrier()

QG0 = 1792
GATE0 = 3072
NEGB = -30000.0


def phase_C(nc, P, Dr, outs):
    with ExitStack() as st:
        def sb(name, shape, dt=F32):
            return T(st.enter_context(nc.sbuf_tensor("c_" + name, list(shape), dt)), name)

        def ps(name, shape, dt=F32):
            return PT(st.enter_context(nc.psum_tensor("c_" + name, list(shape), dt)), name)

        def TT(eng, out, in0, in1, op, rd, wr):
            P.op(eng, lambda e: e.tensor_tensor(out, in0, in1, op), reads=rd, writes=wr)

        def MMUL(out, lhsT, rhs, rd, wr, start=True, stop=True):
            P.op("pe", lambda e: e.matmul(out, lhsT, rhs, start=start, stop=stop, skip_group_check=True), reads=rd, writes=wr)

        identb = sb("identb", [128, 128], BF16)
        P.dma("pool", identb[:], Dr["masks"][:, 896:1024], writes=[identb.r])
        identf = sb("identf", [128, 128])
        P.dma("sp", identf[:], Dr["masks"][:, 896:1024], writes=[identf.r])
        onesf = sb("onesf", [128, 128])
        P.op("pool", lambda e: e.memset(onesf[:], 1.0), writes=[onesf.r])
        NKT = 65
        ksT = [sb("ksT%d" % g, [65, NKT * 128], BF16) for g in range(2)]
        kwT = [sb("kwT%d" % g, [65, NKT * 128], BF16) for g in range(2)]
        vsw = sb("vsw", [128, NKT, 4, 65], BF16)
        kcmpT = [sb("kcmpT%d" % g, [65, 512], BF16) for g in range(2)]
        vcmp = sb("vcmp", [128, 4, 2, 65], BF16)
        nm = sb("nm", [128, 12])
        kmaxb = sb("kmaxb", [128, 1])
        for g in range(2):
            P.op("pool", lambda e: e.memset(ksT[g][64:65, :], 1.0), writes=[ksT[g].r])
            P.op("pool", lambda e: e.memset(kwT[g][64:65, :], 1.0), writes=[kwT[g].r])
            P.op("pool", lambda e: e.memset(kcmpT[g][64:65, :], 1.0), writes=[kcmpT[g].r])
        P.op("pool", lambda e: e.memset(vsw[:, :, :, 64:65], 1.0), writes=[vsw.r])
        P.op("pool", lambda e: e.memset(vcmp[:, :, :, 64:65], 1.0), writes=[vcmp.r])
        kvb = [sb("kvb%d" % i, [128, 768], BF16) for i in range(2)]
        sqt = sb("sqt", [128, 256])
        nt4 = sb("nt4", [128, 4])
        ptr = ps("ptr", [128, 8, 128], BF16)

        r_ya = R("y_a_own")

        def ingest_tile(kb, ti, do_cmp, do_sel, do_win, kcT, vcT, first):
            c0 = ti * 128
            rd = [kb.r, identb.r]
            ing = OPTS.get("ing", 15)
            do_cmp = do_cmp and bool(ing & 1)
            do_sel = do_sel and bool(ing & 2)
            do_win = do_win and bool(ing & 4)
            if do_cmp:
                P.op("pe", lambda e: e.transpose(ptr[:, 0, :], kb[:, 0:128], identb[:]), reads=rd, writes=[ptr.r])
                P.op("pe", lambda e: e.transpose(ptr[:, 1, :], kb[:, 128:256], identb[:]), reads=rd, writes=[ptr.r])
            if do_sel:
                P.op("pe", lambda e: e.transpose(ptr[0:64, 2, :], kb[:, 256:320], identb[:]), reads=rd, writes=[ptr.r])
                P.op("pe", lambda e: e.transpose(ptr[0:64, 3, :], kb[:, 320:384], identb[:]), reads=rd, writes=[ptr.r])
            if do_win:
                P.op("pe", lambda e: e.transpose(ptr[0:64, 4, :], kb[:, 512:576], identb[:]), reads=rd, writes=[ptr.r])
                P.op("pe", lambda e: e.transpose(ptr[0:64, 5, :], kb[:, 576:640], identb[:]), reads=rd, writes=[ptr.r])
            if do_cmp:
                P.op("act", lambda e: e.copy(kcT[:, c0:c0 + 128], ptr[:, 0, :]), reads=[ptr.r], writes=[kcT.r])
                P.op("act", lambda e: e.copy(vcT[:, c0:c0 + 128], ptr[:, 1, :]), reads=[ptr.r], writes=[vcT.r])
            if do_sel:
                P.op("dve", lambda e: e.tensor_copy(ksT[0][0:64, c0:c0 + 128], ptr[0:64, 2, :]), reads=[ptr.r], writes=[ksT[0].r])
                P.op("dve", lambda e: e.tensor_copy(ksT[1][0:64, c0:c0 + 128], ptr[0:64, 3, :]), reads=[ptr.r], writes=[ksT[1].r])
                P.op("pool", lambda e: e.tensor_copy(vsw[:, ti, 0:2, 0:64], kb[:, 384:512].rearrange("p (g d) -> p g d", g=2)),
                     reads=[kb.r], writes=[vsw.r])
            if do_win:
                P.op("dve", lambda e: e.tensor_copy(kwT[0][0:64, c0:c0 + 128], ptr[0:64, 4, :]), reads=[ptr.r], writes=[kwT[0].r])
                P.op("dve", lambda e: e.tensor_copy(kwT[1][0:64, c0:c0 + 128], ptr[0:64, 5, :]), reads=[ptr.r], writes=[kwT[1].r])
                P.op("pool", lambda e: e.tensor_copy(vsw[:, ti, 2:4, 0:64], kb[:, 640:768].rearrange("p (g d) -> p g d", g=2)),
                     reads=[kb.r], writes=[vsw.r])
            if not (ing & 8):
                return
            kk = kb[:, 256:768].rearrange("p (a r) -> p a r", a=2)[:, :, 0:128]
            P.op("pool", lambda e: e.tensor_tensor(sqt[:].rearrange("p (a r) -> p a r", a=2), kk, kk, ALU.mult), reads=[kb.r], writes=[sqt.r])
            P.op("dve", lambda e: e.tensor_reduce(nt4[:], sqt[:].rearrange("p (a d) -> p a d", a=4), AX.X, ALU.add), reads=[sqt.r], writes=[nt4.r])
            if first:
                P.op("dve", lambda e: e.tensor_copy(nm[:, 0:4], nt4[:]), reads=[nt4.r], writes=[nm.r])
            else:
                TT("dve", nm[:, 0:4], nm[:, 0:4], nt4[:], ALU.max, [nm.r, nt4.r], [nm.r])

        def compress_all(kcT, vcT):
            with ExitStack() as st2:
                def sb2(name, shape, dt=F32):
                    return T(st2.enter_context(nc.sbuf_tensor("c2_" + name + "_%d" % P.n_inst, list(shape), dt)), name)

                def ps2(name, shape, dt=F32):
                    return PT(st2.enter_context(nc.psum_tensor("c2_" + name + "_%d" % P.n_inst, list(shape), dt)), name)
                w1c = sb2("w1c", [128, 2, 32, 256], BF16)
                for kv in range(2):
                    src = Dr["cmp_w1"][kv].rearrange("(j d) h -> d j h", d=64)
                    P.dma("pool", w1c[0:64, kv, :, :], src, writes=[w1c.r])
                    P.dma("pool", w1c[64:128, kv, :, :], src, writes=[w1c.r])
                w2c = sb2("w2c", [128, 2, 2, 64], BF16)
                for kv in range(2):
                    P.dma("pool", w2c[:, kv, :, :], Dr["cmp_w2"][kv].rearrange("(c p) d -> p c d", p=128), writes=[w2c.r])
                pef = sb2("pef", [32, 2, 64])
                P.dma("sp", pef[:], Dr["cmp_pe"].rearrange("k j d -> j k d"), writes=[pef.r])
                peT = sb2("peT", [64, 2, 32], BF16)
                b1c = sb2("b1c", [128, 2, 2])
                P.dma("sp", b1c[:], Dr["cmp_b1T"][:, :, :], writes=[b1c.r])
                b2k = sb2("b2k", [64, 1])
                P.dma("sp", b2k[:], Dr["cmp_b2T"][:, :], writes=[b2k.r])
                b2v = sb2("b2v", [128, 64])
                P.dma("sp", b2v[:], Dr["cmp_b2v"][:, :], writes=[b2v.r])
                cb = sb2("cb", [128, 2, 2])
                hx = sb2("hx", [128, 512])
                hu = sb2("hu", [128, 512])
                hT = sb2("hT", [128, 2, 512], BF16)
                kcf = sb2("kcf", [64, 512])
                P.op("pool", lambda e: e.memset(hT[:], 0.0), writes=[hT.r])
                pc = [ps2("pc%d" % i, [128, 512]) for i in range(2)]
                pk = ps2("pk", [128, 512])
                pcm = ps2("pcm", [128, 512])
                for kv in range(2):
                    P.op("pe", lambda e: e.transpose(pcm[0:64, kv * 32:(kv + 1) * 32], pef[:, kv, :], identf[0:32, 0:32]),
                         reads=[pef.r, identf.r], writes=[pcm.r])
                P.op("dve", lambda e: e.tensor_copy(peT[:].rearrange("p k j -> p (k j)"), pcm[0:64, 0:64]), reads=[pcm.r], writes=[peT.r])
                for kv in range(2):
                    for hc in range(2):
                        col = kv * 2 + hc
                        for j in range(32):
                            MMUL(pcm[:, 64 + col:65 + col], w1c[0:64, kv, j, hc * 128:(hc + 1) * 128], peT[:, kv, j:j + 1],
                                 [w1c.r, peT.r], [pcm.r], start=(j == 0), stop=(j == 31))
                TT("dve", cb[:].rearrange("p k c -> p (k c)"), pcm[:, 64:68], b1c[:].rearrange("p k c -> p (k c)"), ALU.add,
                   [pcm.r, b1c.r], [cb.r])
                for kv, srcT in ((0, kcT), (1, vcT)):
                    for g in range(2):
                        rows = slice(g * 64, g * 64 + 64)
                        for hc in range(2):
                            pp = pc[hc]
                            for j in range(32):
                                MMUL(pp[:, 0:511], w1c[rows, kv, j, hc * 128:(hc + 1) * 128],
                                     srcT[rows, j:j + 16 * 510 + 1:16], [w1c.r, srcT.r], [pp.r], start=(j == 0), stop=(j == 31))
                            P.op("act", lambda e: e.activation(hx[:, 0:511], pp[:, 0:511], AF.Identity, bias=cb[:, kv, hc:hc + 1]),
                                 reads=[pp.r, cb.r], writes=[hx.r])
                            TT("dve", hu[:, 0:511], hx[:, 0:511], hx[:, 0:511], ALU.mult, [hx.r], [hu.r])
                            P.op("dve", lambda e: e.tensor_scalar(hu[:, 0:511], hu[:, 0:511], 0.044715, 1.0, ALU.mult, ALU.add), reads=[hu.r], writes=[hu.r])
                            TT("dve", hu[:, 0:511], hu[:, 0:511], hx[:, 0:511], ALU.mult, [hu.r, hx.r], [hu.r])
                            P.op("act", lambda e: e.activation(hu[:, 0:511], hu[:, 0:511], AF.Sigmoid, scale=1.5957691216057308), reads=[hu.r], writes=[hu.r])
                            TT("dve", hT[:, hc, 0:511], hx[:, 0:511], hu[:, 0:511], ALU.mult, [hx.r, hu.r], [hT.r])
                        if kv == 0:
                            for hc in range(2):
                                MMUL(pk[0:64, 0:511], w2c[:, 0, hc, :], hT[:, hc, 0:511], [w2c.r, hT.r], [pk.r], start=(hc == 0), stop=(hc == 1))
                            P.op("act", lambda e: e.activation(kcf[:, 0:511], pk[0:64, 0:511], AF.Identity, bias=b2k[:, 0:1]),
                                 reads=[pk.r, b2k.r], writes=[kcf.r])
                            P.op("pool", lambda e: e.memset(kcf[:, 511:512], 0.0), writes=[kcf.r])
                            P.op("dve", lambda e: e.tensor_copy(kcmpT[g][0:64, :], kcf[:, :]), reads=[kcf.r], writes=[kcmpT[g].r])
                            TT("dve", kcf[:, :], kcf[:, :], kcf[:, :], ALU.mult, [kcf.r], [kcf.r])
                            for bt in range(4):
                                MMUL(pcm[:, 80 + g * 4 + bt:81 + g * 4 + bt], kcf[:, bt * 128:(bt + 1) * 128], onesf[0:64, 0:1],
                                     [kcf.r, onesf.r], [pcm.r])
                            P.op("dve", lambda e: e.tensor_copy(nm[:, 4 + g * 4:8 + g * 4], pcm[:, 80 + g * 4:84 + g * 4]), reads=[pcm.r], writes=[nm.r])
                        else:
                            for bt in range(4):
                                nb = 128
                                for hc in range(2):
                                    MMUL(pk[0:nb, bt * 64:(bt + 1) * 64], hT[:, hc, bt * 128:bt * 128 + nb], w2c[:, 1, hc, :],
                                         [hT.r, w2c.r], [pk.r], start=(hc == 0), stop=(hc == 1))
                                TT("dve", vcmp[0:nb, bt, g, 0:64], pk[0:nb, bt * 64:(bt + 1) * 64], b2v[0:nb, :], ALU.add, [pk.r, b2v.r], [vcmp.r])
                P.op("pe", lambda e: e.transpose(pcm[0:12, 128:256], nm[:, 0:12], identf[:]), reads=[nm.r, identf.r], writes=[pcm.r])
                P.op("dve", lambda e: e.tensor_reduce(hx[0:12, 0:1], pcm[0:12, 128:256], AX.X, ALU.max), reads=[pcm.r], writes=[hx.r])
                P.op("pe", lambda e: e.transpose(pcm[0:1, 256:268], hx[0:12, 0:1], identf[0:12, 0:12]), reads=[hx.r, identf.r], writes=[pcm.r])
                P.op("dve", lambda e: e.tensor_reduce(hx[0:1, 1:2], pcm[0:1, 256:268], AX.X, ALU.max), reads=[pcm.r], writes=[hx.r])
                P.op("act", lambda e: e.activation(hx[0:1, 2:3], hx[0:1, 1:2], AF.Sqrt), reads=[hx.r], writes=[hx.r])
                MMUL(pcm[:, 300:301], onesf[0:1, :], hx[0:1, 2:3], [onesf.r, hx.r], [pcm.r])
                P.op("dve", lambda e: e.tensor_copy(kmaxb[:], pcm[:, 300:301]), reads=[pcm.r], writes=[kmaxb.r])
                P.barrier()

        def attention_scope(run):
            with ExitStack() as st3:
                def sb3(name, shape, dt=F32):
                    return T(st3.enter_context(nc.sbuf_tensor("c3_" + name + "_%d" % P.n_inst, list(shape), dt)), name)

                def ps3(name, shape, dt=F32):
                    return PT(st3.enter_context(nc.psum_tensor("c3_" + name + "_%d" % P.n_inst, list(shape), dt)), name)
                A = {}
                A["G"] = sb3("G", [128, 8192], BF16)
                P.dma("pool", A["G"][:], Dr["Gtab"][:, :], writes=[A["G"].r])
                A["cover"] = sb3("cover", [128, 4, 128], BF16)
                P.dma("pool", A["cover"][:], Dr["cover"][:, :, :], writes=[A["cover"].r])
                A["wq"] = sb3("wq", [128, 8, 536], BF16)
                for kc in range(8):
                    P.dma("pool", A["wq"][:, kc, 0:512], Dr["w_in"][kc * 128:(kc + 1) * 128, QG0:QG0 + 512], writes=[A["wq"].r])
                    P.dma("pool", A["wq"][:, kc, 512:536], Dr["w_in"][kc * 128:(kc + 1) * 128, GATE0:GATE0 + 24], writes=[A["wq"].r])
                A["Ftab"] = sb3("Ftab", [128, 128])
                A["cb2"] = sb3("cb2", [128, 2, 128], BF16)
                A["triS"] = sb3("triS", [128, 4, 512], BF16)
                A["triW"] = sb3("triW", [128, 8, 512], BF16)
                A["xb"] = sb3("xb", [128, 1024], BF16)
                A["xT"] = sb3("xT", [128, 8, 128], BF16)
                A["qf"] = sb3("qf", [128, 512])
                A["rt"] = sb3("rt", [128, 4, 8, 8])
                A["rope"] = sb3("rope", [128, 16])
                A["gts"] = sb3("gts", [128, 24])
                A["qn"] = sb3("qn", [128, 8])
                A["qa"] = sb3("qa", [128, 8, 65], BF16)
                A["qT"] = sb3("qT", [65, 8, 128], BF16)
                A["pT"] = [sb3("pT%d" % i, [128, 512], BF16) for i in range(2)]
                A["ov"] = sb3("ov", [128, 4, 65])
                A["rl"] = sb3("rl", [128, 4])
                A["cf"] = sb3("cf", [128, 4])
                A["imp"] = sb3("imp", [128, 128])
                A["sc"] = sb3("sc", [128, 128])
                A["sc2"] = sb3("sc2", [128, 128])
                A["m8a"] = sb3("m8a", [128, 8])
                A["m8b"] = sb3("m8b", [128, 8])
                A["mb4"] = sb3("mb4", [128, 4, 128], BF16)
                A["ya"] = sb3("ya", [128, 8, 64])
                A["yab"] = sb3("yab", [128, 512], BF16)
                A["tmp"] = sb3("tmp", [128, 4, 64])
                A["sT"] = [ps3("sT%d" % i, [128, 512]) for i in range(2)]
                A["po"] = [ps3("po%d" % i, [128, 512]) for i in range(3)]
                A["ir"] = ps3("ir", [128, 512])
                A["pq"] = ps3("pq", [128, 512])
                A["cnt"] = {"sT": 0, "po": 0, "pT": 0}
                run(A)
                P.barrier()

        def q_prepare(A, n, load_x, load_q, rope_src):
            qf, gts, qa, qT, pq = A["qf"], A["gts"], A["qa"], A["qT"], A["pq"]
            if n < 128:
                P.op("pool", lambda e: e.memset(qa[:], 0.0), writes=[qa.r])
            if load_x is not None:
                load_x(A["xb"])
                for kc in range(8):
                    P.op("pe", lambda e: e.transpose(ptr[:, kc, :], A["xb"][:, kc * 128:(kc + 1) * 128], identb[:]),
                         reads=[A["xb"].r, identb.r], writes=[ptr.r])
                P.op("act", lambda e: e.copy(A["xT"][:], ptr[:]), reads=[ptr.r], writes=[A["xT"].r])
                for kc in range(8):
                    MMUL(pq[:, :], A["xT"][:, kc, :], A["wq"][:, kc, 0:512], [A["xT"].r, A["wq"].r], [pq.r], start=(kc == 0), stop=(kc == 7))
                P.op("act", lambda e: e.activation(qf[:], pq[:], AF.Copy, scale=0.125), reads=[pq.r], writes=[qf.r])
                for kc in range(8):
                    MMUL(pq[:, 0:24], A["xT"][:, kc, :], A["wq"][:, kc, 512:536], [A["xT"].r, A["wq"].r], [pq.r], start=(kc == 0), stop=(kc == 7))
                P.op("act", lambda e: e.activation(gts[:], pq[:, 0:24], AF.Sigmoid), reads=[pq.r], writes=[gts.r])
            else:
                load_q(qf, gts)
                P.op("act", lambda e: e.activation(qf[0:n, :], qf[0:n, :], AF.Copy, scale=0.125), reads=[qf.r], writes=[qf.r])
                P.op("act", lambda e: e.activation(gts[0:n, :], gts[0:n, :], AF.Sigmoid), reads=[gts.r], writes=[gts.r])
            P.dma("sp", A["rope"][0:n, :], rope_src, writes=[A["rope"].r])
            q3 = qf[0:n, :].rearrange("p (h d) -> p h d", h=8)
            x1 = q3[:, :, 0:8]
            x2 = q3[:, :, 8:16]
            cos = A["rope"][0:n, 0:8].unsqueeze(1).to_broadcast([n, 8, 8])
            sin = A["rope"][0:n, 8:16].unsqueeze(1).to_broadcast([n, 8, 8])
            rt = A["rt"]
            rd = [qf.r, rt.r, A["rope"].r]
            TT("dve", rt[0:n, 0], x1, cos, ALU.mult, rd, [rt.r])
            TT("dve", rt[0:n, 1], x2, sin, ALU.mult, rd, [rt.r])
            TT("dve", rt[0:n, 2], x2, cos, ALU.mult, rd, [rt.r])
            TT("dve", rt[0:n, 3], x1, sin, ALU.mult, rd, [rt.r])
            TT("dve", x1, rt[0:n, 0], rt[0:n, 1], ALU.subtract, rd, [qf.r])
            TT("dve", x2, rt[0:n, 2], rt[0:n, 3], ALU.add, rd, [qf.r])
            TT("pool", A["ya"][0:n].rearrange("p h d -> p (h d)"), qf[0:n, :], qf[0:n, :], ALU.mult, [qf.r], [A["ya"].r])
            P.op("dve", lambda e: e.tensor_reduce(A["qn"][0:n, :], A["ya"][0:n], AX.X, ALU.add), reads=[A["ya"].r], writes=[A["qn"].r])
            P.op("act", lambda e: e.activation(A["qn"][0:n, :], A["qn"][0:n, :], AF.Sqrt), reads=[A["qn"].r], writes=[A["qn"].r])
            P.op("dve", lambda e: e.tensor_scalar(A["qn"][0:n, :], A["qn"][0:n, :], kmaxb[0:n, 0:1], -1.0, ALU.mult, ALU.mult),
                 reads=[A["qn"].r, kmaxb.r], writes=[A["qn"].r])
            P.op("dve", lambda e: e.tensor_copy(qa[0:n, :, 0:64], q3), reads=[qf.r], writes=[qa.r])
            P.op("dve", lambda e: e.tensor_copy(qa[0:n, :, 64:65], A["qn"][0:n, :].unsqueeze(2)), reads=[A["qn"].r], writes=[qa.r])
            for h in range(8):
                P.op("pe", lambda e: e.transpose(ptr[0:65, h, :], qa[:, h, :], identb[:]), reads=[qa.r, identb.r], writes=[ptr.r])
            P.op("act", lambda e: e.copy(qT[:], ptr[0:65, :, :]), reads=[ptr.r], writes=[qT.r])

        def nsa_qtile(A, n, cfg):
            qT, gts, ya = A["qT"], A["gts"], A["ya"]
            cnt = A["cnt"]

            def next_sT():
                t = A["sT"][cnt["sT"] % 2]
                cnt["sT"] += 1
                return t

            def next_pT():
                t = A["pT"][cnt["pT"] % 2]
                cnt["pT"] += 1
                return t

            def next_po():
                t = A["po"][cnt["po"] % 3]
                cnt["po"] += 1
                return t

            def finish_branch(po_t, g, br, first):
                ov, rl, cf = A["ov"], A["rl"], A["cf"]
                P.op("act", lambda e: e.copy(ov[:].rearrange("p h d -> p (h d)"), po_t[:, 0:260]), reads=[po_t.r], writes=[ov.r])
                P.op("dve", lambda e: e.tensor_scalar(rl[:], ov[:, :, 64], 1e-30, None, ALU.max), reads=[ov.r], writes=[rl.r])
                P.op("dve", lambda e: e.reciprocal(rl[:], rl[:]), reads=[rl.r], writes=[rl.r])
                g3 = gts[:, :].rearrange("p (h b) -> p h b", b=3)
                TT("dve", cf[:], rl[:], g3[:, 4 * g:4 * g + 4, br], ALU.mult, [rl.r, gts.r], [cf.r])
                dst = ya[:, 4 * g:4 * g + 4, :]
                cfb = cf[:].unsqueeze(2).to_broadcast([128, 4, 64])
                if first:
                    TT("dve", dst, ov[:, :, 0:64], cfb, ALU.mult, [ov.r, cf.r], [ya.r])
                else:
                    TT("dve", A["tmp"][:], ov[:, :, 0:64], cfb, ALU.mult, [ov.r, cf.r], [A["tmp"].r])
                    TT("dve", dst, dst, A["tmp"][:], ALU.add, [ya.r, A["tmp"].r], [ya.r])

            for g in range(2):
                qTg = qT[0:65, 4 * g:4 * g + 4, :].rearrange("p h q -> p (h q)")
                po_t = next_po()
                ir = A["ir"]
                nbt = cfg["nbt"]
                for bt in range(nbt):
                    sT = next_sT()
                    slots = [s for (b_, s) in cfg["cb_tiles"] if b_ == bt]
                    MMUL(sT[:, :], kcmpT[g][0:65, bt * 128:(bt + 1) * 128], qTg, [kcmpT[g].r, qT.r], [sT.r], start=True, stop=(not slots))
                    for s in slots:
                        for h4 in range(4):
                            MMUL(sT[:, h4 * 128:(h4 + 1) * 128], identb[:], A["cb2"][:, s, :], [identb.r, A["cb2"].r], [sT.r],
                                 start=False, stop=(h4 == 3))
                    pT = next_pT()
                    P.op("act", lambda e: e.activation(pT[:], sT[:], AF.Exp), reads=[sT.r], writes=[pT.r])
                    for h4 in range(4):
                        MMUL(po_t[:, h4 * 65:(h4 + 1) * 65], pT[:, h4 * 128:(h4 + 1) * 128], vcmp[:, bt, g, :], [pT.r, vcmp.r], [po_t.r],
                             start=(bt == 0 and h4 == 0), stop=(bt == nbt - 1))
                        MMUL(ir[:, h4 * 128:(h4 + 1) * 128], pT[:, h4 * 128:(h4 + 1) * 128], A["cover"][:, bt, :], [pT.r, A["cover"].r], [ir.r],
                             start=(bt == 0 and h4 == 0), stop=(bt == nbt - 1))
                finish_branch(po_t, g, 0, True)
                rl = A["rl"]
                imp = A["imp"]
                P.op("dve", lambda e: e.tensor_scalar(imp[:], ir[:, 0:128], rl[:, 0:1], None, ALU.mult), reads=[ir.r, rl.r], writes=[imp.r])
                for h4 in range(1, 4):
                    P.op("dve", lambda e: e.scalar_tensor_tensor(imp[:], ir[:, h4 * 128:(h4 + 1) * 128], rl[:, h4:h4 + 1], imp[:], ALU.mult, ALU.add),
                         reads=[ir.r, rl.r, imp.r], writes=[imp.r])
                sc, sc2, m8a, m8b = A["sc"], A["sc2"], A["m8a"], A["m8b"]
                TT("dve", sc[:], imp[:], A["Ftab"][:], ALU.add, [imp.r, A["Ftab"].r], [sc.r])
                P.op("dve", lambda e: e.max(out=m8a[:], in_=sc[:]), reads=[sc.r], writes=[m8a.r])
                P.op("dve", lambda e: e.match_replace(out=sc2[:], in_to_replace=m8a[:], in_values=sc[:], imm_value=-3.0e38),
                     reads=[sc.r, m8a.r], writes=[sc2.r])
                P.op("dve", lambda e: e.max(out=m8b[:], in_=sc2[:]), reads=[sc2.r], writes=[m8b.r])
                tc_ = cfg["topk_col"]
                P.op("dve", lambda e: e.tensor_scalar(sc2[:], sc[:], m8b[:, tc_:tc_ + 1], None, ALU.is_ge), reads=[sc.r, m8b.r], writes=[sc2.r])
                P.op("dve", lambda e: e.tensor_scalar(sc[:], sc[:], -1.0e29, None, ALU.is_gt), reads=[sc.r], writes=[sc.r])
                TT("dve", sc[:], sc[:], sc2[:], ALU.mult, [sc.r, sc2.r], [sc.r])
                P.op("dve", lambda e: e.tensor_scalar(sc[:], sc[:], -NEGB, NEGB, ALU.mult, ALU.add), reads=[sc.r], writes=[sc.r])
                pq = A["pq"]
                P.op("pe", lambda e: e.transpose(pq[:, 0:128], sc[:], identf[:]), reads=[sc.r, identf.r], writes=[pq.r])
                mb4 = A["mb4"]
                P.op("dve", lambda e: e.tensor_copy(mb4[:], pq[:, 0:128].unsqueeze(1).to_broadcast([128, 4, 128])), reads=[pq.r], writes=[mb4.r])
                po_t = next_po()
                tiles = cfg["sel_tiles"]
                for ii, (c, use_G, tri) in enumerate(tiles):
                    sT = next_sT()
                    last = (not use_G) and (tri is None)
                    MMUL(sT[:, :], ksT[g][0:65, c * 128:(c + 1) * 128], qTg, [ksT[g].r, qT.r], [sT.r], start=True, stop=last)
                    if use_G:
                        MMUL(sT[:, :], A["G"][:, c * 128:(c + 1) * 128], mb4[:].rearrange("p h q -> p (h q)"), [A["G"].r, mb4.r], [sT.r],
                             start=False, stop=(tri is None))
                    if tri is not None:
                        MMUL(sT[:, :], identb[:], A["triS"][:, tri, :], [identb.r, A["triS"].r], [sT.r], start=False, stop=True)
                    pT = next_pT()
                    P.op("act", lambda e: e.activation(pT[:], sT[:], AF.Exp), reads=[sT.r], writes=[pT.r])
                    for h4 in range(4):
                        MMUL(po_t[:, h4 * 65:(h4 + 1) * 65], pT[:, h4 * 128:(h4 + 1) * 128], vsw[:, c, g, :], [pT.r, vsw.r], [po_t.r],
                             start=(ii == 0 and h4 == 0), stop=(ii == len(tiles) - 1))
                finish_branch(po_t, g, 1, False)
                po_t = next_po()
                tiles = cfg["win_tiles"]
                for ii, (c, slot) in enumerate(tiles):
                    sT = next_sT()
                    MMUL(sT[:, :], kwT[g][0:65, c * 128:(c + 1) * 128], qTg, [kwT[g].r, qT.r], [sT.r], start=True, stop=False)
                    MMUL(sT[:, :], identb[:], A["triW"][:, slot, :], [identb.r, A["triW"].r], [sT.r], start=False, stop=True)
                    pT = next_pT()
                    P.op("act", lambda e: e.activation(pT[:], sT[:], AF.Exp), reads=[sT.r], writes=[pT.r])
                    for h4 in range(4):
                        MMUL(po_t[:, h4 * 65:(h4 + 1) * 65], pT[:, h4 * 128:(h4 + 1) * 128], vsw[:, c, 2 + g, :], [pT.r, vsw.r], [po_t.r],
                             start=(ii == 0 and h4 == 0), stop=(ii == len(tiles) - 1))
                finish_branch(po_t, g, 2, False)
            P.op("act", lambda e: e.copy(A["yab"][:], ya[:].rearrange("p h d -> p (h d)")), reads=[ya.r], writes=[A["yab"].r])

        def bail():
            Dr["r_ya"] = r_ya
            P.barrier()
        if OPTS.get("cstop", 9) <= 1:
            return bail()
        with ExitStack() as stp:
            kcT = T(stp.enter_context(nc.sbuf_tensor("c_kcT_p", [128, SEQ], BF16)), "kcT")
            vcT = T(stp.enter_context(nc.sbuf_tensor("c_vcT_p", [128, SEQ], BF16)), "vcT")
            for ti in range(NT):
                kb = kvb[ti % 2]
                P.dma("pool", kb[:], Dr["p_full"][ti * 128:(ti + 1) * 128, R_COLS:NA], reads=[Dr["r_pfull"]], writes=[kb.r])
                ingest_tile(kb, ti, True, True, True, kcT, vcT, ti == 0)
            if OPTS.get("cstop", 9) > 2:
                compress_all(kcT, vcT)
        if OPTS.get("cstop", 9) <= 3:
            return bail()

        def run_prompt(A):
            for j in range(OPTS["cq"]):
                P.dma("sp", A["Ftab"][:], Dr["Ftab"][:, j, :], writes=[A["Ftab"].r])
                P.dma("pool", A["cb2"][:], Dr["cbias"][:, j, :, :], writes=[A["cb2"].r])
                if j == 0:
                    P.dma("pool", A["triS"][:], Dr["triS"][:, :, :], writes=[A["triS"].r])
                    P.dma("pool", A["triW"][:], Dr["triW"][:, :, :], writes=[A["triW"].r])

                def load_x(xb, j=j):
                    P.dma("pool", xb[:], Dr["x_own"][j * 128:(j + 1) * 128, :], writes=[xb.r])
                q_prepare(A, 128, load_x, None, Dr["rope_own"][j * 128:(j + 1) * 128, :])
                nbt = (32 * j + 32 + 127) // 128
                cb_tiles = [(nbt - 1, 1)] + ([(nbt - 2, 0)] if nbt >= 2 else [])
                cfg = {"nbt": nbt, "cb_tiles": cb_tiles, "topk_col": 7,
                       "sel_tiles": [(c, True, (c - 4 * j) if c >= 4 * j else None) for c in range(4 * j + 4)],
                       "win_tiles": [(c, c - (4 * j - 4)) for c in range(max(4 * j - 4, 0), 4 * j + 4)]}
                nsa_qtile(A, 128, cfg)
                P.dma("sp", Dr["y_a_own"][j * 128:(j + 1) * 128, :], A["yab"][:], reads=[A["yab"].r], writes=[r_ya])
        attention_scope(run_prompt)

        r_gath = R("gath")
        with ExitStack() as stg_:
            stage = T(stg_.enter_context(nc.sbuf_tensor("c_stage", [64, 8192], F32)), "stage")
            pidx = T(stg_.enter_context(nc.sbuf_tensor("c_pidx", [64, SB], I32)), "pidx")
            pidf = T(stg_.enter_context(nc.sbuf_tensor("c_pidf", [64, SB], F32)), "pidf")
            idx4f = T(stg_.enter_context(nc.sbuf_tensor("c_idx4f", [64, SB, 4], F32)), "idx4f")
            idx4 = T(stg_.enter_context(nc.sbuf_tensor("c_idx4", [64, SB, 4], I32)), "idx4")
            P.op("pool", lambda e: e.memset(pidx[:], 0), writes=[pidx.r])
            for bb in range(OPTS["sb"]):
                P.dma("sp", pidx[:, bb:bb + 1], Dr["pt_col"][bb, :, :], writes=[pidx.r])
            P.op("dve", lambda e: e.tensor_copy(pidf[:], pidx[:]), reads=[pidx.r], writes=[pidf.r])
            for ch in range(4):
                P.op("dve", lambda e: e.tensor_scalar(idx4f[:, :, ch], pidf[:], 4.0, float(ch), ALU.mult, ALU.add), reads=[pidf.r], writes=[idx4f.r])
            P.op("dve", lambda e: e.tensor_copy(idx4[:], idx4f[:]), reads=[idx4f.r], writes=[idx4.r])
            for bb in range(OPTS["sb"]):
                for ci, cache in enumerate((Dr["cache_cmp_pg"], Dr["cache_sel_pg"])):
                    for ch in range(4):
                        P._need("pool", P._deps([idx4.r], [stage.r]))
                        ins = nc.gpsimd.indirect_dma_start(out=stage[:, :], out_offset=None, in_=cache[:, :],
                                                           in_offset=bass.IndirectOffsetOnAxis(ap=idx4[:, bb, ch:ch + 1], axis=0),
                                                           bounds_check=2560 * 4 - 1, oob_is_err=False)
                        pool_, idx_ = P.dq["pool"]
                        key = pool_[idx_ % len(pool_)]
                        P.dq["pool"][1] = idx_ + 1
                        if P.cnt[key] > 0:
                            P._need("pool", [(key, P.cnt[key])])
                        P.cnt[key] += 16
                        ins.then_inc(P.sems[key], 16)
                        P._commit((key, P.cnt[key]), [idx4.r], [stage.r])
                        P.n_inst += 1
                        P.dma("sp", Dr["gath"][bb, ci, :, ch * 8192:(ch + 1) * 8192], stage[:, :], reads=[stage.r], writes=[r_gath])
            P.barrier()
        for bb in range(OPTS["sb"]):
            with ExitStack() as stp:
                kcT = T(stp.enter_context(nc.sbuf_tensor("c_kcT_s%d" % bb, [128, SEQ], BF16)), "kcT")
                vcT = T(stp.enter_context(nc.sbuf_tensor("c_vcT_s%d" % bb, [128, SEQ], BF16)), "vcT")
                for ti in range(64):
                    kb = kvb[ti % 2]
                    P.dma("pool", kb[:, 0:256], Dr["gath"][bb, 0, ti, :].rearrange("(p c) -> p c", p=128), reads=[r_gath], writes=[kb.r])
                    P.dma("pool", kb[:, 256:512], Dr["gath"][bb, 1, ti, :].rearrange("(p c) -> p c", p=128), reads=[r_gath], writes=[kb.r])
                    ingest_tile(kb, ti, True, True, False, kcT, vcT, ti == 0)
                kb = kvb[0]
                P.op("pool", lambda e: e.memset(kb[:], 0.0), writes=[kb.r])
                P.dma("pool", kb[0:DS, 256:512], Dr["p_samp"][bb * DS:(bb + 1) * DS, KV0 + 256:KV0 + 512], reads=[Dr["r_psamp"]], writes=[kb.r])
                ingest_tile(kb, 64, False, True, False, kcT, vcT, False)
                for c in range(5):
                    kb = kvb[(c + 1) % 2]
                    if c < 4:
                        P.dma("pool", kb[:, 512:768], Dr["cache_win"][bb, c * 128:(c + 1) * 128, :], writes=[kb.r])
                    else:
                        P.op("pool", lambda e: e.memset(kb[:], 0.0), writes=[kb.r])
                        P.dma("pool", kb[0:DS, 512:768], Dr["p_samp"][bb * DS:(bb + 1) * DS, KV0 + 512:KV0 + 768], reads=[Dr["r_psamp"]], writes=[kb.r])
                    ingest_tile(kb, c, False, False, True, kcT, vcT, False)
                compress_all(kcT, vcT)

            def run_sample(A, bb=bb):
                P.dma("sp", A["Ftab"][:], Dr["Ftab"][:, 16, :], writes=[A["Ftab"].r])
                P.dma("pool", A["cb2"][:], Dr["cbias"][:, 16, :, :], writes=[A["cb2"].r])
                P.dma("pool", A["triS"][:, 0, :], Dr["triS_s"][:, :], writes=[A["triS"].r])
                P.dma("pool", A["triW"][:, 0:5, :], Dr["triW_s"][:, :, :], writes=[A["triW"].r])

                def load_q(qf, gts):
                    P.dma("sp", qf[0:DS, :], Dr["p_samp"][bb * DS:(bb + 1) * DS, QG0:QG0 + 512], reads=[Dr["r_psamp"]], writes=[qf.r])
                    P.dma("sp", gts[0:DS, :], Dr["p_samp"][bb * DS:(bb + 1) * DS, GATE0:GATE0 + 24], reads=[Dr["r_psamp"]], writes=[gts.r])
                P.op("pool", lambda e: e.memset(A["gts"][:], 0.0), writes=[A["gts"].r])
                q_prepare(A, DS, None, load_q, Dr["rope_s"][0:DS, :])
                cfg = {"nbt": 4, "cb_tiles": [(3, 1), (2, 0)], "topk_col": 6,
                       "sel_tiles": [(c, True, None) for c in range(64)] + [(64, False, 0)],
                       "win_tiles": [(c, c) for c in range(5)]}
                nsa_qtile(A, DS, cfg)
                P.dma("sp", Dr["y_a_own"][2048 + bb * DS:2048 + (bb + 1) * DS, :], A["yab"][0:DS, :], reads=[A["yab"].r], writes=[r_ya])
            attention_scope(run_sample)
        Dr["r_ya"] = r_ya
        P.barrier()

NTOK = 2048 + NS
NTL = 17
MG0 = 3096
DN_ALPHA = 2.0 ** 0.25
NVD = 4 * 1024 + 32


def _tile_rows(u):
    return NS if u == NTL - 1 else 128


def phase_D(nc, P, Dr, outs):
    with ExitStack() as st:
        def sb(name, shape, dt=F32):
            return T(st.enter_context(nc.sbuf_tensor("d_" + name, list(shape), dt)), name)

        def ps(name, shape, dt=F32):
            return PT(st.enter_context(nc.psum_tensor("d_" + name, list(shape), dt)), name)

        w_in = Dr["w_in"]
        identb = sb("identb", [128, 128], BF16)
        P.dma("pool", identb[:], Dr["masks"][:, 896:1024], writes=[identb.r])
        identf = sb("identf", [128, 128])
        P.dma("sp", identf[:], Dr["masks"][:, 896:1024], writes=[identf.r])
        vd = sb("vd", [128, NVD])
        P.dma("sp", vd[:], Dr["vecsD"][:, :], writes=[vd.r])
        sel4 = sb("sel4", [128, 4])
        P.dma("sp", sel4[:], Dr["sel4"][:, :], writes=[sel4.r])
        wmg = sb("wmg", [128, 8, 2048], BF16)
        wo = sb("wo", [128, 8, 1024], BF16)
        for kc in range(8):
            P.dma("pool", wmg[:, kc, :], w_in[kc * 128:(kc + 1) * 128, MG0:MG0 + 2048], writes=[wmg.r])
            P.dma("pool", wo[:, kc, :], Dr["w_o"][kc * 128:(kc + 1) * 128, :], writes=[wo.r])
        wpa = sb("wpa", [128, 4, 1024], BF16)
        wpb = sb("wpb", [128, 4, 1024], BF16)
        for kc in range(4):
            P.dma("pool", wpa[:, kc, :], Dr["w_pa"][kc * 128:(kc + 1) * 128, :], writes=[wpa.r])
            P.dma("pool", wpb[:, kc, :], Dr["w_pb"][kc * 128:(kc + 1) * 128, :], writes=[wpb.r])
        rw = sb("rw", [128, 8, 32])
        P.dma("sp", rw[:], Dr["router_w"].rearrange("(k p) e -> p k e", p=128), writes=[rw.r])

        xf = sb("xf", [128, 1024])
        xb = sb("xb", [128, 1024], BF16)
        xT = sb("xT", [128, 8, 128], BF16)
        sg = sb("sg", [128, 2048])
        yr4 = sb("yr4", [128, 4, 512], BF16)
        yr = sb("yr", [128, 512], BF16)
        ya = sb("ya", [128, 512], BF16)
        yT = sb("yT", [128, 8, 128], BF16)
        mm = sb("mm", [128, 1024])
        mb = sb("mb", [128, 1024], BF16)
        mT = sb("mT", [128, 8, 128], BF16)
        hp_ = sb("hpre", [128, 1024])
        hh = sb("hh", [128, 1024])
        hT = sb("hTf", [128, 8, 128])
        s1 = sb("s1", [128, 1])
        s2 = sb("s2", [128, 1])
        lg = sb("lg", [128, 32])
        m8 = sb("m8", [128, 8])
        msk = sb("msk", [128, 32])
        gt = sb("gt", [128, 32])
        ptr = ps("ptr", [128, 8, 128], BF16)
        ptf = [ps("ptf%d" % i, [128, 512]) for i in range(2)]
        pm = [ps("pm%d" % i, [128, 512]) for i in range(4)]
        pmi = [0]

        def nextpm():
            t = pm[pmi[0] % 4]
            pmi[0] += 1
            return t

        r_h = R("h_own")
        r_g = R("gates_own")
        for u in range(NTL):
            n = _tile_rows(u)
            if u < 16:
                P.dma("sp", xf[:], Dr["x_own"][u * 128:(u + 1) * 128, :], writes=[xf.r])
                P.dma("sp", yr4[:], Dr["y_r"][u * 512:(u + 1) * 512, :].rearrange("(k p) c -> p k c", p=128),
                      reads=[Dr["r_yr"]], writes=[yr4.r])
                P.dma("sp", ya[:], Dr["y_a_own"][u * 128:(u + 1) * 128, :], reads=[Dr["r_ya"]], writes=[ya.r])
                P.op("dve", lambda e: e.tensor_scalar(yr[:], yr4[:, 0, :], sel4[:, 0:1], None, ALU.mult), reads=[yr4.r, sel4.r], writes=[yr.r])
                for k in range(1, 4):
                    P.op("dve", lambda e: e.scalar_tensor_tensor(yr[:], yr4[:, k, :], sel4[:, k:k + 1], yr[:], ALU.mult, ALU.add),
                         reads=[yr4.r, sel4.r, yr.r], writes=[yr.r])
            else:
                P.dma("sp", xf[0:n, :], Dr["x_s"][:, :], writes=[xf.r])
                P.dma("sp", yr[0:n, :], Dr["y_r_s"][:, :], reads=[Dr["r_yr"]], writes=[yr.r])
                P.dma("sp", ya[0:n, :], Dr["y_a_own"][2048:2048 + n, :], reads=[Dr["r_ya"]], writes=[ya.r])
            P.op("act", lambda e: e.copy(xb[0:n, :], xf[0:n, :]), reads=[xf.r], writes=[xb.r])
            for kc in range(8):
                P.op("pe", lambda e: e.transpose(ptr[:, kc, 0:n], xb[0:n, kc * 128:(kc + 1) * 128], identb[0:n, 0:n]),
                     reads=[xb.r, identb.r], writes=[ptr.r])
            P.op("act", lambda e: e.copy(xT[:, :, 0:n], ptr[:, :, 0:n]), reads=[ptr.r], writes=[xT.r])
            for nch in range(4):
                p_ = nextpm()
                for kc in range(8):
                    P.op("pe", lambda e: e.matmul(p_[0:n, :], xT[:, kc, 0:n], wmg[:, kc, nch * 512:(nch + 1) * 512],
                                                  start=(kc == 0), stop=(kc == 7)), reads=[xT.r, wmg.r], writes=[p_.r])
                P.op("act", lambda e: e.activation(sg[0:n, nch * 512:(nch + 1) * 512], p_[0:n, :], AF.Sigmoid), reads=[p_.r], writes=[sg.r])
            for kc in range(4):
                P.op("pe", lambda e: e.transpose(ptr[:, kc, 0:n], yr[0:n, kc * 128:(kc + 1) * 128], identb[0:n, 0:n]),
                     reads=[yr.r, identb.r], writes=[ptr.r])
                P.op("pe", lambda e: e.transpose(ptr[:, 4 + kc, 0:n], ya[0:n, kc * 128:(kc + 1) * 128], identb[0:n, 0:n]),
                     reads=[ya.r, identb.r], writes=[ptr.r])
            P.op("act", lambda e: e.copy(yT[:, :, 0:n], ptr[:, :, 0:n]), reads=[ptr.r], writes=[yT.r])
            for nch in range(2):
                pa = nextpm()
                pb = nextpm()
                for kc in range(4):
                    P.op("pe", lambda e: e.matmul(pa[0:n, :], yT[:, kc, 0:n], wpa[:, kc, nch * 512:(nch + 1) * 512],
                                                  start=(kc == 0), stop=(kc == 3)), reads=[yT.r, wpa.r], writes=[pa.r])
                for kc in range(4):
                    P.op("pe", lambda e: e.matmul(pb[0:n, :], yT[:, 4 + kc, 0:n], wpb[:, kc, nch * 512:(nch + 1) * 512],
                                                  start=(kc == 0), stop=(kc == 3)), reads=[yT.r, wpb.r], writes=[pb.r])
                cs = slice(nch * 512, (nch + 1) * 512)
                P.op("dve", lambda e: e.tensor_tensor(mm[0:n, cs], pa[0:n, :], sg[0:n, nch * 512:(nch + 1) * 512], ALU.mult),
                     reads=[pa.r, sg.r], writes=[mm.r])
                P.op("dve", lambda e: e.tensor_tensor(hp_[0:n, cs], pb[0:n, :], sg[0:n, 1024 + nch * 512:1024 + (nch + 1) * 512], ALU.mult),
                     reads=[pb.r, sg.r], writes=[hp_.r])
                P.op("dve", lambda e: e.tensor_tensor(mb[0:n, cs], mm[0:n, cs], hp_[0:n, cs], ALU.add),
                     reads=[mm.r, hp_.r], writes=[mb.r])
            for kc in range(8):
                P.op("pe", lambda e: e.transpose(ptr[:, kc, 0:n], mb[0:n, kc * 128:(kc + 1) * 128], identb[0:n, 0:n]),
                     reads=[mb.r, identb.r], writes=[ptr.r])
            P.op("act", lambda e: e.copy(mT[:, :, 0:n], ptr[:, :, 0:n]), reads=[ptr.r], writes=[mT.r])
            for nch in range(2):
                p_ = nextpm()
                for kc in range(8):
                    P.op("pe", lambda e: e.matmul(p_[0:n, :], mT[:, kc, 0:n], wo[:, kc, nch * 512:(nch + 1) * 512],
                                                  start=(kc == 0), stop=(kc == 7)), reads=[mT.r, wo.r], writes=[p_.r])
                cs = slice(nch * 512, (nch + 1) * 512)
                P.op("dve", lambda e: e.scalar_tensor_tensor(hp_[0:n, cs], xf[0:n, cs], DN_ALPHA, p_[0:n, :], ALU.mult, ALU.add),
                     reads=[xf.r, p_.r], writes=[hp_.r])
            layer_norm(P, hp_, hh, mm, s1, s2, n, vd[0:n, 0:1024], vd[0:n, 1024:2048], vd.r)
            P.dma("pool", Dr["h_own"][u * 128:u * 128 + n, :], hh[0:n, :], reads=[hh.r], writes=[r_h])
            for kc in range(8):
                pt_ = ptf[kc // 4]
                P.op("pe", lambda e: e.transpose(pt_[:, (kc % 4) * 128:(kc % 4) * 128 + n], hh[0:n, kc * 128:(kc + 1) * 128], identf[0:n, 0:n]),
                     reads=[hh.r, identf.r], writes=[pt_.r])
            P.op("act", lambda e: e.copy(hT[:, 0:4, 0:n], ptf[0][:].rearrange("p (k t) -> p k t", k=4)[:, :, 0:n]), reads=[ptf[0].r], writes=[hT.r])
            P.op("dve", lambda e: e.tensor_copy(hT[:, 4:8, 0:n], ptf[1][:].rearrange("p (k t) -> p k t", k=4)[:, :, 0:n]), reads=[ptf[1].r], writes=[hT.r])
            p_ = nextpm()
            for kc in range(8):
                P.op("pe", lambda e: e.matmul(p_[0:n, 0:32], hT[:, kc, 0:n], rw[:, kc, :], start=(kc == 0), stop=(kc == 7)),
                     reads=[hT.r, rw.r], writes=[p_.r])
            P.op("dve", lambda e: e.tensor_tensor(lg[0:n, :], p_[0:n, 0:32], vd[0:n, 4096:4128], ALU.add), reads=[p_.r, vd.r], writes=[lg.r])
            P.op("dve", lambda e: e.max(out=m8[0:n, :], in_=lg[0:n, :]), reads=[lg.r], writes=[m8.r])
            P.op("dve", lambda e: e.tensor_scalar(msk[0:n, :], lg[0:n, :], m8[0:n, 3:4], None, ALU.is_ge), reads=[lg.r, m8.r], writes=[msk.r])
            P.op("dve", lambda e: e.tensor_scalar(s1[0:n, :], m8[0:n, 0:1], -1.0, None, ALU.mult), reads=[m8.r], writes=[s1.r])
            P.op("act", lambda e: e.activation(gt[0:n, :], lg[0:n, :], AF.Exp, bias=s1[0:n, 0:1]), reads=[lg.r, s1.r], writes=[gt.r])
            P.op("dve", lambda e: e.tensor_tensor(gt[0:n, :], gt[0:n, :], msk[0:n, :], ALU.mult), reads=[gt.r, msk.r], writes=[gt.r])
            P.op("dve", lambda e: e.tensor_reduce(s2[0:n, :], gt[0:n, :], AX.X, ALU.add), reads=[gt.r], writes=[s2.r])
            P.op("dve", lambda e: e.reciprocal(s2[0:n, :], s2[0:n, :]), reads=[s2.r], writes=[s2.r])
            P.op("dve", lambda e: e.tensor_scalar(gt[0:n, :], gt[0:n, :], s2[0:n, 0:1], None, ALU.mult), reads=[gt.r, s2.r], writes=[gt.r])
            P.dma("pool", Dr["gates_own"][u * 128:u * 128 + n, :], gt[0:n, :], reads=[gt.r], writes=[r_g])
        Dr["r_h"] = r_h
        Dr["r_g"] = r_g
        P.barrier()


def layer_norm(P, src, dst, tmp, s1, s2, n, g_ap, b_ap, vr):
    P.op("dve", lambda e: e.tensor_reduce(s1[0:n, :], src[0:n, :], AX.X, ALU.add), reads=[src.r], writes=[s1.r])
    P.op("dve", lambda e: e.tensor_scalar(s1[0:n, :], s1[0:n, :], -1.0 / 1024, None, ALU.mult), reads=[s1.r], writes=[s1.r])
    P.op("dve", lambda e: e.tensor_scalar(dst[0:n, :], src[0:n, :], s1[0:n, 0:1], None, ALU.add), reads=[src.r, s1.r], writes=[dst.r])
    P.op("pool", lambda e: e.tensor_tensor(tmp[0:n, :], dst[0:n, :], dst[0:n, :], ALU.mult), reads=[dst.r], writes=[tmp.r])
    P.op("dve", lambda e: e.tensor_reduce(s2[0:n, :], tmp[0:n, :], AX.X, ALU.add), reads=[tmp.r], writes=[s2.r])
    P.op("dve", lambda e: e.tensor_scalar(s2[0:n, :], s2[0:n, :], 1.0 / 1024, 1e-5, ALU.mult, ALU.add), reads=[s2.r], writes=[s2.r])
    P.op("act", lambda e: e.activation(s2[0:n, :], s2[0:n, :], AF.Sqrt), reads=[s2.r], writes=[s2.r])
    P.op("dve", lambda e: e.reciprocal(s2[0:n, :], s2[0:n, :]), reads=[s2.r], writes=[s2.r])
    P.op("dve", lambda e: e.tensor_scalar(dst[0:n, :], dst[0:n, :], s2[0:n, 0:1], None, ALU.mult), reads=[dst.r, s2.r], writes=[dst.r])
    P.op("pool", lambda e: e.tensor_tensor(dst[0:n, :], dst[0:n, :], g_ap, ALU.mult), reads=[dst.r, vr], writes=[dst.r])
    P.op("pool", lambda e: e.tensor_tensor(dst[0:n, :], dst[0:n, :], b_ap, ALU.add), reads=[dst.r, vr], writes=[dst.r])


def phase_E(nc, P, Dr, outs):
    with ExitStack() as st:
        def sb(name, shape, dt=F32):
            return T(st.enter_context(nc.sbuf_tensor("e_" + name, list(shape), dt)), name)

        def ps(name, shape, dt=F32):
            return PT(st.enter_context(nc.psum_tensor("e_" + name, list(shape), dt)), name)

        identb = sb("identb", [128, 128], BF16)
        P.dma("pool", identb[:], Dr["masks"][:, 896:1024], writes=[identb.r])
        identf = sb("identf", [128, 128])
        P.dma("sp", identf[:], Dr["masks"][:, 896:1024], writes=[identf.r])
        vd = sb("vd", [128, 2048])
        P.dma("sp", vd[:], Dr["vecsD"][:, 2048:4096], writes=[vd.r])
        b1a = sb("b1a", [128, 32, 16])
        P.dma("sp", b1a[:], Dr["mlp1_bT"].rearrange("e p c -> p e c"), writes=[b1a.r])
        b2 = sb("b2", [32, 1024])
        P.dma("sp", b2[:], Dr["mlp2_b"][:, :], writes=[b2.r])

        HT = 9
        hT = sb("hT", [128, 8, 1024 + NS], BF16)
        yacc = sb("yacc", [128, HT, 1024])
        gates = sb("gates", [128, HT, 32])
        gT = sb("gT", [32, HT, 128])
        s1 = sb("s1", [128, 1])
        s2 = sb("s2", [128, 1])
        ptr = ps("ptr", [128, 8, 128], BF16)
        pg_ = [ps("pgl%d" % i, [128, 512]) for i in range(4)]
        po = [ps("po%d" % i, [128, 512]) for i in range(3)]
        cnt = {"pg": 0, "po": 0, "w": 0, "a": 0}
        r_out = R("outE")
        outs.append(r_out)

        def scoped(names):
            stx = ExitStack()
            d = {}
            for nm_, shp, dt_ in names:
                d[nm_] = T(stx.enter_context(nc.sbuf_tensor("e_%s_%d" % (nm_, P.n_inst), list(shp), dt_)), nm_)
            return stx, d

        for half in range(2):
            tiles = list(range(half * 8, half * 8 + 8)) + ([16] if half == 1 else [])
            ntok = sum(_tile_rows(u) for u in tiles)
            groups = [(0, 512), (512, 512)] + ([(1024, NS)] if half == 1 else [])
            stx, dd = scoped([("hf", [128, 1024], F32), ("hb", [128, 1024], BF16)])
            hf, hb = dd["hf"], dd["hb"]
            for li, u in enumerate(tiles):
                n = _tile_rows(u)
                P.dma("sp", hf[0:n, :], Dr["h_own"][u * 128:u * 128 + n, :], reads=[Dr["r_h"]], writes=[hf.r])
                P.dma("sp", gates[0:n, li, :], Dr["gates_own"][u * 128:u * 128 + n, :], reads=[Dr["r_g"]], writes=[gates.r])
                P.op("act", lambda e: e.copy(hb[0:n, :], hf[0:n, :]), reads=[hf.r], writes=[hb.r])
                for kc in range(8):
                    P.op("pe", lambda e: e.transpose(ptr[:, kc, 0:n], hb[0:n, kc * 128:(kc + 1) * 128], identb[0:n, 0:n]),
                         reads=[hb.r, identb.r], writes=[ptr.r])
                P.op("dve", lambda e: e.tensor_copy(hT[:, :, li * 128:li * 128 + n], ptr[:, :, 0:n]), reads=[ptr.r], writes=[hT.r])
                pq = po[cnt["po"] % 3]
                cnt["po"] += 1
                P.op("pe", lambda e: e.transpose(pq[0:32, 0:n], gates[0:n, li, :], identf[0:n, 0:n]), reads=[gates.r, identf.r], writes=[pq.r])
                P.op("act", lambda e: e.copy(gT[:, li, 0:n], pq[0:32, 0:n]), reads=[pq.r], writes=[gT.r])
                for nch in range(2):
                    pq = po[cnt["po"] % 3]
                    cnt["po"] += 1
                    P.op("pe", lambda e: e.matmul(pq[0:n, :], gT[:, li, 0:n], b2[:, nch * 512:(nch + 1) * 512], start=True, stop=True),
                         reads=[gT.r, b2.r], writes=[pq.r])
                    P.op("act", lambda e: e.copy(yacc[0:n, li, nch * 512:(nch + 1) * 512], pq[0:n, :]), reads=[pq.r], writes=[yacc.r])
            P.barrier()
            stx.close()
            stx, dd = scoped([("w1_0", [128, 8, 2048], BF16), ("w1_1", [128, 8, 2048], BF16), ("w2_0", [128, 8, 1024], BF16),
                              ("w2_1", [128, 8, 1024], BF16), ("actT0", [128, 8, 512], BF16), ("actT1", [128, 8, 512], BF16),
                              ("tg0", [128, 512], F32), ("tg1", [128, 512], F32), ("tsg0", [128, 512], F32), ("tsg1", [128, 512], F32),
                              ("tl0", [128, 512], F32), ("tl1", [128, 512], F32)])
            w1 = [dd["w1_0"], dd["w1_1"]]
            w2 = [dd["w2_0"], dd["w2_1"]]
            actT = [dd["actT0"], dd["actT1"]]
            tg = [dd["tg0"], dd["tg1"]]
            tsg = [dd["tsg0"], dd["tsg1"]]
            tl = [dd["tl0"], dd["tl1"]]
            for ex in range(32):
                wb1 = w1[cnt["w"] % 2]
                wb2 = w2[cnt["w"] % 2]
                cnt["w"] += 1
                for kc in range(8):
                    P.dma("pool", wb1[:, kc, :], Dr["mlp1_w"][ex, kc * 128:(kc + 1) * 128, :], writes=[wb1.r])
                for kc in range(8):
                    P.dma("pool", wb2[:, kc, :], Dr["mlp2_w"][ex, kc * 128:(kc + 1) * 128, :], writes=[wb2.r])
                for (g0, gn) in groups:
                    aT = actT[cnt["a"] % 2]
                    cnt["a"] += 1
                    for fc in range(8):
                        pgl = pg_[cnt["pg"] % 4]
                        pll = pg_[(cnt["pg"] + 1) % 4]
                        cnt["pg"] += 2
                        for kc in range(8):
                            P.op("pe", lambda e: e.matmul(pgl[:, 0:gn], wb1[:, kc, fc * 128:(fc + 1) * 128], hT[:, kc, g0:g0 + gn],
                                                          start=(kc == 0), stop=(kc == 7)), reads=[wb1.r, hT.r], writes=[pgl.r])
                        for kc in range(8):
                            P.op("pe", lambda e: e.matmul(pll[:, 0:gn], wb1[:, kc, 1024 + fc * 128:1024 + (fc + 1) * 128], hT[:, kc, g0:g0 + gn],
                                                          start=(kc == 0), stop=(kc == 7)), reads=[wb1.r, hT.r], writes=[pll.r])
                        k2 = fc % 2
                        a_, s_, l_ = tg[k2], tsg[k2], tl[k2]
                        P.op("dve", lambda e: e.tensor_scalar(a_[:, 0:gn], pgl[:, 0:gn], b1a[:, ex, fc:fc + 1], 7.0, ALU.add, ALU.min),
                             reads=[pgl.r, b1a.r], writes=[a_.r])
                        P.op("act", lambda e: e.activation(s_[:, 0:gn], a_[:, 0:gn], AF.Sigmoid, scale=1.702), reads=[a_.r], writes=[s_.r])
                        P.op("dve", lambda e: e.tensor_scalar(l_[:, 0:gn], pll[:, 0:gn], b1a[:, ex, 8 + fc:9 + fc], 7.0, ALU.add, ALU.min),
                             reads=[pll.r, b1a.r], writes=[l_.r])
                        P.op("pool", lambda e: e.tensor_scalar(l_[:, 0:gn], l_[:, 0:gn], -7.0, 1.0, ALU.max, ALU.add), reads=[l_.r], writes=[l_.r])
                        P.op("pool", lambda e: e.tensor_tensor(a_[:, 0:gn], a_[:, 0:gn], s_[:, 0:gn], ALU.mult), reads=[a_.r, s_.r], writes=[a_.r])
                        P.op("dve", lambda e: e.tensor_tensor(aT[:, fc, 0:gn], a_[:, 0:gn], l_[:, 0:gn], ALU.mult), reads=[a_.r, l_.r], writes=[aT.r])
                    nt_in_g = (gn + 127) // 128
                    for tt in range(nt_in_g):
                        li = g0 // 128 + tt
                        n = min(128, gn - tt * 128)
                        for nch in range(2):
                            pq = po[cnt["po"] % 3]
                            cnt["po"] += 1
                            for fc in range(8):
                                P.op("pe", lambda e: e.matmul(pq[0:n, :], aT[:, fc, tt * 128:tt * 128 + n], wb2[:, fc, nch * 512:(nch + 1) * 512],
                                                              start=(fc == 0), stop=(fc == 7)), reads=[aT.r, wb2.r], writes=[pq.r])
                            ya = yacc[0:n, li, nch * 512:(nch + 1) * 512]
                            P.op("dve", lambda e: e.scalar_tensor_tensor(ya, pq[0:n, :], gates[0:n, li, ex:ex + 1], ya, ALU.mult, ALU.add),
                                 reads=[pq.r, gates.r, yacc.r], writes=[yacc.r])
            P.barrier()
            stx.close()
            stx, dd = scoped([("hf", [128, 1024], F32), ("t1", [128, 1024], F32), ("t2", [128, 1024], F32)])
            hf, t1, t2 = dd["hf"], dd["t1"], dd["t2"]
            for li, u in enumerate(tiles):
                n = _tile_rows(u)
                P.dma("sp", hf[0:n, :], Dr["h_own"][u * 128:u * 128 + n, :], reads=[Dr["r_h"]], writes=[hf.r])
                P.op("dve", lambda e: e.scalar_tensor_tensor(t1[0:n, :], hf[0:n, :], DN_ALPHA, yacc[0:n, li, :], ALU.mult, ALU.add),
                     reads=[hf.r, yacc.r], writes=[t1.r])
                layer_norm(P, t1, t2, hf, s1, s2, n, vd[0:n, 0:1024], vd[0:n, 1024:2048], vd.r)
                P.dma("sp", Dr["o_y"][u * 128:u * 128 + n, :], t2[0:n, :], reads=[t2.r], writes=[r_out])
            P.barrier()
            stx.close()
        P.barrier()


def build_program():
    nc = bass.Bass("TRN2", target_bir_lowering=False)
    Dr = {}

    def din(name, shape, dt=F32):
        Dr[name] = nc.dram_tensor(name, list(shape), dt, kind="ExternalInput").ap()
        _INPUT_NAMES.append(name)
    ph = OPTS["phases"]

    def dout(name, shape, dt=F32):
        Dr[name] = nc.dram_tensor(name, list(shape), dt, kind="ExternalOutput").ap()

    def dtmp(name, shape, dt=F32):
        Dr[name] = nc.dram_tensor(name, list(shape), dt).ap()

    din("x_full", [SEQ, D])
    din("x_own", [2048, D])
    din("x_s", [NS, D])
    din("w_in", [D, IN_COLS])
    din("rope_p", [SEQ, 16])
    din("rope_own", [2048, 16])
    din("rope_s", [NS, 16])
    din("cache_win", [SB, 512, 256])
    din("vecs", [128, NVEC])
    din("w_w2", [64, 512])
    din("w_a2", [64, 512])
    din("g_w2", [128, 512])
    din("masks", [128, NMASK])
    din("tmask", [128, 1])
    din("state_shift", [SB, R_COLS])
    din("state_wkv", [SB, 8, 64, 64])
    din("vecsD", [128, NVD])
    din("sel4", [128, 4])
    din("w_o", [D, D])
    din("w_pa", [512, D])
    din("w_pb", [512, D])
    din("router_w", [D, 32])
    if "E" in ph:
        din("mlp1_w", [32, D, 2048])
        din("mlp2_w", [32, D, D])
    din("mlp1_bT", [32, 128, 16])
    din("mlp2_b", [32, D])
    din("cmp_w1", [2, 2048, 256])
    din("cmp_w2", [2, 256, 64])
    din("cmp_pe", [2, 32, 64])
    din("cmp_b1T", [128, 2, 2])
    din("cmp_b2T", [64, 1])
    din("cmp_b2v", [128, 64])
    din("Gtab", [128, 8192])
    din("cover", [128, 4, 128])
    din("Ftab", [128, 17, 128])
    din("cbias", [128, 17, 2, 128])
    din("triS", [128, 4, 512])
    din("triW", [128, 8, 512])
    din("triS_s", [128, 512])
    din("triW_s", [128, 5, 512])
    din("pt_col", [SB, 64, 1], I32)
    if "C" in ph and OPTS["sb"] > 0:
        din("cache_cmp_pg", [2560 * 4, 8192])
        din("cache_sel_pg", [2560 * 4, 8192])

    dout("o_cmp_p", [SEQ, 256])
    dout("o_sel_p", [SEQ, 256])
    dout("o_win_p", [512, 256])
    dout("o_shift_p", [1, R_COLS])
    dout("o_cmp_s", [NS, 256])
    dout("o_sel_s", [NS, 256])
    dout("o_win_s", [SB, 512, 256])
    dout("o_shift_s", [SB, R_COLS])
    dout("o_wkv_p", [8, 64, 64])
    dout("o_wkv_s", [SB, 8, 64, 64])
    dout("o_y", [NTOK, D])

    dtmp("p_full", [SEQ, NA])
    dtmp("p_samp", [NS, IN_COLS])
    dtmp("y_r", [SEQ, 512], BF16)
    dtmp("y_r_s", [NS, 512], BF16)
    if DEBUG:
        dout("gath", [SB, 2, 64, 128 * 256])
    else:
        dtmp("gath", [SB, 2, 64, 128 * 256])
    if DEBUG:
        dout("y_a_own", [NTOK, 512], BF16)
        dout("h_own", [NTOK, D])
        dout("gates_own", [NTOK, 32])
    else:
        dtmp("y_a_own", [NTOK, 512], BF16)
        dtmp("h_own", [NTOK, D])
        dtmp("gates_own", [NTOK, 32])
    outs = []
    with ExitStack() as st:
        P = Prog(nc, st)
        phase_A(nc, P, Dr, outs)
        if "B" in ph:
            phase_B(nc, P, Dr, outs)
        if "C" in ph:
            phase_C(nc, P, Dr, outs)
        if "D" in ph:
            phase_D(nc, P, Dr, outs)
        if "E" in ph:
            phase_E(nc, P, Dr, outs)
        P.finish(outs + [Dr[k] for k in ("r_yr", "r_ya", "r_h", "r_g") if k in Dr])
        P._need("sp", [(k, v) for k, v in P.cnt.items() if v > 0])
        print("epochs", P.epoch, {k: v for k, v in P.cnt.items() if not k.startswith("d_")})
        print("program: n_inst=%d n_wait=%d" % (P.n_inst, P.n_wait))
    return nc


DEBUG = False
OPTS = {"phases": "ABCDE", "cq": 16, "sb": SB, "cores": 8, "cstop": 9}
_NC = None
_INPUT_NAMES = []


def _rope_table(pos):
    half = 8
    inv = (np.float32(500000.0) ** (-np.arange(half, dtype=np.float32) * np.float32(2.0) / np.float32(16))).astype(np.float32)
    ang = pos.astype(np.float32)[:, None] * inv[None, :]
    return np.concatenate([np.cos(ang), np.sin(ang)], axis=1).astype(np.float32)


def _const_masks():
    s = np.arange(128)[:, None]
    t = np.arange(128)[None, :]
    same = (s // 64) == (t // 64)
    Lblk = (same & (s <= t)).astype(np.float32)
    Oblk = same.astype(np.float32)
    strictU = (same & (s < t)).astype(np.float32)
    inclU = Lblk
    maskMA2 = np.concatenate([strictU, inclU, strictU, inclU], axis=1)
    maskNT = (same & (s > t)).astype(np.float32)
    ident = np.eye(128, dtype=np.float32)
    return np.ascontiguousarray(np.concatenate([Lblk, Oblk, maskMA2, maskNT, ident], axis=1))


def _nsa_tables(qq):
    f32 = np.float32
    NB = np.float32(-30000.0)
    kl = np.arange(128)[:, None]
    ql = np.arange(128)[None, :]
    Ftab = np.zeros((128, 17, 128), f32)
    cbias = np.zeros((128, 17, 2, 128), f32)
    sidx = np.arange(128)[None, :]
    for j in range(16):
        i = 4 * j + qq
        qpos = (128 * i + np.arange(128))[:, None]
        causal = (64 * sidx) <= qpos
        cur = qpos // 64
        forced = (sidx == 0) | (sidx == cur) | (sidx == cur - 1)
        F = np.where(forced, 1e6 + 16.0 * sidx, 0.0)
        F = np.where(causal, F, -1e30)
        Ftab[:, j, :] = F
        nbt = (32 * j + 32 + 127) // 128
        for slot, bt in ((1, nbt - 1), (0, nbt - 2)):
            if bt < 0:
                continue
            blk = 128 * bt + kl
            valid = (blk <= 510) & (16 * blk + 31 <= 128 * i + ql)
            cbias[:, j, slot, :] = np.where(valid, 0.0, NB)
    F = np.where((sidx == 0) | (sidx == 127), 1e6 + 16.0 * sidx, 0.0) * np.ones((128, 1))
    Ftab[:, 16, :] = F
    blk = 128 * 3 + kl
    cbias[:, 16, 1, :] = np.where(blk <= 510, 0.0, NB) * np.ones((1, 128))
    cbias[:, 16, 0, :] = 0.0
    rep4 = lambda m: np.tile(m, (1, 4))
    triS = np.zeros((128, 4, 512), f32)
    for rel in range(4):
        if rel < qq:
            m = np.zeros((128, 128), f32)
        elif rel == qq:
            m = np.where(kl <= ql, 0.0, NB)
        else:
            m = np.full((128, 128), NB)
        triS[:, rel, :] = rep4(m)
    triW = np.zeros((128, 8, 512), f32)
    for rel in range(8):
        dlt = qq + 4 - rel
        if dlt < 0 or dlt > 4:
            m = np.full((128, 128), NB)
        elif dlt == 0:
            m = np.where(kl <= ql, 0.0, NB)
        elif dlt == 4:
            m = np.where(kl >= ql, 0.0, NB)
        else:
            m = np.zeros((128, 128), f32)
        triW[:, rel, :] = rep4(m)
    qv = ql < DS
    triS_s = rep4(np.where((kl < DS) & (kl <= ql), 0.0, NB))
    triW_s = np.zeros((128, 5, 512), f32)
    for c in range(5):
        kidx = 128 * c + kl
        ok = (kidx < 512 + DS) & (kidx <= 512 + ql) & (kidx >= ql)
        triW_s[:, c, :] = rep4(np.where(ok, 0.0, NB))
    return (Ftab.astype(f32), cbias.astype(f32), triS.astype(f32), triW.astype(f32), triS_s.astype(f32), triW_s.astype(f32))


def _shared_tables():
    f32 = np.float32
    s = np.arange(128)[:, None]
    x = np.arange(8192)[None, :]
    G = ((x // 64) == s).astype(f32)
    cover = np.zeros((128, 4, 128), f32)
    for bt in range(4):
        blk = 128 * bt + np.arange(128)[:, None]
        ss = np.arange(128)[None, :]
        cover[:, bt, :] = ((blk >= 4 * ss - 1) & (blk <= 4 * ss + 3) & (blk <= 510)).astype(f32)
    return G, cover


def kernel(**inputs):
    global _NC
    if _NC is None:
        _NC = build_program()
    nc = _NC
    g = lambda k: np.asarray(inputs[k])
    f32 = np.float32
    C = np.ascontiguousarray
    x_prompt = g("x_prompt")
    x_sample = g("x_sample")
    w_in = C(g("w_in")[0])
    cache_win = g("cache_win_kv")[0].reshape(32, 512, 256)
    rope_p = _rope_table(np.arange(SEQ))
    rope_s = np.tile(_rope_table(PAST + np.arange(DS)), (SB, 1))
    vec = np.concatenate([g("mu_shift")[0], g("w0")[0], g("a0")[0], g("k_k")[0], g("k_a")[0], g("gn_g")[0],
                          g("gn_b")[0], g("r_k")[0].reshape(-1)]).astype(f32)
    vecs = C(np.broadcast_to(vec[None, :], (128, NVEC)))
    vecD = np.concatenate([g("ln1_g")[0], g("ln1_b")[0], g("ln2_g")[0], g("ln2_b")[0], g("router_b")[0]]).astype(f32)
    vecsD = C(np.broadcast_to(vecD[None, :], (128, NVD)))
    masks = _const_masks()
    tmask = np.zeros((128, 1), f32)
    tmask[0:DS] = 1.0
    tmask[64:64 + DS] = 1.0
    state_shift = g("state_shift")[0]
    state_wkv = g("state_wkv")[0]
    page_table = g("page_table").astype(np.int32)
    Gtab, cover = _shared_tables()
    shared = {
        "w_in": w_in, "rope_p": rope_p, "rope_s": rope_s, "vecs": vecs, "masks": masks, "tmask": tmask,
        "w_w2": C(g("w_w2")[0]), "w_a2": C(g("w_a2")[0]), "g_w2": C(g("g_w2")[0]),
        "vecsD": vecsD, "w_o": C(g("w_o")[0]), "w_pa": C(g("w_pa")[0]), "w_pb": C(g("w_pb")[0]),
        "router_w": C(g("router_w")[0]), "mlp1_w": C(g("mlp1_w")[0]), "mlp2_w": C(g("mlp2_w")[0]),
        "mlp1_bT": C(g("mlp1_b")[0].reshape(32, 16, 128).transpose(0, 2, 1)), "mlp2_b": C(g("mlp2_b")[0]),
        "cmp_w1": C(g("cmp_w1")[0]), "cmp_w2": C(g("cmp_w2")[0]), "cmp_pe": C(g("cmp_pe")[0]),
        "cmp_b1T": C(g("cmp_b1")[0].reshape(2, 2, 128).transpose(2, 0, 1)),
        "cmp_b2T": C(g("cmp_b2")[0][0].reshape(64, 1)),
        "cmp_b2v": C(np.broadcast_to(g("cmp_b2")[0][1][None, :], (128, 64))),
        "Gtab": Gtab, "cover": cover,
        "cache_cmp_pg": g("cache_cmp_kv")[0].reshape(2560 * 4, 8192),
        "cache_sel_pg": g("cache_sel_kv")[0].reshape(2560 * 4, 8192),
    }
    tabs = [_nsa_tables(qq) for qq in range(4)]
    in_maps = []
    for c in range(8):
        b, qq = c // 4, c % 4
        own = np.concatenate([np.arange(128 * (4 * j + qq), 128 * (4 * j + qq) + 128) for j in range(16)])
        Ftab, cbias, triS, triW, triS_s, triW_s = tabs[qq]
        sel4 = np.zeros((128, 4), f32)
        sel4[:, qq] = 1.0
        m = dict(shared)
        m.update({
            "x_full": C(x_prompt[b]),
            "x_own": C(x_prompt[b][own]),
            "rope_own": C(rope_p[own]),
            "x_s": C(x_sample[SB * c:SB * c + SB].reshape(NS, D)),
            "cache_win": C(cache_win[SB * c:SB * c + SB]),
            "state_shift": C(state_shift[SB * c:SB * c + SB]),
            "state_wkv": C(state_wkv[SB * c:SB * c + SB]),
            "sel4": sel4, "Ftab": Ftab, "cbias": cbias, "triS": triS, "triW": triW, "triS_s": triS_s, "triW_s": triW_s,
            "pt_col": C(page_table[SB * c:SB * c + SB].reshape(SB, 64, 1)),
        })
        in_maps.append(m)
    ncores = OPTS["cores"]
    in_maps = [{k: v for k, v in mp.items() if k in _INPUT_NAMES} for mp in in_maps[:ncores]]
    res = run_bass_kernel_spmd(nc, in_maps, core_ids=list(range(ncores)))
    rs = list(res.results)
    global _LAST
    _LAST = rs
    if ncores < 8:
        return None
    y_prompt = np.zeros((2, SEQ, D), f32)
    y_sample = np.zeros((32, DS, D), f32)
    for c in range(8):
        b, qq = c // 4, c % 4
        oy = rs[c]["o_y"]
        for j in range(16):
            i = 4 * j + qq
            y_prompt[b, 128 * i:128 * i + 128] = oy[j * 128:(j + 1) * 128]
        y_sample[SB * c:SB * c + SB] = oy[2048:2048 + NS].reshape(SB, DS, D)
    cmp_p = np.stack([rs[0]["o_cmp_p"], rs[4]["o_cmp_p"]]).reshape(1, 2, SEQ, 2, 2, 64)
    sel_p = np.stack([rs[0]["o_sel_p"], rs[4]["o_sel_p"]]).reshape(1, 2, SEQ, 2, 2, 64)
    win_p = np.stack([rs[0]["o_win_p"], rs[4]["o_win_p"]]).reshape(1, 2, 512, 2, 2, 64)
    wkv_p = np.stack([rs[0]["o_wkv_p"], rs[4]["o_wkv_p"]]).reshape(1, 2, 8, 64, 64)
    shift_p = np.stack([rs[0]["o_shift_p"], rs[4]["o_shift_p"]]).reshape(1, 2, R_COLS)
    cmp_s = np.concatenate([rs[c]["o_cmp_s"] for c in range(8)]).reshape(1, 32, DS, 2, 2, 64)
    sel_s = np.concatenate([rs[c]["o_sel_s"] for c in range(8)]).reshape(1, 32, DS, 2, 2, 64)
    win_s = np.concatenate([rs[c]["o_win_s"] for c in range(8)]).reshape(1, 32, 512, 2, 2, 64)
    wkv_s = np.concatenate([rs[c]["o_wkv_s"] for c in range(8)]).reshape(1, 32, 8, 64, 64)
    shift_s = np.concatenate([rs[c]["o_shift_s"] for c in range(8)]).reshape(1, 32, R_COLS)
    return (y_prompt, y_sample, cmp_p.astype(f32), sel_p.astype(f32), win_p.astype(f32), wkv_p.astype(f32),
            shift_p.astype(f32), cmp_s.astype(f32), sel_s.astype(f32), win_s.astype(f32), wkv_s.astype(f32),
            shift_s.astype(f32))


_LAST = None
```

```python
import numpy as np
from contextlib import ExitStack
import concourse.bass as bass
import concourse.mybir as mybir
from concourse.bass_utils import run_bass_kernel_spmd

F32 = mybir.dt.float32
BF16 = mybir.dt.bfloat16
I32 = mybir.dt.int32
ALU = mybir.AluOpType
AF = mybir.ActivationFunctionType
AX = mybir.AxisListType

D = 1024
SEQ = 8192
NT = SEQ // 128
R_COLS = 1792
KV0 = 1792 + 512
NKV = 768
NA = R_COLS + NKV
IN_COLS = 5144
DS = 4
SB = 4
NS = SB * DS
PAST = 8192


class R:
    __slots__ = ("name", "w", "rs", "excl")

    def __init__(self, name=""):
        self.name = name
        self.w = None
        self.rs = []
        self.excl = False


class Prog:
    NDMA = 24

    def __init__(self, nc, stack):
        self.nc = nc
        self.stack = stack
        self.eng = {"pe": nc.tensor, "dve": nc.vector, "act": nc.scalar,
                    "pool": nc.gpsimd, "sp": nc.sync}
        self.sems = {}
        self.cnt = {}
        self.cur = {}
        self.dead = set()
        self.epoch = 0
        for k in self.eng:
            key = k + "#0"
            self.sems[key] = stack.enter_context(nc.semaphore("prog_" + k + "_0"))
            self.cnt[key] = 0
            self.cur[k] = key
        self.dq = {}
        for q in ("sp", "pool", "act"):
            pool = []
            for i in range(self.NDMA):
                key = "d_%s_%d" % (q, i)
                self.sems[key] = stack.enter_context(nc.semaphore(key))
                self.cnt[key] = 0
                pool.append(key)
            self.dq[q] = [pool, 0]
        self.waited = {k: {} for k in self.eng}
        self.n_inst = 0
        self.n_wait = 0

    def _need(self, e, deps):
        best = {}
        for d in deps:
            if d is None:
                continue
            k, v = d
            if k in self.dead:
                continue
            if v > best.get(k, 0):
                best[k] = v
        for k, v in best.items():
            if self.waited[e].get(k, 0) >= v:
                continue
            self.eng[e].wait_ge(self.sems[k], v)
            self.waited[e][k] = v
            self.n_wait += 1

    def _deps(self, reads, writes):
        deps = []
        for r in reads:
            deps.append(r.w)
            if r.excl:
                deps.extend(r.rs)
        for w in writes:
            deps.append(w.w)
            deps.extend(w.rs)
        return deps

    def _commit(self, tok, reads, writes):
        for r in reads:
            if r.excl:
                r.w = tok
                r.rs = []
                continue
            r.rs.append(tok)
            if len(r.rs) > 48:
                best = {}
                for k, v in r.rs:
                    if v > best.get(k, 0):
                        best[k] = v
                r.rs = list(best.items())
        for w in writes:
            w.w = tok
            w.rs = []

    def op(self, e, fn, reads=(), writes=()):
        self._need(e, self._deps(reads, writes))
        ins = fn(self.eng[e])
        key = self.cur[e]
        self.cnt[key] += 1
        ins.then_inc(self.sems[key], 1)
        tok = (key, self.cnt[key])
        self._commit(tok, reads, writes)
        self.n_inst += 1
        return tok

    def dma(self, q, out, in_, reads=(), writes=(), **kw):
        pool, idx = self.dq[q]
        key = pool[idx % len(pool)]
        self.dq[q][1] = idx + 1
        deps = self._deps(reads, writes)
        if self.cnt[key] > 0:
            deps.append((key, self.cnt[key]))
        self._need(q, deps)
        ins = self.eng[q].dma_start(out=out, in_=in_, **kw)
        self.cnt[key] += 16
        ins.then_inc(self.sems[key], 16)
        tok = (key, self.cnt[key])
        self._commit(tok, reads, writes)
        self.n_inst += 1
        return tok

    def finish(self, regions):
        self._need("sp", [r.w for r in regions])

    def barrier(self):
        allc = [(k, v) for k, v in self.cnt.items() if v > 0 and k not in self.dead]
        for e in self.eng:
            self._need(e, allc)
        for e in self.eng:
            key = self.cur[e]
            if self.cnt[key] > 12000:
                self.dead.add(key)
                self.epoch += 1
                nk = "%s#%d" % (e, self.epoch)
                self.sems[nk] = self.stack.enter_context(self.nc.semaphore("prog_%s_%d" % (e, self.epoch)))
                self.cnt[nk] = 0
                self.cur[e] = nk


def PT(t, name):
    x = T(t, name)
    x.r.excl = True
    return x


class T:
    def __init__(self, t, name):
        self.t = t
        self.r = R(name)

    def __getitem__(self, k):
        return self.t[k]


def phase_A(nc, P, Dr, outs):
    x_full, x_s, w_in = Dr["x_full"], Dr["x_s"], Dr["w_in"]
    p_full, p_samp = Dr["p_full"], Dr["p_samp"]
    with ExitStack() as st:
        def sb(name, shape, dt=F32):
            return T(st.enter_context(nc.sbuf_tensor("a_" + name, list(shape), dt)), name)

        def ps(name, shape, dt=F32):
            return PT(st.enter_context(nc.psum_tensor("a_" + name, list(shape), dt)), name)

        ident = sb("ident", [128, 128], BF16)
        P.op("pool", lambda e: e.memset(ident[:], 0.0), writes=[ident.r])
        P.op("pool", lambda e: e.affine_select(ident[:], ident[:], [[-1, 128]], ALU.not_equal, 1.0,
                                               base=0, channel_multiplier=1),
             reads=[ident.r], writes=[ident.r])
        ropeT = sb("ropeT", [128, NT, 16])
        P.dma("sp", ropeT[:], Dr["rope_p"].rearrange("(n p) d -> p n d", p=128), writes=[ropeT.r])
        ropeS = sb("ropeS", [NS, 16])
        P.dma("sp", ropeS[:], Dr["rope_s"][:, :], writes=[ropeS.r])

        wA = sb("wA", [128, 8, NA], BF16)
        for kc in range(8):
            P.dma("pool", wA[:, kc, 0:R_COLS], w_in[kc * 128:(kc + 1) * 128, 0:R_COLS], writes=[wA.r])
            P.dma("pool", wA[:, kc, R_COLS:NA], w_in[kc * 128:(kc + 1) * 128, KV0:KV0 + NKV], writes=[wA.r])

        xb = [sb("xb%d" % i, [128, D], BF16) for i in range(2)]
        xT = [sb("xT%d" % i, [128, 8, 128], BF16) for i in range(2)]
        pt = [sb("ptile%d" % i, [128, NA]) for i in range(2)]
        rtmp = [sb("rtmp%d" % i, [128, 4, 6, 8]) for i in range(2)]
        ptr = [ps("ptr%d" % i, [128, 8, 128], BF16) for i in range(2)]
        pmm = [ps("pmm%d" % i, [128, 512]) for i in range(5)]
        pmm_i = [0]

        def rope_apply(tile_t, c0, cs_ap, tmp, n):
            v = tile_t.t[0:n, c0:c0 + 768].rearrange("p (a k g d) -> p a k g d", a=3, k=2, g=2)
            x1 = v[:, :, 0, :, 0:8]
            x2 = v[:, :, 0, :, 8:16]
            cos = cs_ap[:, 0:8].unsqueeze(1).unsqueeze(1).to_broadcast([n, 3, 2, 8])
            sin = cs_ap[:, 8:16].unsqueeze(1).unsqueeze(1).to_broadcast([n, 3, 2, 8])
            t = tmp.t[0:n]
            a1, a2, a3, a4 = [t[:, i, :, :].rearrange("p (a g) d -> p a g d", a=3) for i in range(4)]
            rd = [tile_t.r, tmp.r]
            P.op("dve", lambda e: e.tensor_tensor(a1, x1, cos, ALU.mult), reads=rd, writes=[tmp.r])
            P.op("dve", lambda e: e.tensor_tensor(a2, x2, sin, ALU.mult), reads=rd, writes=[tmp.r])
            P.op("dve", lambda e: e.tensor_tensor(a3, x2, cos, ALU.mult), reads=rd, writes=[tmp.r])
            P.op("dve", lambda e: e.tensor_tensor(a4, x1, sin, ALU.mult), reads=rd, writes=[tmp.r])
            P.op("dve", lambda e: e.tensor_tensor(x1, a1, a2, ALU.subtract), reads=rd, writes=[tile_t.r])
            P.op("dve", lambda e: e.tensor_tensor(x2, a3, a4, ALU.add), reads=rd, writes=[tile_t.r])

        r_pfull = R("p_full")
        r_out = R("outsA")
        outs.append(r_out)
        for ti in range(NT):
            b = ti % 2
            t0 = ti * 128
            P.dma("pool", xb[b][:], x_full[t0:t0 + 128, :], writes=[xb[b].r])
            for kc in range(8):
                P.op("pe", lambda e: e.transpose(ptr[b][:, kc, :], xb[b][:, kc * 128:(kc + 1) * 128], ident[:]),
                     reads=[xb[b].r, ident.r], writes=[ptr[b].r])
            P.op("act", lambda e: e.copy(xT[b][:], ptr[b][:]), reads=[ptr[b].r], writes=[xT[b].r])
            for nch in range(5):
                pm = pmm[pmm_i[0] % 5]
                pmm_i[0] += 1
                for kc in range(8):
                    P.op("pe", lambda e: e.matmul(pm[:], xT[b][:, kc, :], wA[:, kc, nch * 512:(nch + 1) * 512],
                                                  start=(kc == 0), stop=(kc == 7)),
                         reads=[xT[b].r, wA.r], writes=[pm.r])
                dst = pt[b][:, nch * 512:(nch + 1) * 512]
                if nch % 2 == 0:
                    P.op("dve", lambda e: e.tensor_copy(dst, pm[:]), reads=[pm.r], writes=[pt[b].r])
                else:
                    P.op("act", lambda e: e.copy(dst, pm[:]), reads=[pm.r], writes=[pt[b].r])
            rope_apply(pt[b], R_COLS, ropeT[:, ti, :], rtmp[b], 128)
            P.dma("sp", p_full[t0:t0 + 128, :], pt[b][:], reads=[pt[b].r], writes=[r_pfull])
            P.dma("sp", Dr["o_cmp_p"][t0:t0 + 128, :], pt[b][:, R_COLS:R_COLS + 256], reads=[pt[b].r], writes=[r_out])
            P.dma("sp", Dr["o_sel_p"][t0:t0 + 128, :], pt[b][:, R_COLS + 256:R_COLS + 512], reads=[pt[b].r], writes=[r_out])
            if ti >= NT - 4:
                w0 = (ti - (NT - 4)) * 128
                P.dma("sp", Dr["o_win_p"][w0:w0 + 128, :], pt[b][:, R_COLS + 512:R_COLS + 768], reads=[pt[b].r], writes=[r_out])
            if ti == NT - 1:
                P.dma("sp", Dr["o_shift_p"][0:1, :], pt[b][127:128, 0:R_COLS], reads=[pt[b].r], writes=[r_out])

        xsb = sb("xsb", [NS, D], BF16)
        xsT = sb("xsT", [128, 8, NS], BF16)
        psT = ptr[0]
        P.dma("pool", xsb[:], x_s[:, :], writes=[xsb.r])
        for kc in range(8):
            P.op("pe", lambda e: e.transpose(psT[:, kc, 0:NS], xsb[:, kc * 128:(kc + 1) * 128], ident[0:NS, 0:NS]),
                 reads=[xsb.r, ident.r], writes=[psT.r])
        P.op("act", lambda e: e.copy(xsT[:], psT[:, :, 0:NS]), reads=[psT.r], writes=[xsT.r])
        psamp = sb("psamp", [NS, IN_COLS])
        wS = [sb("wS%d" % i, [128, 8, 512], BF16) for i in range(2)]
        ncht = (IN_COLS + 511) // 512
        for nch in range(ncht):
            c0 = nch * 512
            cw = min(512, IN_COLS - c0)
            wb = wS[nch % 2]
            for kc in range(8):
                P.dma("pool", wb[:, kc, 0:cw], w_in[kc * 128:(kc + 1) * 128, c0:c0 + cw], writes=[wb.r])
            pm = pmm[pmm_i[0] % 5]
            pmm_i[0] += 1
            for kc in range(8):
                P.op("pe", lambda e: e.matmul(pm[0:NS, 0:cw], xsT[:, kc, :], wb[:, kc, 0:cw],
                                              start=(kc == 0), stop=(kc == 7)),
                     reads=[xsT.r, wb.r], writes=[pm.r])
            P.op("dve", lambda e: e.tensor_copy(psamp[:, c0:c0 + cw], pm[0:NS, 0:cw]), reads=[pm.r], writes=[psamp.r])
        rope_apply(psamp, KV0, ropeS[:, :], rtmp[0], NS)
        r_psamp = R("p_samp")
        P.dma("sp", p_samp[:, :], psamp[:], reads=[psamp.r], writes=[r_psamp])
        P.dma("sp", Dr["o_cmp_s"][:, :], psamp[:, KV0:KV0 + 256], reads=[psamp.r], writes=[r_out])
        P.dma("sp", Dr["o_sel_s"][:, :], psamp[:, KV0 + 256:KV0 + 512], reads=[psamp.r], writes=[r_out])
        for bb in range(SB):
            P.dma("sp", Dr["o_win_s"][bb, 508:512, :], psamp[bb * DS:(bb + 1) * DS, KV0 + 512:KV0 + 768],
                  reads=[psamp.r], writes=[r_out])
            P.dma("sp", Dr["o_win_s"][bb, 0:508, :], Dr["cache_win"][bb, 4:512, :], writes=[r_out])
            P.dma("sp", Dr["o_shift_s"][bb:bb + 1, :], psamp[bb * DS + DS - 1:bb * DS + DS, 0:R_COLS],
                  reads=[psamp.r], writes=[r_out])
        Dr["r_pfull"] = r_pfull
        Dr["r_psamp"] = r_psamp
        P.barrier()

VEC_OFF = {"mu": (0, 1792), "w0": (1792, 512), "a0": (2304, 512), "kk": (2816, 512), "ka": (3328, 512),
           "gng": (3840, 512), "gnb": (4352, 512), "rk": (4864, 512)}
NVEC = 5376
NMASK = 128 + 128 + 512 + 128 + 128


def phase_B(nc, P, Dr, outs):
    with ExitStack() as st:
        def sb(name, shape, dt=F32):
            return T(st.enter_context(nc.sbuf_tensor("b_" + name, list(shape), dt)), name)

        def ps(name, shape, dt=F32):
            return PT(st.enter_context(nc.psum_tensor("b_" + name, list(shape), dt)), name)

        vecs = sb("vecs", [128, NVEC])
        P.dma("sp", vecs[:], Dr["vecs"][:, :], writes=[vecs.r])
        V = lambda k: vecs[:, VEC_OFF[k][0]:VEC_OFF[k][0] + VEC_OFF[k][1]]
        wlora = sb("wlora", [128, 512])
        P.dma("sp", wlora[0:64, :], Dr["w_w2"][:, :], writes=[wlora.r])
        P.dma("sp", wlora[64:128, :], Dr["w_a2"][:, :], writes=[wlora.r])
        gw2 = sb("gw2", [128, 512])
        P.dma("sp", gw2[:], Dr["g_w2"][:, :], writes=[gw2.r])
        masks = sb("masks", [128, NMASK])
        P.dma("sp", masks[:], Dr["masks"][:, :], writes=[masks.r])
        Lblk = masks[:, 0:128]
        Oblk = masks[:, 128:256]
        maskMA2 = masks[:, 256:768]
        maskNT = masks[:, 768:896]
        identF = masks[:, 896:1024]
        tmask = sb("tmask", [128, 1])
        P.dma("sp", tmask[:], Dr["tmask"][:, :], writes=[tmask.r])
        ones = sb("onesc", [128, 1])
        P.op("pool", lambda e: e.memset(ones[:], 1.0), writes=[ones.r])

        pr = sb("pr", [128, R_COLS])
        prev = sb("prev", [128, R_COLS])
        xm = sb("xm", [128, R_COLS])
        la = sb("la", [128, 256])
        laT = sb("laT", [128, 256])
        tA = sb("tA", [128, 512])
        tB = sb("tB", [128, 512])
        lw = sb("lw", [128, 512])
        aicl = sb("aicl", [128, 512])
        g_sb = sb("g_sb", [128, 512])
        kk = sb("kk", [128, 512])
        kkn = sb("kkn", [128, 512])
        kh = sb("kh", [128, 512])
        a_s = sb("a_s", [128, 512])
        b_s = sb("b_s", [128, 512])
        ss = sb("ss", [128, 8])
        rinv = sb("rinv", [128, 8])
        cum_sb = sb("cum_sb", [128, 512])
        e_sb = sb("e_sb", [128, 512])
        einv = sb("einv", [128, 512])
        ea = sb("ea", [128, 512])
        ec = sb("ec", [128, 512])
        at = sb("at", [128, 512])
        rt = sb("rt", [128, 512])
        bt = sb("bt", [128, 512])
        kt = sb("kt", [128, 512])
        bh = sb("bh", [128, 512])
        kh2 = sb("kh2", [128, 512])
        wc = sb("wc", [128, 8])
        FM_ar = sb("FM_ar", [128, 4, 256])
        FM_b = sb("FM_b", [128, 4, 128])
        FM_k = sb("FM_k", [128, 4, 128])
        MM = [sb("MM%d" % h, [128, 512]) for h in range(8)]
        TmA = [sb("TmA%d" % g, [128, 4, 128]) for g in range(2)]
        XAs = [[sb("XA%d_%d" % (g, i), [128, 4, 128]) for i in range(2)] for g in range(2)]
        XTAs = [[sb("XTA%d_%d" % (g, i), [128, 4, 128]) for i in range(2)] for g in range(2)]
        PMAs = [sb("PMA%d" % g, [128, 4, 128]) for g in range(2)]
        ZT_sb = sb("ZT_sb", [128, 512])
        UT_sb = sb("UT_sb", [128, 512])
        y_sb = sb("y_sb", [128, 512])
        yc = sb("yc", [128, 512])
        st1 = sb("st1", [128, 8])
        st2 = sb("st2", [128, 8])
        yo = sb("yo", [128, 512], BF16)
        ST = sb("ST", [128, 256])
        Sio = sb("Sio", [64, 512])

        pg = [ps("pg%d" % i, [128, 512]) for i in range(2)]
        pi = ps("pi", [128, 512])
        pv = [ps("pv%d" % i, [128, 512]) for i in range(2)]
        ZT_ps = ps("ZT_ps", [128, 512])
        UT_ps = ps("UT_ps", [128, 512])
        SN_ps = ps("SN_ps", [128, 512])

        def TT(eng, out, in0, in1, op, rd, wr):
            P.op(eng, lambda e: e.tensor_tensor(out, in0, in1, op), reads=rd, writes=wr)

        def ACT(out, in_, func, rd, wr, **kw):
            P.op("act", lambda e: e.activation(out, in_, func, **kw), reads=rd, writes=wr)

        def MMUL(out, lhsT, rhs, rd, wr, start=True, stop=True):
            P.op("pe", lambda e: e.matmul(out, lhsT, rhs, start=start, stop=stop), reads=rd, writes=wr)

        def TR(out, in_, idn, rd, wr):
            P.op("pe", lambda e: e.transpose(out, in_, idn), reads=rd + [masks.r], writes=wr)

        def load_state(src_ap):
            P.dma("sp", Sio[:].rearrange("i (h j) -> i h j", h=8), src_ap.rearrange("h i j -> i h j"), writes=[Sio.r])
            for hp in range(4):
                TR(pg[0][:, hp * 64:(hp + 1) * 64], Sio[:, hp * 128:(hp + 1) * 128], identF[0:64, 0:64], [Sio.r], [pg[0].r])
            P.op("dve", lambda e: e.tensor_copy(ST[:], pg[0][:, 0:256]), reads=[pg[0].r], writes=[ST.r])

        def store_state(dst_ap, rout):
            for hp in range(4):
                TR(pg[0][0:64, hp * 128:(hp + 1) * 128], ST[:, hp * 64:(hp + 1) * 64], identF, [ST.r], [pg[0].r])
            P.op("dve", lambda e: e.tensor_copy(Sio[:], pg[0][0:64, :]), reads=[pg[0].r], writes=[Sio.r])
            P.dma("sp", dst_ap.rearrange("h i j -> i h j"), Sio[:].rearrange("i (h j) -> i h j", h=8), reads=[Sio.r], writes=[rout])

        def rwkv_tile(load_fn, sample, pre_chunk, post_chunk, y_store):
            load_fn(pr, prev)
            TT("pool", prev[:], prev[:], pr[:], ALU.subtract, [prev.r, pr.r], [prev.r])
            TT("pool", prev[:], prev[:], V("mu"), ALU.mult, [prev.r, vecs.r], [prev.r])
            TT("dve", xm[:], prev[:], pr[:], ALU.add, [prev.r, pr.r], [xm.r])
            r_ = xm[:, 0:512]
            k_ = xm[:, 512:1024]
            v_ = xm[:, 1024:1536]
            ACT(la[:, 0:64], xm[:, 1536:1600], AF.Tanh, [xm.r], [la.r])
            ACT(la[:, 64:128], xm[:, 1600:1664], AF.Copy, [xm.r], [la.r])
            ACT(la[:, 128:256], xm[:, 1664:1792], AF.Sigmoid, [xm.r], [la.r])
            TR(pg[0][:, 0:128], la[:, 0:128], identF, [la.r], [pg[0].r])
            TR(pg[0][:, 128:256], la[:, 128:256], identF, [la.r], [pg[0].r])
            ACT(laT[:], pg[0][:, 0:256], AF.Copy, [pg[0].r], [laT.r])
            MMUL(pg[1][:], laT[0:64, 0:128], wlora[0:64, :], [laT.r, wlora.r], [pg[1].r])
            TT("dve", tA[:], pg[1][:], V("w0"), ALU.add, [pg[1].r, vecs.r], [tA.r])
            ACT(tA[:], tA[:], AF.Sigmoid, [tA.r], [tA.r])
            if sample:
                P.op("dve", lambda e: e.tensor_scalar(lw[:], tA[:], -0.6065306597126334, tmask[:, 0:1], ALU.mult, ALU.mult),
                     reads=[tA.r, tmask.r], writes=[lw.r])
            else:
                P.op("dve", lambda e: e.tensor_scalar(lw[:], tA[:], -0.6065306597126334, None, ALU.mult),
                     reads=[tA.r], writes=[lw.r])
            MMUL(pg[0][:], laT[64:128, 0:128], wlora[64:128, :], [laT.r, wlora.r], [pg[0].r])
            TT("dve", aicl[:], pg[0][:], V("a0"), ALU.add, [pg[0].r, vecs.r], [aicl.r])
            ACT(aicl[:], aicl[:], AF.Sigmoid, [aicl.r], [aicl.r])
            MMUL(pg[1][:], laT[:, 128:256], gw2[:], [laT.r, gw2.r], [pg[1].r])
            ACT(g_sb[:], pg[1][:], AF.Copy, [pg[1].r], [g_sb.r])
            TT("pool", kk[:], k_, V("kk"), ALU.mult, [xm.r, vecs.r], [kk.r])
            TT("pool", kkn[:], kk[:], kk[:], ALU.mult, [kk.r], [kkn.r])
            P.op("dve", lambda e: e.tensor_reduce(ss[:], kkn[:].rearrange("p (h j) -> p h j", h=8), AX.X, ALU.add),
                 reads=[kkn.r], writes=[ss.r])
            P.op("dve", lambda e: e.tensor_scalar(ss[:], ss[:], 1e-24, None, ALU.max), reads=[ss.r], writes=[ss.r])
            ACT(rinv[:], ss[:], AF.Sqrt, [ss.r], [rinv.r])
            P.op("dve", lambda e: e.reciprocal(rinv[:], rinv[:]), reads=[rinv.r], writes=[rinv.r])
            TT("dve", kkn[:].rearrange("p (h j) -> p h j", h=8), kk[:].rearrange("p (h j) -> p h j", h=8),
               rinv[:].unsqueeze(2).to_broadcast([128, 8, 64]), ALU.mult, [kk.r, rinv.r], [kkn.r])
            P.op("dve", lambda e: e.scalar_tensor_tensor(kh[:], aicl[:], -1.0, V("ka"), ALU.add, ALU.mult),
                 reads=[aicl.r, vecs.r], writes=[kh.r])
            P.op("dve", lambda e: e.scalar_tensor_tensor(kh[:], kh[:], 1.0, k_, ALU.add, ALU.mult),
                 reads=[kh.r, xm.r], writes=[kh.r])
            P.op("pool", lambda e: e.tensor_scalar(a_s[:], kkn[:], -1.0, None, ALU.mult), reads=[kkn.r], writes=[a_s.r])
            TT("pool", b_s[:], kkn[:], aicl[:], ALU.mult, [kkn.r, aicl.r], [b_s.r])
            if sample:
                P.op("dve", lambda e: e.tensor_scalar(kh[:], kh[:], tmask[:, 0:1], None, ALU.mult), reads=[kh.r, tmask.r], writes=[kh.r])
                P.op("dve", lambda e: e.tensor_scalar(b_s[:], b_s[:], tmask[:, 0:1], None, ALU.mult), reads=[b_s.r, tmask.r], writes=[b_s.r])
            MMUL(pg[0][:], Lblk, lw[:], [masks.r, lw.r], [pg[0].r])
            MMUL(pg[1][:], Oblk, lw[:], [masks.r, lw.r], [pg[1].r])
            ACT(cum_sb[:], pg[0][:], AF.Copy, [pg[0].r], [cum_sb.r])
            ACT(e_sb[:], pg[0][:], AF.Exp, [pg[0].r], [e_sb.r])
            ACT(einv[:], pg[0][:], AF.Exp, [pg[0].r], [einv.r], scale=-1.0)
            TT("dve", tA[:], pg[0][:], lw[:], ALU.subtract, [pg[0].r, lw.r], [tA.r])
            ACT(ea[:], tA[:], AF.Exp, [tA.r], [ea.r])
            TT("dve", tB[:], pg[1][:], cum_sb[:], ALU.subtract, [pg[1].r, cum_sb.r], [tB.r])
            ACT(ec[:], tB[:], AF.Exp, [tB.r], [ec.r])
            TT("pool", at[:], a_s[:], ea[:], ALU.mult, [a_s.r, ea.r], [at.r])
            TT("dve", rt[:], r_, e_sb[:], ALU.mult, [xm.r, e_sb.r], [rt.r])
            TT("pool", bt[:], b_s[:], einv[:], ALU.mult, [b_s.r, einv.r], [bt.r])
            TT("dve", kt[:], kh[:], einv[:], ALU.mult, [kh.r, einv.r], [kt.r])
            TT("pool", bh[:], b_s[:], ec[:], ALU.mult, [b_s.r, ec.r], [bh.r])
            TT("dve", kh2[:], kh[:], ec[:], ALU.mult, [kh.r, ec.r], [kh2.r])
            for c2 in range(2):
                rows = slice(c2 * 64, c2 * 64 + 64)
                for hp in range(4):
                    MMUL(SN_ps[:, 256 + c2 * 4 + hp:256 + c2 * 4 + hp + 1], lw[rows, hp * 128:(hp + 1) * 128], ones[rows, 0:1],
                         [lw.r, ones.r], [SN_ps.r])
            ACT(wc[:], SN_ps[:, 256:264], AF.Exp, [SN_ps.r], [wc.r])
            for qi, q in enumerate((at, rt)):
                for hp in range(4):
                    TR(pg[qi][:, hp * 128:(hp + 1) * 128], q[:, hp * 128:(hp + 1) * 128], identF, [q.r], [pg[qi].r])
                P.op("act" if qi == 0 else "dve",
                     (lambda e: e.copy(FM_ar[:, :, 0:128], pg[0][:].rearrange("p (h t) -> p h t", h=4))) if qi == 0 else
                     (lambda e: e.tensor_copy(FM_ar[:, :, 128:256], pg[1][:].rearrange("p (h t) -> p h t", h=4))),
                     reads=[pg[qi].r], writes=[FM_ar.r])
            for qi, (q, dst) in enumerate(((bt, FM_b), (kt, FM_k))):
                for hp in range(4):
                    TR(pg[qi][:, hp * 128:(hp + 1) * 128], q[:, hp * 128:(hp + 1) * 128], identF, [q.r], [pg[qi].r])
                if qi == 0:
                    P.op("act", lambda e: e.copy(dst[:].rearrange("p h t -> p (h t)"), pg[0][:]), reads=[pg[0].r], writes=[dst.r])
                else:
                    P.op("dve", lambda e: e.tensor_copy(dst[:].rearrange("p h t -> p (h t)"), pg[1][:]), reads=[pg[1].r], writes=[dst.r])
            for h in range(8):
                hp, h2 = h // 2, h % 2
                rows = slice(h2 * 64, h2 * 64 + 64)
                MMUL(pi[:, 0:256], FM_b[rows, hp, :], FM_ar[rows, hp, :], [FM_b.r, FM_ar.r], [pi.r])
                MMUL(pi[:, 256:512], FM_k[rows, hp, :], FM_ar[rows, hp, :], [FM_k.r, FM_ar.r], [pi.r])
                TT("dve", MM[h][:], pi[:], maskMA2, ALU.mult, [pi.r, masks.r], [MM[h].r])
            banks = [(pv[0], pv[1], pi), (ZT_ps, UT_ps, SN_ps)]
            for grp in range(2):
                bX, bXT, bP = banks[grp]
                XA, XTA, PMA = XAs[grp], XTAs[grp], PMAs[grp]
                for q4 in range(4):
                    h = grp * 4 + q4
                    hp, h2 = h // 2, h % 2
                    rows = slice(h2 * 64, h2 * 64 + 64)
                    MMUL(bXT[:, q4 * 128:(q4 + 1) * 128], FM_ar[rows, hp, 0:128], FM_b[rows, hp, :], [FM_ar.r, FM_b.r], [bXT.r])
                    P.op("pool", lambda e: e.tensor_copy(XA[0][:, q4, :], MM[h][:, 0:128]), reads=[MM[h].r], writes=[XA[0].r])
                    TT("pool", PMA[:, q4, :], MM[h][:, 0:128], identF, ALU.add, [MM[h].r, masks.r], [PMA.r])
                TT("dve", XTA[0][:], bXT[:].rearrange("p (h t) -> p h t", h=4), maskNT.unsqueeze(1).to_broadcast([128, 4, 128]), ALU.mult,
                   [bXT.r, masks.r], [XTA[0].r])
            for k in range(1, 6):
                for grp in range(2):
                    bX, bXT, bP = banks[grp]
                    XA, XTA, PMA = XAs[grp], XTAs[grp], PMAs[grp]
                    Xo, XTo = XA[(k - 1) % 2], XTA[(k - 1) % 2]
                    Xn, XTn = XA[k % 2], XTA[k % 2]
                    for q4 in range(4):
                        MMUL(bXT[:, q4 * 128:(q4 + 1) * 128], Xo[:, q4, :], XTo[:, q4, :], [Xo.r, XTo.r], [bXT.r])
                    if k < 5:
                        for q4 in range(4):
                            MMUL(bX[:, q4 * 128:(q4 + 1) * 128], XTo[:, q4, :], Xo[:, q4, :], [Xo.r, XTo.r], [bX.r])
                    P.op("act", lambda e: e.copy(XTn[:].rearrange("p h t -> p (h t)"), bXT[:]), reads=[bXT.r], writes=[XTn.r])
                    if k < 5:
                        P.op("dve", lambda e: e.tensor_copy(Xn[:].rearrange("p h t -> p (h t)"), bX[:]), reads=[bX.r], writes=[Xn.r])
                    for q4 in range(4):
                        MMUL(bP[:, q4 * 128:(q4 + 1) * 128], XTn[:, q4, :], PMA[:, q4, :], [XTn.r, PMA.r], [bP.r])
                    dstP = TmA[grp] if k == 5 else PMA
                    TT("dve", dstP[:].rearrange("p h t -> p (h t)"), bP[:], PMA[:].rearrange("p h t -> p (h t)"), ALU.add,
                       [bP.r, PMA.r], [dstP.r])
            for c2 in range(2):
                cs = slice(c2 * 64, c2 * 64 + 64)
                cc = slice(c2 * 64, c2 * 64 + 64)
                if pre_chunk is not None:
                    pre_chunk(c2)
                for h in range(8):
                    hp, h2 = h // 2, h % 2
                    rows = slice(h2 * 64, h2 * 64 + 64)
                    hc = slice(h * 64, h * 64 + 64)
                    Sh = ST[rows, hp * 64:(hp + 1) * 64]
                    MMUL(ZT_ps[cs, hc], FM_ar[rows, hp, cc], Sh, [FM_ar.r, ST.r], [ZT_ps.r], start=True, stop=False)
                    MMUL(ZT_ps[cs, hc], MM[h][cs, 256 + c2 * 64:256 + c2 * 64 + 64], xm[cs, 1024 + h * 64:1024 + h * 64 + 64],
                         [MM[h].r, xm.r], [ZT_ps.r], start=False, stop=True)
                P.op("act", lambda e: e.copy(ZT_sb[cs, :], ZT_ps[cs, :]), reads=[ZT_ps.r], writes=[ZT_sb.r])
                for h in range(8):
                    hc = slice(h * 64, h * 64 + 64)
                    MMUL(UT_ps[cs, hc], TmA[h // 4][cs, h % 4, cc], ZT_sb[cs, hc], [TmA[h // 4].r, ZT_sb.r], [UT_ps.r])
                P.op("dve", lambda e: e.tensor_copy(UT_sb[cs, :], UT_ps[cs, :]), reads=[UT_ps.r], writes=[UT_sb.r])
                yps = pg[c2]
                for h in range(8):
                    hp, h2 = h // 2, h % 2
                    rows = slice(h2 * 64, h2 * 64 + 64)
                    hc = slice(h * 64, h * 64 + 64)
                    Sh = ST[rows, hp * 64:(hp + 1) * 64]
                    vh = xm[cs, 1024 + h * 64:1024 + h * 64 + 64]
                    MMUL(yps[cs, hc], FM_ar[rows, hp, 128 + c2 * 64:128 + c2 * 64 + 64], Sh, [FM_ar.r, ST.r], [yps.r], start=True, stop=False)
                    MMUL(yps[cs, hc], MM[h][cs, 128 + c2 * 64:128 + c2 * 64 + 64], UT_sb[cs, hc], [MM[h].r, UT_sb.r], [yps.r], start=False, stop=False)
                    MMUL(yps[cs, hc], MM[h][cs, 384 + c2 * 64:384 + c2 * 64 + 64], vh, [MM[h].r, xm.r], [yps.r], start=False, stop=True)
                    MMUL(SN_ps[rows, hp * 64:(hp + 1) * 64], bh[cs, hc], UT_sb[cs, hc], [bh.r, UT_sb.r], [SN_ps.r], start=True, stop=False)
                    MMUL(SN_ps[rows, hp * 64:(hp + 1) * 64], kh2[cs, hc], vh, [kh2.r, xm.r], [SN_ps.r], start=False, stop=True)
                P.op("act", lambda e: e.copy(y_sb[cs, :], yps[cs, :]), reads=[yps.r], writes=[y_sb.r])
                TT("dve", ST[:].rearrange("p (h i) -> p h i", h=4), ST[:].rearrange("p (h i) -> p h i", h=4),
                   wc[:, c2 * 4:c2 * 4 + 4].unsqueeze(2).to_broadcast([128, 4, 64]), ALU.mult, [ST.r, wc.r], [ST.r])
                TT("dve", ST[:], ST[:], SN_ps[:, 0:256], ALU.add, [ST.r, SN_ps.r], [ST.r])
                if post_chunk is not None:
                    post_chunk(c2)
            y3 = y_sb[:].rearrange("p (h j) -> p h j", h=8)
            yc3 = yc[:].rearrange("p (h j) -> p h j", h=8)
            P.op("dve", lambda e: e.tensor_reduce(st1[:], y3, AX.X, ALU.add), reads=[y_sb.r], writes=[st1.r])
            P.op("dve", lambda e: e.tensor_scalar(st1[:], st1[:], 1.0 / 64, None, ALU.mult), reads=[st1.r], writes=[st1.r])
            TT("dve", yc3, y3, st1[:].unsqueeze(2).to_broadcast([128, 8, 64]), ALU.subtract, [y_sb.r, st1.r], [yc.r])
            TT("pool", tA[:], yc[:], yc[:], ALU.mult, [yc.r], [tA.r])
            P.op("dve", lambda e: e.tensor_reduce(st2[:], tA[:].rearrange("p (h j) -> p h j", h=8), AX.X, ALU.add), reads=[tA.r], writes=[st2.r])
            P.op("dve", lambda e: e.tensor_scalar(st2[:], st2[:], 1.0 / 64, 64e-5, ALU.mult, ALU.add), reads=[st2.r], writes=[st2.r])
            ACT(st2[:], st2[:], AF.Sqrt, [st2.r], [st2.r])
            P.op("dve", lambda e: e.reciprocal(st2[:], st2[:]), reads=[st2.r], writes=[st2.r])
            TT("dve", yc3, yc3, st2[:].unsqueeze(2).to_broadcast([128, 8, 64]), ALU.mult, [yc.r, st2.r], [yc.r])
            TT("pool", yc[:], yc[:], V("gng"), ALU.mult, [yc.r, vecs.r], [yc.r])
            TT("pool", yc[:], yc[:], V("gnb"), ALU.add, [yc.r, vecs.r], [yc.r])
            TT("pool", tB[:], r_, kh[:], ALU.mult, [xm.r, kh.r], [tB.r])
            TT("pool", tB[:], tB[:], V("rk"), ALU.mult, [tB.r, vecs.r], [tB.r])
            P.op("dve", lambda e: e.tensor_reduce(st1[:], tB[:].rearrange("p (h j) -> p h j", h=8), AX.X, ALU.add), reads=[tB.r], writes=[st1.r])
            TT("dve", tB[:].rearrange("p (h j) -> p h j", h=8), v_.rearrange("p (h j) -> p h j", h=8),
               st1[:].unsqueeze(2).to_broadcast([128, 8, 64]), ALU.mult, [xm.r, st1.r], [tB.r])
            TT("dve", yc[:], yc[:], tB[:], ALU.add, [yc.r, tB.r], [yc.r])
            TT("dve", yo[:], yc[:], g_sb[:], ALU.mult, [yc.r, g_sb.r], [yo.r])
            y_store(yo)

        P.op("dve", lambda e: e.memset(ST[:], 0.0), writes=[ST.r])
        r_yr = R("y_r")
        r_out = R("outB")
        outs.append(r_out)
        p_full = Dr["p_full"]
        for ti in range(NT):
            t0 = ti * 128

            def load_fn(pr_t, prev_t, t0=t0, ti=ti):
                P.dma("sp", pr_t[:], p_full[t0:t0 + 128, 0:R_COLS], reads=[Dr["r_pfull"]], writes=[pr_t.r])
                if ti == 0:
                    P.op("dve", lambda e: e.memset(prev_t[0:1, :], 0.0), writes=[prev_t.r])
                    P.dma("sp", prev_t[1:128, :], p_full[0:127, 0:R_COLS], reads=[Dr["r_pfull"]], writes=[prev_t.r])
                else:
                    P.dma("sp", prev_t[:], p_full[t0 - 1:t0 + 127, 0:R_COLS], reads=[Dr["r_pfull"]], writes=[prev_t.r])

            def y_store(yo_t, t0=t0):
                P.dma("pool", Dr["y_r"][t0:t0 + 128, :], yo_t[:], reads=[yo_t.r], writes=[r_yr])

            post = None
            if ti == NT - 1:
                def post(c2):
                    if c2 == 1:
                        store_state(Dr["o_wkv_p"], r_out)
            rwkv_tile(load_fn, False, None, post, y_store)

        p_samp = Dr["p_samp"]
        for tp in range(SB // 2):
            def load_fn(pr_t, prev_t, tp=tp):
                P.op("dve", lambda e: e.memset(pr_t[:], 0.0), writes=[pr_t.r])
                P.op("pool", lambda e: e.memset(prev_t[:], 0.0), writes=[prev_t.r])
                for c2 in range(2):
                    bb = tp * 2 + c2
                    P.dma("sp", pr_t[c2 * 64:c2 * 64 + DS, :], p_samp[bb * DS:(bb + 1) * DS, 0:R_COLS],
                          reads=[Dr["r_psamp"]], writes=[pr_t.r])
                    P.dma("sp", prev_t[c2 * 64:c2 * 64 + 1, :], Dr["state_shift"][bb:bb + 1, :], writes=[prev_t.r])
                    P.dma("sp", prev_t[c2 * 64 + 1:c2 * 64 + DS, :], p_samp[bb * DS:(bb + 1) * DS - 1, 0:R_COLS],
                          reads=[Dr["r_psamp"]], writes=[prev_t.r])

            def pre(c2, tp=tp):
                load_state(Dr["state_wkv"][tp * 2 + c2])

            def post(c2, tp=tp):
                store_state(Dr["o_wkv_s"][tp * 2 + c2], r_out)

            def y_store(yo_t, tp=tp):
                for c2 in range(2):
                    bb = tp * 2 + c2
                    P.dma("pool", Dr["y_r_s"][bb * DS:(bb + 1) * DS, :], yo_t[c2 * 64:c2 * 64 + DS, :], reads=[yo_t.r], writes=[r_yr])

            rwkv_tile(load_fn, True, pre, post, y_store)
        Dr["r_yr"] = r_yr
        P.barrier()

QG0 = 1792
GATE0 = 3072
NEGB = -30000.0


def phase_C(nc, P, Dr, outs):
    with ExitStack() as st:
        def sb(name, shape, dt=F32):
            return T(st.enter_context(nc.sbuf_tensor("c_" + name, list(shape), dt)), name)

        def ps(name, shape, dt=F32):
            return PT(st.enter_context(nc.psum_tensor("c_" + name, list(shape), dt)), name)

        def TT(eng, out, in0, in1, op, rd, wr):
            P.op(eng, lambda e: e.tensor_tensor(out, in0, in1, op), reads=rd, writes=wr)

        def MMUL(out, lhsT, rhs, rd, wr, start=True, stop=True):
            P.op("pe", lambda e: e.matmul(out, lhsT, rhs, start=start, stop=stop, skip_group_check=True), reads=rd, writes=wr)

        identb = sb("identb", [128, 128], BF16)
        P.dma("pool", identb[:], Dr["masks"][:, 896:1024], writes=[identb.r])
        identf = sb("identf", [128, 128])
        P.dma("sp", identf[:], Dr["masks"][:, 896:1024], writes=[identf.r])
        onesf = sb("onesf", [128, 128])
        P.op("pool", lambda e: e.memset(onesf[:], 1.0), writes=[onesf.r])
        NKT = 65
        ksT = [sb("ksT%d" % g, [65, NKT * 128], BF16) for g in range(2)]
        kwT = [sb("kwT%d" % g, [65, NKT * 128], BF16) for g in range(2)]
        vsw = sb("vsw", [128, NKT, 4, 65], BF16)
        kcmpT = [sb("kcmpT%d" % g, [65, 512], BF16) for g in range(2)]
        vcmp = sb("vcmp", [128, 4, 2, 65], BF16)
        nm = sb("nm", [128, 12])
        kmaxb = sb("kmaxb", [128, 1])
        for g in range(2):
            P.op("pool", lambda e: e.memset(ksT[g][64:65, :], 1.0), writes=[ksT[g].r])
            P.op("pool", lambda e: e.memset(kwT[g][64:65, :], 1.0), writes=[kwT[g].r])
            P.op("pool", lambda e: e.memset(kcmpT[g][64:65, :], 1.0), writes=[kcmpT[g].r])
        P.op("pool", lambda e: e.memset(vsw[:, :, :, 64:65], 1.0), writes=[vsw.r])
        P.op("pool", lambda e: e.memset(vcmp[:, :, :, 64:65], 1.0), writes=[vcmp.r])
        kvb = [sb("kvb%d" % i, [128, 768], BF16) for i in range(2)]
        sqt = sb("sqt", [128, 256])
        nt4 = sb("nt4", [128, 4])
        ptr = ps("ptr", [128, 8, 128], BF16)

        r_ya = R("y_a_own")

        def ingest_tile(kb, ti, do_cmp, do_sel, do_win, kcT, vcT, first):
            c0 = ti * 128
            rd = [kb.r, identb.r]
            ing = OPTS.get("ing", 15)
            do_cmp = do_cmp and bool(ing & 1)
            do_sel = do_sel and bool(ing & 2)
            do_win = do_win and bool(ing & 4)
            if do_cmp:
                P.op("pe", lambda e: e.transpose(ptr[:, 0, :], kb[:, 0:128], identb[:]), reads=rd, writes=[ptr.r])
                P.op("pe", lambda e: e.transpose(ptr[:, 1, :], kb[:, 128:256], identb[:]), reads=rd, writes=[ptr.r])
            if do_sel:
                P.op("pe", lambda e: e.transpose(ptr[0:64, 2, :], kb[:, 256:320], identb[:]), reads=rd, writes=[ptr.r])
                P.op("pe", lambda e: e.transpose(ptr[0:64, 3, :], kb[:, 320:384], identb[:]), reads=rd, writes=[ptr.r])
            if do_win:
                P.op("pe", lambda e: e.transpose(ptr[0:64, 4, :], kb[:, 512:576], identb[:]), reads=rd, writes=[ptr.r])
                P.op("pe", lambda e: e.transpose(ptr[0:64, 5, :], kb[:, 576:640], identb[:]), reads=rd, writes=[ptr.r])
            if do_cmp:
                P.op("act", lambda e: e.copy(kcT[:, c0:c0 + 128], ptr[:, 0, :]), reads=[ptr.r], writes=[kcT.r])
                P.op("act", lambda e: e.copy(vcT[:, c0:c0 + 128], ptr[:, 1, :]), reads=[ptr.r], writes=[vcT.r])
            if do_sel:
                P.op("dve", lambda e: e.tensor_copy(ksT[0][0:64, c0:c0 + 128], ptr[0:64, 2, :]), reads=[ptr.r], writes=[ksT[0].r])
                P.op("dve", lambda e: e.tensor_copy(ksT[1][0:64, c0:c0 + 128], ptr[0:64, 3, :]), reads=[ptr.r], writes=[ksT[1].r])
                P.op("pool", lambda e: e.tensor_copy(vsw[:, ti, 0:2, 0:64], kb[:, 384:512].rearrange("p (g d) -> p g d", g=2)),
                     reads=[kb.r], writes=[vsw.r])
            if do_win:
                P.op("dve", lambda e: e.tensor_copy(kwT[0][0:64, c0:c0 + 128], ptr[0:64, 4, :]), reads=[ptr.r], writes=[kwT[0].r])
                P.op("dve", lambda e: e.tensor_copy(kwT[1][0:64, c0:c0 + 128], ptr[0:64, 5, :]), reads=[ptr.r], writes=[kwT[1].r])
                P.op("pool", lambda e: e.tensor_copy(vsw[:, ti, 2:4, 0:64], kb[:, 640:768].rearrange("p (g d) -> p g d", g=2)),
                     reads=[kb.r], writes=[vsw.r])
            if not (ing & 8):
                return
            kk = kb[:, 256:768].rearrange("p (a r) -> p a r", a=2)[:, :, 0:128]
            P.op("pool", lambda e: e.tensor_tensor(sqt[:].rearrange("p (a r) -> p a r", a=2), kk, kk, ALU.mult), reads=[kb.r], writes=[sqt.r])
            P.op("dve", lambda e: e.tensor_reduce(nt4[:], sqt[:].rearrange("p (a d) -> p a d", a=4), AX.X, ALU.add), reads=[sqt.r], writes=[nt4.r])
            if first:
                P.op("dve", lambda e: e.tensor_copy(nm[:, 0:4], nt4[:]), reads=[nt4.r], writes=[nm.r])
            else:
                TT("dve", nm[:, 0:4], nm[:, 0:4], nt4[:], ALU.max, [nm.r, nt4.r], [nm.r])

        def compress_all(kcT, vcT):
            with ExitStack() as st2:
                def sb2(name, shape, dt=F32):
                    return T(st2.enter_context(nc.sbuf_tensor("c2_" + name + "_%d" % P.n_inst, list(shape), dt)), name)

                def ps2(name, shape, dt=F32):
                    return PT(st2.enter_context(nc.psum_tensor("c2_" + name + "_%d" % P.n_inst, list(shape), dt)), name)
                w1c = sb2("w1c", [128, 2, 32, 256], BF16)
                for kv in range(2):
                    src = Dr["cmp_w1"][kv].rearrange("(j d) h -> d j h", d=64)
                    P.dma("pool", w1c[0:64, kv, :, :], src, writes=[w1c.r])
                    P.dma("pool", w1c[64:128, kv, :, :], src, writes=[w1c.r])
                w2c = sb2("w2c", [128, 2, 2, 64], BF16)
                for kv in range(2):
                    P.dma("pool", w2c[:, kv, :, :], Dr["cmp_w2"][kv].rearrange("(c p) d -> p c d", p=128), writes=[w2c.r])
                pef = sb2("pef", [32, 2, 64])
                P.dma("sp", pef[:], Dr["cmp_pe"].rearrange("k j d -> j k d"), writes=[pef.r])
                peT = sb2("peT", [64, 2, 32], BF16)
                b1c = sb2("b1c", [128, 2, 2])
                P.dma("sp", b1c[:], Dr["cmp_b1T"][:, :, :], writes=[b1c.r])
                b2k = sb2("b2k", [64, 1])
                P.dma("sp", b2k[:], Dr["cmp_b2T"][:, :], writes=[b2k.r])
                b2v = sb2("b2v", [128, 64])
                P.dma("sp", b2v[:], Dr["cmp_b2v"][:, :], writes=[b2v.r])
                cb = sb2("cb", [128, 2, 2])
                hx = sb2("hx", [128, 512])
                hu = sb2("hu", [128, 512])
                hT = sb2("hT", [128, 2, 512], BF16)
                kcf = sb2("kcf", [64, 512])
                P.op("pool", lambda e: e.memset(hT[:], 0.0), writes=[hT.r])
                pc = [ps2("pc%d" % i, [128, 512]) for i in range(2)]
                pk = ps2("pk", [128, 512])
                pcm = ps2("pcm", [128, 512])
                for kv in range(2):
                    P.op("pe", lambda e: e.transpose(pcm[0:64, kv * 32:(kv + 1) * 32], pef[:, kv, :], identf[0:32, 0:32]),
                         reads=[pef.r, identf.r], writes=[pcm.r])
                P.op("dve", lambda e: e.tensor_copy(peT[:].rearrange("p k j -> p (k j)"), pcm[0:64, 0:64]), reads=[pcm.r], writes=[peT.r])
                for kv in range(2):
                    for hc in range(2):
                        col = kv * 2 + hc
                        for j in range(32):
                            MMUL(pcm[:, 64 + col:65 + col], w1c[0:64, kv, j, hc * 128:(hc + 1) * 128], peT[:, kv, j:j + 1],
                                 [w1c.r, peT.r], [pcm.r], start=(j == 0), stop=(j == 31))
                TT("dve", cb[:].rearrange("p k c -> p (k c)"), pcm[:, 64:68], b1c[:].rearrange("p k c -> p (k c)"), ALU.add,
                   [pcm.r, b1c.r], [cb.r])
                for kv, srcT in ((0, kcT), (1, vcT)):
                    for g in range(2):
                        rows = slice(g * 64, g * 64 + 64)
                        for hc in range(2):
                            pp = pc[hc]
                            for j in range(32):
                                MMUL(pp[:, 0:511], w1c[rows, kv, j, hc * 128:(hc + 1) * 128],
                                     srcT[rows, j:j + 16 * 510 + 1:16], [w1c.r, srcT.r], [pp.r], start=(j == 0), stop=(j == 31))
                            P.op("act", lambda e: e.activation(hx[:, 0:511], pp[:, 0:511], AF.Identity, bias=cb[:, kv, hc:hc + 1]),
                                 reads=[pp.r, cb.r], writes=[hx.r])
                            TT("dve", hu[:, 0:511], hx[:, 0:511], hx[:, 0:511], ALU.mult, [hx.r], [hu.r])
                            P.op("dve", lambda e: e.tensor_scalar(hu[:, 0:511], hu[:, 0:511], 0.044715, 1.0, ALU.mult, ALU.add), reads=[hu.r], writes=[hu.r])
                            TT("dve", hu[:, 0:511], hu[:, 0:511], hx[:, 0:511], ALU.mult, [hu.r, hx.r], [hu.r])
                            P.op("act", lambda e: e.activation(hu[:, 0:511], hu[:, 0:511], AF.Sigmoid, scale=1.5957691216057308), reads=[hu.r], writes=[hu.r])
                            TT("dve", hT[:, hc, 0:511], hx[:, 0:511], hu[:, 0:511], ALU.mult, [hx.r, hu.r], [hT.r])
                        if kv == 0:
                            for hc in range(2):
                                MMUL(pk[0:64, 0:511], w2c[:, 0, hc, :], hT[:, hc, 0:511], [w2c.r, hT.r], [pk.r], start=(hc == 0), stop=(hc == 1))
                            P.op("act", lambda e: e.activation(kcf[:, 0:511], pk[0:64, 0:511], AF.Identity, bias=b2k[:, 0:1]),
                                 reads=[pk.r, b2k.r], writes=[kcf.r])
                            P.op("pool", lambda e: e.memset(kcf[:, 511:512], 0.0), writes=[kcf.r])
                            P.op("dve", lambda e: e.tensor_copy(kcmpT[g][0:64, :], kcf[:, :]), reads=[kcf.r], writes=[kcmpT[g].r])
                            TT("dve", kcf[:, :], kcf[:, :], kcf[:, :], ALU.mult, [kcf.r], [kcf.r])
                            for bt in range(4):
                                MMUL(pcm[:, 80 + g * 4 + bt:81 + g * 4 + bt], kcf[:, bt * 128:(bt + 1) * 128], onesf[0:64, 0:1],
                                     [kcf.r, onesf.r], [pcm.r])
                            P.op("dve", lambda e: e.tensor_copy(nm[:, 4 + g * 4:8 + g * 4], pcm[:, 80 + g * 4:84 + g * 4]), reads=[pcm.r], writes=[nm.r])
                        else:
                            for bt in range(4):
                                nb = 128
                                for hc in range(2):
                                    MMUL(pk[0:nb, bt * 64:(bt + 1) * 64], hT[:, hc, bt * 128:bt * 128 + nb], w2c[:, 1, hc, :],
                                         [hT.r, w2c.r], [pk.r], start=(hc == 0), stop=(hc == 1))
                                TT("dve", vcmp[0:nb, bt, g, 0:64], pk[0:nb, bt * 64:(bt + 1) * 64], b2v[0:nb, :], ALU.add, [pk.r, b2v.r], [vcmp.r])
                P.op("pe", lambda e: e.transpose(pcm[0:12, 128:256], nm[:, 0:12], identf[:]), reads=[nm.r, identf.r], writes=[pcm.r])
                P.op("dve", lambda e: e.tensor_reduce(hx[0:12, 0:1], pcm[0:12, 128:256], AX.X, ALU.max), reads=[pcm.r], writes=[hx.r])
                P.op("pe", lambda e: e.transpose(pcm[0:1, 256:268], hx[0:12, 0:1], identf[0:12, 0:12]), reads=[hx.r, identf.r], writes=[pcm.r])
                P.op("dve", lambda e: e.tensor_reduce(hx[0:1, 1:2], pcm[0:1, 256:268], AX.X, ALU.max), reads=[pcm.r], writes=[hx.r])
                P.op("act", lambda e: e.activation(hx[0:1, 2:3], hx[0:1, 1:2], AF.Sqrt), reads=[hx.r], writes=[hx.r])
                MMUL(pcm[:, 300:301], onesf[0:1, :], hx[0:1, 2:3], [onesf.r, hx.r], [pcm.r])
                P.op("dve", lambda e: e.tensor_copy(kmaxb[:], pcm[:, 300:301]), reads=[pcm.r], writes=[kmaxb.r])
                P.barrier()

        def attention_scope(run):
            with ExitStack() as st3:
                def sb3(name, shape, dt=F32):
                    return T(st3.enter_context(nc.sbuf_tensor("c3_" + name + "_%d" % P.n_inst, list(shape), dt)), name)

                def ps3(name, shape, dt=F32):
                    return PT(st3.enter_context(nc.psum_tensor("c3_" + name + "_%d" % P.n_inst, list(shape), dt)), name)
                A = {}
                A["G"] = sb3("G", [128, 8192], BF16)
                P.dma("pool", A["G"][:], Dr["Gtab"][:, :], writes=[A["G"].r])
                A["cover"] = sb3("cover", [128, 4, 128], BF16)
                P.dma("pool", A["cover"][:], Dr["cover"][:, :, :], writes=[A["cover"].r])
                A["wq"] = sb3("wq", [128, 8, 536], BF16)
                for kc in range(8):
                    P.dma("pool", A["wq"][:, kc, 0:512], Dr["w_in"][kc * 128:(kc + 1) * 128, QG0:QG0 + 512], writes=[A["wq"].r])
                    P.dma("pool", A["wq"][:, kc, 512:536], Dr["w_in"][kc * 128:(kc + 1) * 128, GATE0:GATE0 + 24], writes=[A["wq"].r])
                A["Ftab"] = sb3("Ftab", [128, 128])
                A["cb2"] = sb3("cb2", [128, 2, 128], BF16)
                A["triS"] = sb3("triS", [128, 4, 512], BF16)
                A["triW"] = sb3("triW", [128, 8, 512], BF16)
                A["xb"] = sb3("xb", [128, 1024], BF16)
                A["xT"] = sb3("xT", [128, 8, 128], BF16)
                A["qf"] = sb3("qf", [128, 512])
                A["rt"] = sb3("rt", [128, 4, 8, 8])
                A["rope"] = sb3("rope", [128, 16])
                A["gts"] = sb3("gts", [128, 24])
                A["qn"] = sb3("qn", [128, 8])
                A["qa"] = sb3("qa", [128, 8, 65], BF16)
                A["qT"] = sb3("qT", [65, 8, 128], BF16)
                A["pT"] = [sb3("pT%d" % i, [128, 512], BF16) for i in range(2)]
                A["ov"] = sb3("ov", [128, 4, 65])
                A["rl"] = sb3("rl", [128, 4])
                A["cf"] = sb3("cf", [128, 4])
                A["imp"] = sb3("imp", [128, 128])
                A["sc"] = sb3("sc", [128, 128])
                A["sc2"] = sb3("sc2", [128, 128])
                A["m8a"] = sb3("m8a", [128, 8])
                A["m8b"] = sb3("m8b", [128, 8])
                A["mb4"] = sb3("mb4", [128, 4, 128], BF16)
                A["ya"] = sb3("ya", [128, 8, 64])
                A["yab"] = sb3("yab", [128, 512], BF16)
                A["tmp"] = sb3("tmp", [128, 4, 64])
                A["sT"] = [ps3("sT%d" % i, [128, 512]) for i in range(2)]
                A["po"] = [ps3("po%d" % i, [128, 512]) for i in range(3)]
                A["ir"] = ps3("ir", [128, 512])
                A["pq"] = ps3("pq", [128, 512])
                A["cnt"] = {"sT": 0, "po": 0, "pT": 0}
                run(A)
                P.barrier()

        def q_prepare(A, n, load_x, load_q, rope_src):
            qf, gts, qa, qT, pq = A["qf"], A["gts"], A["qa"], A["qT"], A["pq"]
            if n < 128:
                P.op("pool", lambda e: e.memset(qa[:], 0.0), writes=[qa.r])
            if load_x is not None:
                load_x(A["xb"])
                for kc in range(8):
                    P.op("pe", lambda e: e.transpose(ptr[:, kc, :], A["xb"][:, kc * 128:(kc + 1) * 128], identb[:]),
                         reads=[A["xb"].r, identb.r], writes=[ptr.r])
                P.op("act", lambda e: e.copy(A["xT"][:], ptr[:]), reads=[ptr.r], writes=[A["xT"].r])
                for kc in range(8):
                    MMUL(pq[:, :], A["xT"][:, kc, :], A["wq"][:, kc, 0:512], [A["xT"].r, A["wq"].r], [pq.r], start=(kc == 0), stop=(kc == 7))
                P.op("act", lambda e: e.activation(qf[:], pq[:], AF.Copy, scale=0.125), reads=[pq.r], writes=[qf.r])
                for kc in range(8):
                    MMUL(pq[:, 0:24], A["xT"][:, kc, :], A["wq"][:, kc, 512:536], [A["xT"].r, A["wq"].r], [pq.r], start=(kc == 0), stop=(kc == 7))
                P.op("act", lambda e: e.activation(gts[:], pq[:, 0:24], AF.Sigmoid), reads=[pq.r], writes=[gts.r])
            else:
                load_q(qf, gts)
                P.op("act", lambda e: e.activation(qf[0:n, :], qf[0:n, :], AF.Copy, scale=0.125), reads=[qf.r], writes=[qf.r])
                P.op("act", lambda e: e.activation(gts[0:n, :], gts[0:n, :], AF.Sigmoid), reads=[gts.r], writes=[gts.r])
            P.dma("sp", A["rope"][0:n, :], rope_src, writes=[A["rope"].r])
            q3 = qf[0:n, :].rearrange("p (h d) -> p h d", h=8)
            x1 = q3[:, :, 0:8]
            x2 = q3[:, :, 8:16]
            cos = A["rope"][0:n, 0:8].unsqueeze(1).to_broadcast([n, 8, 8])
            sin = A["rope"][0:n, 8:16].unsqueeze(1).to_broadcast([n, 8, 8])
            rt = A["rt"]
            rd = [qf.r, rt.r, A["rope"].r]
            TT("dve", rt[0:n, 0], x1, cos, ALU.mult, rd, [rt.r])
            TT("dve", rt[0:n, 1], x2, sin, ALU.mult, rd, [rt.r])
            TT("dve", rt[0:n, 2], x2, cos, ALU.mult, rd, [rt.r])
            TT("dve", rt[0:n, 3], x1, sin, ALU.mult, rd, [rt.r])
            TT("dve", x1, rt[0:n, 0], rt[0:n, 1], ALU.subtract, rd, [qf.r])
            TT("dve", x2, rt[0:n, 2], rt[0:n, 3], ALU.add, rd, [qf.r])
            TT("pool", A["ya"][0:n].rearrange("p h d -> p (h d)"), qf[0:n, :], qf[0:n, :], ALU.mult, [qf.r], [A["ya"].r])
            P.op("dve", lambda e: e.tensor_reduce(A["qn"][0:n, :], A["ya"][0:n], AX.X, ALU.add), reads=[A["ya"].r], writes=[A["qn"].r])
            P.op("act", lambda e: e.activation(A["qn"][0:n, :], A["qn"][0:n, :], AF.Sqrt), reads=[A["qn"].r], writes=[A["qn"].r])
            P.op("dve", lambda e: e.tensor_scalar(A["qn"][0:n, :], A["qn"][0:n, :], kmaxb[0:n, 0:1], -1.0, ALU.mult, ALU.mult),
                 reads=[A["qn"].r, kmaxb.r], writes=[A["qn"].r])
            P.op("dve", lambda e: e.tensor_copy(qa[0:n, :, 0:64], q3), reads=[qf.r], writes=[qa.r])
            P.op("dve", lambda e: e.tensor_copy(qa[0:n, :, 64:65], A["qn"][0:n, :].unsqueeze(2)), reads=[A["qn"].r], writes=[qa.r])
            for h in range(8):
                P.op("pe", lambda e: e.transpose(ptr[0:65, h, :], qa[:, h, :], identb[:]), reads=[qa.r, identb.r], writes=[ptr.r])
            P.op("act", lambda e: e.copy(qT[:], ptr[0:65, :, :]), reads=[ptr.r], writes=[qT.r])

        def nsa_qtile(A, n, cfg):
            qT, gts, ya = A["qT"], A["gts"], A["ya"]
            cnt = A["cnt"]

            def next_sT():
                t = A["sT"][cnt["sT"] % 2]
                cnt["sT"] += 1
                return t

            def next_pT():
                t = A["pT"][cnt["pT"] % 2]
                cnt["pT"] += 1
                return t

            def next_po():
                t = A["po"][cnt["po"] % 3]
                cnt["po"] += 1
                return t

            def finish_branch(po_t, g, br, first):
                ov, rl, cf = A["ov"], A["rl"], A["cf"]
                P.op("act", lambda e: e.copy(ov[:].rearrange("p h d -> p (h d)"), po_t[:, 0:260]), reads=[po_t.r], writes=[ov.r])
                P.op("dve", lambda e: e.tensor_scalar(rl[:], ov[:, :, 64], 1e-30, None, ALU.max), reads=[ov.r], writes=[rl.r])
                P.op("dve", lambda e: e.reciprocal(rl[:], rl[:]), reads=[rl.r], writes=[rl.r])
                g3 = gts[:, :].rearrange("p (h b) -> p h b", b=3)
                TT("dve", cf[:], rl[:], g3[:, 4 * g:4 * g + 4, br], ALU.mult, [rl.r, gts.r], [cf.r])
                dst = ya[:, 4 * g:4 * g + 4, :]
                cfb = cf[:].unsqueeze(2).to_broadcast([128, 4, 64])
                if first:
                    TT("dve", dst, ov[:, :, 0:64], cfb, ALU.mult, [ov.r, cf.r], [ya.r])
                else:
                    TT("dve", A["tmp"][:], ov[:, :, 0:64], cfb, ALU.mult, [ov.r, cf.r], [A["tmp"].r])
                    TT("dve", dst, dst, A["tmp"][:], ALU.add, [ya.r, A["tmp"].r], [ya.r])

            for g in range(2):
                qTg = qT[0:65, 4 * g:4 * g + 4, :].rearrange("p h q -> p (h q)")
                po_t = next_po()
                ir = A["ir"]
                nbt = cfg["nbt"]
                for bt in range(nbt):
                    sT = next_sT()
                    slots = [s for (b_, s) in cfg["cb_tiles"] if b_ == bt]
                    MMUL(sT[:, :], kcmpT[g][0:65, bt * 128:(bt + 1) * 128], qTg, [kcmpT[g].r, qT.r], [sT.r], start=True, stop=(not slots))
                    for s in slots:
                        for h4 in range(4):
                            MMUL(sT[:, h4 * 128:(h4 + 1) * 128], identb[:], A["cb2"][:, s, :], [identb.r, A["cb2"].r], [sT.r],
                                 start=False, stop=(h4 == 3))
                    pT = next_pT()
                    P.op("act", lambda e: e.activation(pT[:], sT[:], AF.Exp), reads=[sT.r], writes=[pT.r])
                    for h4 in range(4):
                        MMUL(po_t[:, h4 * 65:(h4 + 1) * 65], pT[:, h4 * 128:(h4 + 1) * 128], vcmp[:, bt, g, :], [pT.r, vcmp.r], [po_t.r],
                             start=(bt == 0 and h4 == 0), stop=(bt == nbt - 1))
                        MMUL(ir[:, h4 * 128:(h4 + 1) * 128], pT[:, h4 * 128:(h4 + 1) * 128], A["cover"][:, bt, :], [pT.r, A["cover"].r], [ir.r],
                             start=(bt == 0 and h4 == 0), stop=(bt == nbt - 1))
                finish_branch(po_t, g, 0, True)
                rl = A["rl"]
                imp = A["imp"]
                P.op("dve", lambda e: e.tensor_scalar(imp[:], ir[:, 0:128], rl[:, 0:1], None, ALU.mult), reads=[ir.r, rl.r], writes=[imp.r])
                for h4 in range(1, 4):
                    P.op("dve", lambda e: e.scalar_tensor_tensor(imp[:], ir[:, h4 * 128:(h4 + 1) * 128], rl[:, h4:h4 + 1], imp[:], ALU.mult, ALU.add),
                         reads=[ir.r, rl.r, imp.r], writes=[imp.r])
                sc, sc2, m8a, m8b = A["sc"], A["sc2"], A["m8a"], A["m8b"]
                TT("dve", sc[:], imp[:], A["Ftab"][:], ALU.add, [imp.r, A["Ftab"].r], [sc.r])
                P.op("dve", lambda e: e.max(out=m8a[:], in_=sc[:]), reads=[sc.r], writes=[m8a.r])
                P.op("dve", lambda e: e.match_replace(out=sc2[:], in_to_replace=m8a[:], in_values=sc[:], imm_value=-3.0e38),
                     reads=[sc.r, m8a.r], writes=[sc2.r])
                P.op("dve", lambda e: e.max(out=m8b[:], in_=sc2[:]), reads=[sc2.r], writes=[m8b.r])
                tc_ = cfg["topk_col"]
                P.op("dve", lambda e: e.tensor_scalar(sc2[:], sc[:], m8b[:, tc_:tc_ + 1], None, ALU.is_ge), reads=[sc.r, m8b.r], writes=[sc2.r])
                P.op("dve", lambda e: e.tensor_scalar(sc[:], sc[:], -1.0e29, None, ALU.is_gt), reads=[sc.r], writes=[sc.r])
                TT("dve", sc[:], sc[:], sc2[:], ALU.mult, [sc.r, sc2.r], [sc.r])
                P.op("dve", lambda e: e.tensor_scalar(sc[:], sc[:], -NEGB, NEGB, ALU.mult, ALU.add), reads=[sc.r], writes=[sc.r])
                pq = A["pq"]
                P.op("pe", lambda e: e.transpose(pq[:, 0:128], sc[:], identf[:]), reads=[sc.r, identf.r], writes=[pq.r])
                mb4 = A["mb4"]
                P.op("dve", lambda e: e.tensor_copy(mb4[:], pq[:, 0:128].unsqueeze(1).to_broadcast([128, 4, 128])), reads=[pq.r], writes=[mb4.r])
                po_t = next_po()
                tiles = cfg["sel_tiles"]
                for ii, (c, use_G, tri) in enumerate(tiles):
                    sT = next_sT()
                    last = (not use_G) and (tri is None)
                    MMUL(sT[:, :], ksT[g][0:65, c * 128:(c + 1) * 128], qTg, [ksT[g].r, qT.r], [sT.r], start=True, stop=last)
                    if use_G:
                        MMUL(sT[:, :], A["G"][:, c * 128:(c + 1) * 128], mb4[:].rearrange("p h q -> p (h q)"), [A["G"].r, mb4.r], [sT.r],
                             start=False, stop=(tri is None))
                    if tri is not None:
                        MMUL(sT[:, :], identb[:], A["triS"][:, tri, :], [identb.r, A["triS"].r], [sT.r], start=False, stop=True)
                    pT = next_pT()
                    P.op("act", lambda e: e.activation(pT[:], sT[:], AF.Exp), reads=[sT.r], writes=[pT.r])
                    for h4 in range(4):
                        MMUL(po_t[:, h4 * 65:(h4 + 1) * 65], pT[:, h4 * 128:(h4 + 1) * 128], vsw[:, c, g, :], [pT.r, vsw.r], [po_t.r],
                             start=(ii == 0 and h4 == 0), stop=(ii == len(tiles) - 1))
                finish_branch(po_t, g, 1, False)
                po_t = next_po()
                tiles = cfg["win_tiles"]
                for ii, (c, slot) in enumerate(tiles):
                    sT = next_sT()
                    MMUL(sT[:, :], kwT[g][0:65, c * 128:(c + 1) * 128], qTg, [kwT[g].r, qT.r], [sT.r], start=True, stop=False)
                    MMUL(sT[:, :], identb[:], A["triW"][:, slot, :], [identb.r, A["triW"].r], [sT.r], start=False, stop=True)
                    pT = next_pT()
                    P.op("act", lambda e: e.activation(pT[:], sT[:], AF.Exp), reads=[sT.r], writes=[pT.r])
                    for h4 in range(4):
                        MMUL(po_t[:, h4 * 65:(h4 + 1) * 65], pT[:, h4 * 128:(h4 + 1) * 128], vsw[:, c, 2 + g, :], [pT.r, vsw.r], [po_t.r],
                             start=(ii == 0 and h4 == 0), stop=(ii == len(tiles) - 1))
                finish_branch(po_t, g, 2, False)
            P.op("act", lambda e: e.copy(A["yab"][:], ya[:].rearrange("p h d -> p (h d)")), reads=[ya.r], writes=[A["yab"].r])

        def bail():
            Dr["r_ya"] = r_ya
            P.barrier()
        if OPTS.get("cstop", 9) <= 1:
            return bail()
        with ExitStack() as stp:
            kcT = T(stp.enter_context(nc.sbuf_tensor("c_kcT_p", [128, SEQ], BF16)), "kcT")
            vcT = T(stp.enter_context(nc.sbuf_tensor("c_vcT_p", [128, SEQ], BF16)), "vcT")
            for ti in range(NT):
                kb = kvb[ti % 2]
                P.dma("pool", kb[:], Dr["p_full"][ti * 128:(ti + 1) * 128, R_COLS:NA], reads=[Dr["r_pfull"]], writes=[kb.r])
                ingest_tile(kb, ti, True, True, True, kcT, vcT, ti == 0)
            if OPTS.get("cstop", 9) > 2:
                compress_all(kcT, vcT)
        if OPTS.get("cstop", 9) <= 3:
            return bail()

        def run_prompt(A):
            for j in range(OPTS["cq"]):
                P.dma("sp", A["Ftab"][:], Dr["Ftab"][:, j, :], writes=[A["Ftab"].r])
                P.dma("pool", A["cb2"][:], Dr["cbias"][:, j, :, :], writes=[A["cb2"].r])
                if j == 0:
                    P.dma("pool", A["triS"][:], Dr["triS"][:, :, :], writes=[A["triS"].r])
                    P.dma("pool", A["triW"][:], Dr["triW"][:, :, :], writes=[A["triW"].r])

                def load_x(xb, j=j):
                    P.dma("pool", xb[:], Dr["x_own"][j * 128:(j + 1) * 128, :], writes=[xb.r])
                q_prepare(A, 128, load_x, None, Dr["rope_own"][j * 128:(j + 1) * 128, :])
                nbt = (32 * j + 32 + 127) // 128
                cb_tiles = [(nbt - 1, 1)] + ([(nbt - 2, 0)] if nbt >= 2 else [])
                cfg = {"nbt": nbt, "cb_tiles": cb_tiles, "topk_col": 7,
                       "sel_tiles": [(c, True, (c - 4 * j) if c >= 4 * j else None) for c in range(4 * j + 4)],
                       "win_tiles": [(c, c - (4 * j - 4)) for c in range(max(4 * j - 4, 0), 4 * j + 4)]}
                nsa_qtile(A, 128, cfg)
                P.dma("sp", Dr["y_a_own"][j * 128:(j + 1) * 128, :], A["yab"][:], reads=[A["yab"].r], writes=[r_ya])
        attention_scope(run_prompt)

        r_gath = R("gath")
        with ExitStack() as stg_:
            stage = T(stg_.enter_context(nc.sbuf_tensor("c_stage", [64, 8192], F32)), "stage")
            pidx = T(stg_.enter_context(nc.sbuf_tensor("c_pidx", [64, SB], I32)), "pidx")
            pidf = T(stg_.enter_context(nc.sbuf_tensor("c_pidf", [64, SB], F32)), "pidf")
            idx4f = T(stg_.enter_context(nc.sbuf_tensor("c_idx4f", [64, SB, 4], F32)), "idx4f")
            idx4 = T(stg_.enter_context(nc.sbuf_tensor("c_idx4", [64, SB, 4], I32)), "idx4")
            P.op("pool", lambda e: e.memset(pidx[:], 0), writes=[pidx.r])
            for bb in range(OPTS["sb"]):
                P.dma("sp", pidx[:, bb:bb + 1], Dr["pt_col"][bb, :, :], writes=[pidx.r])
            P.op("dve", lambda e: e.tensor_copy(pidf[:], pidx[:]), reads=[pidx.r], writes=[pidf.r])
            for ch in range(4):
                P.op("dve", lambda e: e.tensor_scalar(idx4f[:, :, ch], pidf[:], 4.0, float(ch), ALU.mult, ALU.add), reads=[pidf.r], writes=[idx4f.r])
            P.op("dve", lambda e: e.tensor_copy(idx4[:], idx4f[:]), reads=[idx4f.r], writes=[idx4.r])
            for bb in range(OPTS["sb"]):
                for ci, cache in enumerate((Dr["cache_cmp_pg"], Dr["cache_sel_pg"])):
                    for ch in range(4):
                        P._need("pool", P._deps([idx4.r], [stage.r]))
                        ins = nc.gpsimd.indirect_dma_start(out=stage[:, :], out_offset=None, in_=cache[:, :],
                                                           in_offset=bass.IndirectOffsetOnAxis(ap=idx4[:, bb, ch:ch + 1], axis=0),
                                                           bounds_check=2560 * 4 - 1, oob_is_err=False)
                        pool_, idx_ = P.dq["pool"]
                        key = pool_[idx_ % len(pool_)]
                        P.dq["pool"][1] = idx_ + 1
                        if P.cnt[key] > 0:
                            P._need("pool", [(key, P.cnt[key])])
                        P.cnt[key] += 16
                        ins.then_inc(P.sems[key], 16)
                        P._commit((key, P.cnt[key]), [idx4.r], [stage.r])
                        P.n_inst += 1
                        P.dma("sp", Dr["gath"][bb, ci, :, ch * 8192:(ch + 1) * 8192], stage[:, :], reads=[stage.r], writes=[r_gath])
            P.barrier()
        for bb in range(OPTS["sb"]):
            with ExitStack() as stp:
                kcT = T(stp.enter_context(nc.sbuf_tensor("c_kcT_s%d" % bb, [128, SEQ], BF16)), "kcT")
                vcT = T(stp.enter_context(nc.sbuf_tensor("c_vcT_s%d" % bb, [128, SEQ], BF16)), "vcT")
                for ti in range(64):
                    kb = kvb[ti % 2]
                    P.dma("pool", kb[:, 0:256], Dr["gath"][bb, 0, ti, :].rearrange("(p c) -> p c", p=128), reads=[r_gath], writes=[kb.r])
                    P.dma("pool", kb[:, 256:512], Dr["gath"][bb, 1, ti, :].rearrange("(p c) -> p c", p=128), reads=[r_gath], writes=[kb.r])
                    ingest_tile(kb, ti, True, True, False, kcT, vcT, ti == 0)
                kb = kvb[0]
                P.op("pool", lambda e: e.memset(kb[:], 0.0), writes=[kb.r])
                P.dma("pool", kb[0:DS, 256:512], Dr["p_samp"][bb * DS:(bb + 1) * DS, KV0 + 256:KV0 + 512], reads=[Dr["r_psamp"]], writes=[kb.r])
                ingest_tile(kb, 64, False, True, False, kcT, vcT, False)
                for c in range(5):
                    kb = kvb[(c + 1) % 2]
                    if c < 4:
                        P.dma("pool", kb[:, 512:768], Dr["cache_win"][bb, c * 128:(c + 1) * 128, :], writes=[kb.r])
                    else:
                        P.op("pool", lambda e: e.memset(kb[:], 0.0), writes=[kb.r])
                        P.dma("pool", kb[0:DS, 512:768], Dr["p_samp"][bb * DS:(bb + 1) * DS, KV0 + 512:KV0 + 768], reads=[Dr["r_psamp"]], writes=[kb.r])
                    ingest_tile(kb, c, False, False, True, kcT, vcT, False)
                compress_all(kcT, vcT)

            def run_sample(A, bb=bb):
                P.dma("sp", A["Ftab"][:], Dr["Ftab"][:, 16, :], writes=[A["Ftab"].r])
                P.dma("pool", A["cb2"][:], Dr["cbias"][:, 16, :, :], writes=[A["cb2"].r])
                P.dma("pool", A["triS"][:, 0, :], Dr["triS_s"][:, :], writes=[A["triS"].r])
                P.dma("pool", A["triW"][:, 0:5, :], Dr["triW_s"][:, :, :], writes=[A["triW"].r])

                def load_q(qf, gts):
                    P.dma("sp", qf[0:DS, :], Dr["p_samp"][bb * DS:(bb + 1) * DS, QG0:QG0 + 512], reads=[Dr["r_psamp"]], writes=[qf.r])
                    P.dma("sp", gts[0:DS, :], Dr["p_samp"][bb * DS:(bb + 1) * DS, GATE0:GATE0 + 24], reads=[Dr["r_psamp"]], writes=[gts.r])
                P.op("pool", lambda e: e.memset(A["gts"][:], 0.0), writes=[A["gts"].r])
                q_prepare(A, DS, None, load_q, Dr["rope_s"][0:DS, :])
                cfg = {"nbt": 4, "cb_tiles": [(3, 1), (2, 0)], "topk_col": 6,
                       "sel_tiles": [(c, True, None) for c in range(64)] + [(64, False, 0)],
                       "win_tiles": [(c, c) for c in range(5)]}
                nsa_qtile(A, DS, cfg)
                P.dma("sp", Dr["y_a_own"][2048 + bb * DS:2048 + (bb + 1) * DS, :], A["yab"][0:DS, :], reads=[A["yab"].r], writes=[r_ya])
            attention_scope(run_sample)
        Dr["r_ya"] = r_ya
        P.barrier()

NTOK = 2048 + NS
NTL = 17
MG0 = 3096
DN_ALPHA = 2.0 ** 0.25
NVD = 4 * 1024 + 32


def _tile_rows(u):
    return NS if u == NTL - 1 else 128


def phase_D(nc, P, Dr, outs):
    with ExitStack() as st:
        def sb(name, shape, dt=F32):
            return T(st.enter_context(nc.sbuf_tensor("d_" + name, list(shape), dt)), name)

        def ps(name, shape, dt=F32):
            return PT(st.enter_context(nc.psum_tensor("d_" + name, list(shape), dt)), name)

        w_in = Dr["w_in"]
        identb = sb("identb", [128, 128], BF16)
        P.dma("pool", identb[:], Dr["masks"][:, 896:1024], writes=[identb.r])
        identf = sb("identf", [128, 128])
        P.dma("sp", identf[:], Dr["masks"][:, 896:1024], writes=[identf.r])
        vd = sb("vd", [128, NVD])
        P.dma("sp", vd[:], Dr["vecsD"][:, :], writes=[vd.r])
        sel4 = sb("sel4", [128, 4])
        P.dma("sp", sel4[:], Dr["sel4"][:, :], writes=[sel4.r])
        wmg = sb("wmg", [128, 8, 2048], BF16)
        wo = sb("wo", [128, 8, 1024], BF16)
        for kc in range(8):
            P.dma("pool", wmg[:, kc, :], w_in[kc * 128:(kc + 1) * 128, MG0:MG0 + 2048], writes=[wmg.r])
            P.dma("pool", wo[:, kc, :], Dr["w_o"][kc * 128:(kc + 1) * 128, :], writes=[wo.r])
        wpa = sb("wpa", [128, 4, 1024], BF16)
        wpb = sb("wpb", [128, 4, 1024], BF16)
        for kc in range(4):
            P.dma("pool", wpa[:, kc, :], Dr["w_pa"][kc * 128:(kc + 1) * 128, :], writes=[wpa.r])
            P.dma("pool", wpb[:, kc, :], Dr["w_pb"][kc * 128:(kc + 1) * 128, :], writes=[wpb.r])
        rw = sb("rw", [128, 8, 32])
        P.dma("sp", rw[:], Dr["router_w"].rearrange("(k p) e -> p k e", p=128), writes=[rw.r])

        xf = sb("xf", [128, 1024])
        xb = sb("xb", [128, 1024], BF16)
        xT = sb("xT", [128, 8, 128], BF16)
        sg = sb("sg", [128, 2048])
        yr4 = sb("yr4", [128, 4, 512], BF16)
        yr = sb("yr", [128, 512], BF16)
        ya = sb("ya", [128, 512], BF16)
        yT = sb("yT", [128, 8, 128], BF16)
        mm = sb("mm", [128, 1024])
        mb = sb("mb", [128, 1024], BF16)
        mT = sb("mT", [128, 8, 128], BF16)
        hp_ = sb("hpre", [128, 1024])
        hh = sb("hh", [128, 1024])
        hT = sb("hTf", [128, 8, 128])
        s1 = sb("s1", [128, 1])
        s2 = sb("s2", [128, 1])
        lg = sb("lg", [128, 32])
        m8 = sb("m8", [128, 8])
        msk = sb("msk", [128, 32])
        gt = sb("gt", [128, 32])
        ptr = ps("ptr", [128, 8, 128], BF16)
        ptf = [ps("ptf%d" % i, [128, 512]) for i in range(2)]
        pm = [ps("pm%d" % i, [128, 512]) for i in range(4)]
        pmi = [0]

        def nextpm():
            t = pm[pmi[0] % 4]
            pmi[0] += 1
            return t

        r_h = R("h_own")
        r_g = R("gates_own")
        for u in range(NTL):
            n = _tile_rows(u)
            if u < 16:
                P.dma("sp", xf[:], Dr["x_own"][u * 128:(u + 1) * 128, :], writes=[xf.r])
                P.dma("sp", yr4[:], Dr["y_r"][u * 512:(u + 1) * 512, :].rearrange("(k p) c -> p k c", p=128),
                      reads=[Dr["r_yr"]], writes=[yr4.r])
                P.dma("sp", ya[:], Dr["y_a_own"][u * 128:(u + 1) * 128, :], reads=[Dr["r_ya"]], writes=[ya.r])
                P.op("dve", lambda e: e.tensor_scalar(yr[:], yr4[:, 0, :], sel4[:, 0:1], None, ALU.mult), reads=[yr4.r, sel4.r], writes=[yr.r])
                for k in range(1, 4):
                    P.op("dve", lambda e: e.scalar_tensor_tensor(yr[:], yr4[:, k, :], sel4[:, k:k + 1], yr[:], ALU.mult, ALU.add),
                         reads=[yr4.r, sel4.r, yr.r], writes=[yr.r])
            else:
                P.dma("sp", xf[0:n, :], Dr["x_s"][:, :], writes=[xf.r])
                P.dma("sp", yr[0:n, :], Dr["y_r_s"][:, :], reads=[Dr["r_yr"]], writes=[yr.r])
                P.dma("sp", ya[0:n, :], Dr["y_a_own"][2048:2048 + n, :], reads=[Dr["r_ya"]], writes=[ya.r])
            P.op("act", lambda e: e.copy(xb[0:n, :], xf[0:n, :]), reads=[xf.r], writes=[xb.r])
            for kc in range(8):
                P.op("pe", lambda e: e.transpose(ptr[:, kc, 0:n], xb[0:n, kc * 128:(kc + 1) * 128], identb[0:n, 0:n]),
                     reads=[xb.r, identb.r], writes=[ptr.r])
            P.op("act", lambda e: e.copy(xT[:, :, 0:n], ptr[:, :, 0:n]), reads=[ptr.r], writes=[xT.r])
            for nch in range(4):
                p_ = nextpm()
                for kc in range(8):
                    P.op("pe", lambda e: e.matmul(p_[0:n, :], xT[:, kc, 0:n], wmg[:, kc, nch * 512:(nch + 1) * 512],
                                                  start=(kc == 0), stop=(kc == 7)), reads=[xT.r, wmg.r], writes=[p_.r])
                P.op("act", lambda e: e.activation(sg[0:n, nch * 512:(nch + 1) * 512], p_[0:n, :], AF.Sigmoid), reads=[p_.r], writes=[sg.r])
            for kc in range(4):
                P.op("pe", lambda e: e.transpose(ptr[:, kc, 0:n], yr[0:n, kc * 128:(kc + 1) * 128], identb[0:n, 0:n]),
                     reads=[yr.r, identb.r], writes=[ptr.r])
                P.op("pe", lambda e: e.transpose(ptr[:, 4 + kc, 0:n], ya[0:n, kc * 128:(kc + 1) * 128], identb[0:n, 0:n]),
                     reads=[ya.r, identb.r], writes=[ptr.r])
            P.op("act", lambda e: e.copy(yT[:, :, 0:n], ptr[:, :, 0:n]), reads=[ptr.r], writes=[yT.r])
            for nch in range(2):
                pa = nextpm()
                pb = nextpm()
                for kc in range(4):
                    P.op("pe", lambda e: e.matmul(pa[0:n, :], yT[:, kc, 0:n], wpa[:, kc, nch * 512:(nch + 1) * 512],
                                                  start=(kc == 0), stop=(kc == 3)), reads=[yT.r, wpa.r], writes=[pa.r])
                for kc in range(4):
                    P.op("pe", lambda e: e.matmul(pb[0:n, :], yT[:, 4 + kc, 0:n], wpb[:, kc, nch * 512:(nch + 1) * 512],
                                                  start=(kc == 0), stop=(kc == 3)), reads=[yT.r, wpb.r], writes=[pb.r])
                cs = slice(nch * 512, (nch + 1) * 512)
                P.op("dve", lambda e: e.tensor_tensor(mm[0:n, cs], pa[0:n, :], sg[0:n, nch * 512:(nch + 1) * 512], ALU.mult),
                     reads=[pa.r, sg.r], writes=[mm.r])
                P.op("dve", lambda e: e.tensor_tensor(hp_[0:n, cs], pb[0:n, :], sg[0:n, 1024 + nch * 512:1024 + (nch + 1) * 512], ALU.mult),
                     reads=[pb.r, sg.r], writes=[hp_.r])
                P.op("dve", lambda e: e.tensor_tensor(mb[0:n, cs], mm[0:n, cs], hp_[0:n, cs], ALU.add),
                     reads=[mm.r, hp_.r], writes=[mb.r])
            for kc in range(8):
                P.op("pe", lambda e: e.transpose(ptr[:, kc, 0:n], mb[0:n, kc * 128:(kc + 1) * 128], identb[0:n, 0:n]),
                     reads=[mb.r, identb.r], writes=[ptr.r])
            P.op("act", lambda e: e.copy(mT[:, :, 0:n], ptr[:, :, 0:n]), reads=[ptr.r], writes=[mT.r])
            for nch in range(2):
                p_ = nextpm()
                for kc in range(8):
                    P.op("pe", lambda e: e.matmul(p_[0:n, :], mT[:, kc, 0:n], wo[:, kc, nch * 512:(nch + 1) * 512],
                                                  start=(kc == 0), stop=(kc == 7)), reads=[mT.r, wo.r], writes=[p_.r])
                cs = slice(nch * 512, (nch + 1) * 512)
                P.op("dve", lambda e: e.scalar_tensor_tensor(hp_[0:n, cs], xf[0:n, cs], DN_ALPHA, p_[0:n, :], ALU.mult, ALU.add),
                     reads=[xf.r, p_.r], writes=[hp_.r])
            layer_norm(P, hp_, hh, mm, s1, s2, n, vd[0:n, 0:1024], vd[0:n, 1024:2048], vd.r)
            P.dma("pool", Dr["h_own"][u * 128:u * 128 + n, :], hh[0:n, :], reads=[hh.r], writes=[r_h])
            for kc in range(8):
                pt_ = ptf[kc // 4]
                P.op("pe", lambda e: e.transpose(pt_[:, (kc % 4) * 128:(kc % 4) * 128 + n], hh[0:n, kc * 128:(kc + 1) * 128], identf[0:n, 0:n]),
                     reads=[hh.r, identf.r], writes=[pt_.r])
            P.op("act", lambda e: e.copy(hT[:, 0:4, 0:n], ptf[0][:].rearrange("p (k t) -> p k t", k=4)[:, :, 0:n]), reads=[ptf[0].r], writes=[hT.r])
            P.op("dve", lambda e: e.tensor_copy(hT[:, 4:8, 0:n], ptf[1][:].rearrange("p (k t) -> p k t", k=4)[:, :, 0:n]), reads=[ptf[1].r], writes=[hT.r])
            p_ = nextpm()
            for kc in range(8):
                P.op("pe", lambda e: e.matmul(p_[0:n, 0:32], hT[:, kc, 0:n], rw[:, kc, :], start=(kc == 0), stop=(kc == 7)),
                     reads=[hT.r, rw.r], writes=[p_.r])
            P.op("dve", lambda e: e.tensor_tensor(lg[0:n, :], p_[0:n, 0:32], vd[0:n, 4096:4128], ALU.add), reads=[p_.r, vd.r], writes=[lg.r])
            P.op("dve", lambda e: e.max(out=m8[0:n, :], in_=lg[0:n, :]), reads=[lg.r], writes=[m8.r])
            P.op("dve", lambda e: e.tensor_scalar(msk[0:n, :], lg[0:n, :], m8[0:n, 3:4], None, ALU.is_ge), reads=[lg.r, m8.r], writes=[msk.r])
            P.op("dve", lambda e: e.tensor_scalar(s1[0:n, :], m8[0:n, 0:1], -1.0, None, ALU.mult), reads=[m8.r], writes=[s1.r])
            P.op("act", lambda e: e.activation(gt[0:n, :], lg[0:n, :], AF.Exp, bias=s1[0:n, 0:1]), reads=[lg.r, s1.r], writes=[gt.r])
            P.op("dve", lambda e: e.tensor_tensor(gt[0:n, :], gt[0:n, :], msk[0:n, :], ALU.mult), reads=[gt.r, msk.r], writes=[gt.r])
            P.op("dve", lambda e: e.tensor_reduce(s2[0:n, :], gt[0:n, :], AX.X, ALU.add), reads=[gt.r], writes=[s2.r])
            P.op("dve", lambda e: e.reciprocal(s2[0:n, :], s2[0:n, :]), reads=[s2.r], writes=[s2.r])
            P.op("dve", lambda e: e.tensor_scalar(gt[0:n, :], gt[0:n, :], s2[0:n, 0:1], None, ALU.mult), reads=[gt.r, s2.r], writes=[gt.r])
            P.dma("pool", Dr["gates_own"][u * 128:u * 128 + n, :], gt[0:n, :], reads=[gt.r], writes=[r_g])
        Dr["r_h"] = r_h
        Dr["r_g"] = r_g
        P.barrier()


def layer_norm(P, src, dst, tmp, s1, s2, n, g_ap, b_ap, vr):
    P.op("dve", lambda e: e.tensor_reduce(s1[0:n, :], src[0:n, :], AX.X, ALU.add), reads=[src.r], writes=[s1.r])
    P.op("dve", lambda e: e.tensor_scalar(s1[0:n, :], s1[0:n, :], -1.0 / 1024, None, ALU.mult), reads=[s1.r], writes=[s1.r])
    P.op("dve", lambda e: e.tensor_scalar(dst[0:n, :], src[0:n, :], s1[0:n, 0:1], None, ALU.add), reads=[src.r, s1.r], writes=[dst.r])
    P.op("pool", lambda e: e.tensor_tensor(tmp[0:n, :], dst[0:n, :], dst[0:n, :], ALU.mult), reads=[dst.r], writes=[tmp.r])
    P.op("dve", lambda e: e.tensor_reduce(s2[0:n, :], tmp[0:n, :], AX.X, ALU.add), reads=[tmp.r], writes=[s2.r])
    P.op("dve", lambda e: e.tensor_scalar(s2[0:n, :], s2[0:n, :], 1.0 / 1024, 1e-5, ALU.mult, ALU.add), reads=[s2.r], writes=[s2.r])
    P.op("act", lambda e: e.activation(s2[0:n, :], s2[0:n, :], AF.Sqrt), reads=[s2.r], writes=[s2.r])
    P.op("dve", lambda e: e.reciprocal(s2[0:n, :], s2[0:n, :]), reads=[s2.r], writes=[s2.r])
    P.op("dve", lambda e: e.tensor_scalar(dst[0:n, :], dst[0:n, :], s2[0:n, 0:1], None, ALU.mult), reads=[dst.r, s2.r], writes=[dst.r])
    P.op("pool", lambda e: e.tensor_tensor(dst[0:n, :], dst[0:n, :], g_ap, ALU.mult), reads=[dst.r, vr], writes=[dst.r])
    P.op("pool", lambda e: e.tensor_tensor(dst[0:n, :], dst[0:n, :], b_ap, ALU.add), reads=[dst.r, vr], writes=[dst.r])


def phase_E(nc, P, Dr, outs):
    with ExitStack() as st:
        def sb(name, shape, dt=F32):
            return T(st.enter_context(nc.sbuf_tensor("e_" + name, list(shape), dt)), name)

        def ps(name, shape, dt=F32):
            return PT(st.enter_context(nc.psum_tensor("e_" + name, list(shape), dt)), name)

        identb = sb("identb", [128, 128], BF16)
        P.dma("pool", identb[:], Dr["masks"][:, 896:1024], writes=[identb.r])
        identf = sb("identf", [128, 128])
        P.dma("sp", identf[:], Dr["masks"][:, 896:1024], writes=[identf.r])
        vd = sb("vd", [128, 2048])
        P.dma("sp", vd[:], Dr["vecsD"][:, 2048:4096], writes=[vd.r])
        b1a = sb("b1a", [128, 32, 16])
        P.dma("sp", b1a[:], Dr["mlp1_bT"].rearrange("e p c -> p e c"), writes=[b1a.r])
        b2 = sb("b2", [32, 1024])
        P.dma("sp", b2[:], Dr["mlp2_b"][:, :], writes=[b2.r])

        HT = 9
        hT = sb("hT", [128, 8, 1024 + NS], BF16)
        yacc = sb("yacc", [128, HT, 1024])
        gates = sb("gates", [128, HT, 32])
        gT = sb("gT", [32, HT, 128])
        s1 = sb("s1", [128, 1])
        s2 = sb("s2", [128, 1])
        ptr = ps("ptr", [128, 8, 128], BF16)
        pg_ = [ps("pgl%d" % i, [128, 512]) for i in range(4)]
        po = [ps("po%d" % i, [128, 512]) for i in range(3)]
        cnt = {"pg": 0, "po": 0, "w": 0, "a": 0}
        r_out = R("outE")
        outs.append(r_out)

        def scoped(names):
            stx = ExitStack()
            d = {}
            for nm_, shp, dt_ in names:
                d[nm_] = T(stx.enter_context(nc.sbuf_tensor("e_%s_%d" % (nm_, P.n_inst), list(shp), dt_)), nm_)
            return stx, d

        for half in range(2):
            tiles = list(range(half * 8, half * 8 + 8)) + ([16] if half == 1 else [])
            ntok = sum(_tile_rows(u) for u in tiles)
            groups = [(0, 512), (512, 512)] + ([(1024, NS)] if half == 1 else [])
            stx, dd = scoped([("hf", [128, 1024], F32), ("hb", [128, 1024], BF16)])
            hf, hb = dd["hf"], dd["hb"]
            for li, u in enumerate(tiles):
                n = _tile_rows(u)
                P.dma("sp", hf[0:n, :], Dr["h_own"][u * 128:u * 128 + n, :], reads=[Dr["r_h"]], writes=[hf.r])
                P.dma("sp", gates[0:n, li, :], Dr["gates_own"][u * 128:u * 128 + n, :], reads=[Dr["r_g"]], writes=[gates.r])
                P.op("act", lambda e: e.copy(hb[0:n, :], hf[0:n, :]), reads=[hf.r], writes=[hb.r])
                for kc in range(8):
                    P.op("pe", lambda e: e.transpose(ptr[:, kc, 0:n], hb[0:n, kc * 128:(kc + 1) * 128], identb[0:n, 0:n]),
                         reads=[hb.r, identb.r], writes=[ptr.r])
                P.op("dve", lambda e: e.tensor_copy(hT[:, :, li * 128:li * 128 + n], ptr[:, :, 0:n]), reads=[ptr.r], writes=[hT.r])
                pq = po[cnt["po"] % 3]
                cnt["po"] += 1
                P.op("pe", lambda e: e.transpose(pq[0:32, 0:n], gates[0:n, li, :], identf[0:n, 0:n]), reads=[gates.r, identf.r], writes=[pq.r])
                P.op("act", lambda e: e.copy(gT[:, li, 0:n], pq[0:32, 0:n]), reads=[pq.r], writes=[gT.r])
                for nch in range(2):
                    pq = po[cnt["po"] % 3]
                    cnt["po"] += 1
                    P.op("pe", lambda e: e.matmul(pq[0:n, :], gT[:, li, 0:n], b2[:, nch * 512:(nch + 1) * 512], start=True, stop=True),
                         reads=[gT.r, b2.r], writes=[pq.r])
                    P.op("act", lambda e: e.copy(yacc[0:n, li, nch * 512:(nch + 1) * 512], pq[0:n, :]), reads=[pq.r], writes=[yacc.r])
            P.barrier()
            stx.close()
            stx, dd = scoped([("w1_0", [128, 8, 2048], BF16), ("w1_1", [128, 8, 2048], BF16), ("w2_0", [128, 8, 1024], BF16),
                              ("w2_1", [128, 8, 1024], BF16), ("actT0", [128, 8, 512], BF16), ("actT1", [128, 8, 512], BF16),
                              ("tg0", [128, 512], F32), ("tg1", [128, 512], F32), ("tsg0", [128, 512], F32), ("tsg1", [128, 512], F32),
                              ("tl0", [128, 512], F32), ("tl1", [128, 512], F32)])
            w1 = [dd["w1_0"], dd["w1_1"]]
            w2 = [dd["w2_0"], dd["w2_1"]]
            actT = [dd["actT0"], dd["actT1"]]
            tg = [dd["tg0"], dd["tg1"]]
            tsg = [dd["tsg0"], dd["tsg1"]]
            tl = [dd["tl0"], dd["tl1"]]
            for ex in range(32):
                wb1 = w1[cnt["w"] % 2]
                wb2 = w2[cnt["w"] % 2]
                cnt["w"] += 1
                for kc in range(8):
                    P.dma("pool", wb1[:, kc, :], Dr["mlp1_w"][ex, kc * 128:(kc + 1) * 128, :], writes=[wb1.r])
                for kc in range(8):
                    P.dma("pool", wb2[:, kc, :], Dr["mlp2_w"][ex, kc * 128:(kc + 1) * 128, :], writes=[wb2.r])
                for (g0, gn) in groups:
                    aT = actT[cnt["a"] % 2]
                    cnt["a"] += 1
                    for fc in range(8):
                        pgl = pg_[cnt["pg"] % 4]
                        pll = pg_[(cnt["pg"] + 1) % 4]
                        cnt["pg"] += 2
                        for kc in range(8):
                            P.op("pe", lambda e: e.matmul(pgl[:, 0:gn], wb1[:, kc, fc * 128:(fc + 1) * 128], hT[:, kc, g0:g0 + gn],
                                                          start=(kc == 0), stop=(kc == 7)), reads=[wb1.r, hT.r], writes=[pgl.r])
                        for kc in range(8):
                            P.op("pe", lambda e: e.matmul(pll[:, 0:gn], wb1[:, kc, 1024 + fc * 128:1024 + (fc + 1) * 128], hT[:, kc, g0:g0 + gn],
                                                          start=(kc == 0), stop=(kc == 7)), reads=[wb1.r, hT.r], writes=[pll.r])
                        k2 = fc % 2
                        a_, s_, l_ = tg[k2], tsg[k2], tl[k2]
                        P.op("dve", lambda e: e.tensor_scalar(a_[:, 0:gn], pgl[:, 0:gn], b1a[:, ex, fc:fc + 1], 7.0, ALU.add, ALU.min),
                             reads=[pgl.r, b1a.r], writes=[a_.r])
                        P.op("act", lambda e: e.activation(s_[:, 0:gn], a_[:, 0:gn], AF.Sigmoid, scale=1.702), reads=[a_.r], writes=[s_.r])
                        P.op("dve", lambda e: e.tensor_scalar(l_[:, 0:gn], pll[:, 0:gn], b1a[:, ex, 8 + fc:9 + fc], 7.0, ALU.add, ALU.min),
                             reads=[pll.r, b1a.r], writes=[l_.r])
                        P.op("pool", lambda e: e.tensor_scalar(l_[:, 0:gn], l_[:, 0:gn], -7.0, 1.0, ALU.max, ALU.add), reads=[l_.r], writes=[l_.r])
                        P.op("pool", lambda e: e.tensor_tensor(a_[:, 0:gn], a_[:, 0:gn], s_[:, 0:gn], ALU.mult), reads=[a_.r, s_.r], writes=[a_.r])
                        P.op("dve", lambda e: e.tensor_tensor(aT[:, fc, 0:gn], a_[:, 0:gn], l_[:, 0:gn], ALU.mult), reads=[a_.r, l_.r], writes=[aT.r])
                    nt_in_g = (gn + 127) // 128
                    for tt in range(nt_in_g):
                        li = g0 // 128 + tt
                        n = min(128, gn - tt * 128)
                        for nch in range(2):
                            pq = po[cnt["po"] % 3]
                            cnt["po"] += 1
                            for fc in range(8):
                                P.op("pe", lambda e: e.matmul(pq[0:n, :], aT[:, fc, tt * 128:tt * 128 + n], wb2[:, fc, nch * 512:(nch + 1) * 512],
                                                              start=(fc == 0), stop=(fc == 7)), reads=[aT.r, wb2.r], writes=[pq.r])
                            ya = yacc[0:n, li, nch * 512:(nch + 1) * 512]
                            P.op("dve", lambda e: e.scalar_tensor_tensor(ya, pq[0:n, :], gates[0:n, li, ex:ex + 1], ya, ALU.mult, ALU.add),
                                 reads=[pq.r, gates.r, yacc.r], writes=[yacc.r])
            P.barrier()
            stx.close()
            stx, dd = scoped([("hf", [128, 1024], F32), ("t1", [128, 1024], F32), ("t2", [128, 1024], F32)])
            hf, t1, t2 = dd["hf"], dd["t1"], dd["t2"]
            for li, u in enumerate(tiles):
                n = _tile_rows(u)
                P.dma("sp", hf[0:n, :], Dr["h_own"][u * 128:u * 128 + n, :], reads=[Dr["r_h"]], writes=[hf.r])
                P.op("dve", lambda e: e.scalar_tensor_tensor(t1[0:n, :], hf[0:n, :], DN_ALPHA, yacc[0:n, li, :], ALU.mult, ALU.add),
                     reads=[hf.r, yacc.r], writes=[t1.r])
                layer_norm(P, t1, t2, hf, s1, s2, n, vd[0:n, 0:1024], vd[0:n, 1024:2048], vd.r)
                P.dma("sp", Dr["o_y"][u * 128:u * 128 + n, :], t2[0:n, :], reads=[t2.r], writes=[r_out])
            P.barrier()
            stx.close()
        P.barrier()


def build_program():
    nc = bass.Bass("TRN2", target_bir_lowering=False)
    Dr = {}

    def din(name, shape, dt=F32):
        Dr[name] = nc.dram_tensor(name, list(shape), dt, kind="ExternalInput").ap()
        _INPUT_NAMES.append(name)
    ph = OPTS["phases"]

    def dout(name, shape, dt=F32):
        Dr[name] = nc.dram_tensor(name, list(shape), dt, kind="ExternalOutput").ap()

    def dtmp(name, shape, dt=F32):
        Dr[name] = nc.dram_tensor(name, list(shape), dt).ap()

    din("x_full", [SEQ, D])
    din("x_own", [2048, D])
    din("x_s", [NS, D])
    din("w_in", [D, IN_COLS])
    din("rope_p", [SEQ, 16])
    din("rope_own", [2048, 16])
    din("rope_s", [NS, 16])
    din("cache_win", [SB, 512, 256])
    din("vecs", [128, NVEC])
    din("w_w2", [64, 512])
    din("w_a2", [64, 512])
    din("g_w2", [128, 512])
    din("masks", [128, NMASK])
    din("tmask", [128, 1])
    din("state_shift", [SB, R_COLS])
    din("state_wkv", [SB, 8, 64, 64])
    din("vecsD", [128, NVD])
    din("sel4", [128, 4])
    din("w_o", [D, D])
    din("w_pa", [512, D])
    din("w_pb", [512, D])
    din("router_w", [D, 32])
    if "E" in ph:
        din("mlp1_w", [32, D, 2048])
        din("mlp2_w", [32, D, D])
    din("mlp1_bT", [32, 128, 16])
    din("mlp2_b", [32, D])
    din("cmp_w1", [2, 2048, 256])
    din("cmp_w2", [2, 256, 64])
    din("cmp_pe", [2, 32, 64])
    din("cmp_b1T", [128, 2, 2])
    din("cmp_b2T", [64, 1])
    din("cmp_b2v", [128, 64])
    din("Gtab", [128, 8192])
    din("cover", [128, 4, 128])
    din("Ftab", [128, 17, 128])
    din("cbias", [128, 17, 2, 128])
    din("triS", [128, 4, 512])
    din("triW", [128, 8, 512])
    din("triS_s", [128, 512])
    din("triW_s", [128, 5, 512])
    din("pt_col", [SB, 64, 1], I32)
    if "C" in ph and OPTS["sb"] > 0:
        din("cache_cmp_pg", [2560 * 4, 8192])
        din("cache_sel_pg", [2560 * 4, 8192])

    dout("o_cmp_p", [SEQ, 256])
    dout("o_sel_p", [SEQ, 256])
    dout("o_win_p", [512, 256])
    dout("o_shift_p", [1, R_COLS])
    dout("o_cmp_s", [NS, 256])
    dout("o_sel_s", [NS, 256])
    dout("o_win_s", [SB, 512, 256])
    dout("o_shift_s", [SB, R_COLS])
    dout("o_wkv_p", [8, 64, 64])
    dout("o_wkv_s", [SB, 8, 64, 64])
    dout("o_y", [NTOK, D])

    dtmp("p_full", [SEQ, NA])
    dtmp("p_samp", [NS, IN_COLS])
    dtmp("y_r", [SEQ, 512], BF16)
    dtmp("y_r_s", [NS, 512], BF16)
    if DEBUG:
        dout("gath", [SB, 2, 64, 128 * 256])
    else:
        dtmp("gath", [SB, 2, 64, 128 * 256])
    if DEBUG:
        dout("y_a_own", [NTOK, 512], BF16)
        dout("h_own", [NTOK, D])
        dout("gates_own", [NTOK, 32])
    else:
        dtmp("y_a_own", [NTOK, 512], BF16)
        dtmp("h_own", [NTOK, D])
        dtmp("gates_own", [NTOK, 32])
    outs = []
    with ExitStack() as st:
        P = Prog(nc, st)
        phase_A(nc, P, Dr, outs)
        if "B" in ph:
            phase_B(nc, P, Dr, outs)
        if "C" in ph:
            phase_C(nc, P, Dr, outs)
        if "D" in ph:
            phase_D(nc, P, Dr, outs)
        if "E" in ph:
            phase_E(nc, P, Dr, outs)
        P.finish(outs + [Dr[k] for k in ("r_yr", "r_ya", "r_h", "r_g") if k in Dr])
        P._need("sp", [(k, v) for k, v in P.cnt.items() if v > 0])
        print("epochs", P.epoch, {k: v for k, v in P.cnt.items() if not k.startswith("d_")})
        print("program: n_inst=%d n_wait=%d" % (P.n_inst, P.n_wait))
    return nc


DEBUG = False
OPTS = {"phases": "ABCDE", "cq": 16, "sb": SB, "cores": 8, "cstop": 9}
_NC = None
_INPUT_NAMES = []


def _rope_table(pos):
    half = 8
    inv = (np.float32(500000.0) ** (-np.arange(half, dtype=np.float32) * np.float32(2.0) / np.float32(16))).astype(np.float32)
    ang = pos.astype(np.float32)[:, None] * inv[None, :]
    return np.concatenate([np.cos(ang), np.sin(ang)], axis=1).astype(np.float32)


def _const_masks():
    s = np.arange(128)[:, None]
    t = np.arange(128)[None, :]
    same = (s // 64) == (t // 64)
    Lblk = (same & (s <= t)).astype(np.float32)
    Oblk = same.astype(np.float32)
    strictU = (same & (s < t)).astype(np.float32)
    inclU = Lblk
    maskMA2 = np.concatenate([strictU, inclU, strictU, inclU], axis=1)
    maskNT = (same & (s > t)).astype(np.float32)
    ident = np.eye(128, dtype=np.float32)
    return np.ascontiguousarray(np.concatenate([Lblk, Oblk, maskMA2, maskNT, ident], axis=1))


def _nsa_tables(qq):
    f32 = np.float32
    NB = np.float32(-30000.0)
    kl = np.arange(128)[:, None]
    ql = np.arange(128)[None, :]
    Ftab = np.zeros((128, 17, 128), f32)
    cbias = np.zeros((128, 17, 2, 128), f32)
    sidx = np.arange(128)[None, :]
    for j in range(16):
        i = 4 * j + qq
        qpos = (128 * i + np.arange(128))[:, None]
        causal = (64 * sidx) <= qpos
        cur = qpos // 64
        forced = (sidx == 0) | (sidx == cur) | (sidx == cur - 1)
        F = np.where(forced, 1e6 + 16.0 * sidx, 0.0)
        F = np.where(causal, F, -1e30)
        Ftab[:, j, :] = F
        nbt = (32 * j + 32 + 127) // 128
        for slot, bt in ((1, nbt - 1), (0, nbt - 2)):
            if bt < 0:
                continue
            blk = 128 * bt + kl
            valid = (blk <= 510) & (16 * blk + 31 <= 128 * i + ql)
            cbias[:, j, slot, :] = np.where(valid, 0.0, NB)
    F = np.where((sidx == 0) | (sidx == 127), 1e6 + 16.0 * sidx, 0.0) * np.ones((128, 1))
    Ftab[:, 16, :] = F
    blk = 128 * 3 + kl
    cbias[:, 16, 1, :] = np.where(blk <= 510, 0.0, NB) * np.ones((1, 128))
    cbias[:, 16, 0, :] = 0.0
    rep4 = lambda m: np.tile(m, (1, 4))
    triS = np.zeros((128, 4, 512), f32)
    for rel in range(4):
        if rel < qq:
            m = np.zeros((128, 128), f32)
        elif rel == qq:
            m = np.where(kl <= ql, 0.0, NB)
        else:
            m = np.full((128, 128), NB)
        triS[:, rel, :] = rep4(m)
    triW = np.zeros((128, 8, 512), f32)
    for rel in range(8):
        dlt = qq + 4 - rel
        if dlt < 0 or dlt > 4:
            m = np.full((128, 128), NB)
        elif dlt == 0:
            m = np.where(kl <= ql, 0.0, NB)
        elif dlt == 4:
            m = np.where(kl >= ql, 0.0, NB)
        else:
            m = np.zeros((128, 128), f32)
        triW[:, rel, :] = rep4(m)
    qv = ql < DS
    triS_s = rep4(np.where((kl < DS) & (kl <= ql), 0.0, NB))
    triW_s = np.zeros((128, 5, 512), f32)
    for c in range(5):
        kidx = 128 * c + kl
        ok = (kidx < 512 + DS) & (kidx <= 512 + ql) & (kidx >= ql)
        triW_s[:, c, :] = rep4(np.where(ok, 0.0, NB))
    return (Ftab.astype(f32), cbias.astype(f32), triS.astype(f32), triW.astype(f32), triS_s.astype(f32), triW_s.astype(f32))


def _shared_tables():
    f32 = np.float32
    s = np.arange(128)[:, None]
    x = np.arange(8192)[None, :]
    G = ((x // 64) == s).astype(f32)
    cover = np.zeros((128, 4, 128), f32)
    for bt in range(4):
        blk = 128 * bt + np.arange(128)[:, None]
        ss = np.arange(128)[None, :]
        cover[:, bt, :] = ((blk >= 4 * ss - 1) & (blk <= 4 * ss + 3) & (blk <= 510)).astype(f32)
    return G, cover


def kernel(**inputs):
    global _NC
    if _NC is None:
        _NC = build_program()
    nc = _NC
    g = lambda k: np.asarray(inputs[k])
    f32 = np.float32
    C = np.ascontiguousarray
    x_prompt = g("x_prompt")
    x_sample = g("x_sample")
    w_in = C(g("w_in")[0])
    cache_win = g("cache_win_kv")[0].reshape(32, 512, 256)
    rope_p = _rope_table(np.arange(SEQ))
    rope_s = np.tile(_rope_table(PAST + np.arange(DS)), (SB, 1))
    vec = np.concatenate([g("mu_shift")[0], g("w0")[0], g("a0")[0], g("k_k")[0], g("k_a")[0], g("gn_g")[0],
                          g("gn_b")[0], g("r_k")[0].reshape(-1)]).astype(f32)
    vecs = C(np.broadcast_to(vec[None, :], (128, NVEC)))
    vecD = np.concatenate([g("ln1_g")[0], g("ln1_b")[0], g("ln2_g")[0], g("ln2_b")[0], g("router_b")[0]]).astype(f32)
    vecsD = C(np.broadcast_to(vecD[None, :], (128, NVD)))
    masks = _const_masks()
    tmask = np.zeros((128, 1), f32)
    tmask[0:DS] = 1.0
    tmask[64:64 + DS] = 1.0
    state_shift = g("state_shift")[0]
    state_wkv = g("state_wkv")[0]
    page_table = g("page_table").astype(np.int32)
    Gtab, cover = _shared_tables()
    shared = {
        "w_in": w_in, "rope_p": rope_p, "rope_s": rope_s, "vecs": vecs, "masks": masks, "tmask": tmask,
        "w_w2": C(g("w_w2")[0]), "w_a2": C(g("w_a2")[0]), "g_w2": C(g("g_w2")[0]),
        "vecsD": vecsD, "w_o": C(g("w_o")[0]), "w_pa": C(g("w_pa")[0]), "w_pb": C(g("w_pb")[0]),
        "router_w": C(g("router_w")[0]), "mlp1_w": C(g("mlp1_w")[0]), "mlp2_w": C(g("mlp2_w")[0]),
        "mlp1_bT": C(g("mlp1_b")[0].reshape(32, 16, 128).transpose(0, 2, 1)), "mlp2_b": C(g("mlp2_b")[0]),
        "cmp_w1": C(g("cmp_w1")[0]), "cmp_w2": C(g("cmp_w2")[0]), "cmp_pe": C(g("cmp_pe")[0]),
        "cmp_b1T": C(g("cmp_b1")[0].reshape(2, 2, 128).transpose(2, 0, 1)),
        "cmp_b2T": C(g("cmp_b2")[0][0].reshape(64, 1)),
        "cmp_b2v": C(np.broadcast_to(g("cmp_b2")[0][1][None, :], (128, 64))),
        "Gtab": Gtab, "cover": cover,
        "cache_cmp_pg": g("cache_cmp_kv")[0].reshape(2560 * 4, 8192),
        "cache_sel_pg": g("cache_sel_kv")[0].reshape(2560 * 4, 8192),
    }
    tabs = [_nsa_tables(qq) for qq in range(4)]
    in_maps = []
    for c in range(8):
        b, qq = c // 4, c % 4
        own = np.concatenate([np.arange(128 * (4 * j + qq), 128 * (4 * j + qq) + 128) for j in range(16)])
        Ftab, cbias, triS, triW, triS_s, triW_s = tabs[qq]
        sel4 = np.zeros((128, 4), f32)
        sel4[:, qq] = 1.0
        m = dict(shared)
        m.update({
            "x_full": C(x_prompt[b]),
            "x_own": C(x_prompt[b][own]),
            "rope_own": C(rope_p[own]),
            "x_s": C(x_sample[SB * c:SB * c + SB].reshape(NS, D)),
            "cache_win": C(cache_win[SB * c:SB * c + SB]),
            "state_shift": C(state_shift[SB * c:SB * c + SB]),
            "state_wkv": C(state_wkv[SB * c:SB * c + SB]),
            "sel4": sel4, "Ftab": Ftab, "cbias": cbias, "triS": triS, "triW": triW, "triS_s": triS_s, "triW_s": triW_s,
            "pt_col": C(page_table[SB * c:SB * c + SB].reshape(SB, 64, 1)),
        })
        in_maps.append(m)
    ncores = OPTS["cores"]
    in_maps = [{k: v for k, v in mp.items() if k in _INPUT_NAMES} for mp in in_maps[:ncores]]
    res = run_bass_kernel_spmd(nc, in_maps, core_ids=list(range(ncores)))
    rs = list(res.results)
    global _LAST
    _LAST = rs
    if ncores < 8:
        return None
    y_prompt = np.zeros((2, SEQ, D), f32)
    y_sample = np.zeros((32, DS, D), f32)
    for c in range(8):
        b, qq = c // 4, c % 4
        oy = rs[c]["o_y"]
        for j in range(16):
            i = 4 * j + qq
            y_prompt[b, 128 * i:128 * i + 128] = oy[j * 128:(j + 1) * 128]
        y_sample[SB * c:SB * c + SB] = oy[2048:2048 + NS].reshape(SB, DS, D)
    cmp_p = np.stack([rs[0]["o_cmp_p"], rs[4]["o_cmp_p"]]).reshape(1, 2, SEQ, 2, 2, 64)
    sel_p = np.stack([rs[0]["o_sel_p"], rs[4]["o_sel_p"]]).reshape(1, 2, SEQ, 2, 2, 64)
    win_p = np.stack([rs[0]["o_win_p"], rs[4]["o_win_p"]]).reshape(1, 2, 512, 2, 2, 64)
    wkv_p = np.stack([rs[0]["o_wkv_p"], rs[4]["o_wkv_p"]]).reshape(1, 2, 8, 64, 64)
    shift_p = np.stack([rs[0]["o_shift_p"], rs[4]["o_shift_p"]]).reshape(1, 2, R_COLS)
    cmp_s = np.concatenate([rs[c]["o_cmp_s"] for c in range(8)]).reshape(1, 32, DS, 2, 2, 64)
    sel_s = np.concatenate([rs[c]["o_sel_s"] for c in range(8)]).reshape(1, 32, DS, 2, 2, 64)
    win_s = np.concatenate([rs[c]["o_win_s"] for c in range(8)]).reshape(1, 32, 512, 2, 2, 64)
    wkv_s = np.concatenate([rs[c]["o_wkv_s"] for c in range(8)]).reshape(1, 32, 8, 64, 64)
    shift_s = np.concatenate([rs[c]["o_shift_s"] for c in range(8)]).reshape(1, 32, R_COLS)
    return (y_prompt, y_sample, cmp_p.astype(f32), sel_p.astype(f32), win_p.astype(f32), wkv_p.astype(f32),
            shift_p.astype(f32), cmp_s.astype(f32), sel_s.astype(f32), win_s.astype(f32), wkv_s.astype(f32),
            shift_s.astype(f32))


_LAST = None
```

```python
import numpy as np
from contextlib import ExitStack
import concourse.bass as bass
import concourse.mybir as mybir
from concourse.bass_utils import run_bass_kernel_spmd

F32 = mybir.dt.float32
BF16 = mybir.dt.bfloat16
I32 = mybir.dt.int32
ALU = mybir.AluOpType
AF = mybir.ActivationFunctionType
AX = mybir.AxisListType

D = 1024
SEQ = 8192
NT = SEQ // 128
R_COLS = 1792
KV0 = 1792 + 512
NKV = 768
NA = R_COLS + NKV
IN_COLS = 5144
DS = 4
SB = 4
NS = SB * DS
PAST = 8192


class R:
    __slots__ = ("name", "w", "rs", "excl")

    def __init__(self, name=""):
        self.name = name
        self.w = None
        self.rs = []
        self.excl = False


class Prog:
    NDMA = 24

    def __init__(self, nc, stack):
        self.nc = nc
        self.stack = stack
        self.eng = {"pe": nc.tensor, "dve": nc.vector, "act": nc.scalar,
                    "pool": nc.gpsimd, "sp": nc.sync}
        self.sems = {}
        self.cnt = {}
        self.cur = {}
        self.dead = set()
        self.epoch = 0
        for k in self.eng:
            key = k + "#0"
            self.sems[key] = stack.enter_context(nc.semaphore("prog_" + k + "_0"))
            self.cnt[key] = 0
            self.cur[k] = key
        self.dq = {}
        for q in ("sp", "pool", "act"):
            pool = []
            for i in range(self.NDMA):
                key = "d_%s_%d" % (q, i)
                self.sems[key] = stack.enter_context(nc.semaphore(key))
                self.cnt[key] = 0
                pool.append(key)
            self.dq[q] = [pool, 0]
        self.waited = {k: {} for k in self.eng}
        self.n_inst = 0
        self.n_wait = 0
        self.pe_selfsync = False

    def _need(self, e, deps):
        best = {}
        for d in deps:
            if d is None:
                continue
            k, v = d
            if k in self.dead:
                continue
            if e == "pe" and k.startswith("pe#") and not self.pe_selfsync:
                continue
            if v > best.get(k, 0):
                best[k] = v
        for k, v in best.items():
            if self.waited[e].get(k, 0) >= v:
                continue
            self.eng[e].wait_ge(self.sems[k], v)
            self.waited[e][k] = v
            self.n_wait += 1

    def _deps(self, reads, writes):
        deps = []
        for r in reads:
            deps.append(r.w)
            if r.excl:
                deps.extend(r.rs)
        for w in writes:
            deps.append(w.w)
            deps.extend(w.rs)
        return deps

    def _commit(self, tok, reads, writes):
        for r in reads:
            if r.excl:
                r.w = tok
                r.rs = []
                continue
            r.rs.append(tok)
            if len(r.rs) > 48:
                best = {}
                for k, v in r.rs:
                    if v > best.get(k, 0):
                        best[k] = v
                r.rs = list(best.items())
        for w in writes:
            w.w = tok
            w.rs = []

    def op(self, e, fn, reads=(), writes=()):
        self._need(e, self._deps(reads, writes))
        ins = fn(self.eng[e])
        key = self.cur[e]
        self.cnt[key] += 1
        ins.then_inc(self.sems[key], 1)
        tok = (key, self.cnt[key])
        self._commit(tok, reads, writes)
        self.n_inst += 1
        return tok

    def dma(self, q, out, in_, reads=(), writes=(), **kw):
        pool, idx = self.dq[q]
        key = pool[idx % len(pool)]
        self.dq[q][1] = idx + 1
        deps = self._deps(reads, writes)
        if self.cnt[key] > 0:
            deps.append((key, self.cnt[key]))
        self._need(q, deps)
        ins = self.eng[q].dma_start(out=out, in_=in_, **kw)
        self.cnt[key] += 16
        ins.then_inc(self.sems[key], 16)
        tok = (key, self.cnt[key])
        self._commit(tok, reads, writes)
        self.n_inst += 1
        return tok

    def finish(self, regions):
        self._need("sp", [r.w for r in regions])

    def barrier(self):
        allc = [(k, v) for k, v in self.cnt.items() if v > 0 and k not in self.dead]
        for e in self.eng:
            self._need(e, allc)
        for e in self.eng:
            key = self.cur[e]
            if self.cnt[key] > 12000:
                self.dead.add(key)
                self.epoch += 1
                nk = "%s#%d" % (e, self.epoch)
                self.sems[nk] = self.stack.enter_context(self.nc.semaphore("prog_%s_%d" % (e, self.epoch)))
                self.cnt[nk] = 0
                self.cur[e] = nk


def PT(t, name):
    x = T(t, name)
    x.r.excl = True
    return x


class T:
    def __init__(self, t, name):
        self.t = t
        self.r = R(name)

    def __getitem__(self, k):
        return self.t[k]


def phase_A(nc, P, Dr, outs):
    x_full, x_s, w_in = Dr["x_full"], Dr["x_s"], Dr["w_in"]
    p_full, p_samp = Dr["p_full"], Dr["p_samp"]
    with ExitStack() as st:
        def sb(name, shape, dt=F32):
            return T(st.enter_context(nc.sbuf_tensor("a_" + name, list(shape), dt)), name)

        def ps(name, shape, dt=F32):
            return PT(st.enter_context(nc.psum_tensor("a_" + name, list(shape), dt)), name)

        ident = sb("ident", [128, 128], BF16)
        P.op("pool", lambda e: e.memset(ident[:], 0.0), writes=[ident.r])
        P.op("pool", lambda e: e.affine_select(ident[:], ident[:], [[-1, 128]], ALU.not_equal, 1.0,
                                               base=0, channel_multiplier=1),
             reads=[ident.r], writes=[ident.r])
        ropeT = sb("ropeT", [128, NT, 16])
        P.dma("sp", ropeT[:], Dr["rope_p"].rearrange("(n p) d -> p n d", p=128), writes=[ropeT.r])
        ropeS = sb("ropeS", [NS, 16])
        P.dma("sp", ropeS[:], Dr["rope_s"][:, :], writes=[ropeS.r])

        wA = sb("wA", [128, 8, NA], BF16)
        for kc in range(8):
            P.dma("pool", wA[:, kc, 0:R_COLS], w_in[kc * 128:(kc + 1) * 128, 0:R_COLS], writes=[wA.r])
            P.dma("pool", wA[:, kc, R_COLS:NA], w_in[kc * 128:(kc + 1) * 128, KV0:KV0 + NKV], writes=[wA.r])

        xb = [sb("xb%d" % i, [128, D], BF16) for i in range(2)]
        xT = [sb("xT%d" % i, [128, 8, 128], BF16) for i in range(2)]
        pt = [sb("ptile%d" % i, [128, NA]) for i in range(2)]
        rtmp = [sb("rtmp%d" % i, [128, 4, 6, 8]) for i in range(2)]
        ptr = [ps("ptr%d" % i, [128, 8, 128], BF16) for i in range(2)]
        pmm = [ps("pmm%d" % i, [128, 512]) for i in range(5)]
        pmm_i = [0]

        def rope_apply(tile_t, c0, cs_ap, tmp, n):
            v = tile_t.t[0:n, c0:c0 + 768].rearrange("p (a k g d) -> p a k g d", a=3, k=2, g=2)
            x1 = v[:, :, 0, :, 0:8]
            x2 = v[:, :, 0, :, 8:16]
            cos = cs_ap[:, 0:8].unsqueeze(1).unsqueeze(1).to_broadcast([n, 3, 2, 8])
            sin = cs_ap[:, 8:16].unsqueeze(1).unsqueeze(1).to_broadcast([n, 3, 2, 8])
            t = tmp.t[0:n]
            a1, a2, a3, a4 = [t[:, i, :, :].rearrange("p (a g) d -> p a g d", a=3) for i in range(4)]
            rd = [tile_t.r, tmp.r]
            P.op("dve", lambda e: e.tensor_tensor(a1, x1, cos, ALU.mult), reads=rd, writes=[tmp.r])
            P.op("dve", lambda e: e.tensor_tensor(a2, x2, sin, ALU.mult), reads=rd, writes=[tmp.r])
            P.op("dve", lambda e: e.tensor_tensor(a3, x2, cos, ALU.mult), reads=rd, writes=[tmp.r])
            P.op("dve", lambda e: e.tensor_tensor(a4, x1, sin, ALU.mult), reads=rd, writes=[tmp.r])
            P.op("dve", lambda e: e.tensor_tensor(x1, a1, a2, ALU.subtract), reads=rd, writes=[tile_t.r])
            P.op("dve", lambda e: e.tensor_tensor(x2, a3, a4, ALU.add), reads=rd, writes=[tile_t.r])

        r_pfull = R("p_full")
        r_out = R("outsA")
        outs.append(r_out)
        for ti in range(NT):
            b = ti % 2
            t0 = ti * 128
            P.dma("pool", xb[b][:], x_full[t0:t0 + 128, :], writes=[xb[b].r])
            for kc in range(8):
                P.op("pe", lambda e: e.transpose(ptr[b][:, kc, :], xb[b][:, kc * 128:(kc + 1) * 128], ident[:]),
                     reads=[xb[b].r, ident.r], writes=[ptr[b].r])
            P.op("act", lambda e: e.copy(xT[b][:], ptr[b][:]), reads=[ptr[b].r], writes=[xT[b].r])
            for nch in range(5):
                pm = pmm[pmm_i[0] % 5]
                pmm_i[0] += 1
                for kc in range(8):
                    P.op("pe", lambda e: e.matmul(pm[:], xT[b][:, kc, :], wA[:, kc, nch * 512:(nch + 1) * 512],
                                                  start=(kc == 0), stop=(kc == 7)),
                         reads=[xT[b].r, wA.r], writes=[pm.r])
                dst = pt[b][:, nch * 512:(nch + 1) * 512]
                if nch % 2 == 0:
                    P.op("dve", lambda e: e.tensor_copy(dst, pm[:]), reads=[pm.r], writes=[pt[b].r])
                else:
                    P.op("act", lambda e: e.copy(dst, pm[:]), reads=[pm.r], writes=[pt[b].r])
            rope_apply(pt[b], R_COLS, ropeT[:, ti, :], rtmp[b], 128)
            P.dma("sp", p_full[t0:t0 + 128, :], pt[b][:], reads=[pt[b].r], writes=[r_pfull])
            P.dma("sp", Dr["o_cmp_p"][t0:t0 + 128, :], pt[b][:, R_COLS:R_COLS + 256], reads=[pt[b].r], writes=[r_out])
            P.dma("sp", Dr["o_sel_p"][t0:t0 + 128, :], pt[b][:, R_COLS + 256:R_COLS + 512], reads=[pt[b].r], writes=[r_out])
            if ti >= NT - 4:
                w0 = (ti - (NT - 4)) * 128
                P.dma("sp", Dr["o_win_p"][w0:w0 + 128, :], pt[b][:, R_COLS + 512:R_COLS + 768], reads=[pt[b].r], writes=[r_out])
            if ti == NT - 1:
                P.dma("sp", Dr["o_shift_p"][0:1, :], pt[b][127:128, 0:R_COLS], reads=[pt[b].r], writes=[r_out])

        xsb = sb("xsb", [NS, D], BF16)
        xsT = sb("xsT", [128, 8, NS], BF16)
        psT = ptr[0]
        P.dma("pool", xsb[:], x_s[:, :], writes=[xsb.r])
        for kc in range(8):
            P.op("pe", lambda e: e.transpose(psT[:, kc, 0:NS], xsb[:, kc * 128:(kc + 1) * 128], ident[0:NS, 0:NS]),
                 reads=[xsb.r, ident.r], writes=[psT.r])
        P.op("act", lambda e: e.copy(xsT[:], psT[:, :, 0:NS]), reads=[psT.r], writes=[xsT.r])
        psamp = sb("psamp", [NS, IN_COLS])
        wS = [sb("wS%d" % i, [128, 8, 512], BF16) for i in range(2)]
        ncht = (IN_COLS + 511) // 512
        for nch in range(ncht):
            c0 = nch * 512
            cw = min(512, IN_COLS - c0)
            wb = wS[nch % 2]
            for kc in range(8):
                P.dma("pool", wb[:, kc, 0:cw], w_in[kc * 128:(kc + 1) * 128, c0:c0 + cw], writes=[wb.r])
            pm = pmm[pmm_i[0] % 5]
            pmm_i[0] += 1
            for kc in range(8):
                P.op("pe", lambda e: e.matmul(pm[0:NS, 0:cw], xsT[:, kc, :], wb[:, kc, 0:cw],
                                              start=(kc == 0), stop=(kc == 7)),
                     reads=[xsT.r, wb.r], writes=[pm.r])
            P.op("dve", lambda e: e.tensor_copy(psamp[:, c0:c0 + cw], pm[0:NS, 0:cw]), reads=[pm.r], writes=[psamp.r])
        rope_apply(psamp, KV0, ropeS[:, :], rtmp[0], NS)
        r_psamp = R("p_samp")
        P.dma("sp", p_samp[:, :], psamp[:], reads=[psamp.r], writes=[r_psamp])
        P.dma("sp", Dr["o_cmp_s"][:, :], psamp[:, KV0:KV0 + 256], reads=[psamp.r], writes=[r_out])
        P.dma("sp", Dr["o_sel_s"][:, :], psamp[:, KV0 + 256:KV0 + 512], reads=[psamp.r], writes=[r_out])
        for bb in range(SB):
            P.dma("sp", Dr["o_win_s"][bb, 508:512, :], psamp[bb * DS:(bb + 1) * DS, KV0 + 512:KV0 + 768],
                  reads=[psamp.r], writes=[r_out])
            P.dma("sp", Dr["o_win_s"][bb, 0:508, :], Dr["cache_win"][bb, 4:512, :], writes=[r_out])
            P.dma("sp", Dr["o_shift_s"][bb:bb + 1, :], psamp[bb * DS + DS - 1:bb * DS + DS, 0:R_COLS],
                  reads=[psamp.r], writes=[r_out])
        Dr["r_pfull"] = r_pfull
        Dr["r_psamp"] = r_psamp
        P.barrier()

VEC_OFF = {"mu": (0, 1792), "w0": (1792, 512), "a0": (2304, 512), "kk": (2816, 512), "ka": (3328, 512),
           "gng": (3840, 512), "gnb": (4352, 512), "rk": (4864, 512)}
NVEC = 5376
NMASK = 128 + 128 + 512 + 128 + 128


def phase_B(nc, P, Dr, outs):
    P.pe_selfsync = True
    with ExitStack() as st:
        def sb(name, shape, dt=F32):
            return T(st.enter_context(nc.sbuf_tensor("b_" + name, list(shape), dt)), name)

        def ps(name, shape, dt=F32):
            return PT(st.enter_context(nc.psum_tensor("b_" + name, list(shape), dt)), name)

        vecs = sb("vecs", [128, NVEC])
        P.dma("sp", vecs[:], Dr["vecs"][:, :], writes=[vecs.r])
        V = lambda k: vecs[:, VEC_OFF[k][0]:VEC_OFF[k][0] + VEC_OFF[k][1]]
        wlora = sb("wlora", [128, 512])
        P.dma("sp", wlora[0:64, :], Dr["w_w2"][:, :], writes=[wlora.r])
        P.dma("sp", wlora[64:128, :], Dr["w_a2"][:, :], writes=[wlora.r])
        gw2 = sb("gw2", [128, 512])
        P.dma("sp", gw2[:], Dr["g_w2"][:, :], writes=[gw2.r])
        masks = sb("masks", [128, NMASK])
        P.dma("sp", masks[:], Dr["masks"][:, :], writes=[masks.r])
        Lblk = masks[:, 0:128]
        Oblk = masks[:, 128:256]
        maskMA2 = masks[:, 256:768]
        maskNT = masks[:, 768:896]
        identF = masks[:, 896:1024]
        tmask = sb("tmask", [128, 1])
        P.dma("sp", tmask[:], Dr["tmask"][:, :], writes=[tmask.r])
        ones = sb("onesc", [128, 1])
        P.op("pool", lambda e: e.memset(ones[:], 1.0), writes=[ones.r])

        pr = sb("pr", [128, R_COLS])
        prev = sb("prev", [128, R_COLS])
        xm = sb("xm", [128, R_COLS])
        la = sb("la", [128, 256])
        laT = sb("laT", [128, 256])
        tA = sb("tA", [128, 512])
        tB = sb("tB", [128, 512])
        lw = sb("lw", [128, 512])
        aicl = sb("aicl", [128, 512])
        g_sb = sb("g_sb", [128, 512])
        kk = sb("kk", [128, 512])
        kkn = sb("kkn", [128, 512])
        kh = sb("kh", [128, 512])
        a_s = sb("a_s", [128, 512])
        b_s = sb("b_s", [128, 512])
        ss = sb("ss", [128, 8])
        rinv = sb("rinv", [128, 8])
        cum_sb = sb("cum_sb", [128, 512])
        e_sb = sb("e_sb", [128, 512])
        einv = sb("einv", [128, 512])
        ea = sb("ea", [128, 512])
        ec = sb("ec", [128, 512])
        at = sb("at", [128, 512])
        rt = sb("rt", [128, 512])
        bt = sb("bt", [128, 512])
        kt = sb("kt", [128, 512])
        bh = sb("bh", [128, 512])
        kh2 = sb("kh2", [128, 512])
        wc = sb("wc", [128, 8])
        FM_ar = sb("FM_ar", [128, 4, 256])
        FM_b = sb("FM_b", [128, 4, 128])
        FM_k = sb("FM_k", [128, 4, 128])
        MM = [sb("MM%d" % h, [128, 512]) for h in range(8)]
        TmA = [sb("TmA%d" % g, [128, 4, 128]) for g in range(2)]
        XAs = [[sb("XA%d_%d" % (g, i), [128, 4, 128]) for i in range(2)] for g in range(2)]
        XTAs = [[sb("XTA%d_%d" % (g, i), [128, 4, 128]) for i in range(2)] for g in range(2)]
        PMAs = [sb("PMA%d" % g, [128, 4, 128]) for g in range(2)]
        ZT_sb = sb("ZT_sb", [128, 512])
        UT_sb = sb("UT_sb", [128, 512])
        y_sb = sb("y_sb", [128, 512])
        yc = sb("yc", [128, 512])
        st1 = sb("st1", [128, 8])
        st2 = sb("st2", [128, 8])
        yo = sb("yo", [128, 512], BF16)
        ST = sb("ST", [128, 256])
        Sio = sb("Sio", [64, 512])

        pg = [ps("pg%d" % i, [128, 512]) for i in range(2)]
        pi = ps("pi", [128, 512])
        pv = [ps("pv%d" % i, [128, 512]) for i in range(2)]
        ZT_ps = ps("ZT_ps", [128, 512])
        UT_ps = ps("UT_ps", [128, 512])
        SN_ps = ps("SN_ps", [128, 512])

        def TT(eng, out, in0, in1, op, rd, wr):
            P.op(eng, lambda e: e.tensor_tensor(out, in0, in1, op), reads=rd, writes=wr)

        def ACT(out, in_, func, rd, wr, **kw):
            P.op("act", lambda e: e.activation(out, in_, func, **kw), reads=rd, writes=wr)

        def MMUL(out, lhsT, rhs, rd, wr, start=True, stop=True):
            P.op("pe", lambda e: e.matmul(out, lhsT, rhs, start=start, stop=stop), reads=rd, writes=wr)

        def TR(out, in_, idn, rd, wr):
            P.op("pe", lambda e: e.transpose(out, in_, idn), reads=rd + [masks.r], writes=wr)

        def load_state(src_ap):
            P.dma("sp", Sio[:].rearrange("i (h j) -> i h j", h=8), src_ap.rearrange("h i j -> i h j"), writes=[Sio.r])
            for hp in range(4):
                TR(pg[0][:, hp * 64:(hp + 1) * 64], Sio[:, hp * 128:(hp + 1) * 128], identF[0:64, 0:64], [Sio.r], [pg[0].r])
            P.op("dve", lambda e: e.tensor_copy(ST[:], pg[0][:, 0:256]), reads=[pg[0].r], writes=[ST.r])

        def store_state(dst_ap, rout):
            for hp in range(4):
                TR(pg[0][0:64, hp * 128:(hp + 1) * 128], ST[:, hp * 64:(hp + 1) * 64], identF, [ST.r], [pg[0].r])
            P.op("dve", lambda e: e.tensor_copy(Sio[:], pg[0][0:64, :]), reads=[pg[0].r], writes=[Sio.r])
            P.dma("sp", dst_ap.rearrange("h i j -> i h j"), Sio[:].rearrange("i (h j) -> i h j", h=8), reads=[Sio.r], writes=[rout])

        def rwkv_tile(load_fn, sample, pre_chunk, post_chunk, y_store):
            load_fn(pr, prev)
            TT("dve", prev[:], prev[:], pr[:], ALU.subtract, [prev.r, pr.r], [prev.r])
            TT("dve", prev[:], prev[:], V("mu"), ALU.mult, [prev.r, vecs.r], [prev.r])
            TT("dve", xm[:], prev[:], pr[:], ALU.add, [prev.r, pr.r], [xm.r])
            r_ = xm[:, 0:512]
            k_ = xm[:, 512:1024]
            v_ = xm[:, 1024:1536]
            ACT(la[:, 0:64], xm[:, 1536:1600], AF.Tanh, [xm.r], [la.r])
            ACT(la[:, 64:128], xm[:, 1600:1664], AF.Copy, [xm.r], [la.r])
            ACT(la[:, 128:256], xm[:, 1664:1792], AF.Sigmoid, [xm.r], [la.r])
            TR(pg[0][:, 0:128], la[:, 0:128], identF, [la.r], [pg[0].r])
            TR(pg[0][:, 128:256], la[:, 128:256], identF, [la.r], [pg[0].r])
            ACT(laT[:], pg[0][:, 0:256], AF.Copy, [pg[0].r], [laT.r])
            MMUL(pg[1][:], laT[0:64, 0:128], wlora[0:64, :], [laT.r, wlora.r], [pg[1].r])
            TT("dve", tA[:], pg[1][:], V("w0"), ALU.add, [pg[1].r, vecs.r], [tA.r])
            ACT(tA[:], tA[:], AF.Sigmoid, [tA.r], [tA.r])
            if sample:
                P.op("dve", lambda e: e.tensor_scalar(lw[:], tA[:], -0.6065306597126334, tmask[:, 0:1], ALU.mult, ALU.mult),
                     reads=[tA.r, tmask.r], writes=[lw.r])
            else:
                P.op("dve", lambda e: e.tensor_scalar(lw[:], tA[:], -0.6065306597126334, None, ALU.mult),
                     reads=[tA.r], writes=[lw.r])
            MMUL(pg[0][:], laT[64:128, 0:128], wlora[64:128, :], [laT.r, wlora.r], [pg[0].r])
            TT("dve", aicl[:], pg[0][:], V("a0"), ALU.add, [pg[0].r, vecs.r], [aicl.r])
            ACT(aicl[:], aicl[:], AF.Sigmoid, [aicl.r], [aicl.r])
            MMUL(pg[1][:], laT[:, 128:256], gw2[:], [laT.r, gw2.r], [pg[1].r])
            ACT(g_sb[:], pg[1][:], AF.Copy, [pg[1].r], [g_sb.r])
            TT("dve", kk[:], k_, V("kk"), ALU.mult, [xm.r, vecs.r], [kk.r])
            TT("dve", kkn[:], kk[:], kk[:], ALU.mult, [kk.r], [kkn.r])
            P.op("dve", lambda e: e.tensor_reduce(ss[:], kkn[:].rearrange("p (h j) -> p h j", h=8), AX.X, ALU.add),
                 reads=[kkn.r], writes=[ss.r])
            P.op("dve", lambda e: e.tensor_scalar(ss[:], ss[:], 1e-24, None, ALU.max), reads=[ss.r], writes=[ss.r])
            ACT(rinv[:], ss[:], AF.Sqrt, [ss.r], [rinv.r])
            P.op("dve", lambda e: e.reciprocal(rinv[:], rinv[:]), reads=[rinv.r], writes=[rinv.r])
            TT("dve", kkn[:].rearrange("p (h j) -> p h j", h=8), kk[:].rearrange("p (h j) -> p h j", h=8),
               rinv[:].unsqueeze(2).to_broadcast([128, 8, 64]), ALU.mult, [kk.r, rinv.r], [kkn.r])
            P.op("dve", lambda e: e.scalar_tensor_tensor(kh[:], aicl[:], -1.0, V("ka"), ALU.add, ALU.mult),
                 reads=[aicl.r, vecs.r], writes=[kh.r])
            P.op("dve", lambda e: e.scalar_tensor_tensor(kh[:], kh[:], 1.0, k_, ALU.add, ALU.mult),
                 reads=[kh.r, xm.r], writes=[kh.r])
            P.op("dve", lambda e: e.tensor_scalar(a_s[:], kkn[:], -1.0, None, ALU.mult), reads=[kkn.r], writes=[a_s.r])
            TT("dve", b_s[:], kkn[:], aicl[:], ALU.mult, [kkn.r, aicl.r], [b_s.r])
            if sample:
                P.op("dve", lambda e: e.tensor_scalar(kh[:], kh[:], tmask[:, 0:1], None, ALU.mult), reads=[kh.r, tmask.r], writes=[kh.r])
                P.op("dve", lambda e: e.tensor_scalar(b_s[:], b_s[:], tmask[:, 0:1], None, ALU.mult), reads=[b_s.r, tmask.r], writes=[b_s.r])
            MMUL(pg[0][:], Lblk, lw[:], [masks.r, lw.r], [pg[0].r])
            MMUL(pg[1][:], Oblk, lw[:], [masks.r, lw.r], [pg[1].r])
            ACT(cum_sb[:], pg[0][:], AF.Copy, [pg[0].r], [cum_sb.r])
            ACT(e_sb[:], pg[0][:], AF.Exp, [pg[0].r], [e_sb.r])
            ACT(einv[:], pg[0][:], AF.Exp, [pg[0].r], [einv.r], scale=-1.0)
            TT("dve", tA[:], pg[0][:], lw[:], ALU.subtract, [pg[0].r, lw.r], [tA.r])
            ACT(ea[:], tA[:], AF.Exp, [tA.r], [ea.r])
            TT("dve", tB[:], pg[1][:], cum_sb[:], ALU.subtract, [pg[1].r, cum_sb.r], [tB.r])
            ACT(ec[:], tB[:], AF.Exp, [tB.r], [ec.r])
            TT("dve", at[:], a_s[:], ea[:], ALU.mult, [a_s.r, ea.r], [at.r])
            TT("dve", rt[:], r_, e_sb[:], ALU.mult, [xm.r, e_sb.r], [rt.r])
            TT("dve", bt[:], b_s[:], einv[:], ALU.mult, [b_s.r, einv.r], [bt.r])
            TT("dve", kt[:], kh[:], einv[:], ALU.mult, [kh.r, einv.r], [kt.r])
            TT("dve", bh[:], b_s[:], ec[:], ALU.mult, [b_s.r, ec.r], [bh.r])
            TT("dve", kh2[:], kh[:], ec[:], ALU.mult, [kh.r, ec.r], [kh2.r])
            for c2 in range(2):
                rows = slice(c2 * 64, c2 * 64 + 64)
                for hp in range(4):
                    MMUL(SN_ps[:, 256 + c2 * 4 + hp:256 + c2 * 4 + hp + 1], lw[rows, hp * 128:(hp + 1) * 128], ones[rows, 0:1],
                         [lw.r, ones.r], [SN_ps.r])
            ACT(wc[:], SN_ps[:, 256:264], AF.Exp, [SN_ps.r], [wc.r])
            for qi, q in enumerate((at, rt)):
                for hp in range(4):
                    TR(pg[qi][:, hp * 128:(hp + 1) * 128], q[:, hp * 128:(hp + 1) * 128], identF, [q.r], [pg[qi].r])
                P.op("act" if qi == 0 else "dve",
                     (lambda e: e.copy(FM_ar[:, :, 0:128], pg[0][:].rearrange("p (h t) -> p h t", h=4))) if qi == 0 else
                     (lambda e: e.tensor_copy(FM_ar[:, :, 128:256], pg[1][:].rearrange("p (h t) -> p h t", h=4))),
                     reads=[pg[qi].r], writes=[FM_ar.r])
            for qi, (q, dst) in enumerate(((bt, FM_b), (kt, FM_k))):
                for hp in range(4):
                    TR(pg[qi][:, hp * 128:(hp + 1) * 128], q[:, hp * 128:(hp + 1) * 128], identF, [q.r], [pg[qi].r])
                if qi == 0:
                    P.op("act", lambda e: e.copy(dst[:].rearrange("p h t -> p (h t)"), pg[0][:]), reads=[pg[0].r], writes=[dst.r])
                else:
                    P.op("dve", lambda e: e.tensor_copy(dst[:].rearrange("p h t -> p (h t)"), pg[1][:]), reads=[pg[1].r], writes=[dst.r])
            for h in range(8):
                hp, h2 = h // 2, h % 2
                rows = slice(h2 * 64, h2 * 64 + 64)
                MMUL(pi[:, 0:256], FM_b[rows, hp, :], FM_ar[rows, hp, :], [FM_b.r, FM_ar.r], [pi.r])
                MMUL(pi[:, 256:512], FM_k[rows, hp, :], FM_ar[rows, hp, :], [FM_k.r, FM_ar.r], [pi.r])
                TT("dve", MM[h][:], pi[:], maskMA2, ALU.mult, [pi.r, masks.r], [MM[h].r])
            banks = [(pv[0], pv[1], pi), (ZT_ps, UT_ps, SN_ps)]
            for grp in range(2):
                bX, bXT, bP = banks[grp]
                XA, XTA, PMA = XAs[grp], XTAs[grp], PMAs[grp]
                for q4 in range(4):
                    h = grp * 4 + q4
                    hp, h2 = h // 2, h % 2
                    rows = slice(h2 * 64, h2 * 64 + 64)
                    MMUL(bXT[:, q4 * 128:(q4 + 1) * 128], FM_ar[rows, hp, 0:128], FM_b[rows, hp, :], [FM_ar.r, FM_b.r], [bXT.r])
                    P.op("dve", lambda e: e.tensor_copy(XA[0][:, q4, :], MM[h][:, 0:128]), reads=[MM[h].r], writes=[XA[0].r])
                    TT("dve", PMA[:, q4, :], MM[h][:, 0:128], identF, ALU.add, [MM[h].r, masks.r], [PMA.r])
                TT("dve", XTA[0][:], bXT[:].rearrange("p (h t) -> p h t", h=4), maskNT.unsqueeze(1).to_broadcast([128, 4, 128]), ALU.mult,
                   [bXT.r, masks.r], [XTA[0].r])
            for k in range(1, 6):
                for grp in range(2):
                    bX, bXT, bP = banks[grp]
                    XA, XTA, PMA = XAs[grp], XTAs[grp], PMAs[grp]
                    Xo, XTo = XA[(k - 1) % 2], XTA[(k - 1) % 2]
                    Xn, XTn = XA[k % 2], XTA[k % 2]
                    for q4 in range(4):
                        MMUL(bXT[:, q4 * 128:(q4 + 1) * 128], Xo[:, q4, :], XTo[:, q4, :], [Xo.r, XTo.r], [bXT.r])
                    if k < 5:
                        for q4 in range(4):
                            MMUL(bX[:, q4 * 128:(q4 + 1) * 128], XTo[:, q4, :], Xo[:, q4, :], [Xo.r, XTo.r], [bX.r])
                    P.op("act", lambda e: e.copy(XTn[:].rearrange("p h t -> p (h t)"), bXT[:]), reads=[bXT.r], writes=[XTn.r])
                    if k < 5:
                        P.op("dve", lambda e: e.tensor_copy(Xn[:].rearrange("p h t -> p (h t)"), bX[:]), reads=[bX.r], writes=[Xn.r])
                    for q4 in range(4):
                        MMUL(bP[:, q4 * 128:(q4 + 1) * 128], XTn[:, q4, :], PMA[:, q4, :], [XTn.r, PMA.r], [bP.r])
                    dstP = TmA[grp] if k == 5 else PMA
                    TT("dve", dstP[:].rearrange("p h t -> p (h t)"), bP[:], PMA[:].rearrange("p h t -> p (h t)"), ALU.add,
                       [bP.r, PMA.r], [dstP.r])
            for c2 in range(2):
                cs = slice(c2 * 64, c2 * 64 + 64)
                cc = slice(c2 * 64, c2 * 64 + 64)
                if pre_chunk is not None:
                    pre_chunk(c2)
                for h in range(8):
                    hp, h2 = h // 2, h % 2
                    rows = slice(h2 * 64, h2 * 64 + 64)
                    hc = slice(h * 64, h * 64 + 64)
                    Sh = ST[rows, hp * 64:(hp + 1) * 64]
                    MMUL(ZT_ps[cs, hc], FM_ar[rows, hp, cc], Sh, [FM_ar.r, ST.r], [ZT_ps.r], start=True, stop=False)
                    MMUL(ZT_ps[cs, hc], MM[h][cs, 256 + c2 * 64:256 + c2 * 64 + 64], xm[cs, 1024 + h * 64:1024 + h * 64 + 64],
                         [MM[h].r, xm.r], [ZT_ps.r], start=False, stop=True)
                P.op("act", lambda e: e.copy(ZT_sb[cs, :], ZT_ps[cs, :]), reads=[ZT_ps.r], writes=[ZT_sb.r])
                for h in range(8):
                    hc = slice(h * 64, h * 64 + 64)
                    MMUL(UT_ps[cs, hc], TmA[h // 4][cs, h % 4, cc], ZT_sb[cs, hc], [TmA[h // 4].r, ZT_sb.r], [UT_ps.r])
                P.op("dve", lambda e: e.tensor_copy(UT_sb[cs, :], UT_ps[cs, :]), reads=[UT_ps.r], writes=[UT_sb.r])
                yps = pg[c2]
                for h in range(8):
                    hp, h2 = h // 2, h % 2
                    rows = slice(h2 * 64, h2 * 64 + 64)
                    hc = slice(h * 64, h * 64 + 64)
                    Sh = ST[rows, hp * 64:(hp + 1) * 64]
                    vh = xm[cs, 1024 + h * 64:1024 + h * 64 + 64]
                    MMUL(yps[cs, hc], FM_ar[rows, hp, 128 + c2 * 64:128 + c2 * 64 + 64], Sh, [FM_ar.r, ST.r], [yps.r], start=True, stop=False)
                    MMUL(yps[cs, hc], MM[h][cs, 128 + c2 * 64:128 + c2 * 64 + 64], UT_sb[cs, hc], [MM[h].r, UT_sb.r], [yps.r], start=False, stop=False)
                    MMUL(yps[cs, hc], MM[h][cs, 384 + c2 * 64:384 + c2 * 64 + 64], vh, [MM[h].r, xm.r], [yps.r], start=False, stop=True)
                    MMUL(SN_ps[rows, hp * 64:(hp + 1) * 64], bh[cs, hc], UT_sb[cs, hc], [bh.r, UT_sb.r], [SN_ps.r], start=True, stop=False)
                    MMUL(SN_ps[rows, hp * 64:(hp + 1) * 64], kh2[cs, hc], vh, [kh2.r, xm.r], [SN_ps.r], start=False, stop=True)
                P.op("act", lambda e: e.copy(y_sb[cs, :], yps[cs, :]), reads=[yps.r], writes=[y_sb.r])
                TT("dve", ST[:].rearrange("p (h i) -> p h i", h=4), ST[:].rearrange("p (h i) -> p h i", h=4),
                   wc[:, c2 * 4:c2 * 4 + 4].unsqueeze(2).to_broadcast([128, 4, 64]), ALU.mult, [ST.r, wc.r], [ST.r])
                TT("dve", ST[:], ST[:], SN_ps[:, 0:256], ALU.add, [ST.r, SN_ps.r], [ST.r])
                if post_chunk is not None:
                    post_chunk(c2)
            y3 = y_sb[:].rearrange("p (h j) -> p h j", h=8)
            yc3 = yc[:].rearrange("p (h j) -> p h j", h=8)
            P.op("dve", lambda e: e.tensor_reduce(st1[:], y3, AX.X, ALU.add), reads=[y_sb.r], writes=[st1.r])
            P.op("dve", lambda e: e.tensor_scalar(st1[:], st1[:], 1.0 / 64, None, ALU.mult), reads=[st1.r], writes=[st1.r])
            TT("dve", yc3, y3, st1[:].unsqueeze(2).to_broadcast([128, 8, 64]), ALU.subtract, [y_sb.r, st1.r], [yc.r])
            TT("dve", tA[:], yc[:], yc[:], ALU.mult, [yc.r], [tA.r])
            P.op("dve", lambda e: e.tensor_reduce(st2[:], tA[:].rearrange("p (h j) -> p h j", h=8), AX.X, ALU.add), reads=[tA.r], writes=[st2.r])
            P.op("dve", lambda e: e.tensor_scalar(st2[:], st2[:], 1.0 / 64, 64e-5, ALU.mult, ALU.add), reads=[st2.r], writes=[st2.r])
            ACT(st2[:], st2[:], AF.Sqrt, [st2.r], [st2.r])
            P.op("dve", lambda e: e.reciprocal(st2[:], st2[:]), reads=[st2.r], writes=[st2.r])
            TT("dve", yc3, yc3, st2[:].unsqueeze(2).to_broadcast([128, 8, 64]), ALU.mult, [yc.r, st2.r], [yc.r])
            TT("dve", yc[:], yc[:], V("gng"), ALU.mult, [yc.r, vecs.r], [yc.r])
            TT("dve", yc[:], yc[:], V("gnb"), ALU.add, [yc.r, vecs.r], [yc.r])
            TT("dve", tB[:], r_, kh[:], ALU.mult, [xm.r, kh.r], [tB.r])
            TT("dve", tB[:], tB[:], V("rk"), ALU.mult, [tB.r, vecs.r], [tB.r])
            P.op("dve", lambda e: e.tensor_reduce(st1[:], tB[:].rearrange("p (h j) -> p h j", h=8), AX.X, ALU.add), reads=[tB.r], writes=[st1.r])
            TT("dve", tB[:].rearrange("p (h j) -> p h j", h=8), v_.rearrange("p (h j) -> p h j", h=8),
               st1[:].unsqueeze(2).to_broadcast([128, 8, 64]), ALU.mult, [xm.r, st1.r], [tB.r])
            TT("dve", yc[:], yc[:], tB[:], ALU.add, [yc.r, tB.r], [yc.r])
            TT("dve", yo[:], yc[:], g_sb[:], ALU.mult, [yc.r, g_sb.r], [yo.r])
            y_store(yo)

        P.op("dve", lambda e: e.memset(ST[:], 0.0), writes=[ST.r])
        r_yr = R("y_r")
        r_out = R("outB")
        outs.append(r_out)
        p_full = Dr["p_full"]
        for ti in range(NT):
            t0 = ti * 128

            def load_fn(pr_t, prev_t, t0=t0, ti=ti):
                P.dma("sp", pr_t[:], p_full[t0:t0 + 128, 0:R_COLS], reads=[Dr["r_pfull"]], writes=[pr_t.r])
                if ti == 0:
                    P.op("dve", lambda e: e.memset(prev_t[0:1, :], 0.0), writes=[prev_t.r])
                    P.dma("sp", prev_t[1:128, :], p_full[0:127, 0:R_COLS], reads=[Dr["r_pfull"]], writes=[prev_t.r])
                else:
                    P.dma("sp", prev_t[:], p_full[t0 - 1:t0 + 127, 0:R_COLS], reads=[Dr["r_pfull"]], writes=[prev_t.r])

            def y_store(yo_t, t0=t0):
                P.dma("pool", Dr["y_r"][t0:t0 + 128, :], yo_t[:], reads=[yo_t.r], writes=[r_yr])

            post = None
            if ti == NT - 1:
                def post(c2):
                    if c2 == 1:
                        store_state(Dr["o_wkv_p"], r_out)
            rwkv_tile(load_fn, False, None, post, y_store)

        p_samp = Dr["p_samp"]
        for tp in range(SB // 2):
            def load_fn(pr_t, prev_t, tp=tp):
                P.op("dve", lambda e: e.memset(pr_t[:], 0.0), writes=[pr_t.r])
                P.op("dve", lambda e: e.memset(prev_t[:], 0.0), writes=[prev_t.r])
                for c2 in range(2):
                    bb = tp * 2 + c2
                    P.dma("sp", pr_t[c2 * 64:c2 * 64 + DS, :], p_samp[bb * DS:(bb + 1) * DS, 0:R_COLS],
                          reads=[Dr["r_psamp"]], writes=[pr_t.r])
                    P.dma("sp", prev_t[c2 * 64:c2 * 64 + 1, :], Dr["state_shift"][bb:bb + 1, :], writes=[prev_t.r])
                    P.dma("sp", prev_t[c2 * 64 + 1:c2 * 64 + DS, :], p_samp[bb * DS:(bb + 1) * DS - 1, 0:R_COLS],
                          reads=[Dr["r_psamp"]], writes=[prev_t.r])

            def pre(c2, tp=tp):
                load_state(Dr["state_wkv"][tp * 2 + c2])

            def post(c2, tp=tp):
                store_state(Dr["o_wkv_s"][tp * 2 + c2], r_out)

            def y_store(yo_t, tp=tp):
                for c2 in range(2):
                    bb = tp * 2 + c2
                    P.dma("pool", Dr["y_r_s"][bb * DS:(bb + 1) * DS, :], yo_t[c2 * 64:c2 * 64 + DS, :], reads=[yo_t.r], writes=[r_yr])

            rwkv_tile(load_fn, True, pre, post, y_store)
        Dr["r_yr"] = r_yr
        P.barrier()
    P.pe_selfsync = False

QG0 = 1792
GATE0 = 3072
NEGB = -30000.0


def phase_C(nc, P, Dr, outs):
    with ExitStack() as st:
        def sb(name, shape, dt=F32):
            return T(st.enter_context(nc.sbuf_tensor("c_" + name, list(shape), dt)), name)

        def ps(name, shape, dt=F32):
            return PT(st.enter_context(nc.psum_tensor("c_" + name, list(shape), dt)), name)

        def TT(eng, out, in0, in1, op, rd, wr):
            P.op(eng, lambda e: e.tensor_tensor(out, in0, in1, op), reads=rd, writes=wr)

        def MMUL(out, lhsT, rhs, rd, wr, start=True, stop=True):
            P.op("pe", lambda e: e.matmul(out, lhsT, rhs, start=start, stop=stop, skip_group_check=True), reads=rd, writes=wr)

        identb = sb("identb", [128, 128], BF16)
        P.dma("pool", identb[:], Dr["masks"][:, 896:1024], writes=[identb.r])
        identf = sb("identf", [128, 128])
        P.dma("sp", identf[:], Dr["masks"][:, 896:1024], writes=[identf.r])
        onesf = sb("onesf", [128, 128])
        P.op("pool", lambda e: e.memset(onesf[:], 1.0), writes=[onesf.r])
        NKT = 65
        ksT = [sb("ksT%d" % g, [65, NKT * 128], BF16) for g in range(2)]
        kwT = [sb("kwT%d" % g, [65, NKT * 128], BF16) for g in range(2)]
        vsw = sb("vsw", [128, NKT, 4, 65], BF16)
        kcmpT = [sb("kcmpT%d" % g, [65, 512], BF16) for g in range(2)]
        vcmp = sb("vcmp", [128, 4, 2, 65], BF16)
        nm = sb("nm", [128, 12])
        kmaxb = sb("kmaxb", [128, 1])
        for g in range(2):
            P.op("pool", lambda e: e.memset(ksT[g][64:65, :], 1.0), writes=[ksT[g].r])
            P.op("pool", lambda e: e.memset(kwT[g][64:65, :], 1.0), writes=[kwT[g].r])
            P.op("pool", lambda e: e.memset(kcmpT[g][64:65, :], 1.0), writes=[kcmpT[g].r])
        P.op("pool", lambda e: e.memset(vsw[:, :, :, 64:65], 1.0), writes=[vsw.r])
        P.op("pool", lambda e: e.memset(vcmp[:, :, :, 64:65], 1.0), writes=[vcmp.r])
        kvb = [sb("kvb%d" % i, [128, 768], BF16) for i in range(2)]
        sqt = sb("sqt", [128, 256])
        nt4 = sb("nt4", [128, 4])
        ptr = ps("ptr", [128, 8, 128], BF16)

        r_ya = R("y_a_own")

        def ingest_tile(kb, ti, do_cmp, do_sel, do_win, kcT, vcT, first):
            c0 = ti * 128
            rd = [kb.r, identb.r]
            ing = OPTS.get("ing", 15)
            do_cmp = do_cmp and bool(ing & 1)
            do_sel = do_sel and bool(ing & 2)
            do_win = do_win and bool(ing & 4)
            if do_cmp:
                P.op("pe", lambda e: e.transpose(ptr[:, 0, :], kb[:, 0:128], identb[:]), reads=rd, writes=[ptr.r])
                P.op("pe", lambda e: e.transpose(ptr[:, 1, :], kb[:, 128:256], identb[:]), reads=rd, writes=[ptr.r])
            if do_sel:
                P.op("pe", lambda e: e.transpose(ptr[0:64, 2, :], kb[:, 256:320], identb[:]), reads=rd, writes=[ptr.r])
                P.op("pe", lambda e: e.transpose(ptr[0:64, 3, :], kb[:, 320:384], identb[:]), reads=rd, writes=[ptr.r])
            if do_win:
                P.op("pe", lambda e: e.transpose(ptr[0:64, 4, :], kb[:, 512:576], identb[:]), reads=rd, writes=[ptr.r])
                P.op("pe", lambda e: e.transpose(ptr[0:64, 5, :], kb[:, 576:640], identb[:]), reads=rd, writes=[ptr.r])
            if do_cmp:
                P.op("act", lambda e: e.copy(kcT[:, c0:c0 + 128], ptr[:, 0, :]), reads=[ptr.r], writes=[kcT.r])
                P.op("act", lambda e: e.copy(vcT[:, c0:c0 + 128], ptr[:, 1, :]), reads=[ptr.r], writes=[vcT.r])
            if do_sel:
                P.op("dve", lambda e: e.tensor_copy(ksT[0][0:64, c0:c0 + 128], ptr[0:64, 2, :]), reads=[ptr.r], writes=[ksT[0].r])
                P.op("dve", lambda e: e.tensor_copy(ksT[1][0:64, c0:c0 + 128], ptr[0:64, 3, :]), reads=[ptr.r], writes=[ksT[1].r])
                P.op("act", lambda e: e.copy(vsw[:, ti, 0:2, 0:64], kb[:, 384:512].rearrange("p (g d) -> p g d", g=2)),
                     reads=[kb.r], writes=[vsw.r])
            if do_win:
                P.op("dve", lambda e: e.tensor_copy(kwT[0][0:64, c0:c0 + 128], ptr[0:64, 4, :]), reads=[ptr.r], writes=[kwT[0].r])
                P.op("dve", lambda e: e.tensor_copy(kwT[1][0:64, c0:c0 + 128], ptr[0:64, 5, :]), reads=[ptr.r], writes=[kwT[1].r])
                P.op("act", lambda e: e.copy(vsw[:, ti, 2:4, 0:64], kb[:, 640:768].rearrange("p (g d) -> p g d", g=2)),
                     reads=[kb.r], writes=[vsw.r])
            if not (ing & 8):
                return
            kk = kb[:, 256:768].rearrange("p (a r) -> p a r", a=2)[:, :, 0:128]
            P.op("dve", lambda e: e.tensor_tensor(sqt[:].rearrange("p (a r) -> p a r", a=2), kk, kk, ALU.mult), reads=[kb.r], writes=[sqt.r])
            P.op("dve", lambda e: e.tensor_reduce(nt4[:], sqt[:].rearrange("p (a d) -> p a d", a=4), AX.X, ALU.add), reads=[sqt.r], writes=[nt4.r])
            if first:
                P.op("dve", lambda e: e.tensor_copy(nm[:, 0:4], nt4[:]), reads=[nt4.r], writes=[nm.r])
            else:
                TT("dve", nm[:, 0:4], nm[:, 0:4], nt4[:], ALU.max, [nm.r, nt4.r], [nm.r])

        def compress_all(kcT, vcT):
            with ExitStack() as st2:
                def sb2(name, shape, dt=F32):
                    return T(st2.enter_context(nc.sbuf_tensor("c2_" + name + "_%d" % P.n_inst, list(shape), dt)), name)

                def ps2(name, shape, dt=F32):
                    return PT(st2.enter_context(nc.psum_tensor("c2_" + name + "_%d" % P.n_inst, list(shape), dt)), name)
                w1c = sb2("w1c", [128, 2, 32, 256], BF16)
                for kv in range(2):
                    src = Dr["cmp_w1"][kv].rearrange("(j d) h -> d j h", d=64)
                    P.dma("pool", w1c[0:64, kv, :, :], src, writes=[w1c.r])
                    P.dma("pool", w1c[64:128, kv, :, :], src, writes=[w1c.r])
                w2c = sb2("w2c", [128, 2, 2, 64], BF16)
                for kv in range(2):
                    P.dma("pool", w2c[:, kv, :, :], Dr["cmp_w2"][kv].rearrange("(c p) d -> p c d", p=128), writes=[w2c.r])
                pef = sb2("pef", [32, 2, 64])
                P.dma("sp", pef[:], Dr["cmp_pe"].rearrange("k j d -> j k d"), writes=[pef.r])
                peT = sb2("peT", [64, 2, 32], BF16)
                b1c = sb2("b1c", [128, 2, 2])
                P.dma("sp", b1c[:], Dr["cmp_b1T"][:, :, :], writes=[b1c.r])
                b2k = sb2("b2k", [64, 1])
                P.dma("sp", b2k[:], Dr["cmp_b2T"][:, :], writes=[b2k.r])
                b2v = sb2("b2v", [128, 64])
                P.dma("sp", b2v[:], Dr["cmp_b2v"][:, :], writes=[b2v.r])
                cb = sb2("cb", [128, 2, 2])
                hx = sb2("hx", [128, 512])
                hu = sb2("hu", [128, 512])
                hT = sb2("hT", [128, 2, 512], BF16)
                kcf = sb2("kcf", [64, 512])
                P.op("pool", lambda e: e.memset(hT[:], 0.0), writes=[hT.r])
                pc = [ps2("pc%d" % i, [128, 512]) for i in range(2)]
                pk = ps2("pk", [128, 512])
                pcm = ps2("pcm", [128, 512])
                for kv in range(2):
                    P.op("pe", lambda e: e.transpose(pcm[0:64, kv * 32:(kv + 1) * 32], pef[:, kv, :], identf[0:32, 0:32]),
                         reads=[pef.r, identf.r], writes=[pcm.r])
                P.op("dve", lambda e: e.tensor_copy(peT[:].rearrange("p k j -> p (k j)"), pcm[0:64, 0:64]), reads=[pcm.r], writes=[peT.r])
                for kv in range(2):
                    for hc in range(2):
                        col = kv * 2 + hc
                        for j in range(32):
                            MMUL(pcm[:, 64 + col:65 + col], w1c[0:64, kv, j, hc * 128:(hc + 1) * 128], peT[:, kv, j:j + 1],
                                 [w1c.r, peT.r], [pcm.r], start=(j == 0), stop=(j == 31))
                TT("dve", cb[:].rearrange("p k c -> p (k c)"), pcm[:, 64:68], b1c[:].rearrange("p k c -> p (k c)"), ALU.add,
                   [pcm.r, b1c.r], [cb.r])
                for kv, srcT in ((0, kcT), (1, vcT)):
                    for g in range(2):
                        rows = slice(g * 64, g * 64 + 64)
                        for hc in range(2):
                            pp = pc[hc]
                            for j in range(32):
                                MMUL(pp[:, 0:511], w1c[rows, kv, j, hc * 128:(hc + 1) * 128],
                                     srcT[rows, j:j + 16 * 510 + 1:16], [w1c.r, srcT.r], [pp.r], start=(j == 0), stop=(j == 31))
                            P.op("act", lambda e: e.activation(hx[:, 0:511], pp[:, 0:511], AF.Identity, bias=cb[:, kv, hc:hc + 1]),
                                 reads=[pp.r, cb.r], writes=[hx.r])
                            TT("dve", hu[:, 0:511], hx[:, 0:511], hx[:, 0:511], ALU.mult, [hx.r], [hu.r])
                            P.op("dve", lambda e: e.tensor_scalar(hu[:, 0:511], hu[:, 0:511], 0.044715, 1.0, ALU.mult, ALU.add), reads=[hu.r], writes=[hu.r])
                            TT("dve", hu[:, 0:511], hu[:, 0:511], hx[:, 0:511], ALU.mult, [hu.r, hx.r], [hu.r])
                            P.op("act", lambda e: e.activation(hu[:, 0:511], hu[:, 0:511], AF.Sigmoid, scale=1.5957691216057308), reads=[hu.r], writes=[hu.r])
                            TT("dve", hT[:, hc, 0:511], hx[:, 0:511], hu[:, 0:511], ALU.mult, [hx.r, hu.r], [hT.r])
                        if kv == 0:
                            for hc in range(2):
                                MMUL(pk[0:64, 0:511], w2c[:, 0, hc, :], hT[:, hc, 0:511], [w2c.r, hT.r], [pk.r], start=(hc == 0), stop=(hc == 1))
                            P.op("act", lambda e: e.activation(kcf[:, 0:511], pk[0:64, 0:511], AF.Identity, bias=b2k[:, 0:1]),
                                 reads=[pk.r, b2k.r], writes=[kcf.r])
                            P.op("pool", lambda e: e.memset(kcf[:, 511:512], 0.0), writes=[kcf.r])
                            P.op("dve", lambda e: e.tensor_copy(kcmpT[g][0:64, :], kcf[:, :]), reads=[kcf.r], writes=[kcmpT[g].r])
                            TT("dve", kcf[:, :], kcf[:, :], kcf[:, :], ALU.mult, [kcf.r], [kcf.r])
                            for bt in range(4):
                                MMUL(pcm[:, 80 + g * 4 + bt:81 + g * 4 + bt], kcf[:, bt * 128:(bt + 1) * 128], onesf[0:64, 0:1],
                                     [kcf.r, onesf.r], [pcm.r])
                            P.op("dve", lambda e: e.tensor_copy(nm[:, 4 + g * 4:8 + g * 4], pcm[:, 80 + g * 4:84 + g * 4]), reads=[pcm.r], writes=[nm.r])
                        else:
                            for bt in range(4):
                                nb = 128
                                for hc in range(2):
                                    MMUL(pk[0:nb, bt * 64:(bt + 1) * 64], hT[:, hc, bt * 128:bt * 128 + nb], w2c[:, 1, hc, :],
                                         [hT.r, w2c.r], [pk.r], start=(hc == 0), stop=(hc == 1))
                                TT("dve", vcmp[0:nb, bt, g, 0:64], pk[0:nb, bt * 64:(bt + 1) * 64], b2v[0:nb, :], ALU.add, [pk.r, b2v.r], [vcmp.r])
                P.op("pe", lambda e: e.transpose(pcm[0:12, 128:256], nm[:, 0:12], identf[:]), reads=[nm.r, identf.r], writes=[pcm.r])
                P.op("dve", lambda e: e.tensor_reduce(hx[0:12, 0:1], pcm[0:12, 128:256], AX.X, ALU.max), reads=[pcm.r], writes=[hx.r])
                P.op("pe", lambda e: e.transpose(pcm[0:1, 256:268], hx[0:12, 0:1], identf[0:12, 0:12]), reads=[hx.r, identf.r], writes=[pcm.r])
                P.op("dve", lambda e: e.tensor_reduce(hx[0:1, 1:2], pcm[0:1, 256:268], AX.X, ALU.max), reads=[pcm.r], writes=[hx.r])
                P.op("act", lambda e: e.activation(hx[0:1, 2:3], hx[0:1, 1:2], AF.Sqrt), reads=[hx.r], writes=[hx.r])
                MMUL(pcm[:, 300:301], onesf[0:1, :], hx[0:1, 2:3], [onesf.r, hx.r], [pcm.r])
                P.op("dve", lambda e: e.tensor_copy(kmaxb[:], pcm[:, 300:301]), reads=[pcm.r], writes=[kmaxb.r])
                P.barrier()

        def attention_scope(run):
            with ExitStack() as st3:
                def sb3(name, shape, dt=F32):
                    return T(st3.enter_context(nc.sbuf_tensor("c3_" + name + "_%d" % P.n_inst, list(shape), dt)), name)

                def ps3(name, shape, dt=F32):
                    return PT(st3.enter_context(nc.psum_tensor("c3_" + name + "_%d" % P.n_inst, list(shape), dt)), name)
                A = {}
                A["G"] = sb3("G", [128, 8192], BF16)
                P.dma("pool", A["G"][:], Dr["Gtab"][:, :], writes=[A["G"].r])
                A["cover"] = sb3("cover", [128, 4, 128], BF16)
                P.dma("pool", A["cover"][:], Dr["cover"][:, :, :], writes=[A["cover"].r])
                A["wq"] = sb3("wq", [128, 8, 536], BF16)
                for kc in range(8):
                    P.dma("pool", A["wq"][:, kc, 0:512], Dr["w_in"][kc * 128:(kc + 1) * 128, QG0:QG0 + 512], writes=[A["wq"].r])
                    P.dma("pool", A["wq"][:, kc, 512:536], Dr["w_in"][kc * 128:(kc + 1) * 128, GATE0:GATE0 + 24], writes=[A["wq"].r])
                A["Ftab"] = sb3("Ftab", [128, 128])
                A["cb2"] = sb3("cb2", [128, 2, 128], BF16)
                A["triS"] = sb3("triS", [128, 4, 512], BF16)
                A["triW"] = sb3("triW", [128, 8, 512], BF16)
                A["xb"] = sb3("xb", [128, 1024], BF16)
                A["xT"] = sb3("xT", [128, 8, 128], BF16)
                A["qf"] = sb3("qf", [128, 512])
                A["rt"] = sb3("rt", [128, 4, 8, 8])
                A["rope"] = sb3("rope", [128, 16])
                A["gts"] = sb3("gts", [128, 24])
                A["qn"] = sb3("qn", [128, 8])
                A["qa"] = sb3("qa", [128, 8, 65], BF16)
                A["qT"] = sb3("qT", [65, 8, 128], BF16)
                A["pT"] = [sb3("pT%d" % i, [128, 512], BF16) for i in range(2)]
                A["ov"] = sb3("ov", [128, 4, 65])
                A["rl"] = sb3("rl", [128, 4])
                A["cf"] = sb3("cf", [128, 4])
                A["imp"] = sb3("imp", [128, 128])
                A["sc"] = sb3("sc", [128, 128])
                A["sc2"] = sb3("sc2", [128, 128])
                A["m8a"] = sb3("m8a", [128, 8])
                A["m8b"] = sb3("m8b", [128, 8])
                A["mb4"] = sb3("mb4", [128, 4, 128], BF16)
                A["ya"] = sb3("ya", [128, 8, 64])
                A["yab"] = sb3("yab", [128, 512], BF16)
                A["tmp"] = sb3("tmp", [128, 4, 64])
                A["sT"] = [ps3("sT%d" % i, [128, 512]) for i in range(2)]
                A["po"] = [ps3("po%d" % i, [128, 512]) for i in range(3)]
                A["ir"] = ps3("ir", [128, 512])
                A["pq"] = ps3("pq", [128, 512])
                A["cnt"] = {"sT": 0, "po": 0, "pT": 0}
                run(A)
                P.barrier()

        def q_prepare(A, n, load_x, load_q, rope_src):
            qf, gts, qa, qT, pq = A["qf"], A["gts"], A["qa"], A["qT"], A["pq"]
            if n < 128:
                P.op("pool", lambda e: e.memset(qa[:], 0.0), writes=[qa.r])
            if load_x is not None:
                load_x(A["xb"])
                for kc in range(8):
                    P.op("pe", lambda e: e.transpose(ptr[:, kc, :], A["xb"][:, kc * 128:(kc + 1) * 128], identb[:]),
                         reads=[A["xb"].r, identb.r], writes=[ptr.r])
                P.op("act", lambda e: e.copy(A["xT"][:], ptr[:]), reads=[ptr.r], writes=[A["xT"].r])
                for kc in range(8):
                    MMUL(pq[:, :], A["xT"][:, kc, :], A["wq"][:, kc, 0:512], [A["xT"].r, A["wq"].r], [pq.r], start=(kc == 0), stop=(kc == 7))
                P.op("act", lambda e: e.activation(qf[:], pq[:], AF.Copy, scale=0.125), reads=[pq.r], writes=[qf.r])
                for kc in range(8):
                    MMUL(pq[:, 0:24], A["xT"][:, kc, :], A["wq"][:, kc, 512:536], [A["xT"].r, A["wq"].r], [pq.r], start=(kc == 0), stop=(kc == 7))
                P.op("act", lambda e: e.activation(gts[:], pq[:, 0:24], AF.Sigmoid), reads=[pq.r], writes=[gts.r])
            else:
                load_q(qf, gts)
                P.op("act", lambda e: e.activation(qf[0:n, :], qf[0:n, :], AF.Copy, scale=0.125), reads=[qf.r], writes=[qf.r])
                P.op("act", lambda e: e.activation(gts[0:n, :], gts[0:n, :], AF.Sigmoid), reads=[gts.r], writes=[gts.r])
            P.dma("sp", A["rope"][0:n, :], rope_src, writes=[A["rope"].r])
            q3 = qf[0:n, :].rearrange("p (h d) -> p h d", h=8)
            x1 = q3[:, :, 0:8]
            x2 = q3[:, :, 8:16]
            cos = A["rope"][0:n, 0:8].unsqueeze(1).to_broadcast([n, 8, 8])
            sin = A["rope"][0:n, 8:16].unsqueeze(1).to_broadcast([n, 8, 8])
            rt = A["rt"]
            rd = [qf.r, rt.r, A["rope"].r]
            TT("dve", rt[0:n, 0], x1, cos, ALU.mult, rd, [rt.r])
            TT("dve", rt[0:n, 1], x2, sin, ALU.mult, rd, [rt.r])
            TT("dve", rt[0:n, 2], x2, cos, ALU.mult, rd, [rt.r])
            TT("dve", rt[0:n, 3], x1, sin, ALU.mult, rd, [rt.r])
            TT("dve", x1, rt[0:n, 0], rt[0:n, 1], ALU.subtract, rd, [qf.r])
            TT("dve", x2, rt[0:n, 2], rt[0:n, 3], ALU.add, rd, [qf.r])
            TT("dve", A["ya"][0:n].rearrange("p h d -> p (h d)"), qf[0:n, :], qf[0:n, :], ALU.mult, [qf.r], [A["ya"].r])
            P.op("dve", lambda e: e.tensor_reduce(A["qn"][0:n, :], A["ya"][0:n], AX.X, ALU.add), reads=[A["ya"].r], writes=[A["qn"].r])
            P.op("act", lambda e: e.activation(A["qn"][0:n, :], A["qn"][0:n, :], AF.Sqrt), reads=[A["qn"].r], writes=[A["qn"].r])
            P.op("dve", lambda e: e.tensor_scalar(A["qn"][0:n, :], A["qn"][0:n, :], kmaxb[0:n, 0:1], -1.0, ALU.mult, ALU.mult),
                 reads=[A["qn"].r, kmaxb.r], writes=[A["qn"].r])
            P.op("dve", lambda e: e.tensor_copy(qa[0:n, :, 0:64], q3), reads=[qf.r], writes=[qa.r])
            P.op("dve", lambda e: e.tensor_copy(qa[0:n, :, 64:65], A["qn"][0:n, :].unsqueeze(2)), reads=[A["qn"].r], writes=[qa.r])
            for h in range(8):
                P.op("pe", lambda e: e.transpose(ptr[0:65, h, :], qa[:, h, :], identb[:]), reads=[qa.r, identb.r], writes=[ptr.r])
            P.op("act", lambda e: e.copy(qT[:], ptr[0:65, :, :]), reads=[ptr.r], writes=[qT.r])

        def nsa_qtile(A, n, cfg):
            qT, gts, ya = A["qT"], A["gts"], A["ya"]
            cnt = A["cnt"]

            def next_sT():
                t = A["sT"][cnt["sT"] % 2]
                cnt["sT"] += 1
                return t

            def next_pT():
                t = A["pT"][cnt["pT"] % 2]
                cnt["pT"] += 1
                return t

            def next_po():
                t = A["po"][cnt["po"] % 3]
                cnt["po"] += 1
                return t

            def finish_branch(po_t, g, br, first):
                ov, rl, cf = A["ov"], A["rl"], A["cf"]
                P.op("act", lambda e: e.copy(ov[:].rearrange("p h d -> p (h d)"), po_t[:, 0:260]), reads=[po_t.r], writes=[ov.r])
                P.op("dve", lambda e: e.tensor_scalar(rl[:], ov[:, :, 64], 1e-30, None, ALU.max), reads=[ov.r], writes=[rl.r])
                P.op("dve", lambda e: e.reciprocal(rl[:], rl[:]), reads=[rl.r], writes=[rl.r])
                g3 = gts[:, :].rearrange("p (h b) -> p h b", b=3)
                TT("dve", cf[:], rl[:], g3[:, 4 * g:4 * g + 4, br], ALU.mult, [rl.r, gts.r], [cf.r])
                dst = ya[:, 4 * g:4 * g + 4, :]
                cfb = cf[:].unsqueeze(2).to_broadcast([128, 4, 64])
                if first:
                    TT("dve", dst, ov[:, :, 0:64], cfb, ALU.mult, [ov.r, cf.r], [ya.r])
                else:
                    TT("dve", A["tmp"][:], ov[:, :, 0:64], cfb, ALU.mult, [ov.r, cf.r], [A["tmp"].r])
                    TT("dve", dst, dst, A["tmp"][:], ALU.add, [ya.r, A["tmp"].r], [ya.r])

            for g in range(2):
                qTg = qT[0:65, 4 * g:4 * g + 4, :].rearrange("p h q -> p (h q)")
                po_t = next_po()
                ir = A["ir"]
                nbt = cfg["nbt"]
                for bt in range(nbt):
                    sT = next_sT()
                    slots = [s for (b_, s) in cfg["cb_tiles"] if b_ == bt]
                    MMUL(sT[:, :], kcmpT[g][0:65, bt * 128:(bt + 1) * 128], qTg, [kcmpT[g].r, qT.r], [sT.r], start=True, stop=(not slots))
                    for s in slots:
                        for h4 in range(4):
                            MMUL(sT[:, h4 * 128:(h4 + 1) * 128], identb[:], A["cb2"][:, s, :], [identb.r, A["cb2"].r], [sT.r],
                                 start=False, stop=(h4 == 3))
                    pT = next_pT()
                    P.op("act", lambda e: e.activation(pT[:], sT[:], AF.Exp), reads=[sT.r], writes=[pT.r])
                    for h4 in range(4):
                        MMUL(po_t[:, h4 * 65:(h4 + 1) * 65], pT[:, h4 * 128:(h4 + 1) * 128], vcmp[:, bt, g, :], [pT.r, vcmp.r], [po_t.r],
                             start=(bt == 0 and h4 == 0), stop=(bt == nbt - 1))
                        MMUL(ir[:, h4 * 128:(h4 + 1) * 128], pT[:, h4 * 128:(h4 + 1) * 128], A["cover"][:, bt, :], [pT.r, A["cover"].r], [ir.r],
                             start=(bt == 0 and h4 == 0), stop=(bt == nbt - 1))
                finish_branch(po_t, g, 0, True)
                rl = A["rl"]
                imp = A["imp"]
                P.op("dve", lambda e: e.tensor_scalar(imp[:], ir[:, 0:128], rl[:, 0:1], None, ALU.mult), reads=[ir.r, rl.r], writes=[imp.r])
                for h4 in range(1, 4):
                    P.op("dve", lambda e: e.scalar_tensor_tensor(imp[:], ir[:, h4 * 128:(h4 + 1) * 128], rl[:, h4:h4 + 1], imp[:], ALU.mult, ALU.add),
                         reads=[ir.r, rl.r, imp.r], writes=[imp.r])
                sc, sc2, m8a, m8b = A["sc"], A["sc2"], A["m8a"], A["m8b"]
                TT("dve", sc[:], imp[:], A["Ftab"][:], ALU.add, [imp.r, A["Ftab"].r], [sc.r])
                P.op("dve", lambda e: e.max(out=m8a[:], in_=sc[:]), reads=[sc.r], writes=[m8a.r])
                P.op("dve", lambda e: e.match_replace(out=sc2[:], in_to_replace=m8a[:], in_values=sc[:], imm_value=-3.0e38),
                     reads=[sc.r, m8a.r], writes=[sc2.r])
                P.op("dve", lambda e: e.max(out=m8b[:], in_=sc2[:]), reads=[sc2.r], writes=[m8b.r])
                tc_ = cfg["topk_col"]
                P.op("dve", lambda e: e.tensor_scalar(sc2[:], sc[:], m8b[:, tc_:tc_ + 1], None, ALU.is_ge), reads=[sc.r, m8b.r], writes=[sc2.r])
                P.op("dve", lambda e: e.tensor_scalar(sc[:], sc[:], -1.0e29, None, ALU.is_gt), reads=[sc.r], writes=[sc.r])
                TT("dve", sc[:], sc[:], sc2[:], ALU.mult, [sc.r, sc2.r], [sc.r])
                P.op("dve", lambda e: e.tensor_scalar(sc[:], sc[:], -NEGB, NEGB, ALU.mult, ALU.add), reads=[sc.r], writes=[sc.r])
                pq = A["pq"]
                P.op("pe", lambda e: e.transpose(pq[:, 0:128], sc[:], identf[:]), reads=[sc.r, identf.r], writes=[pq.r])
                mb4 = A["mb4"]
                P.op("dve", lambda e: e.tensor_copy(mb4[:], pq[:, 0:128].unsqueeze(1).to_broadcast([128, 4, 128])), reads=[pq.r], writes=[mb4.r])
                po_t = next_po()
                tiles = cfg["sel_tiles"]
                for ii, (c, use_G, tri) in enumerate(tiles):
                    sT = next_sT()
                    last = (not use_G) and (tri is None)
                    MMUL(sT[:, :], ksT[g][0:65, c * 128:(c + 1) * 128], qTg, [ksT[g].r, qT.r], [sT.r], start=True, stop=last)
                    if use_G:
                        MMUL(sT[:, :], A["G"][:, c * 128:(c + 1) * 128], mb4[:].rearrange("p h q -> p (h q)"), [A["G"].r, mb4.r], [sT.r],
                             start=False, stop=(tri is None))
                    if tri is not None:
                        MMUL(sT[:, :], identb[:], A["triS"][:, tri, :], [identb.r, A["triS"].r], [sT.r], start=False, stop=True)
                    pT = next_pT()
                    P.op("act", lambda e: e.activation(pT[:], sT[:], AF.Exp), reads=[sT.r], writes=[pT.r])
                    for h4 in range(4):
                        MMUL(po_t[:, h4 * 65:(h4 + 1) * 65], pT[:, h4 * 128:(h4 + 1) * 128], vsw[:, c, g, :], [pT.r, vsw.r], [po_t.r],
                             start=(ii == 0 and h4 == 0), stop=(ii == len(tiles) - 1))
                finish_branch(po_t, g, 1, False)
                po_t = next_po()
                tiles = cfg["win_tiles"]
                for ii, (c, slot) in enumerate(tiles):
                    sT = next_sT()
                    MMUL(sT[:, :], kwT[g][0:65, c * 128:(c + 1) * 128], qTg, [kwT[g].r, qT.r], [sT.r], start=True, stop=False)
                    MMUL(sT[:, :], identb[:], A["triW"][:, slot, :], [identb.r, A["triW"].r], [sT.r], start=False, stop=True)
                    pT = next_pT()
                    P.op("act", lambda e: e.activation(pT[:], sT[:], AF.Exp), reads=[sT.r], writes=[pT.r])
                    for h4 in range(4):
                        MMUL(po_t[:, h4 * 65:(h4 + 1) * 65], pT[:, h4 * 128:(h4 + 1) * 128], vsw[:, c, 2 + g, :], [pT.r, vsw.r], [po_t.r],
                             start=(ii == 0 and h4 == 0), stop=(ii == len(tiles) - 1))
                finish_branch(po_t, g, 2, False)
            P.op("act", lambda e: e.copy(A["yab"][:], ya[:].rearrange("p h d -> p (h d)")), reads=[ya.r], writes=[A["yab"].r])

        def bail():
            Dr["r_ya"] = r_ya
            P.barrier()
        if OPTS.get("cstop", 9) <= 1:
            return bail()
        with ExitStack() as stp:
            kcT = T(stp.enter_context(nc.sbuf_tensor("c_kcT_p", [128, SEQ], BF16)), "kcT")
            vcT = T(stp.enter_context(nc.sbuf_tensor("c_vcT_p", [128, SEQ], BF16)), "vcT")
            for ti in range(NT):
                kb = kvb[ti % 2]
                P.dma("pool", kb[:], Dr["p_full"][ti * 128:(ti + 1) * 128, R_COLS:NA], reads=[Dr["r_pfull"]], writes=[kb.r])
                ingest_tile(kb, ti, True, True, True, kcT, vcT, ti == 0)
            if OPTS.get("cstop", 9) > 2:
                compress_all(kcT, vcT)
        if OPTS.get("cstop", 9) <= 3:
            return bail()

        def run_prompt(A):
            for j in range(OPTS["cq"]):
                P.dma("sp", A["Ftab"][:], Dr["Ftab"][:, j, :], writes=[A["Ftab"].r])
                P.dma("pool", A["cb2"][:], Dr["cbias"][:, j, :, :], writes=[A["cb2"].r])
                if j == 0:
                    P.dma("pool", A["triS"][:], Dr["triS"][:, :, :], writes=[A["triS"].r])
                    P.dma("pool", A["triW"][:], Dr["triW"][:, :, :], writes=[A["triW"].r])

                def load_x(xb, j=j):
                    P.dma("pool", xb[:], Dr["x_own"][j * 128:(j + 1) * 128, :], writes=[xb.r])
                q_prepare(A, 128, load_x, None, Dr["rope_own"][j * 128:(j + 1) * 128, :])
                nbt = (32 * j + 32 + 127) // 128
                cb_tiles = [(nbt - 1, 1)] + ([(nbt - 2, 0)] if nbt >= 2 else [])
                cfg = {"nbt": nbt, "cb_tiles": cb_tiles, "topk_col": 7,
                       "sel_tiles": [(c, True, (c - 4 * j) if c >= 4 * j else None) for c in range(4 * j + 4)],
                       "win_tiles": [(c, c - (4 * j - 4)) for c in range(max(4 * j - 4, 0), 4 * j + 4)]}
                nsa_qtile(A, 128, cfg)
                P.dma("sp", Dr["y_a_own"][j * 128:(j + 1) * 128, :], A["yab"][:], reads=[A["yab"].r], writes=[r_ya])
        attention_scope(run_prompt)

        r_gath = R("gath")
        with ExitStack() as stg_:
            stage = T(stg_.enter_context(nc.sbuf_tensor("c_stage", [64, 8192], F32)), "stage")
            pidx = T(stg_.enter_context(nc.sbuf_tensor("c_pidx", [64, SB], I32)), "pidx")
            pidf = T(stg_.enter_context(nc.sbuf_tensor("c_pidf", [64, SB], F32)), "pidf")
            idx4f = T(stg_.enter_context(nc.sbuf_tensor("c_idx4f", [64, SB, 4], F32)), "idx4f")
            idx4 = T(stg_.enter_context(nc.sbuf_tensor("c_idx4", [64, SB, 4], I32)), "idx4")
            P.op("pool", lambda e: e.memset(pidx[:], 0), writes=[pidx.r])
            for bb in range(OPTS["sb"]):
                P.dma("sp", pidx[:, bb:bb + 1], Dr["pt_col"][bb, :, :], writes=[pidx.r])
            P.op("dve", lambda e: e.tensor_copy(pidf[:], pidx[:]), reads=[pidx.r], writes=[pidf.r])
            for ch in range(4):
                P.op("dve", lambda e: e.tensor_scalar(idx4f[:, :, ch], pidf[:], 4.0, float(ch), ALU.mult, ALU.add), reads=[pidf.r], writes=[idx4f.r])
            P.op("dve", lambda e: e.tensor_copy(idx4[:], idx4f[:]), reads=[idx4f.r], writes=[idx4.r])
            for bb in range(OPTS["sb"]):
                for ci, cache in enumerate((Dr["cache_cmp_pg"], Dr["cache_sel_pg"])):
                    for ch in range(4):
                        P._need("pool", P._deps([idx4.r], [stage.r]))
                        ins = nc.gpsimd.indirect_dma_start(out=stage[:, :], out_offset=None, in_=cache[:, :],
                                                           in_offset=bass.IndirectOffsetOnAxis(ap=idx4[:, bb, ch:ch + 1], axis=0),
                                                           bounds_check=2560 * 4 - 1, oob_is_err=False)
                        pool_, idx_ = P.dq["pool"]
                        key = pool_[idx_ % len(pool_)]
                        P.dq["pool"][1] = idx_ + 1
                        if P.cnt[key] > 0:
                            P._need("pool", [(key, P.cnt[key])])
                        P.cnt[key] += 16
                        ins.then_inc(P.sems[key], 16)
                        P._commit((key, P.cnt[key]), [idx4.r], [stage.r])
                        P.n_inst += 1
                        P.dma("sp", Dr["gath"][bb, ci, :, ch * 8192:(ch + 1) * 8192], stage[:, :], reads=[stage.r], writes=[r_gath])
            P.barrier()
        for bb in range(OPTS["sb"]):
            with ExitStack() as stp:
                kcT = T(stp.enter_context(nc.sbuf_tensor("c_kcT_s%d" % bb, [128, SEQ], BF16)), "kcT")
                vcT = T(stp.enter_context(nc.sbuf_tensor("c_vcT_s%d" % bb, [128, SEQ], BF16)), "vcT")
                for ti in range(64):
                    kb = kvb[ti % 2]
                    P.dma("pool", kb[:, 0:256], Dr["gath"][bb, 0, ti, :].rearrange("(p c) -> p c", p=128), reads=[r_gath], writes=[kb.r])
                    P.dma("pool", kb[:, 256:512], Dr["gath"][bb, 1, ti, :].rearrange("(p c) -> p c", p=128), reads=[r_gath], writes=[kb.r])
                    ingest_tile(kb, ti, True, True, False, kcT, vcT, ti == 0)
                kb = kvb[0]
                P.op("pool", lambda e: e.memset(kb[:], 0.0), writes=[kb.r])
                P.dma("pool", kb[0:DS, 256:512], Dr["p_samp"][bb * DS:(bb + 1) * DS, KV0 + 256:KV0 + 512], reads=[Dr["r_psamp"]], writes=[kb.r])
                ingest_tile(kb, 64, False, True, False, kcT, vcT, False)
                for c in range(5):
                    kb = kvb[(c + 1) % 2]
                    if c < 4:
                        P.dma("pool", kb[:, 512:768], Dr["cache_win"][bb, c * 128:(c + 1) * 128, :], writes=[kb.r])
                    else:
                        P.op("pool", lambda e: e.memset(kb[:], 0.0), writes=[kb.r])
                        P.dma("pool", kb[0:DS, 512:768], Dr["p_samp"][bb * DS:(bb + 1) * DS, KV0 + 512:KV0 + 768], reads=[Dr["r_psamp"]], writes=[kb.r])
                    ingest_tile(kb, c, False, False, True, kcT, vcT, False)
                compress_all(kcT, vcT)

            def run_sample(A, bb=bb):
                P.dma("sp", A["Ftab"][:], Dr["Ftab"][:, 16, :], writes=[A["Ftab"].r])
                P.dma("pool", A["cb2"][:], Dr["cbias"][:, 16, :, :], writes=[A["cb2"].r])
                P.dma("pool", A["triS"][:, 0, :], Dr["triS_s"][:, :], writes=[A["triS"].r])
                P.dma("pool", A["triW"][:, 0:5, :], Dr["triW_s"][:, :, :], writes=[A["triW"].r])

                def load_q(qf, gts):
                    P.dma("sp", qf[0:DS, :], Dr["p_samp"][bb * DS:(bb + 1) * DS, QG0:QG0 + 512], reads=[Dr["r_psamp"]], writes=[qf.r])
                    P.dma("sp", gts[0:DS, :], Dr["p_samp"][bb * DS:(bb + 1) * DS, GATE0:GATE0 + 24], reads=[Dr["r_psamp"]], writes=[gts.r])
                P.op("pool", lambda e: e.memset(A["gts"][:], 0.0), writes=[A["gts"].r])
                q_prepare(A, DS, None, load_q, Dr["rope_s"][0:DS, :])
                cfg = {"nbt": 4, "cb_tiles": [(3, 1), (2, 0)], "topk_col": 6,
                       "sel_tiles": [(c, True, None) for c in range(64)] + [(64, False, 0)],
                       "win_tiles": [(c, c) for c in range(5)]}
                nsa_qtile(A, DS, cfg)
                P.dma("sp", Dr["y_a_own"][2048 + bb * DS:2048 + (bb + 1) * DS, :], A["yab"][0:DS, :], reads=[A["yab"].r], writes=[r_ya])
            attention_scope(run_sample)
        Dr["r_ya"] = r_ya
        P.barrier()

NTOK = 2048 + NS
NTL = 17
MG0 = 3096
DN_ALPHA = 2.0 ** 0.25
NVD = 4 * 1024 + 32


def _tile_rows(u):
    return NS if u == NTL - 1 else 128


def phase_D(nc, P, Dr, outs):
    with ExitStack() as st:
        def sb(name, shape, dt=F32):
            return T(st.enter_context(nc.sbuf_tensor("d_" + name, list(shape), dt)), name)

        def ps(name, shape, dt=F32):
            return PT(st.enter_context(nc.psum_tensor("d_" + name, list(shape), dt)), name)

        w_in = Dr["w_in"]
        identb = sb("identb", [128, 128], BF16)
        P.dma("pool", identb[:], Dr["masks"][:, 896:1024], writes=[identb.r])
        identf = sb("identf", [128, 128])
        P.dma("sp", identf[:], Dr["masks"][:, 896:1024], writes=[identf.r])
        vd = sb("vd", [128, NVD])
        P.dma("sp", vd[:], Dr["vecsD"][:, :], writes=[vd.r])
        sel4 = sb("sel4", [128, 4])
        P.dma("sp", sel4[:], Dr["sel4"][:, :], writes=[sel4.r])
        wmg = sb("wmg", [128, 8, 2048], BF16)
        wo = sb("wo", [128, 8, 1024], BF16)
        for kc in range(8):
            P.dma("pool", wmg[:, kc, :], w_in[kc * 128:(kc + 1) * 128, MG0:MG0 + 2048], writes=[wmg.r])
            P.dma("pool", wo[:, kc, :], Dr["w_o"][kc * 128:(kc + 1) * 128, :], writes=[wo.r])
        wpa = sb("wpa", [128, 4, 1024], BF16)
        wpb = sb("wpb", [128, 4, 1024], BF16)
        for kc in range(4):
            P.dma("pool", wpa[:, kc, :], Dr["w_pa"][kc * 128:(kc + 1) * 128, :], writes=[wpa.r])
            P.dma("pool", wpb[:, kc, :], Dr["w_pb"][kc * 128:(kc + 1) * 128, :], writes=[wpb.r])
        rw = sb("rw", [128, 8, 32])
        P.dma("sp", rw[:], Dr["router_w"].rearrange("(k p) e -> p k e", p=128), writes=[rw.r])

        xf = sb("xf", [128, 1024])
        xb = sb("xb", [128, 1024], BF16)
        xT = sb("xT", [128, 8, 128], BF16)
        sg = sb("sg", [128, 2048])
        yr4 = sb("yr4", [128, 4, 512], BF16)
        yr = sb("yr", [128, 512], BF16)
        ya = sb("ya", [128, 512], BF16)
        yT = sb("yT", [128, 8, 128], BF16)
        mm = sb("mm", [128, 1024])
        mb = sb("mb", [128, 1024], BF16)
        mT = sb("mT", [128, 8, 128], BF16)
        hp_ = sb("hpre", [128, 1024])
        hh = sb("hh", [128, 1024])
        hT = sb("hTf", [128, 8, 128])
        s1 = sb("s1", [128, 1])
        s2 = sb("s2", [128, 1])
        lg = sb("lg", [128, 32])
        m8 = sb("m8", [128, 8])
        msk = sb("msk", [128, 32])
        gt = sb("gt", [128, 32])
        ptr = ps("ptr", [128, 8, 128], BF16)
        ptf = [ps("ptf%d" % i, [128, 512]) for i in range(2)]
        pm = [ps("pm%d" % i, [128, 512]) for i in range(4)]
        pmi = [0]

        def nextpm():
            t = pm[pmi[0] % 4]
            pmi[0] += 1
            return t

        r_h = R("h_own")
        r_g = R("gates_own")
        for u in range(NTL):
            n = _tile_rows(u)
            if u < 16:
                P.dma("sp", xf[:], Dr["x_own"][u * 128:(u + 1) * 128, :], writes=[xf.r])
                P.dma("sp", yr4[:], Dr["y_r"][u * 512:(u + 1) * 512, :].rearrange("(k p) c -> p k c", p=128),
                      reads=[Dr["r_yr"]], writes=[yr4.r])
                P.dma("sp", ya[:], Dr["y_a_own"][u * 128:(u + 1) * 128, :], reads=[Dr["r_ya"]], writes=[ya.r])
                P.op("dve", lambda e: e.tensor_scalar(yr[:], yr4[:, 0, :], sel4[:, 0:1], None, ALU.mult), reads=[yr4.r, sel4.r], writes=[yr.r])
                for k in range(1, 4):
                    P.op("dve", lambda e: e.scalar_tensor_tensor(yr[:], yr4[:, k, :], sel4[:, k:k + 1], yr[:], ALU.mult, ALU.add),
                         reads=[yr4.r, sel4.r, yr.r], writes=[yr.r])
            else:
                P.dma("sp", xf[0:n, :], Dr["x_s"][:, :], writes=[xf.r])
                P.dma("sp", yr[0:n, :], Dr["y_r_s"][:, :], reads=[Dr["r_yr"]], writes=[yr.r])
                P.dma("sp", ya[0:n, :], Dr["y_a_own"][2048:2048 + n, :], reads=[Dr["r_ya"]], writes=[ya.r])
            P.op("act", lambda e: e.copy(xb[0:n, :], xf[0:n, :]), reads=[xf.r], writes=[xb.r])
            for kc in range(8):
                P.op("pe", lambda e: e.transpose(ptr[:, kc, 0:n], xb[0:n, kc * 128:(kc + 1) * 128], identb[0:n, 0:n]),
                     reads=[xb.r, identb.r], writes=[ptr.r])
            P.op("act", lambda e: e.copy(xT[:, :, 0:n], ptr[:, :, 0:n]), reads=[ptr.r], writes=[xT.r])
            for nch in range(4):
                p_ = nextpm()
                for kc in range(8):
                    P.op("pe", lambda e: e.matmul(p_[0:n, :], xT[:, kc, 0:n], wmg[:, kc, nch * 512:(nch + 1) * 512],
                                                  start=(kc == 0), stop=(kc == 7)), reads=[xT.r, wmg.r], writes=[p_.r])
                P.op("act", lambda e: e.activation(sg[0:n, nch * 512:(nch + 1) * 512], p_[0:n, :], AF.Sigmoid), reads=[p_.r], writes=[sg.r])
            for kc in range(4):
                P.op("pe", lambda e: e.transpose(ptr[:, kc, 0:n], yr[0:n, kc * 128:(kc + 1) * 128], identb[0:n, 0:n]),
                     reads=[yr.r, identb.r], writes=[ptr.r])
                P.op("pe", lambda e: e.transpose(ptr[:, 4 + kc, 0:n], ya[0:n, kc * 128:(kc + 1) * 128], identb[0:n, 0:n]),
                     reads=[ya.r, identb.r], writes=[ptr.r])
            P.op("act", lambda e: e.copy(yT[:, :, 0:n], ptr[:, :, 0:n]), reads=[ptr.r], writes=[yT.r])
            for nch in range(2):
                pa = nextpm()
                pb = nextpm()
                for kc in range(4):
                    P.op("pe", lambda e: e.matmul(pa[0:n, :], yT[:, kc, 0:n], wpa[:, kc, nch * 512:(nch + 1) * 512],
                                                  start=(kc == 0), stop=(kc == 3)), reads=[yT.r, wpa.r], writes=[pa.r])
                for kc in range(4):
                    P.op("pe", lambda e: e.matmul(pb[0:n, :], yT[:, 4 + kc, 0:n], wpb[:, kc, nch * 512:(nch + 1) * 512],
                                                  start=(kc == 0), stop=(kc == 3)), reads=[yT.r, wpb.r], writes=[pb.r])
                cs = slice(nch * 512, (nch + 1) * 512)
                P.op("dve", lambda e: e.tensor_tensor(mm[0:n, cs], pa[0:n, :], sg[0:n, nch * 512:(nch + 1) * 512], ALU.mult),
                     reads=[pa.r, sg.r], writes=[mm.r])
                P.op("dve", lambda e: e.tensor_tensor(hp_[0:n, cs], pb[0:n, :], sg[0:n, 1024 + nch * 512:1024 + (nch + 1) * 512], ALU.mult),
                     reads=[pb.r, sg.r], writes=[hp_.r])
                P.op("dve", lambda e: e.tensor_tensor(mb[0:n, cs], mm[0:n, cs], hp_[0:n, cs], ALU.add),
                     reads=[mm.r, hp_.r], writes=[mb.r])
            for kc in range(8):
                P.op("pe", lambda e: e.transpose(ptr[:, kc, 0:n], mb[0:n, kc * 128:(kc + 1) * 128], identb[0:n, 0:n]),
                     reads=[mb.r, identb.r], writes=[ptr.r])
            P.op("act", lambda e: e.copy(mT[:, :, 0:n], ptr[:, :, 0:n]), reads=[ptr.r], writes=[mT.r])
            for nch in range(2):
                p_ = nextpm()
                for kc in range(8):
                    P.op("pe", lambda e: e.matmul(p_[0:n, :], mT[:, kc, 0:n], wo[:, kc, nch * 512:(nch + 1) * 512],
                                                  start=(kc == 0), stop=(kc == 7)), reads=[mT.r, wo.r], writes=[p_.r])
                cs = slice(nch * 512, (nch + 1) * 512)
                P.op("dve", lambda e: e.scalar_tensor_tensor(hp_[0:n, cs], xf[0:n, cs], DN_ALPHA, p_[0:n, :], ALU.mult, ALU.add),
                     reads=[xf.r, p_.r], writes=[hp_.r])
            layer_norm(P, hp_, hh, mm, s1, s2, n, vd[0:n, 0:1024], vd[0:n, 1024:2048], vd.r)
            P.dma("pool", Dr["h_own"][u * 128:u * 128 + n, :], hh[0:n, :], reads=[hh.r], writes=[r_h])
            for kc in range(8):
                pt_ = ptf[kc // 4]
                P.op("pe", lambda e: e.transpose(pt_[:, (kc % 4) * 128:(kc % 4) * 128 + n], hh[0:n, kc * 128:(kc + 1) * 128], identf[0:n, 0:n]),
                     reads=[hh.r, identf.r], writes=[pt_.r])
            P.op("act", lambda e: e.copy(hT[:, 0:4, 0:n], ptf[0][:].rearrange("p (k t) -> p k t", k=4)[:, :, 0:n]), reads=[ptf[0].r], writes=[hT.r])
            P.op("dve", lambda e: e.tensor_copy(hT[:, 4:8, 0:n], ptf[1][:].rearrange("p (k t) -> p k t", k=4)[:, :, 0:n]), reads=[ptf[1].r], writes=[hT.r])
            p_ = nextpm()
            for kc in range(8):
                P.op("pe", lambda e: e.matmul(p_[0:n, 0:32], hT[:, kc, 0:n], rw[:, kc, :], start=(kc == 0), stop=(kc == 7)),
                     reads=[hT.r, rw.r], writes=[p_.r])
            P.op("dve", lambda e: e.tensor_tensor(lg[0:n, :], p_[0:n, 0:32], vd[0:n, 4096:4128], ALU.add), reads=[p_.r, vd.r], writes=[lg.r])
            P.op("dve", lambda e: e.max(out=m8[0:n, :], in_=lg[0:n, :]), reads=[lg.r], writes=[m8.r])
            P.op("dve", lambda e: e.tensor_scalar(msk[0:n, :], lg[0:n, :], m8[0:n, 3:4], None, ALU.is_ge), reads=[lg.r, m8.r], writes=[msk.r])
            P.op("dve", lambda e: e.tensor_scalar(s1[0:n, :], m8[0:n, 0:1], -1.0, None, ALU.mult), reads=[m8.r], writes=[s1.r])
            P.op("act", lambda e: e.activation(gt[0:n, :], lg[0:n, :], AF.Exp, bias=s1[0:n, 0:1]), reads=[lg.r, s1.r], writes=[gt.r])
            P.op("dve", lambda e: e.tensor_tensor(gt[0:n, :], gt[0:n, :], msk[0:n, :], ALU.mult), reads=[gt.r, msk.r], writes=[gt.r])
            P.op("dve", lambda e: e.tensor_reduce(s2[0:n, :], gt[0:n, :], AX.X, ALU.add), reads=[gt.r], writes=[s2.r])
            P.op("dve", lambda e: e.reciprocal(s2[0:n, :], s2[0:n, :]), reads=[s2.r], writes=[s2.r])
            P.op("dve", lambda e: e.tensor_scalar(gt[0:n, :], gt[0:n, :], s2[0:n, 0:1], None, ALU.mult), reads=[gt.r, s2.r], writes=[gt.r])
            P.dma("pool", Dr["gates_own"][u * 128:u * 128 + n, :], gt[0:n, :], reads=[gt.r], writes=[r_g])
        Dr["r_h"] = r_h
        Dr["r_g"] = r_g
        P.barrier()


def layer_norm(P, src, dst, tmp, s1, s2, n, g_ap, b_ap, vr):
    P.op("dve", lambda e: e.tensor_reduce(s1[0:n, :], src[0:n, :], AX.X, ALU.add), reads=[src.r], writes=[s1.r])
    P.op("dve", lambda e: e.tensor_scalar(s1[0:n, :], s1[0:n, :], -1.0 / 1024, None, ALU.mult), reads=[s1.r], writes=[s1.r])
    P.op("dve", lambda e: e.tensor_scalar(dst[0:n, :], src[0:n, :], s1[0:n, 0:1], None, ALU.add), reads=[src.r, s1.r], writes=[dst.r])
    P.op("dve", lambda e: e.tensor_tensor(tmp[0:n, :], dst[0:n, :], dst[0:n, :], ALU.mult), reads=[dst.r], writes=[tmp.r])
    P.op("dve", lambda e: e.tensor_reduce(s2[0:n, :], tmp[0:n, :], AX.X, ALU.add), reads=[tmp.r], writes=[s2.r])
    P.op("dve", lambda e: e.tensor_scalar(s2[0:n, :], s2[0:n, :], 1.0 / 1024, 1e-5, ALU.mult, ALU.add), reads=[s2.r], writes=[s2.r])
    P.op("act", lambda e: e.activation(s2[0:n, :], s2[0:n, :], AF.Sqrt), reads=[s2.r], writes=[s2.r])
    P.op("dve", lambda e: e.reciprocal(s2[0:n, :], s2[0:n, :]), reads=[s2.r], writes=[s2.r])
    P.op("dve", lambda e: e.tensor_scalar(dst[0:n, :], dst[0:n, :], s2[0:n, 0:1], None, ALU.mult), reads=[dst.r, s2.r], writes=[dst.r])
    P.op("dve", lambda e: e.tensor_tensor(dst[0:n, :], dst[0:n, :], g_ap, ALU.mult), reads=[dst.r, vr], writes=[dst.r])
    P.op("dve", lambda e: e.tensor_tensor(dst[0:n, :], dst[0:n, :], b_ap, ALU.add), reads=[dst.r, vr], writes=[dst.r])


def phase_E(nc, P, Dr, outs):
    with ExitStack() as st:
        def sb(name, shape, dt=F32):
            return T(st.enter_context(nc.sbuf_tensor("e_" + name, list(shape), dt)), name)

        def ps(name, shape, dt=F32):
            return PT(st.enter_context(nc.psum_tensor("e_" + name, list(shape), dt)), name)

        identb = sb("identb", [128, 128], BF16)
        P.dma("pool", identb[:], Dr["masks"][:, 896:1024], writes=[identb.r])
        identf = sb("identf", [128, 128])
        P.dma("sp", identf[:], Dr["masks"][:, 896:1024], writes=[identf.r])
        vd = sb("vd", [128, 2048])
        P.dma("sp", vd[:], Dr["vecsD"][:, 2048:4096], writes=[vd.r])
        b1a = sb("b1a", [128, 32, 16])
        P.dma("sp", b1a[:], Dr["mlp1_bT"].rearrange("e p c -> p e c"), writes=[b1a.r])
        b2 = sb("b2", [32, 1024])
        P.dma("sp", b2[:], Dr["mlp2_b"][:, :], writes=[b2.r])

        HT = 9
        hT = sb("hT", [128, 8, 1024 + NS], BF16)
        yacc = sb("yacc", [128, HT, 1024])
        gates = sb("gates", [128, HT, 32])
        gT = sb("gT", [32, HT, 128])
        s1 = sb("s1", [128, 1])
        s2 = sb("s2", [128, 1])
        ptr = ps("ptr", [128, 8, 128], BF16)
        pg_ = [ps("pgl%d" % i, [128, 512]) for i in range(4)]
        po = [ps("po%d" % i, [128, 512]) for i in range(3)]
        cnt = {"pg": 0, "po": 0, "w": 0, "a": 0}
        r_out = R("outE")
        outs.append(r_out)

        def scoped(names):
            stx = ExitStack()
            d = {}
            for nm_, shp, dt_ in names:
                d[nm_] = T(stx.enter_context(nc.sbuf_tensor("e_%s_%d" % (nm_, P.n_inst), list(shp), dt_)), nm_)
            return stx, d

        for half in range(2):
            tiles = list(range(half * 8, half * 8 + 8)) + ([16] if half == 1 else [])
            ntok = sum(_tile_rows(u) for u in tiles)
            groups = [(0, 512), (512, 512)] + ([(1024, NS)] if half == 1 else [])
            stx, dd = scoped([("hf", [128, 1024], F32), ("hb", [128, 1024], BF16)])
            hf, hb = dd["hf"], dd["hb"]
            for li, u in enumerate(tiles):
                n = _tile_rows(u)
                P.dma("sp", hf[0:n, :], Dr["h_own"][u * 128:u * 128 + n, :], reads=[Dr["r_h"]], writes=[hf.r])
                P.dma("sp", gates[0:n, li, :], Dr["gates_own"][u * 128:u * 128 + n, :], reads=[Dr["r_g"]], writes=[gates.r])
                P.op("act", lambda e: e.copy(hb[0:n, :], hf[0:n, :]), reads=[hf.r], writes=[hb.r])
                for kc in range(8):
                    P.op("pe", lambda e: e.transpose(ptr[:, kc, 0:n], hb[0:n, kc * 128:(kc + 1) * 128], identb[0:n, 0:n]),
                         reads=[hb.r, identb.r], writes=[ptr.r])
                P.op("dve", lambda e: e.tensor_copy(hT[:, :, li * 128:li * 128 + n], ptr[:, :, 0:n]), reads=[ptr.r], writes=[hT.r])
                pq = po[cnt["po"] % 3]
                cnt["po"] += 1
                P.op("pe", lambda e: e.transpose(pq[0:32, 0:n], gates[0:n, li, :], identf[0:n, 0:n]), reads=[gates.r, identf.r], writes=[pq.r])
                P.op("act", lambda e: e.copy(gT[:, li, 0:n], pq[0:32, 0:n]), reads=[pq.r], writes=[gT.r])
                for nch in range(2):
                    pq = po[cnt["po"] % 3]
                    cnt["po"] += 1
                    P.op("pe", lambda e: e.matmul(pq[0:n, :], gT[:, li, 0:n], b2[:, nch * 512:(nch + 1) * 512], start=True, stop=True),
                         reads=[gT.r, b2.r], writes=[pq.r])
                    P.op("act", lambda e: e.copy(yacc[0:n, li, nch * 512:(nch + 1) * 512], pq[0:n, :]), reads=[pq.r], writes=[yacc.r])
            P.barrier()
            stx.close()
            stx, dd = scoped([("w1_0", [128, 8, 2048], BF16), ("w1_1", [128, 8, 2048], BF16), ("w2_0", [128, 8, 1024], BF16),
                              ("w2_1", [128, 8, 1024], BF16), ("actT0", [128, 8, 512], BF16), ("actT1", [128, 8, 512], BF16),
                              ("tg0", [128, 512], F32), ("tg1", [128, 512], F32), ("tsg0", [128, 512], F32), ("tsg1", [128, 512], F32),
                              ("tl0", [128, 512], F32), ("tl1", [128, 512], F32)])
            w1 = [dd["w1_0"], dd["w1_1"]]
            w2 = [dd["w2_0"], dd["w2_1"]]
            actT = [dd["actT0"], dd["actT1"]]
            tg = [dd["tg0"], dd["tg1"]]
            tsg = [dd["tsg0"], dd["tsg1"]]
            tl = [dd["tl0"], dd["tl1"]]
            for ex in range(32):
                wb1 = w1[cnt["w"] % 2]
                wb2 = w2[cnt["w"] % 2]
                cnt["w"] += 1
                for kc in range(8):
                    P.dma("pool", wb1[:, kc, :], Dr["mlp1_w"][ex, kc * 128:(kc + 1) * 128, :], writes=[wb1.r])
                for kc in range(8):
                    P.dma("pool", wb2[:, kc, :], Dr["mlp2_w"][ex, kc * 128:(kc + 1) * 128, :], writes=[wb2.r])
                for (g0, gn) in groups:
                    aT = actT[cnt["a"] % 2]
                    cnt["a"] += 1
                    for fc in range(8):
                        pgl = pg_[cnt["pg"] % 4]
                        pll = pg_[(cnt["pg"] + 1) % 4]
                        cnt["pg"] += 2
                        for kc in range(8):
                            P.op("pe", lambda e: e.matmul(pgl[:, 0:gn], wb1[:, kc, fc * 128:(fc + 1) * 128], hT[:, kc, g0:g0 + gn],
                                                          start=(kc == 0), stop=(kc == 7)), reads=[wb1.r, hT.r], writes=[pgl.r])
                        for kc in range(8):
                            P.op("pe", lambda e: e.matmul(pll[:, 0:gn], wb1[:, kc, 1024 + fc * 128:1024 + (fc + 1) * 128], hT[:, kc, g0:g0 + gn],
                                                          start=(kc == 0), stop=(kc == 7)), reads=[wb1.r, hT.r], writes=[pll.r])
                        k2 = fc % 2
                        a_, s_, l_ = tg[k2], tsg[k2], tl[k2]
                        P.op("dve", lambda e: e.tensor_scalar(a_[:, 0:gn], pgl[:, 0:gn], b1a[:, ex, fc:fc + 1], 7.0, ALU.add, ALU.min),
                             reads=[pgl.r, b1a.r], writes=[a_.r])
                        P.op("act", lambda e: e.activation(s_[:, 0:gn], a_[:, 0:gn], AF.Sigmoid, scale=1.702), reads=[a_.r], writes=[s_.r])
                        P.op("dve", lambda e: e.tensor_scalar(l_[:, 0:gn], pll[:, 0:gn], b1a[:, ex, 8 + fc:9 + fc], 7.0, ALU.add, ALU.min),
                             reads=[pll.r, b1a.r], writes=[l_.r])
                        P.op("dve", lambda e: e.tensor_scalar(l_[:, 0:gn], l_[:, 0:gn], -7.0, 1.0, ALU.max, ALU.add), reads=[l_.r], writes=[l_.r])
                        P.op("dve", lambda e: e.tensor_tensor(a_[:, 0:gn], a_[:, 0:gn], s_[:, 0:gn], ALU.mult), reads=[a_.r, s_.r], writes=[a_.r])
                        P.op("dve", lambda e: e.tensor_tensor(aT[:, fc, 0:gn], a_[:, 0:gn], l_[:, 0:gn], ALU.mult), reads=[a_.r, l_.r], writes=[aT.r])
                    nt_in_g = (gn + 127) // 128
                    for tt in range(nt_in_g):
                        li = g0 // 128 + tt
                        n = min(128, gn - tt * 128)
                        for nch in range(2):
                            pq = po[cnt["po"] % 3]
                            cnt["po"] += 1
                            for fc in range(8):
                                P.op("pe", lambda e: e.matmul(pq[0:n, :], aT[:, fc, tt * 128:tt * 128 + n], wb2[:, fc, nch * 512:(nch + 1) * 512],
                                                              start=(fc == 0), stop=(fc == 7)), reads=[aT.r, wb2.r], writes=[pq.r])
                            ya = yacc[0:n, li, nch * 512:(nch + 1) * 512]
                            P.op("dve", lambda e: e.scalar_tensor_tensor(ya, pq[0:n, :], gates[0:n, li, ex:ex + 1], ya, ALU.mult, ALU.add),
                                 reads=[pq.r, gates.r, yacc.r], writes=[yacc.r])
            P.barrier()
            stx.close()
            stx, dd = scoped([("hf", [128, 1024], F32), ("t1", [128, 1024], F32), ("t2", [128, 1024], F32)])
            hf, t1, t2 = dd["hf"], dd["t1"], dd["t2"]
            for li, u in enumerate(tiles):
                n = _tile_rows(u)
                P.dma("sp", hf[0:n, :], Dr["h_own"][u * 128:u * 128 + n, :], reads=[Dr["r_h"]], writes=[hf.r])
                P.op("dve", lambda e: e.scalar_tensor_tensor(t1[0:n, :], hf[0:n, :], DN_ALPHA, yacc[0:n, li, :], ALU.mult, ALU.add),
                     reads=[hf.r, yacc.r], writes=[t1.r])
                layer_norm(P, t1, t2, hf, s1, s2, n, vd[0:n, 0:1024], vd[0:n, 1024:2048], vd.r)
                P.dma("sp", Dr["o_y"][u * 128:u * 128 + n, :], t2[0:n, :], reads=[t2.r], writes=[r_out])
            P.barrier()
            stx.close()
        P.barrier()


def build_program():
    nc = bass.Bass("TRN2", target_bir_lowering=False)
    Dr = {}

    def din(name, shape, dt=F32):
        Dr[name] = nc.dram_tensor(name, list(shape), dt, kind="ExternalInput").ap()
        _INPUT_NAMES.append(name)
    ph = OPTS["phases"]

    def dout(name, shape, dt=F32):
        Dr[name] = nc.dram_tensor(name, list(shape), dt, kind="ExternalOutput").ap()

    def dtmp(name, shape, dt=F32):
        Dr[name] = nc.dram_tensor(name, list(shape), dt).ap()

    din("x_full", [SEQ, D])
    din("x_own", [2048, D])
    din("x_s", [NS, D])
    din("w_in", [D, IN_COLS])
    din("rope_p", [SEQ, 16])
    din("rope_own", [2048, 16])
    din("rope_s", [NS, 16])
    din("cache_win", [SB, 512, 256])
    din("vecs", [128, NVEC])
    din("w_w2", [64, 512])
    din("w_a2", [64, 512])
    din("g_w2", [128, 512])
    din("masks", [128, NMASK])
    din("tmask", [128, 1])
    din("state_shift", [SB, R_COLS])
    din("state_wkv", [SB, 8, 64, 64])
    din("vecsD", [128, NVD])
    din("sel4", [128, 4])
    din("w_o", [D, D])
    din("w_pa", [512, D])
    din("w_pb", [512, D])
    din("router_w", [D, 32])
    if "E" in ph:
        din("mlp1_w", [32, D, 2048])
        din("mlp2_w", [32, D, D])
    din("mlp1_bT", [32, 128, 16])
    din("mlp2_b", [32, D])
    din("cmp_w1", [2, 2048, 256])
    din("cmp_w2", [2, 256, 64])
    din("cmp_pe", [2, 32, 64])
    din("cmp_b1T", [128, 2, 2])
    din("cmp_b2T", [64, 1])
    din("cmp_b2v", [128, 64])
    din("Gtab", [128, 8192])
    din("cover", [128, 4, 128])
    din("Ftab", [128, 17, 128])
    din("cbias", [128, 17, 2, 128])
    din("triS", [128, 4, 512])
    din("triW", [128, 8, 512])
    din("triS_s", [128, 512])
    din("triW_s", [128, 5, 512])
    din("pt_col", [SB, 64, 1], I32)
    if "C" in ph and OPTS["sb"] > 0:
        din("cache_cmp_pg", [2560 * 4, 8192])
        din("cache_sel_pg", [2560 * 4, 8192])

    dout("o_cmp_p", [SEQ, 256])
    dout("o_sel_p", [SEQ, 256])
    dout("o_win_p", [512, 256])
    dout("o_shift_p", [1, R_COLS])
    dout("o_cmp_s", [NS, 256])
    dout("o_sel_s", [NS, 256])
    dout("o_win_s", [SB, 512, 256])
    dout("o_shift_s", [SB, R_COLS])
    dout("o_wkv_p", [8, 64, 64])
    dout("o_wkv_s", [SB, 8, 64, 64])
    dout("o_y", [NTOK, D])

    dtmp("p_full", [SEQ, NA])
    dtmp("p_samp", [NS, IN_COLS])
    dtmp("y_r", [SEQ, 512], BF16)
    dtmp("y_r_s", [NS, 512], BF16)
    if DEBUG:
        dout("gath", [SB, 2, 64, 128 * 256])
    else:
        dtmp("gath", [SB, 2, 64, 128 * 256])
    if DEBUG:
        dout("y_a_own", [NTOK, 512], BF16)
        dout("h_own", [NTOK, D])
        dout("gates_own", [NTOK, 32])
    else:
        dtmp("y_a_own", [NTOK, 512], BF16)
        dtmp("h_own", [NTOK, D])
        dtmp("gates_own", [NTOK, 32])
    outs = []
    with ExitStack() as st:
        P = Prog(nc, st)
        phase_A(nc, P, Dr, outs)
        if "B" in ph:
            phase_B(nc, P, Dr, outs)
        if "C" in ph:
            phase_C(nc, P, Dr, outs)
        if "D" in ph:
            phase_D(nc, P, Dr, outs)
        if "E" in ph:
            phase_E(nc, P, Dr, outs)
        P.finish(outs + [Dr[k] for k in ("r_yr", "r_ya", "r_h", "r_g") if k in Dr])
        P._need("sp", [(k, v) for k, v in P.cnt.items() if v > 0])
        print("epochs", P.epoch, {k: v for k, v in P.cnt.items() if not k.startswith("d_")})
        print("program: n_inst=%d n_wait=%d" % (P.n_inst, P.n_wait))
    return nc


DEBUG = False
OPTS = {"phases": "ABCDE", "cq": 16, "sb": SB, "cores": 8, "cstop": 9}
_NC = None
_INPUT_NAMES = []


def _rope_table(pos):
    half = 8
    inv = (np.float32(500000.0) ** (-np.arange(half, dtype=np.float32) * np.float32(2.0) / np.float32(16))).astype(np.float32)
    ang = pos.astype(np.float32)[:, None] * inv[None, :]
    return np.concatenate([np.cos(ang), np.sin(ang)], axis=1).astype(np.float32)


def _const_masks():
    s = np.arange(128)[:, None]
    t = np.arange(128)[None, :]
    same = (s // 64) == (t // 64)
    Lblk = (same & (s <= t)).astype(np.float32)
    Oblk = same.astype(np.float32)
    strictU = (same & (s < t)).astype(np.float32)
    inclU = Lblk
    maskMA2 = np.concatenate([strictU, inclU, strictU, inclU], axis=1)
    maskNT = (same & (s > t)).astype(np.float32)
    ident = np.eye(128, dtype=np.float32)
    return np.ascontiguousarray(np.concatenate([Lblk, Oblk, maskMA2, maskNT, ident], axis=1))


def _nsa_tables(qq):
    f32 = np.float32
    NB = np.float32(-30000.0)
    kl = np.arange(128)[:, None]
    ql = np.arange(128)[None, :]
    Ftab = np.zeros((128, 17, 128), f32)
    cbias = np.zeros((128, 17, 2, 128), f32)
    sidx = np.arange(128)[None, :]
    for j in range(16):
        i = 4 * j + qq
        qpos = (128 * i + np.arange(128))[:, None]
        causal = (64 * sidx) <= qpos
        cur = qpos // 64
        forced = (sidx == 0) | (sidx == cur) | (sidx == cur - 1)
        F = np.where(forced, 1e6 + 16.0 * sidx, 0.0)
        F = np.where(causal, F, -1e30)
        Ftab[:, j, :] = F
        nbt = (32 * j + 32 + 127) // 128
        for slot, bt in ((1, nbt - 1), (0, nbt - 2)):
            if bt < 0:
                continue
            blk = 128 * bt + kl
            valid = (blk <= 510) & (16 * blk + 31 <= 128 * i + ql)
            cbias[:, j, slot, :] = np.where(valid, 0.0, NB)
    F = np.where((sidx == 0) | (sidx == 127), 1e6 + 16.0 * sidx, 0.0) * np.ones((128, 1))
    Ftab[:, 16, :] = F
    blk = 128 * 3 + kl
    cbias[:, 16, 1, :] = np.where(blk <= 510, 0.0, NB) * np.ones((1, 128))
    cbias[:, 16, 0, :] = 0.0
    rep4 = lambda m: np.tile(m, (1, 4))
    triS = np.zeros((128, 4, 512), f32)
    for rel in range(4):
        if rel < qq:
            m = np.zeros((128, 128), f32)
        elif rel == qq:
            m = np.where(kl <= ql, 0.0, NB)
        else:
            m = np.full((128, 128), NB)
        triS[:, rel, :] = rep4(m)
    triW = np.zeros((128, 8, 512), f32)
    for rel in range(8):
        dlt = qq + 4 - rel
        if dlt < 0 or dlt > 4:
            m = np.full((128, 128), NB)
        elif dlt == 0:
            m = np.where(kl <= ql, 0.0, NB)
        elif dlt == 4:
            m = np.where(kl >= ql, 0.0, NB)
        else:
            m = np.zeros((128, 128), f32)
        triW[:, rel, :] = rep4(m)
    qv = ql < DS
    triS_s = rep4(np.where((kl < DS) & (kl <= ql), 0.0, NB))
    triW_s = np.zeros((128, 5, 512), f32)
    for c in range(5):
        kidx = 128 * c + kl
        ok = (kidx < 512 + DS) & (kidx <= 512 + ql) & (kidx >= ql)
        triW_s[:, c, :] = rep4(np.where(ok, 0.0, NB))
    return (Ftab.astype(f32), cbias.astype(f32), triS.astype(f32), triW.astype(f32), triS_s.astype(f32), triW_s.astype(f32))


def _shared_tables():
    f32 = np.float32
    s = np.arange(128)[:, None]
    x = np.arange(8192)[None, :]
    G = ((x // 64) == s).astype(f32)
    cover = np.zeros((128, 4, 128), f32)
    for bt in range(4):
        blk = 128 * bt + np.arange(128)[:, None]
        ss = np.arange(128)[None, :]
        cover[:, bt, :] = ((blk >= 4 * ss - 1) & (blk <= 4 * ss + 3) & (blk <= 510)).astype(f32)
    return G, cover


def kernel(**inputs):
    global _NC
    if _NC is None:
        _NC = build_program()
    nc = _NC
    g = lambda k: np.asarray(inputs[k])
    f32 = np.float32
    C = np.ascontiguousarray
    x_prompt = g("x_prompt")
    x_sample = g("x_sample")
    w_in = C(g("w_in")[0])
    cache_win = g("cache_win_kv")[0].reshape(32, 512, 256)
    rope_p = _rope_table(np.arange(SEQ))
    rope_s = np.tile(_rope_table(PAST + np.arange(DS)), (SB, 1))
    vec = np.concatenate([g("mu_shift")[0], g("w0")[0], g("a0")[0], g("k_k")[0], g("k_a")[0], g("gn_g")[0],
                          g("gn_b")[0], g("r_k")[0].reshape(-1)]).astype(f32)
    vecs = C(np.broadcast_to(vec[None, :], (128, NVEC)))
    vecD = np.concatenate([g("ln1_g")[0], g("ln1_b")[0], g("ln2_g")[0], g("ln2_b")[0], g("router_b")[0]]).astype(f32)
    vecsD = C(np.broadcast_to(vecD[None, :], (128, NVD)))
    masks = _const_masks()
    tmask = np.zeros((128, 1), f32)
    tmask[0:DS] = 1.0
    tmask[64:64 + DS] = 1.0
    state_shift = g("state_shift")[0]
    state_wkv = g("state_wkv")[0]
    page_table = g("page_table").astype(np.int32)
    Gtab, cover = _shared_tables()
    shared = {
        "w_in": w_in, "rope_p": rope_p, "rope_s": rope_s, "vecs": vecs, "masks": masks, "tmask": tmask,
        "w_w2": C(g("w_w2")[0]), "w_a2": C(g("w_a2")[0]), "g_w2": C(g("g_w2")[0]),
        "vecsD": vecsD, "w_o": C(g("w_o")[0]), "w_pa": C(g("w_pa")[0]), "w_pb": C(g("w_pb")[0]),
        "router_w": C(g("router_w")[0]), "mlp1_w": C(g("mlp1_w")[0]), "mlp2_w": C(g("mlp2_w")[0]),
        "mlp1_bT": C(g("mlp1_b")[0].reshape(32, 16, 128).transpose(0, 2, 1)), "mlp2_b": C(g("mlp2_b")[0]),
        "cmp_w1": C(g("cmp_w1")[0]), "cmp_w2": C(g("cmp_w2")[0]), "cmp_pe": C(g("cmp_pe")[0]),
        "cmp_b1T": C(g("cmp_b1")[0].reshape(2, 2, 128).transpose(2, 0, 1)),
        "cmp_b2T": C(g("cmp_b2")[0][0].reshape(64, 1)),
        "cmp_b2v": C(np.broadcast_to(g("cmp_b2")[0][1][None, :], (128, 64))),
        "Gtab": Gtab, "cover": cover,
        "cache_cmp_pg": g("cache_cmp_kv")[0].reshape(2560 * 4, 8192),
        "cache_sel_pg": g("cache_sel_kv")[0].reshape(2560 * 4, 8192),
    }
    tabs = [_nsa_tables(qq) for qq in range(4)]
    in_maps = []
    for c in range(8):
        b, qq = c // 4, c % 4
        own = np.concatenate([np.arange(128 * (4 * j + qq), 128 * (4 * j + qq) + 128) for j in range(16)])
        Ftab, cbias, triS, triW, triS_s, triW_s = tabs[qq]
        sel4 = np.zeros((128, 4), f32)
        sel4[:, qq] = 1.0
        m = dict(shared)
        m.update({
            "x_full": C(x_prompt[b]),
            "x_own": C(x_prompt[b][own]),
            "rope_own": C(rope_p[own]),
            "x_s": C(x_sample[SB * c:SB * c + SB].reshape(NS, D)),
            "cache_win": C(cache_win[SB * c:SB * c + SB]),
            "state_shift": C(state_shift[SB * c:SB * c + SB]),
            "state_wkv": C(state_wkv[SB * c:SB * c + SB]),
            "sel4": sel4, "Ftab": Ftab, "cbias": cbias, "triS": triS, "triW": triW, "triS_s": triS_s, "triW_s": triW_s,
            "pt_col": C(page_table[SB * c:SB * c + SB].reshape(SB, 64, 1)),
        })
        in_maps.append(m)
    ncores = OPTS["cores"]
    in_maps = [{k: v for k, v in mp.items() if k in _INPUT_NAMES} for mp in in_maps[:ncores]]
    res = run_bass_kernel_spmd(nc, in_maps, core_ids=list(range(ncores)))
    rs = list(res.results)
    global _LAST
    _LAST = rs
    if ncores < 8:
        return None
    y_prompt = np.zeros((2, SEQ, D), f32)
    y_sample = np.zeros((32, DS, D), f32)
    for c in range(8):
        b, qq = c // 4, c % 4
        oy = rs[c]["o_y"]
        for j in range(16):
            i = 4 * j + qq
            y_prompt[b, 128 * i:128 * i + 128] = oy[j * 128:(j + 1) * 128]
        y_sample[SB * c:SB * c + SB] = oy[2048:2048 + NS].reshape(SB, DS, D)
    cmp_p = np.stack([rs[0]["o_cmp_p"], rs[4]["o_cmp_p"]]).reshape(1, 2, SEQ, 2, 2, 64)
    sel_p = np.stack([rs[0]["o_sel_p"], rs[4]["o_sel_p"]]).reshape(1, 2, SEQ, 2, 2, 64)
    win_p = np.stack([rs[0]["o_win_p"], rs[4]["o_win_p"]]).reshape(1, 2, 512, 2, 2, 64)
    wkv_p = np.stack([rs[0]["o_wkv_p"], rs[4]["o_wkv_p"]]).reshape(1, 2, 8, 64, 64)
    shift_p = np.stack([rs[0]["o_shift_p"], rs[4]["o_shift_p"]]).reshape(1, 2, R_COLS)
    cmp_s = np.concatenate([rs[c]["o_cmp_s"] for c in range(8)]).reshape(1, 32, DS, 2, 2, 64)
    sel_s = np.concatenate([rs[c]["o_sel_s"] for c in range(8)]).reshape(1, 32, DS, 2, 2, 64)
    win_s = np.concatenate([rs[c]["o_win_s"] for c in range(8)]).reshape(1, 32, 512, 2, 2, 64)
    wkv_s = np.concatenate([rs[c]["o_wkv_s"] for c in range(8)]).reshape(1, 32, 8, 64, 64)
    shift_s = np.concatenate([rs[c]["o_shift_s"] for c in range(8)]).reshape(1, 32, R_COLS)
    return (y_prompt, y_sample, cmp_p.astype(f32), sel_p.astype(f32), win_p.astype(f32), wkv_p.astype(f32),
            shift_p.astype(f32), cmp_s.astype(f32), sel_s.astype(f32), win_s.astype(f32), wkv_s.astype(f32),
            shift_s.astype(f32))


_LAST = None
```

```python
import numpy as np
from contextlib import ExitStack
import concourse.bass as bass
import concourse.mybir as mybir
from concourse.bass_utils import run_bass_kernel_spmd

F32 = mybir.dt.float32
BF16 = mybir.dt.bfloat16
I32 = mybir.dt.int32
ALU = mybir.AluOpType
AF = mybir.ActivationFunctionType
AX = mybir.AxisListType

D = 1024
SEQ = 8192
NT = SEQ // 128
R_COLS = 1792
KV0 = 1792 + 512
NKV = 768
NA = R_COLS + NKV
IN_COLS = 5144
DS = 4
SB = 4
NS = SB * DS
PAST = 8192


class R:
    __slots__ = ("name", "w", "rs", "excl")

    def __init__(self, name=""):
        self.name = name
        self.w = None
        self.rs = []
        self.excl = False


class Prog:
    NDMA = 24

    def __init__(self, nc, stack):
        self.nc = nc
        self.stack = stack
        self.eng = {"pe": nc.tensor, "dve": nc.vector, "act": nc.scalar,
                    "pool": nc.gpsimd, "sp": nc.sync}
        self.sems = {}
        self.cnt = {}
        self.cur = {}
        self.dead = set()
        self.epoch = 0
        for k in self.eng:
            key = k + "#0"
            self.sems[key] = stack.enter_context(nc.semaphore("prog_" + k + "_0"))
            self.cnt[key] = 0
            self.cur[k] = key
        self.dq = {}
        for q in ("sp", "pool", "act"):
            pool = []
            for i in range(self.NDMA):
                key = "d_%s_%d" % (q, i)
                self.sems[key] = stack.enter_context(nc.semaphore(key))
                self.cnt[key] = 0
                pool.append(key)
            self.dq[q] = [pool, 0]
        self.waited = {k: {} for k in self.eng}
        self.n_inst = 0
        self.n_wait = 0
        self.pe_selfsync = False

    def _need(self, e, deps):
        best = {}
        for d in deps:
            if d is None:
                continue
            k, v = d
            if k in self.dead:
                continue
            if e == "pe" and k.startswith("pe#") and not self.pe_selfsync:
                continue
            if v > best.get(k, 0):
                best[k] = v
        for k, v in best.items():
            if self.waited[e].get(k, 0) >= v:
                continue
            self.eng[e].wait_ge(self.sems[k], v)
            self.waited[e][k] = v
            self.n_wait += 1

    def _deps(self, reads, writes):
        deps = []
        for r in reads:
            deps.append(r.w)
            if r.excl:
                deps.extend(r.rs)
        for w in writes:
            deps.append(w.w)
            deps.extend(w.rs)
        return deps

    def _commit(self, tok, reads, writes):
        for r in reads:
            if r.excl:
                r.w = tok
                r.rs = []
                continue
            r.rs.append(tok)
            if len(r.rs) > 48:
                best = {}
                for k, v in r.rs:
                    if v > best.get(k, 0):
                        best[k] = v
                r.rs = list(best.items())
        for w in writes:
            w.w = tok
            w.rs = []

    def op(self, e, fn, reads=(), writes=()):
        self._need(e, self._deps(reads, writes))
        ins = fn(self.eng[e])
        key = self.cur[e]
        self.cnt[key] += 1
        ins.then_inc(self.sems[key], 1)
        tok = (key, self.cnt[key])
        self._commit(tok, reads, writes)
        self.n_inst += 1
        return tok

    def dma(self, q, out, in_, reads=(), writes=(), **kw):
        pool, idx = self.dq[q]
        key = pool[idx % len(pool)]
        self.dq[q][1] = idx + 1
        deps = self._deps(reads, writes)
        if self.cnt[key] > 0:
            deps.append((key, self.cnt[key]))
        self._need(q, deps)
        ins = self.eng[q].dma_start(out=out, in_=in_, **kw)
        self.cnt[key] += 16
        ins.then_inc(self.sems[key], 16)
        tok = (key, self.cnt[key])
        self._commit(tok, reads, writes)
        self.n_inst += 1
        return tok

    def finish(self, regions):
        self._need("sp", [r.w for r in regions])

    def barrier(self):
        allc = [(k, v) for k, v in self.cnt.items() if v > 0 and k not in self.dead]
        for e in self.eng:
            self._need(e, allc)
        for e in self.eng:
            key = self.cur[e]
            if self.cnt[key] > 12000:
                self.dead.add(key)
                self.epoch += 1
                nk = "%s#%d" % (e, self.epoch)
                self.sems[nk] = self.stack.enter_context(self.nc.semaphore("prog_%s_%d" % (e, self.epoch)))
                self.cnt[nk] = 0
                self.cur[e] = nk


def PT(t, name):
    x = T(t, name)
    x.r.excl = True
    return x


class T:
    def __init__(self, t, name):
        self.t = t
        self.r = R(name)

    def __getitem__(self, k):
        return self.t[k]


def phase_A(nc, P, Dr, outs):
    x_full, x_s, w_in = Dr["x_full"], Dr["x_s"], Dr["w_in"]
    p_full, p_samp = Dr["p_full"], Dr["p_samp"]
    with ExitStack() as st:
        def sb(name, shape, dt=F32):
            return T(st.enter_context(nc.sbuf_tensor("a_" + name, list(shape), dt)), name)

        def ps(name, shape, dt=F32):
            return PT(st.enter_context(nc.psum_tensor("a_" + name, list(shape), dt)), name)

        ident = sb("ident", [128, 128], BF16)
        P.op("pool", lambda e: e.memset(ident[:], 0.0), writes=[ident.r])
        P.op("pool", lambda e: e.affine_select(ident[:], ident[:], [[-1, 128]], ALU.not_equal, 1.0,
                                               base=0, channel_multiplier=1),
             reads=[ident.r], writes=[ident.r])
        ropeT = sb("ropeT", [128, NT, 16])
        P.dma("sp", ropeT[:], Dr["rope_p"].rearrange("(n p) d -> p n d", p=128), writes=[ropeT.r])
        ropeS = sb("ropeS", [NS, 16])
        P.dma("sp", ropeS[:], Dr["rope_s"][:, :], writes=[ropeS.r])

        wA = sb("wA", [128, 8, NA], BF16)
        for kc in range(8):
            P.dma("pool", wA[:, kc, 0:R_COLS], w_in[kc * 128:(kc + 1) * 128, 0:R_COLS], writes=[wA.r])
            P.dma("pool", wA[:, kc, R_COLS:NA], w_in[kc * 128:(kc + 1) * 128, KV0:KV0 + NKV], writes=[wA.r])

        xb = [sb("xb%d" % i, [128, D], BF16) for i in range(2)]
        xT = [sb("xT%d" % i, [128, 8, 128], BF16) for i in range(2)]
        pt = [sb("ptile%d" % i, [128, NA]) for i in range(2)]
        rtmp = [sb("rtmp%d" % i, [128, 4, 6, 8]) for i in range(2)]
        ptr = [ps("ptr%d" % i, [128, 8, 128], BF16) for i in range(2)]
        pmm = [ps("pmm%d" % i, [128, 512]) for i in range(5)]
        pmm_i = [0]

        def rope_apply(tile_t, c0, cs_ap, tmp, n):
            v = tile_t.t[0:n, c0:c0 + 768].rearrange("p (a k g d) -> p a k g d", a=3, k=2, g=2)
            x1 = v[:, :, 0, :, 0:8]
            x2 = v[:, :, 0, :, 8:16]
            cos = cs_ap[:, 0:8].unsqueeze(1).unsqueeze(1).to_broadcast([n, 3, 2, 8])
            sin = cs_ap[:, 8:16].unsqueeze(1).unsqueeze(1).to_broadcast([n, 3, 2, 8])
            t = tmp.t[0:n]
            a1, a2, a3, a4 = [t[:, i, :, :].rearrange("p (a g) d -> p a g d", a=3) for i in range(4)]
            rd = [tile_t.r, tmp.r]
            P.op("dve", lambda e: e.tensor_tensor(a1, x1, cos, ALU.mult), reads=rd, writes=[tmp.r])
            P.op("dve", lambda e: e.tensor_tensor(a2, x2, sin, ALU.mult), reads=rd, writes=[tmp.r])
            P.op("dve", lambda e: e.tensor_tensor(a3, x2, cos, ALU.mult), reads=rd, writes=[tmp.r])
            P.op("dve", lambda e: e.tensor_tensor(a4, x1, sin, ALU.mult), reads=rd, writes=[tmp.r])
            P.op("dve", lambda e: e.tensor_tensor(x1, a1, a2, ALU.subtract), reads=rd, writes=[tile_t.r])
            P.op("dve", lambda e: e.tensor_tensor(x2, a3, a4, ALU.add), reads=rd, writes=[tile_t.r])

        r_pfull = R("p_full")
        r_out = R("outsA")
        outs.append(r_out)
        for ti in range(NT):
            b = ti % 2
            t0 = ti * 128
            P.dma("pool", xb[b][:], x_full[t0:t0 + 128, :], writes=[xb[b].r])
            for kc in range(8):
                P.op("pe", lambda e: e.transpose(ptr[b][:, kc, :], xb[b][:, kc * 128:(kc + 1) * 128], ident[:]),
                     reads=[xb[b].r, ident.r], writes=[ptr[b].r])
            P.op("act", lambda e: e.copy(xT[b][:], ptr[b][:]), reads=[ptr[b].r], writes=[xT[b].r])
            for nch in range(5):
                pm = pmm[pmm_i[0] % 5]
                pmm_i[0] += 1
                for kc in range(8):
                    P.op("pe", lambda e: e.matmul(pm[:], xT[b][:, kc, :], wA[:, kc, nch * 512:(nch + 1) * 512],
                                                  start=(kc == 0), stop=(kc == 7)),
                         reads=[xT[b].r, wA.r], writes=[pm.r])
                dst = pt[b][:, nch * 512:(nch + 1) * 512]
                if nch % 2 == 0:
                    P.op("dve", lambda e: e.tensor_copy(dst, pm[:]), reads=[pm.r], writes=[pt[b].r])
                else:
                    P.op("act", lambda e: e.copy(dst, pm[:]), reads=[pm.r], writes=[pt[b].r])
            rope_apply(pt[b], R_COLS, ropeT[:, ti, :], rtmp[b], 128)
            P.dma("sp", p_full[t0:t0 + 128, :], pt[b][:], reads=[pt[b].r], writes=[r_pfull])
            P.dma("sp", Dr["o_cmp_p"][t0:t0 + 128, :], pt[b][:, R_COLS:R_COLS + 256], reads=[pt[b].r], writes=[r_out])
            P.dma("sp", Dr["o_sel_p"][t0:t0 + 128, :], pt[b][:, R_COLS + 256:R_COLS + 512], reads=[pt[b].r], writes=[r_out])
            if ti >= NT - 4:
                w0 = (ti - (NT - 4)) * 128
                P.dma("sp", Dr["o_win_p"][w0:w0 + 128, :], pt[b][:, R_COLS + 512:R_COLS + 768], reads=[pt[b].r], writes=[r_out])
            if ti == NT - 1:
                P.dma("sp", Dr["o_shift_p"][0:1, :], pt[b][127:128, 0:R_COLS], reads=[pt[b].r], writes=[r_out])

        xsb = sb("xsb", [NS, D], BF16)
        xsT = sb("xsT", [128, 8, NS], BF16)
        psT = ptr[0]
        P.dma("pool", xsb[:], x_s[:, :], writes=[xsb.r])
        for kc in range(8):
            P.op("pe", lambda e: e.transpose(psT[:, kc, 0:NS], xsb[:, kc * 128:(kc + 1) * 128], ident[0:NS, 0:NS]),
                 reads=[xsb.r, ident.r], writes=[psT.r])
        P.op("act", lambda e: e.copy(xsT[:], psT[:, :, 0:NS]), reads=[psT.r], writes=[xsT.r])
        psamp = sb("psamp", [NS, IN_COLS])
        wS = [sb("wS%d" % i, [128, 8, 512], BF16) for i in range(2)]
        ncht = (IN_COLS + 511) // 512
        for nch in range(ncht):
            c0 = nch * 512
            cw = min(512, IN_COLS - c0)
            wb = wS[nch % 2]
            for kc in range(8):
                P.dma("pool", wb[:, kc, 0:cw], w_in[kc * 128:(kc + 1) * 128, c0:c0 + cw], writes=[wb.r])
            pm = pmm[pmm_i[0] % 5]
            pmm_i[0] += 1
            for kc in range(8):
                P.op("pe", lambda e: e.matmul(pm[0:NS, 0:cw], xsT[:, kc, :], wb[:, kc, 0:cw],
                                              start=(kc == 0), stop=(kc == 7)),
                     reads=[xsT.r, wb.r], writes=[pm.r])
            P.op("dve", lambda e: e.tensor_copy(psamp[:, c0:c0 + cw], pm[0:NS, 0:cw]), reads=[pm.r], writes=[psamp.r])
        rope_apply(psamp, KV0, ropeS[:, :], rtmp[0], NS)
        r_psamp = R("p_samp")
        P.dma("sp", p_samp[:, :], psamp[:], reads=[psamp.r], writes=[r_psamp])
        P.dma("sp", Dr["o_cmp_s"][:, :], psamp[:, KV0:KV0 + 256], reads=[psamp.r], writes=[r_out])
        P.dma("sp", Dr["o_sel_s"][:, :], psamp[:, KV0 + 256:KV0 + 512], reads=[psamp.r], writes=[r_out])
        for bb in range(SB):
            P.dma("sp", Dr["o_win_s"][bb, 508:512, :], psamp[bb * DS:(bb + 1) * DS, KV0 + 512:KV0 + 768],
                  reads=[psamp.r], writes=[r_out])
            P.dma("sp", Dr["o_win_s"][bb, 0:508, :], Dr["cache_win"][bb, 4:512, :], writes=[r_out])
            P.dma("sp", Dr["o_shift_s"][bb:bb + 1, :], psamp[bb * DS + DS - 1:bb * DS + DS, 0:R_COLS],
                  reads=[psamp.r], writes=[r_out])
        Dr["r_pfull"] = r_pfull
        Dr["r_psamp"] = r_psamp
        P.barrier()

VEC_OFF = {"mu": (0, 1792), "w0": (1792, 512), "a0": (2304, 512), "kk": (2816, 512), "ka": (3328, 512),
           "gng": (3840, 512), "gnb": (4352, 512), "rk": (4864, 512)}
NVEC = 5376
NMASK = 128 + 128 + 512 + 128 + 128


def phase_B(nc, P, Dr, outs):
    P.pe_selfsync = False
    with ExitStack() as st:
        def sb(name, shape, dt=F32):
            return T(st.enter_context(nc.sbuf_tensor("b_" + name, list(shape), dt)), name)

        def ps(name, shape, dt=F32):
            return PT(st.enter_context(nc.psum_tensor("b_" + name, list(shape), dt)), name)

        vecs = sb("vecs", [128, NVEC])
        P.dma("sp", vecs[:], Dr["vecs"][:, :], writes=[vecs.r])
        V = lambda k: vecs[:, VEC_OFF[k][0]:VEC_OFF[k][0] + VEC_OFF[k][1]]
        wlora = sb("wlora", [128, 512])
        P.dma("sp", wlora[0:64, :], Dr["w_w2"][:, :], writes=[wlora.r])
        P.dma("sp", wlora[64:128, :], Dr["w_a2"][:, :], writes=[wlora.r])
        gw2 = sb("gw2", [128, 512])
        P.dma("sp", gw2[:], Dr["g_w2"][:, :], writes=[gw2.r])
        masks = sb("masks", [128, NMASK])
        P.dma("sp", masks[:], Dr["masks"][:, :], writes=[masks.r])
        Lblk = masks[:, 0:128]
        Oblk = masks[:, 128:256]
        maskMA2 = masks[:, 256:768]
        maskNT = masks[:, 768:896]
        identF = masks[:, 896:1024]
        tmask = sb("tmask", [128, 1])
        P.dma("sp", tmask[:], Dr["tmask"][:, :], writes=[tmask.r])
        ones = sb("onesc", [128, 1])
        P.op("pool", lambda e: e.memset(ones[:], 1.0), writes=[ones.r])

        pr = sb("pr", [128, R_COLS])
        prev = sb("prev", [128, R_COLS])
        xm = sb("xm", [128, R_COLS])
        la = sb("la", [128, 256])
        laT = sb("laT", [128, 256])
        tA = sb("tA", [128, 512])
        tB = sb("tB", [128, 512])
        lw = sb("lw", [128, 512])
        aicl = sb("aicl", [128, 512])
        g_sb = sb("g_sb", [128, 512])
        kk = sb("kk", [128, 512])
        kkn = sb("kkn", [128, 512])
        kh = sb("kh", [128, 512])
        a_s = sb("a_s", [128, 512])
        b_s = sb("b_s", [128, 512])
        ss = sb("ss", [128, 8])
        rinv = sb("rinv", [128, 8])
        cum_sb = sb("cum_sb", [128, 512])
        e_sb = sb("e_sb", [128, 512])
        einv = sb("einv", [128, 512])
        ea = sb("ea", [128, 512])
        ec = sb("ec", [128, 512])
        at = sb("at", [128, 512])
        rt = sb("rt", [128, 512])
        bt = sb("bt", [128, 512])
        kt = sb("kt", [128, 512])
        bh = sb("bh", [128, 512])
        kh2 = sb("kh2", [128, 512])
        wc = sb("wc", [128, 8])
        FM_ar = sb("FM_ar", [128, 4, 256])
        FM_b = sb("FM_b", [128, 4, 128])
        FM_k = sb("FM_k", [128, 4, 128])
        MM = [sb("MM%d" % h, [128, 512]) for h in range(8)]
        TmA = [sb("TmA%d" % g, [128, 4, 128]) for g in range(2)]
        XAs = [[sb("XA%d_%d" % (g, i), [128, 4, 128]) for i in range(2)] for g in range(2)]
        XTAs = [[sb("XTA%d_%d" % (g, i), [128, 4, 128]) for i in range(2)] for g in range(2)]
        PMAs = [sb("PMA%d" % g, [128, 4, 128]) for g in range(2)]
        ZT_sb = sb("ZT_sb", [128, 512])
        UT_sb = sb("UT_sb", [128, 512])
        y_sb = sb("y_sb", [128, 512])
        yc = sb("yc", [128, 512])
        st1 = sb("st1", [128, 8])
        st2 = sb("st2", [128, 8])
        yo = sb("yo", [128, 512], BF16)
        ST = sb("ST", [128, 256])
        Sio = sb("Sio", [64, 512])

        pg = [ps("pg%d" % i, [128, 512]) for i in range(2)]
        pi = ps("pi", [128, 512])
        pv = [ps("pv%d" % i, [128, 512]) for i in range(2)]
        ZT_ps = ps("ZT_ps", [128, 512])
        UT_ps = ps("UT_ps", [128, 512])
        SN_ps = ps("SN_ps", [128, 512])

        def TT(eng, out, in0, in1, op, rd, wr):
            P.op(eng, lambda e: e.tensor_tensor(out, in0, in1, op), reads=rd, writes=wr)

        def ACT(out, in_, func, rd, wr, **kw):
            P.op("act", lambda e: e.activation(out, in_, func, **kw), reads=rd, writes=wr)

        def MMUL(out, lhsT, rhs, rd, wr, start=True, stop=True, sync=False):
            P.pe_selfsync = sync
            P.op("pe", lambda e: e.matmul(out, lhsT, rhs, start=start, stop=stop), reads=rd, writes=wr)
            P.pe_selfsync = False

        def TR(out, in_, idn, rd, wr):
            P.op("pe", lambda e: e.transpose(out, in_, idn), reads=rd + [masks.r], writes=wr)

        def load_state(src_ap):
            P.dma("sp", Sio[:].rearrange("i (h j) -> i h j", h=8), src_ap.rearrange("h i j -> i h j"), writes=[Sio.r])
            for hp in range(4):
                TR(pg[0][:, hp * 64:(hp + 1) * 64], Sio[:, hp * 128:(hp + 1) * 128], identF[0:64, 0:64], [Sio.r], [pg[0].r])
            P.op("dve", lambda e: e.tensor_copy(ST[:], pg[0][:, 0:256]), reads=[pg[0].r], writes=[ST.r])

        def store_state(dst_ap, rout):
            for hp in range(4):
                TR(pg[0][0:64, hp * 128:(hp + 1) * 128], ST[:, hp * 64:(hp + 1) * 64], identF, [ST.r], [pg[0].r])
            P.op("dve", lambda e: e.tensor_copy(Sio[:], pg[0][0:64, :]), reads=[pg[0].r], writes=[Sio.r])
            P.dma("sp", dst_ap.rearrange("h i j -> i h j"), Sio[:].rearrange("i (h j) -> i h j", h=8), reads=[Sio.r], writes=[rout])

        def rwkv_tile(load_fn, sample, pre_chunk, post_chunk, y_store):
            load_fn(pr, prev)
            TT("dve", prev[:], prev[:], pr[:], ALU.subtract, [prev.r, pr.r], [prev.r])
            TT("dve", prev[:], prev[:], V("mu"), ALU.mult, [prev.r, vecs.r], [prev.r])
            TT("dve", xm[:], prev[:], pr[:], ALU.add, [prev.r, pr.r], [xm.r])
            r_ = xm[:, 0:512]
            k_ = xm[:, 512:1024]
            v_ = xm[:, 1024:1536]
            ACT(la[:, 0:64], xm[:, 1536:1600], AF.Tanh, [xm.r], [la.r])
            ACT(la[:, 64:128], xm[:, 1600:1664], AF.Copy, [xm.r], [la.r])
            ACT(la[:, 128:256], xm[:, 1664:1792], AF.Sigmoid, [xm.r], [la.r])
            TR(pg[0][:, 0:128], la[:, 0:128], identF, [la.r], [pg[0].r])
            TR(pg[0][:, 128:256], la[:, 128:256], identF, [la.r], [pg[0].r])
            ACT(laT[:], pg[0][:, 0:256], AF.Copy, [pg[0].r], [laT.r])
            MMUL(pg[1][:], laT[0:64, 0:128], wlora[0:64, :], [laT.r, wlora.r], [pg[1].r])
            TT("dve", tA[:], pg[1][:], V("w0"), ALU.add, [pg[1].r, vecs.r], [tA.r])
            ACT(tA[:], tA[:], AF.Sigmoid, [tA.r], [tA.r])
            if sample:
                P.op("dve", lambda e: e.tensor_scalar(lw[:], tA[:], -0.6065306597126334, tmask[:, 0:1], ALU.mult, ALU.mult),
                     reads=[tA.r, tmask.r], writes=[lw.r])
            else:
                P.op("dve", lambda e: e.tensor_scalar(lw[:], tA[:], -0.6065306597126334, None, ALU.mult),
                     reads=[tA.r], writes=[lw.r])
            MMUL(pg[0][:], laT[64:128, 0:128], wlora[64:128, :], [laT.r, wlora.r], [pg[0].r])
            TT("dve", aicl[:], pg[0][:], V("a0"), ALU.add, [pg[0].r, vecs.r], [aicl.r])
            ACT(aicl[:], aicl[:], AF.Sigmoid, [aicl.r], [aicl.r])
            MMUL(pg[1][:], laT[:, 128:256], gw2[:], [laT.r, gw2.r], [pg[1].r])
            ACT(g_sb[:], pg[1][:], AF.Copy, [pg[1].r], [g_sb.r])
            TT("dve", kk[:], k_, V("kk"), ALU.mult, [xm.r, vecs.r], [kk.r])
            TT("dve", kkn[:], kk[:], kk[:], ALU.mult, [kk.r], [kkn.r])
            P.op("dve", lambda e: e.tensor_reduce(ss[:], kkn[:].rearrange("p (h j) -> p h j", h=8), AX.X, ALU.add),
                 reads=[kkn.r], writes=[ss.r])
            P.op("dve", lambda e: e.tensor_scalar(ss[:], ss[:], 1e-24, None, ALU.max), reads=[ss.r], writes=[ss.r])
            ACT(rinv[:], ss[:], AF.Sqrt, [ss.r], [rinv.r])
            P.op("dve", lambda e: e.reciprocal(rinv[:], rinv[:]), reads=[rinv.r], writes=[rinv.r])
            TT("dve", kkn[:].rearrange("p (h j) -> p h j", h=8), kk[:].rearrange("p (h j) -> p h j", h=8),
               rinv[:].unsqueeze(2).to_broadcast([128, 8, 64]), ALU.mult, [kk.r, rinv.r], [kkn.r])
            P.op("dve", lambda e: e.scalar_tensor_tensor(kh[:], aicl[:], -1.0, V("ka"), ALU.add, ALU.mult),
                 reads=[aicl.r, vecs.r], writes=[kh.r])
            P.op("dve", lambda e: e.scalar_tensor_tensor(kh[:], kh[:], 1.0, k_, ALU.add, ALU.mult),
                 reads=[kh.r, xm.r], writes=[kh.r])
            P.op("dve", lambda e: e.tensor_scalar(a_s[:], kkn[:], -1.0, None, ALU.mult), reads=[kkn.r], writes=[a_s.r])
            TT("dve", b_s[:], kkn[:], aicl[:], ALU.mult, [kkn.r, aicl.r], [b_s.r])
            if sample:
                P.op("dve", lambda e: e.tensor_scalar(kh[:], kh[:], tmask[:, 0:1], None, ALU.mult), reads=[kh.r, tmask.r], writes=[kh.r])
                P.op("dve", lambda e: e.tensor_scalar(b_s[:], b_s[:], tmask[:, 0:1], None, ALU.mult), reads=[b_s.r, tmask.r], writes=[b_s.r])
            MMUL(pg[0][:], Lblk, lw[:], [masks.r, lw.r], [pg[0].r])
            MMUL(pg[1][:], Oblk, lw[:], [masks.r, lw.r], [pg[1].r])
            ACT(cum_sb[:], pg[0][:], AF.Copy, [pg[0].r], [cum_sb.r])
            ACT(e_sb[:], pg[0][:], AF.Exp, [pg[0].r], [e_sb.r])
            ACT(einv[:], pg[0][:], AF.Exp, [pg[0].r], [einv.r], scale=-1.0)
            TT("dve", tA[:], pg[0][:], lw[:], ALU.subtract, [pg[0].r, lw.r], [tA.r])
            ACT(ea[:], tA[:], AF.Exp, [tA.r], [ea.r])
            TT("dve", tB[:], pg[1][:], cum_sb[:], ALU.subtract, [pg[1].r, cum_sb.r], [tB.r])
            ACT(ec[:], tB[:], AF.Exp, [tB.r], [ec.r])
            TT("dve", at[:], a_s[:], ea[:], ALU.mult, [a_s.r, ea.r], [at.r])
            TT("dve", rt[:], r_, e_sb[:], ALU.mult, [xm.r, e_sb.r], [rt.r])
            TT("dve", bt[:], b_s[:], einv[:], ALU.mult, [b_s.r, einv.r], [bt.r])
            TT("dve", kt[:], kh[:], einv[:], ALU.mult, [kh.r, einv.r], [kt.r])
            TT("dve", bh[:], b_s[:], ec[:], ALU.mult, [b_s.r, ec.r], [bh.r])
            TT("dve", kh2[:], kh[:], ec[:], ALU.mult, [kh.r, ec.r], [kh2.r])
            for c2 in range(2):
                rows = slice(c2 * 64, c2 * 64 + 64)
                for hp in range(4):
                    MMUL(SN_ps[:, 256 + c2 * 4 + hp:256 + c2 * 4 + hp + 1], lw[rows, hp * 128:(hp + 1) * 128], ones[rows, 0:1],
                         [lw.r, ones.r], [SN_ps.r], sync=True)
            ACT(wc[:], SN_ps[:, 256:264], AF.Exp, [SN_ps.r], [wc.r])
            for qi, q in enumerate((at, rt)):
                for hp in range(4):
                    TR(pg[qi][:, hp * 128:(hp + 1) * 128], q[:, hp * 128:(hp + 1) * 128], identF, [q.r], [pg[qi].r])
                P.op("act" if qi == 0 else "dve",
                     (lambda e: e.copy(FM_ar[:, :, 0:128], pg[0][:].rearrange("p (h t) -> p h t", h=4))) if qi == 0 else
                     (lambda e: e.tensor_copy(FM_ar[:, :, 128:256], pg[1][:].rearrange("p (h t) -> p h t", h=4))),
                     reads=[pg[qi].r], writes=[FM_ar.r])
            for qi, (q, dst) in enumerate(((bt, FM_b), (kt, FM_k))):
                for hp in range(4):
                    TR(pg[qi][:, hp * 128:(hp + 1) * 128], q[:, hp * 128:(hp + 1) * 128], identF, [q.r], [pg[qi].r])
                if qi == 0:
                    P.op("act", lambda e: e.copy(dst[:].rearrange("p h t -> p (h t)"), pg[0][:]), reads=[pg[0].r], writes=[dst.r])
                else:
                    P.op("dve", lambda e: e.tensor_copy(dst[:].rearrange("p h t -> p (h t)"), pg[1][:]), reads=[pg[1].r], writes=[dst.r])
            for h in range(8):
                hp, h2 = h // 2, h % 2
                rows = slice(h2 * 64, h2 * 64 + 64)
                MMUL(pi[:, 0:256], FM_b[rows, hp, :], FM_ar[rows, hp, :], [FM_b.r, FM_ar.r], [pi.r])
                MMUL(pi[:, 256:512], FM_k[rows, hp, :], FM_ar[rows, hp, :], [FM_k.r, FM_ar.r], [pi.r])
                TT("dve", MM[h][:], pi[:], maskMA2, ALU.mult, [pi.r, masks.r], [MM[h].r])
            banks = [(pv[0], pv[1], pi), (ZT_ps, UT_ps, SN_ps)]
            for grp in range(2):
                bX, bXT, bP = banks[grp]
                XA, XTA, PMA = XAs[grp], XTAs[grp], PMAs[grp]
                for q4 in range(4):
                    h = grp * 4 + q4
                    hp, h2 = h // 2, h % 2
                    rows = slice(h2 * 64, h2 * 64 + 64)
                    MMUL(bXT[:, q4 * 128:(q4 + 1) * 128], FM_ar[rows, hp, 0:128], FM_b[rows, hp, :], [FM_ar.r, FM_b.r], [bXT.r], sync=True)
                    P.op("dve", lambda e: e.tensor_copy(XA[0][:, q4, :], MM[h][:, 0:128]), reads=[MM[h].r], writes=[XA[0].r])
                    TT("dve", PMA[:, q4, :], MM[h][:, 0:128], identF, ALU.add, [MM[h].r, masks.r], [PMA.r])
                TT("dve", XTA[0][:], bXT[:].rearrange("p (h t) -> p h t", h=4), maskNT.unsqueeze(1).to_broadcast([128, 4, 128]), ALU.mult,
                   [bXT.r, masks.r], [XTA[0].r])
            for k in range(1, 6):
                for grp in range(2):
                    bX, bXT, bP = banks[grp]
                    XA, XTA, PMA = XAs[grp], XTAs[grp], PMAs[grp]
                    Xo, XTo = XA[(k - 1) % 2], XTA[(k - 1) % 2]
                    Xn, XTn = XA[k % 2], XTA[k % 2]
                    for q4 in range(4):
                        MMUL(bXT[:, q4 * 128:(q4 + 1) * 128], Xo[:, q4, :], XTo[:, q4, :], [Xo.r, XTo.r], [bXT.r])
                    if k < 5:
                        for q4 in range(4):
                            MMUL(bX[:, q4 * 128:(q4 + 1) * 128], XTo[:, q4, :], Xo[:, q4, :], [Xo.r, XTo.r], [bX.r])
                    P.op("act", lambda e: e.copy(XTn[:].rearrange("p h t -> p (h t)"), bXT[:]), reads=[bXT.r], writes=[XTn.r])
                    if k < 5:
                        P.op("dve", lambda e: e.tensor_copy(Xn[:].rearrange("p h t -> p (h t)"), bX[:]), reads=[bX.r], writes=[Xn.r])
                    for q4 in range(4):
                        MMUL(bP[:, q4 * 128:(q4 + 1) * 128], XTn[:, q4, :], PMA[:, q4, :], [XTn.r, PMA.r], [bP.r])
                    dstP = TmA[grp] if k == 5 else PMA
                    TT("dve", dstP[:].rearrange("p h t -> p (h t)"), bP[:], PMA[:].rearrange("p h t -> p (h t)"), ALU.add,
                       [bP.r, PMA.r], [dstP.r])
            for c2 in range(2):
                cs = slice(c2 * 64, c2 * 64 + 64)
                cc = slice(c2 * 64, c2 * 64 + 64)
                if pre_chunk is not None:
                    pre_chunk(c2)
                for h in range(8):
                    hp, h2 = h // 2, h % 2
                    rows = slice(h2 * 64, h2 * 64 + 64)
                    hc = slice(h * 64, h * 64 + 64)
                    Sh = ST[rows, hp * 64:(hp + 1) * 64]
                    MMUL(ZT_ps[cs, hc], FM_ar[rows, hp, cc], Sh, [FM_ar.r, ST.r], [ZT_ps.r], start=True, stop=False, sync=True)
                    MMUL(ZT_ps[cs, hc], MM[h][cs, 256 + c2 * 64:256 + c2 * 64 + 64], xm[cs, 1024 + h * 64:1024 + h * 64 + 64],
                         [MM[h].r, xm.r], [ZT_ps.r], start=False, stop=True, sync=True)
                P.op("act", lambda e: e.copy(ZT_sb[cs, :], ZT_ps[cs, :]), reads=[ZT_ps.r], writes=[ZT_sb.r])
                for h in range(8):
                    hc = slice(h * 64, h * 64 + 64)
                    MMUL(UT_ps[cs, hc], TmA[h // 4][cs, h % 4, cc], ZT_sb[cs, hc], [TmA[h // 4].r, ZT_sb.r], [UT_ps.r], sync=True)
                P.op("dve", lambda e: e.tensor_copy(UT_sb[cs, :], UT_ps[cs, :]), reads=[UT_ps.r], writes=[UT_sb.r])
                yps = pg[c2]
                for h in range(8):
                    hp, h2 = h // 2, h % 2
                    rows = slice(h2 * 64, h2 * 64 + 64)
                    hc = slice(h * 64, h * 64 + 64)
                    Sh = ST[rows, hp * 64:(hp + 1) * 64]
                    vh = xm[cs, 1024 + h * 64:1024 + h * 64 + 64]
                    MMUL(yps[cs, hc], FM_ar[rows, hp, 128 + c2 * 64:128 + c2 * 64 + 64], Sh, [FM_ar.r, ST.r], [yps.r], start=True, stop=False, sync=True)
                    MMUL(yps[cs, hc], MM[h][cs, 128 + c2 * 64:128 + c2 * 64 + 64], UT_sb[cs, hc], [MM[h].r, UT_sb.r], [yps.r], start=False, stop=False, sync=True)
                    MMUL(yps[cs, hc], MM[h][cs, 384 + c2 * 64:384 + c2 * 64 + 64], vh, [MM[h].r, xm.r], [yps.r], start=False, stop=True, sync=True)
                    MMUL(SN_ps[rows, hp * 64:(hp + 1) * 64], bh[cs, hc], UT_sb[cs, hc], [bh.r, UT_sb.r], [SN_ps.r], start=True, stop=False, sync=True)
                    MMUL(SN_ps[rows, hp * 64:(hp + 1) * 64], kh2[cs, hc], vh, [kh2.r, xm.r], [SN_ps.r], start=False, stop=True, sync=True)
                P.op("act", lambda e: e.copy(y_sb[cs, :], yps[cs, :]), reads=[yps.r], writes=[y_sb.r])
                TT("dve", ST[:].rearrange("p (h i) -> p h i", h=4), ST[:].rearrange("p (h i) -> p h i", h=4),
                   wc[:, c2 * 4:c2 * 4 + 4].unsqueeze(2).to_broadcast([128, 4, 64]), ALU.mult, [ST.r, wc.r], [ST.r])
                TT("dve", ST[:], ST[:], SN_ps[:, 0:256], ALU.add, [ST.r, SN_ps.r], [ST.r])
                if post_chunk is not None:
                    post_chunk(c2)
            y3 = y_sb[:].rearrange("p (h j) -> p h j", h=8)
            yc3 = yc[:].rearrange("p (h j) -> p h j", h=8)
            P.op("dve", lambda e: e.tensor_reduce(st1[:], y3, AX.X, ALU.add), reads=[y_sb.r], writes=[st1.r])
            P.op("dve", lambda e: e.tensor_scalar(st1[:], st1[:], 1.0 / 64, None, ALU.mult), reads=[st1.r], writes=[st1.r])
            TT("dve", yc3, y3, st1[:].unsqueeze(2).to_broadcast([128, 8, 64]), ALU.subtract, [y_sb.r, st1.r], [yc.r])
            TT("dve", tA[:], yc[:], yc[:], ALU.mult, [yc.r], [tA.r])
            P.op("dve", lambda e: e.tensor_reduce(st2[:], tA[:].rearrange("p (h j) -> p h j", h=8), AX.X, ALU.add), reads=[tA.r], writes=[st2.r])
            P.op("dve", lambda e: e.tensor_scalar(st2[:], st2[:], 1.0 / 64, 64e-5, ALU.mult, ALU.add), reads=[st2.r], writes=[st2.r])
            ACT(st2[:], st2[:], AF.Sqrt, [st2.r], [st2.r])
            P.op("dve", lambda e: e.reciprocal(st2[:], st2[:]), reads=[st2.r], writes=[st2.r])
            TT("dve", yc3, yc3, st2[:].unsqueeze(2).to_broadcast([128, 8, 64]), ALU.mult, [yc.r, st2.r], [yc.r])
            TT("dve", yc[:], yc[:], V("gng"), ALU.mult, [yc.r, vecs.r], [yc.r])
            TT("dve", yc[:], yc[:], V("gnb"), ALU.add, [yc.r, vecs.r], [yc.r])
            TT("dve", tB[:], r_, kh[:], ALU.mult, [xm.r, kh.r], [tB.r])
            TT("dve", tB[:], tB[:], V("rk"), ALU.mult, [tB.r, vecs.r], [tB.r])
            P.op("dve", lambda e: e.tensor_reduce(st1[:], tB[:].rearrange("p (h j) -> p h j", h=8), AX.X, ALU.add), reads=[tB.r], writes=[st1.r])
            TT("dve", tB[:].rearrange("p (h j) -> p h j", h=8), v_.rearrange("p (h j) -> p h j", h=8),
               st1[:].unsqueeze(2).to_broadcast([128, 8, 64]), ALU.mult, [xm.r, st1.r], [tB.r])
            TT("dve", yc[:], yc[:], tB[:], ALU.add, [yc.r, tB.r], [yc.r])
            TT("dve", yo[:], yc[:], g_sb[:], ALU.mult, [yc.r, g_sb.r], [yo.r])
            y_store(yo)

        P.op("dve", lambda e: e.memset(ST[:], 0.0), writes=[ST.r])
        r_yr = R("y_r")
        r_out = R("outB")
        outs.append(r_out)
        p_full = Dr["p_full"]
        for ti in range(NT):
            t0 = ti * 128

            def load_fn(pr_t, prev_t, t0=t0, ti=ti):
                P.dma("sp", pr_t[:], p_full[t0:t0 + 128, 0:R_COLS], reads=[Dr["r_pfull"]], writes=[pr_t.r])
                if ti == 0:
                    P.op("dve", lambda e: e.memset(prev_t[0:1, :], 0.0), writes=[prev_t.r])
                    P.dma("sp", prev_t[1:128, :], p_full[0:127, 0:R_COLS], reads=[Dr["r_pfull"]], writes=[prev_t.r])
                else:
                    P.dma("sp", prev_t[:], p_full[t0 - 1:t0 + 127, 0:R_COLS], reads=[Dr["r_pfull"]], writes=[prev_t.r])

            def y_store(yo_t, t0=t0):
                P.dma("pool", Dr["y_r"][t0:t0 + 128, :], yo_t[:], reads=[yo_t.r], writes=[r_yr])

            post = None
            if ti == NT - 1:
                def post(c2):
                    if c2 == 1:
                        store_state(Dr["o_wkv_p"], r_out)
            rwkv_tile(load_fn, False, None, post, y_store)
            if ti % 2 == 1:
                next(Dr["gather_gen"], None)

        p_samp = Dr["p_samp"]
        for tp in range(SB // 2):
            def load_fn(pr_t, prev_t, tp=tp):
                P.op("dve", lambda e: e.memset(pr_t[:], 0.0), writes=[pr_t.r])
                P.op("dve", lambda e: e.memset(prev_t[:], 0.0), writes=[prev_t.r])
                for c2 in range(2):
                    bb = tp * 2 + c2
                    P.dma("sp", pr_t[c2 * 64:c2 * 64 + DS, :], p_samp[bb * DS:(bb + 1) * DS, 0:R_COLS],
                          reads=[Dr["r_psamp"]], writes=[pr_t.r])
                    P.dma("sp", prev_t[c2 * 64:c2 * 64 + 1, :], Dr["state_shift"][bb:bb + 1, :], writes=[prev_t.r])
                    P.dma("sp", prev_t[c2 * 64 + 1:c2 * 64 + DS, :], p_samp[bb * DS:(bb + 1) * DS - 1, 0:R_COLS],
                          reads=[Dr["r_psamp"]], writes=[prev_t.r])

            def pre(c2, tp=tp):
                load_state(Dr["state_wkv"][tp * 2 + c2])

            def post(c2, tp=tp):
                store_state(Dr["o_wkv_s"][tp * 2 + c2], r_out)

            def y_store(yo_t, tp=tp):
                for c2 in range(2):
                    bb = tp * 2 + c2
                    P.dma("pool", Dr["y_r_s"][bb * DS:(bb + 1) * DS, :], yo_t[c2 * 64:c2 * 64 + DS, :], reads=[yo_t.r], writes=[r_yr])

            rwkv_tile(load_fn, True, pre, post, y_store)
        Dr["r_yr"] = r_yr
        P.barrier()
    P.pe_selfsync = False

QG0 = 1792
GATE0 = 3072
NEGB = -30000.0


def phase_C(nc, P, Dr, outs):
    for _ in Dr["gather_gen"]:
        pass
    P.barrier()
    Dr["gather_stack"].close()
    with ExitStack() as st:
        def sb(name, shape, dt=F32):
            return T(st.enter_context(nc.sbuf_tensor("c_" + name, list(shape), dt)), name)

        def ps(name, shape, dt=F32):
            return PT(st.enter_context(nc.psum_tensor("c_" + name, list(shape), dt)), name)

        def TT(eng, out, in0, in1, op, rd, wr):
            P.op(eng, lambda e: e.tensor_tensor(out, in0, in1, op), reads=rd, writes=wr)

        def MMUL(out, lhsT, rhs, rd, wr, start=True, stop=True):
            P.op("pe", lambda e: e.matmul(out, lhsT, rhs, start=start, stop=stop, skip_group_check=True), reads=rd, writes=wr)

        identb = sb("identb", [128, 128], BF16)
        P.dma("pool", identb[:], Dr["masks"][:, 896:1024], writes=[identb.r])
        identf = sb("identf", [128, 128])
        P.dma("sp", identf[:], Dr["masks"][:, 896:1024], writes=[identf.r])
        onesf = sb("onesf", [128, 128])
        P.op("pool", lambda e: e.memset(onesf[:], 1.0), writes=[onesf.r])
        NKT = 65
        ksT = [sb("ksT%d" % g, [65, NKT * 128], BF16) for g in range(2)]
        kwT = [sb("kwT%d" % g, [65, NKT * 128], BF16) for g in range(2)]
        vsw = sb("vsw", [128, NKT, 4, 65], BF16)
        kcmpT = [sb("kcmpT%d" % g, [65, 512], BF16) for g in range(2)]
        vcmp = sb("vcmp", [128, 4, 2, 65], BF16)
        nm = sb("nm", [128, 12])
        kmaxb = sb("kmaxb", [128, 1])
        for g in range(2):
            P.op("pool", lambda e: e.memset(ksT[g][64:65, :], 1.0), writes=[ksT[g].r])
            P.op("pool", lambda e: e.memset(kwT[g][64:65, :], 1.0), writes=[kwT[g].r])
            P.op("pool", lambda e: e.memset(kcmpT[g][64:65, :], 1.0), writes=[kcmpT[g].r])
        P.op("pool", lambda e: e.memset(vsw[:, :, :, 64:65], 1.0), writes=[vsw.r])
        P.op("pool", lambda e: e.memset(vcmp[:, :, :, 64:65], 1.0), writes=[vcmp.r])
        kvb = [sb("kvb%d" % i, [128, 768], BF16) for i in range(2)]
        sqt = sb("sqt", [128, 256])
        nt4 = sb("nt4", [128, 4])
        ptr = ps("ptr", [128, 8, 128], BF16)

        r_ya = R("y_a_own")

        def ingest_tile(kb, ti, do_cmp, do_sel, do_win, kcT, vcT, first):
            c0 = ti * 128
            rd = [kb.r, identb.r]
            ing = OPTS.get("ing", 15)
            do_cmp = do_cmp and bool(ing & 1)
            do_sel = do_sel and bool(ing & 2)
            do_win = do_win and bool(ing & 4)
            if do_cmp:
                P.op("pe", lambda e: e.transpose(ptr[:, 0, :], kb[:, 0:128], identb[:]), reads=rd, writes=[ptr.r])
                P.op("pe", lambda e: e.transpose(ptr[:, 1, :], kb[:, 128:256], identb[:]), reads=rd, writes=[ptr.r])
            if do_sel:
                P.op("pe", lambda e: e.transpose(ptr[0:64, 2, :], kb[:, 256:320], identb[:]), reads=rd, writes=[ptr.r])
                P.op("pe", lambda e: e.transpose(ptr[0:64, 3, :], kb[:, 320:384], identb[:]), reads=rd, writes=[ptr.r])
            if do_win:
                P.op("pe", lambda e: e.transpose(ptr[0:64, 4, :], kb[:, 512:576], identb[:]), reads=rd, writes=[ptr.r])
                P.op("pe", lambda e: e.transpose(ptr[0:64, 5, :], kb[:, 576:640], identb[:]), reads=rd, writes=[ptr.r])
            if do_cmp:
                P.op("act", lambda e: e.copy(kcT[:, c0:c0 + 128], ptr[:, 0, :]), reads=[ptr.r], writes=[kcT.r])
                P.op("act", lambda e: e.copy(vcT[:, c0:c0 + 128], ptr[:, 1, :]), reads=[ptr.r], writes=[vcT.r])
            if do_sel:
                P.op("dve", lambda e: e.tensor_copy(ksT[0][0:64, c0:c0 + 128], ptr[0:64, 2, :]), reads=[ptr.r], writes=[ksT[0].r])
                P.op("dve", lambda e: e.tensor_copy(ksT[1][0:64, c0:c0 + 128], ptr[0:64, 3, :]), reads=[ptr.r], writes=[ksT[1].r])
                P.op("act", lambda e: e.copy(vsw[:, ti, 0:2, 0:64], kb[:, 384:512].rearrange("p (g d) -> p g d", g=2)),
                     reads=[kb.r], writes=[vsw.r])
            if do_win:
                P.op("dve", lambda e: e.tensor_copy(kwT[0][0:64, c0:c0 + 128], ptr[0:64, 4, :]), reads=[ptr.r], writes=[kwT[0].r])
                P.op("dve", lambda e: e.tensor_copy(kwT[1][0:64, c0:c0 + 128], ptr[0:64, 5, :]), reads=[ptr.r], writes=[kwT[1].r])
                P.op("act", lambda e: e.copy(vsw[:, ti, 2:4, 0:64], kb[:, 640:768].rearrange("p (g d) -> p g d", g=2)),
                     reads=[kb.r], writes=[vsw.r])
            if not (ing & 8):
                return
            kk = kb[:, 256:768].rearrange("p (a r) -> p a r", a=2)[:, :, 0:128]
            P.op("dve", lambda e: e.tensor_tensor(sqt[:].rearrange("p (a r) -> p a r", a=2), kk, kk, ALU.mult), reads=[kb.r], writes=[sqt.r])
            P.op("dve", lambda e: e.tensor_reduce(nt4[:], sqt[:].rearrange("p (a d) -> p a d", a=4), AX.X, ALU.add), reads=[sqt.r], writes=[nt4.r])
            if first:
                P.op("dve", lambda e: e.tensor_copy(nm[:, 0:4], nt4[:]), reads=[nt4.r], writes=[nm.r])
            else:
                TT("dve", nm[:, 0:4], nm[:, 0:4], nt4[:], ALU.max, [nm.r, nt4.r], [nm.r])

        def compress_all(kcT, vcT):
            with ExitStack() as st2:
                def sb2(name, shape, dt=F32):
                    return T(st2.enter_context(nc.sbuf_tensor("c2_" + name + "_%d" % P.n_inst, list(shape), dt)), name)

                def ps2(name, shape, dt=F32):
                    return PT(st2.enter_context(nc.psum_tensor("c2_" + name + "_%d" % P.n_inst, list(shape), dt)), name)
                w1c = sb2("w1c", [128, 2, 32, 256], BF16)
                for kv in range(2):
                    src = Dr["cmp_w1"][kv].rearrange("(j d) h -> d j h", d=64)
                    P.dma("pool", w1c[0:64, kv, :, :], src, writes=[w1c.r])
                    P.dma("pool", w1c[64:128, kv, :, :], src, writes=[w1c.r])
                w2c = sb2("w2c", [128, 2, 2, 64], BF16)
                for kv in range(2):
                    P.dma("pool", w2c[:, kv, :, :], Dr["cmp_w2"][kv].rearrange("(c p) d -> p c d", p=128), writes=[w2c.r])
                pef = sb2("pef", [32, 2, 64])
                P.dma("sp", pef[:], Dr["cmp_pe"].rearrange("k j d -> j k d"), writes=[pef.r])
                peT = sb2("peT", [64, 2, 32], BF16)
                b1c = sb2("b1c", [128, 2, 2])
                P.dma("sp", b1c[:], Dr["cmp_b1T"][:, :, :], writes=[b1c.r])
                b2k = sb2("b2k", [64, 1])
                P.dma("sp", b2k[:], Dr["cmp_b2T"][:, :], writes=[b2k.r])
                b2v = sb2("b2v", [128, 64])
                P.dma("sp", b2v[:], Dr["cmp_b2v"][:, :], writes=[b2v.r])
                cb = sb2("cb", [128, 2, 2])
                hx = sb2("hx", [128, 512])
                hu = sb2("hu", [128, 512])
                hT = sb2("hT", [128, 2, 512], BF16)
                kcf = sb2("kcf", [64, 512])
                P.op("pool", lambda e: e.memset(hT[:], 0.0), writes=[hT.r])
                pc = [ps2("pc%d" % i, [128, 512]) for i in range(2)]
                pk = ps2("pk", [128, 512])
                pcm = ps2("pcm", [128, 512])
                for kv in range(2):
                    P.op("pe", lambda e: e.transpose(pcm[0:64, kv * 32:(kv + 1) * 32], pef[:, kv, :], identf[0:32, 0:32]),
                         reads=[pef.r, identf.r], writes=[pcm.r])
                P.op("dve", lambda e: e.tensor_copy(peT[:].rearrange("p k j -> p (k j)"), pcm[0:64, 0:64]), reads=[pcm.r], writes=[peT.r])
                for kv in range(2):
                    for hc in range(2):
                        col = kv * 2 + hc
                        for j in range(32):
                            MMUL(pcm[:, 64 + col:65 + col], w1c[0:64, kv, j, hc * 128:(hc + 1) * 128], peT[:, kv, j:j + 1],
                                 [w1c.r, peT.r], [pcm.r], start=(j == 0), stop=(j == 31))
                TT("dve", cb[:].rearrange("p k c -> p (k c)"), pcm[:, 64:68], b1c[:].rearrange("p k c -> p (k c)"), ALU.add,
                   [pcm.r, b1c.r], [cb.r])
                for kv, srcT in ((0, kcT), (1, vcT)):
                    for g in range(2):
                        rows = slice(g * 64, g * 64 + 64)
                        for hc in range(2):
                            pp = pc[hc]
                            for j in range(32):
                                MMUL(pp[:, 0:511], w1c[rows, kv, j, hc * 128:(hc + 1) * 128],
                                     srcT[rows, j:j + 16 * 510 + 1:16], [w1c.r, srcT.r], [pp.r], start=(j == 0), stop=(j == 31))
                            P.op("act", lambda e: e.activation(hx[:, 0:511], pp[:, 0:511], AF.Identity, bias=cb[:, kv, hc:hc + 1]),
                                 reads=[pp.r, cb.r], writes=[hx.r])
                            TT("dve", hu[:, 0:511], hx[:, 0:511], hx[:, 0:511], ALU.mult, [hx.r], [hu.r])
                            P.op("dve", lambda e: e.tensor_scalar(hu[:, 0:511], hu[:, 0:511], 0.044715, 1.0, ALU.mult, ALU.add), reads=[hu.r], writes=[hu.r])
                            TT("dve", hu[:, 0:511], hu[:, 0:511], hx[:, 0:511], ALU.mult, [hu.r, hx.r], [hu.r])
                            P.op("act", lambda e: e.activation(hu[:, 0:511], hu[:, 0:511], AF.Sigmoid, scale=1.5957691216057308), reads=[hu.r], writes=[hu.r])
                            TT("dve", hT[:, hc, 0:511], hx[:, 0:511], hu[:, 0:511], ALU.mult, [hx.r, hu.r], [hT.r])
                        if kv == 0:
                            for hc in range(2):
                                MMUL(pk[0:64, 0:511], w2c[:, 0, hc, :], hT[:, hc, 0:511], [w2c.r, hT.r], [pk.r], start=(hc == 0), stop=(hc == 1))
                            P.op("act", lambda e: e.activation(kcf[:, 0:511], pk[0:64, 0:511], AF.Identity, bias=b2k[:, 0:1]),
                                 reads=[pk.r, b2k.r], writes=[kcf.r])
                            P.op("pool", lambda e: e.memset(kcf[:, 511:512], 0.0), writes=[kcf.r])
                            P.op("dve", lambda e: e.tensor_copy(kcmpT[g][0:64, :], kcf[:, :]), reads=[kcf.r], writes=[kcmpT[g].r])
                            TT("dve", kcf[:, :], kcf[:, :], kcf[:, :], ALU.mult, [kcf.r], [kcf.r])
                            for bt in range(4):
                                MMUL(pcm[:, 80 + g * 4 + bt:81 + g * 4 + bt], kcf[:, bt * 128:(bt + 1) * 128], onesf[0:64, 0:1],
                                     [kcf.r, onesf.r], [pcm.r])
                            P.op("dve", lambda e: e.tensor_copy(nm[:, 4 + g * 4:8 + g * 4], pcm[:, 80 + g * 4:84 + g * 4]), reads=[pcm.r], writes=[nm.r])
                        else:
                            for bt in range(4):
                                nb = 128
                                for hc in range(2):
                                    MMUL(pk[0:nb, bt * 64:(bt + 1) * 64], hT[:, hc, bt * 128:bt * 128 + nb], w2c[:, 1, hc, :],
                                         [hT.r, w2c.r], [pk.r], start=(hc == 0), stop=(hc == 1))
                                TT("dve", vcmp[0:nb, bt, g, 0:64], pk[0:nb, bt * 64:(bt + 1) * 64], b2v[0:nb, :], ALU.add, [pk.r, b2v.r], [vcmp.r])
                P.op("pe", lambda e: e.transpose(pcm[0:12, 128:256], nm[:, 0:12], identf[:]), reads=[nm.r, identf.r], writes=[pcm.r])
                P.op("dve", lambda e: e.tensor_reduce(hx[0:12, 0:1], pcm[0:12, 128:256], AX.X, ALU.max), reads=[pcm.r], writes=[hx.r])
                P.op("pe", lambda e: e.transpose(pcm[0:1, 256:268], hx[0:12, 0:1], identf[0:12, 0:12]), reads=[hx.r, identf.r], writes=[pcm.r])
                P.op("dve", lambda e: e.tensor_reduce(hx[0:1, 1:2], pcm[0:1, 256:268], AX.X, ALU.max), reads=[pcm.r], writes=[hx.r])
                P.op("act", lambda e: e.activation(hx[0:1, 2:3], hx[0:1, 1:2], AF.Sqrt), reads=[hx.r], writes=[hx.r])
                MMUL(pcm[:, 300:301], onesf[0:1, :], hx[0:1, 2:3], [onesf.r, hx.r], [pcm.r])
                P.op("dve", lambda e: e.tensor_copy(kmaxb[:], pcm[:, 300:301]), reads=[pcm.r], writes=[kmaxb.r])
                P.barrier()

        def attention_scope(run):
            with ExitStack() as st3:
                def sb3(name, shape, dt=F32):
                    return T(st3.enter_context(nc.sbuf_tensor("c3_" + name + "_%d" % P.n_inst, list(shape), dt)), name)

                def ps3(name, shape, dt=F32):
                    return PT(st3.enter_context(nc.psum_tensor("c3_" + name + "_%d" % P.n_inst, list(shape), dt)), name)
                A = {}
                A["G"] = sb3("G", [128, 8192], BF16)
                P.dma("pool", A["G"][:], Dr["Gtab"][:, :], writes=[A["G"].r])
                A["cover"] = sb3("cover", [128, 4, 128], BF16)
                P.dma("pool", A["cover"][:], Dr["cover"][:, :, :], writes=[A["cover"].r])
                A["wq"] = sb3("wq", [128, 8, 536], BF16)
                for kc in range(8):
                    P.dma("pool", A["wq"][:, kc, 0:512], Dr["w_in"][kc * 128:(kc + 1) * 128, QG0:QG0 + 512], writes=[A["wq"].r])
                    P.dma("pool", A["wq"][:, kc, 512:536], Dr["w_in"][kc * 128:(kc + 1) * 128, GATE0:GATE0 + 24], writes=[A["wq"].r])
                A["Ftab"] = sb3("Ftab", [128, 128])
                A["cb2"] = sb3("cb2", [128, 2, 128], BF16)
                A["triS"] = sb3("triS", [128, 4, 512], BF16)
                A["triW"] = sb3("triW", [128, 8, 512], BF16)
                A["xb"] = sb3("xb", [128, 1024], BF16)
                A["xT"] = sb3("xT", [128, 8, 128], BF16)
                A["qf"] = sb3("qf", [128, 512])
                A["rt"] = sb3("rt", [128, 4, 8, 8])
                A["rope"] = sb3("rope", [128, 16])
                A["gts"] = sb3("gts", [128, 24])
                A["qn"] = sb3("qn", [128, 8])
                A["qa"] = sb3("qa", [128, 8, 65], BF16)
                A["qT"] = sb3("qT", [65, 8, 128], BF16)
                A["pT"] = [sb3("pT%d" % i, [128, 512], BF16) for i in range(2)]
                A["ov"] = sb3("ov", [128, 4, 65])
                A["rl"] = sb3("rl", [128, 4])
                A["cf"] = sb3("cf", [128, 4])
                A["imp"] = sb3("imp", [128, 128])
                A["sc"] = sb3("sc", [128, 128])
                A["sc2"] = sb3("sc2", [128, 128])
                A["m8a"] = sb3("m8a", [128, 8])
                A["m8b"] = sb3("m8b", [128, 8])
                A["mb4"] = sb3("mb4", [128, 4, 128], BF16)
                A["ya"] = sb3("ya", [128, 8, 64])
                A["yab"] = sb3("yab", [128, 512], BF16)
                A["tmp"] = sb3("tmp", [128, 4, 64])
                A["sT"] = [ps3("sT%d" % i, [128, 512]) for i in range(2)]
                A["po"] = [ps3("po%d" % i, [128, 512]) for i in range(3)]
                A["ir"] = ps3("ir", [128, 512])
                A["pq"] = ps3("pq", [128, 512])
                A["cnt"] = {"sT": 0, "po": 0, "pT": 0}
                run(A)
                P.barrier()

        def q_prepare(A, n, load_x, load_q, rope_src):
            qf, gts, qa, qT, pq = A["qf"], A["gts"], A["qa"], A["qT"], A["pq"]
            if n < 128:
                P.op("pool", lambda e: e.memset(qa[:], 0.0), writes=[qa.r])
            if load_x is not None:
                load_x(A["xb"])
                for kc in range(8):
                    P.op("pe", lambda e: e.transpose(ptr[:, kc, :], A["xb"][:, kc * 128:(kc + 1) * 128], identb[:]),
                         reads=[A["xb"].r, identb.r], writes=[ptr.r])
                P.op("act", lambda e: e.copy(A["xT"][:], ptr[:]), reads=[ptr.r], writes=[A["xT"].r])
                for kc in range(8):
                    MMUL(pq[:, :], A["xT"][:, kc, :], A["wq"][:, kc, 0:512], [A["xT"].r, A["wq"].r], [pq.r], start=(kc == 0), stop=(kc == 7))
                P.op("act", lambda e: e.activation(qf[:], pq[:], AF.Copy, scale=0.125), reads=[pq.r], writes=[qf.r])
                for kc in range(8):
                    MMUL(pq[:, 0:24], A["xT"][:, kc, :], A["wq"][:, kc, 512:536], [A["xT"].r, A["wq"].r], [pq.r], start=(kc == 0), stop=(kc == 7))
                P.op("act", lambda e: e.activation(gts[:], pq[:, 0:24], AF.Sigmoid), reads=[pq.r], writes=[gts.r])
            else:
                load_q(qf, gts)
                P.op("act", lambda e: e.activation(qf[0:n, :], qf[0:n, :], AF.Copy, scale=0.125), reads=[qf.r], writes=[qf.r])
                P.op("act", lambda e: e.activation(gts[0:n, :], gts[0:n, :], AF.Sigmoid), reads=[gts.r], writes=[gts.r])
            P.dma("sp", A["rope"][0:n, :], rope_src, writes=[A["rope"].r])
            q3 = qf[0:n, :].rearrange("p (h d) -> p h d", h=8)
            x1 = q3[:, :, 0:8]
            x2 = q3[:, :, 8:16]
            cos = A["rope"][0:n, 0:8].unsqueeze(1).to_broadcast([n, 8, 8])
            sin = A["rope"][0:n, 8:16].unsqueeze(1).to_broadcast([n, 8, 8])
            rt = A["rt"]
            rd = [qf.r, rt.r, A["rope"].r]
            TT("dve", rt[0:n, 0], x1, cos, ALU.mult, rd, [rt.r])
            TT("dve", rt[0:n, 1], x2, sin, ALU.mult, rd, [rt.r])
            TT("dve", rt[0:n, 2], x2, cos, ALU.mult, rd, [rt.r])
            TT("dve", rt[0:n, 3], x1, sin, ALU.mult, rd, [rt.r])
            TT("dve", x1, rt[0:n, 0], rt[0:n, 1], ALU.subtract, rd, [qf.r])
            TT("dve", x2, rt[0:n, 2], rt[0:n, 3], ALU.add, rd, [qf.r])
            TT("dve", A["ya"][0:n].rearrange("p h d -> p (h d)"), qf[0:n, :], qf[0:n, :], ALU.mult, [qf.r], [A["ya"].r])
            P.op("dve", lambda e: e.tensor_reduce(A["qn"][0:n, :], A["ya"][0:n], AX.X, ALU.add), reads=[A["ya"].r], writes=[A["qn"].r])
            P.op("act", lambda e: e.activation(A["qn"][0:n, :], A["qn"][0:n, :], AF.Sqrt), reads=[A["qn"].r], writes=[A["qn"].r])
            P.op("dve", lambda e: e.tensor_scalar(A["qn"][0:n, :], A["qn"][0:n, :], kmaxb[0:n, 0:1], -1.0, ALU.mult, ALU.mult),
                 reads=[A["qn"].r, kmaxb.r], writes=[A["qn"].r])
            P.op("dve", lambda e: e.tensor_copy(qa[0:n, :, 0:64], q3), reads=[qf.r], writes=[qa.r])
            P.op("dve", lambda e: e.tensor_copy(qa[0:n, :, 64:65], A["qn"][0:n, :].unsqueeze(2)), reads=[A["qn"].r], writes=[qa.r])
            for h in range(8):
                P.op("pe", lambda e: e.transpose(ptr[0:65, h, :], qa[:, h, :], identb[:]), reads=[qa.r, identb.r], writes=[ptr.r])
            P.op("act", lambda e: e.copy(qT[:], ptr[0:65, :, :]), reads=[ptr.r], writes=[qT.r])

        def nsa_qtile(A, n, cfg):
            qT, gts, ya = A["qT"], A["gts"], A["ya"]
            cnt = A["cnt"]

            def next_sT():
                t = A["sT"][cnt["sT"] % 2]
                cnt["sT"] += 1
                return t

            def next_pT():
                t = A["pT"][cnt["pT"] % 2]
                cnt["pT"] += 1
                return t

            def next_po():
                t = A["po"][cnt["po"] % 3]
                cnt["po"] += 1
                return t

            def finish_branch(po_t, g, br, first):
                ov, rl, cf = A["ov"], A["rl"], A["cf"]
                P.op("act", lambda e: e.copy(ov[:].rearrange("p h d -> p (h d)"), po_t[:, 0:260]), reads=[po_t.r], writes=[ov.r])
                P.op("dve", lambda e: e.tensor_scalar(rl[:], ov[:, :, 64], 1e-30, None, ALU.max), reads=[ov.r], writes=[rl.r])
                P.op("dve", lambda e: e.reciprocal(rl[:], rl[:]), reads=[rl.r], writes=[rl.r])
                g3 = gts[:, :].rearrange("p (h b) -> p h b", b=3)
                TT("dve", cf[:], rl[:], g3[:, 4 * g:4 * g + 4, br], ALU.mult, [rl.r, gts.r], [cf.r])
                dst = ya[:, 4 * g:4 * g + 4, :]
                cfb = cf[:].unsqueeze(2).to_broadcast([128, 4, 64])
                if first:
                    TT("dve", dst, ov[:, :, 0:64], cfb, ALU.mult, [ov.r, cf.r], [ya.r])
                else:
                    TT("dve", A["tmp"][:], ov[:, :, 0:64], cfb, ALU.mult, [ov.r, cf.r], [A["tmp"].r])
                    TT("dve", dst, dst, A["tmp"][:], ALU.add, [ya.r, A["tmp"].r], [ya.r])

            for g in range(2):
                qTg = qT[0:65, 4 * g:4 * g + 4, :].rearrange("p h q -> p (h q)")
                po_t = next_po()
                ir = A["ir"]
                nbt = cfg["nbt"]
                for bt in range(nbt):
                    sT = next_sT()
                    slots = [s for (b_, s) in cfg["cb_tiles"] if b_ == bt]
                    MMUL(sT[:, :], kcmpT[g][0:65, bt * 128:(bt + 1) * 128], qTg, [kcmpT[g].r, qT.r], [sT.r], start=True, stop=(not slots))
                    for s in slots:
                        for h4 in range(4):
                            MMUL(sT[:, h4 * 128:(h4 + 1) * 128], identb[:], A["cb2"][:, s, :], [identb.r, A["cb2"].r], [sT.r],
                                 start=False, stop=(h4 == 3))
                    pT = next_pT()
                    P.op("act", lambda e: e.activation(pT[:], sT[:], AF.Exp), reads=[sT.r], writes=[pT.r])
                    for h4 in range(4):
                        MMUL(po_t[:, h4 * 65:(h4 + 1) * 65], pT[:, h4 * 128:(h4 + 1) * 128], vcmp[:, bt, g, :], [pT.r, vcmp.r], [po_t.r],
                             start=(bt == 0 and h4 == 0), stop=(bt == nbt - 1))
                        MMUL(ir[:, h4 * 128:(h4 + 1) * 128], pT[:, h4 * 128:(h4 + 1) * 128], A["cover"][:, bt, :], [pT.r, A["cover"].r], [ir.r],
                             start=(bt == 0 and h4 == 0), stop=(bt == nbt - 1))
                finish_branch(po_t, g, 0, True)
                rl = A["rl"]
                imp = A["imp"]
                P.op("dve", lambda e: e.tensor_scalar(imp[:], ir[:, 0:128], rl[:, 0:1], None, ALU.mult), reads=[ir.r, rl.r], writes=[imp.r])
                for h4 in range(1, 4):
                    P.op("dve", lambda e: e.scalar_tensor_tensor(imp[:], ir[:, h4 * 128:(h4 + 1) * 128], rl[:, h4:h4 + 1], imp[:], ALU.mult, ALU.add),
                         reads=[ir.r, rl.r, imp.r], writes=[imp.r])
                sc, sc2, m8a, m8b = A["sc"], A["sc2"], A["m8a"], A["m8b"]
                TT("dve", sc[:], imp[:], A["Ftab"][:], ALU.add, [imp.r, A["Ftab"].r], [sc.r])
                P.op("dve", lambda e: e.max(out=m8a[:], in_=sc[:]), reads=[sc.r], writes=[m8a.r])
                P.op("dve", lambda e: e.match_replace(out=sc2[:], in_to_replace=m8a[:], in_values=sc[:], imm_value=-3.0e38),
                     reads=[sc.r, m8a.r], writes=[sc2.r])
                P.op("dve", lambda e: e.max(out=m8b[:], in_=sc2[:]), reads=[sc2.r], writes=[m8b.r])
                tc_ = cfg["topk_col"]
                P.op("dve", lambda e: e.tensor_scalar(sc2[:], sc[:], m8b[:, tc_:tc_ + 1], None, ALU.is_ge), reads=[sc.r, m8b.r], writes=[sc2.r])
                P.op("dve", lambda e: e.tensor_scalar(sc[:], sc[:], -1.0e29, None, ALU.is_gt), reads=[sc.r], writes=[sc.r])
                TT("dve", sc[:], sc[:], sc2[:], ALU.mult, [sc.r, sc2.r], [sc.r])
                P.op("dve", lambda e: e.tensor_scalar(sc[:], sc[:], -NEGB, NEGB, ALU.mult, ALU.add), reads=[sc.r], writes=[sc.r])
                pq = A["pq"]
                P.op("pe", lambda e: e.transpose(pq[:, 0:128], sc[:], identf[:]), reads=[sc.r, identf.r], writes=[pq.r])
                mb4 = A["mb4"]
                P.op("dve", lambda e: e.tensor_copy(mb4[:], pq[:, 0:128].unsqueeze(1).to_broadcast([128, 4, 128])), reads=[pq.r], writes=[mb4.r])
                po_t = next_po()
                tiles = cfg["sel_tiles"]
                for ii, (c, use_G, tri) in enumerate(tiles):
                    sT = next_sT()
                    last = (not use_G) and (tri is None)
                    MMUL(sT[:, :], ksT[g][0:65, c * 128:(c + 1) * 128], qTg, [ksT[g].r, qT.r], [sT.r], start=True, stop=last)
                    if use_G:
                        MMUL(sT[:, :], A["G"][:, c * 128:(c + 1) * 128], mb4[:].rearrange("p h q -> p (h q)"), [A["G"].r, mb4.r], [sT.r],
                             start=False, stop=(tri is None))
                    if tri is not None:
                        MMUL(sT[:, :], identb[:], A["triS"][:, tri, :], [identb.r, A["triS"].r], [sT.r], start=False, stop=True)
                    pT = next_pT()
                    P.op("act", lambda e: e.activation(pT[:], sT[:], AF.Exp), reads=[sT.r], writes=[pT.r])
                    for h4 in range(4):
                        MMUL(po_t[:, h4 * 65:(h4 + 1) * 65], pT[:, h4 * 128:(h4 + 1) * 128], vsw[:, c, g, :], [pT.r, vsw.r], [po_t.r],
                             start=(ii == 0 and h4 == 0), stop=(ii == len(tiles) - 1))
                finish_branch(po_t, g, 1, False)
                po_t = next_po()
                tiles = cfg["win_tiles"]
                for ii, (c, slot) in enumerate(tiles):
                    sT = next_sT()
                    MMUL(sT[:, :], kwT[g][0:65, c * 128:(c + 1) * 128], qTg, [kwT[g].r, qT.r], [sT.r], start=True, stop=False)
                    MMUL(sT[:, :], identb[:], A["triW"][:, slot, :], [identb.r, A["triW"].r], [sT.r], start=False, stop=True)
                    pT = next_pT()
                    P.op("act", lambda e: e.activation(pT[:], sT[:], AF.Exp), reads=[sT.r], writes=[pT.r])
                    for h4 in range(4):
                        MMUL(po_t[:, h4 * 65:(h4 + 1) * 65], pT[:, h4 * 128:(h4 + 1) * 128], vsw[:, c, 2 + g, :], [pT.r, vsw.r], [po_t.r],
                             start=(ii == 0 and h4 == 0), stop=(ii == len(tiles) - 1))
                finish_branch(po_t, g, 2, False)
            P.op("act", lambda e: e.copy(A["yab"][:], ya[:].rearrange("p h d -> p (h d)")), reads=[ya.r], writes=[A["yab"].r])

        def bail():
            Dr["r_ya"] = r_ya
            P.barrier()
        if OPTS.get("cstop", 9) <= 1:
            return bail()
        with ExitStack() as stp:
            kcT = T(stp.enter_context(nc.sbuf_tensor("c_kcT_p", [128, SEQ], BF16)), "kcT")
            vcT = T(stp.enter_context(nc.sbuf_tensor("c_vcT_p", [128, SEQ], BF16)), "vcT")
            for ti in range(NT):
                kb = kvb[ti % 2]
                P.dma("pool", kb[:], Dr["p_full"][ti * 128:(ti + 1) * 128, R_COLS:NA], reads=[Dr["r_pfull"]], writes=[kb.r])
                ingest_tile(kb, ti, True, True, True, kcT, vcT, ti == 0)
            if OPTS.get("cstop", 9) > 2:
                compress_all(kcT, vcT)
        if OPTS.get("cstop", 9) <= 3:
            return bail()

        def run_prompt(A):
            for j in range(OPTS["cq"]):
                P.dma("sp", A["Ftab"][:], Dr["Ftab"][:, j, :], writes=[A["Ftab"].r])
                P.dma("pool", A["cb2"][:], Dr["cbias"][:, j, :, :], writes=[A["cb2"].r])
                if j == 0:
                    P.dma("pool", A["triS"][:], Dr["triS"][:, :, :], writes=[A["triS"].r])
                    P.dma("pool", A["triW"][:], Dr["triW"][:, :, :], writes=[A["triW"].r])

                def load_x(xb, j=j):
                    P.dma("pool", xb[:], Dr["x_own"][j * 128:(j + 1) * 128, :], writes=[xb.r])
                q_prepare(A, 128, load_x, None, Dr["rope_own"][j * 128:(j + 1) * 128, :])
                nbt = (32 * j + 32 + 127) // 128
                cb_tiles = [(nbt - 1, 1)] + ([(nbt - 2, 0)] if nbt >= 2 else [])
                cfg = {"nbt": nbt, "cb_tiles": cb_tiles, "topk_col": 7,
                       "sel_tiles": [(c, True, (c - 4 * j) if c >= 4 * j else None) for c in range(4 * j + 4)],
                       "win_tiles": [(c, c - (4 * j - 4)) for c in range(max(4 * j - 4, 0), 4 * j + 4)]}
                nsa_qtile(A, 128, cfg)
                P.dma("sp", Dr["y_a_own"][j * 128:(j + 1) * 128, :], A["yab"][:], reads=[A["yab"].r], writes=[r_ya])
        attention_scope(run_prompt)

        r_gath = Dr["r_gath"]
        for bb in range(OPTS["sb"]):
            with ExitStack() as stp:
                kcT = T(stp.enter_context(nc.sbuf_tensor("c_kcT_s%d" % bb, [128, SEQ], BF16)), "kcT")
                vcT = T(stp.enter_context(nc.sbuf_tensor("c_vcT_s%d" % bb, [128, SEQ], BF16)), "vcT")
                for ti in range(64):
                    kb = kvb[ti % 2]
                    P.dma("pool", kb[:, 0:256], Dr["gath"][bb, 0, ti, :].rearrange("(p c) -> p c", p=128), reads=[r_gath], writes=[kb.r])
                    P.dma("pool", kb[:, 256:512], Dr["gath"][bb, 1, ti, :].rearrange("(p c) -> p c", p=128), reads=[r_gath], writes=[kb.r])
                    ingest_tile(kb, ti, True, True, False, kcT, vcT, ti == 0)
                kb = kvb[0]
                P.op("pool", lambda e: e.memset(kb[:], 0.0), writes=[kb.r])
                P.dma("pool", kb[0:DS, 256:512], Dr["p_samp"][bb * DS:(bb + 1) * DS, KV0 + 256:KV0 + 512], reads=[Dr["r_psamp"]], writes=[kb.r])
                ingest_tile(kb, 64, False, True, False, kcT, vcT, False)
                for c in range(5):
                    kb = kvb[(c + 1) % 2]
                    if c < 4:
                        P.dma("pool", kb[:, 512:768], Dr["cache_win"][bb, c * 128:(c + 1) * 128, :], writes=[kb.r])
                    else:
                        P.op("pool", lambda e: e.memset(kb[:], 0.0), writes=[kb.r])
                        P.dma("pool", kb[0:DS, 512:768], Dr["p_samp"][bb * DS:(bb + 1) * DS, KV0 + 512:KV0 + 768], reads=[Dr["r_psamp"]], writes=[kb.r])
                    ingest_tile(kb, c, False, False, True, kcT, vcT, False)
                compress_all(kcT, vcT)

            def run_sample(A, bb=bb):
                P.dma("sp", A["Ftab"][:], Dr["Ftab"][:, 16, :], writes=[A["Ftab"].r])
                P.dma("pool", A["cb2"][:], Dr["cbias"][:, 16, :, :], writes=[A["cb2"].r])
                P.dma("pool", A["triS"][:, 0, :], Dr["triS_s"][:, :], writes=[A["triS"].r])
                P.dma("pool", A["triW"][:, 0:5, :], Dr["triW_s"][:, :, :], writes=[A["triW"].r])

                def load_q(qf, gts):
                    P.dma("sp", qf[0:DS, :], Dr["p_samp"][bb * DS:(bb + 1) * DS, QG0:QG0 + 512], reads=[Dr["r_psamp"]], writes=[qf.r])
                    P.dma("sp", gts[0:DS, :], Dr["p_samp"][bb * DS:(bb + 1) * DS, GATE0:GATE0 + 24], reads=[Dr["r_psamp"]], writes=[gts.r])
                P.op("pool", lambda e: e.memset(A["gts"][:], 0.0), writes=[A["gts"].r])
                q_prepare(A, DS, None, load_q, Dr["rope_s"][0:DS, :])
                cfg = {"nbt": 4, "cb_tiles": [(3, 1), (2, 0)], "topk_col": 6,
                       "sel_tiles": [(c, True, None) for c in range(64)] + [(64, False, 0)],
                       "win_tiles": [(c, c) for c in range(5)]}
                nsa_qtile(A, DS, cfg)
                P.dma("sp", Dr["y_a_own"][2048 + bb * DS:2048 + (bb + 1) * DS, :], A["yab"][0:DS, :], reads=[A["yab"].r], writes=[r_ya])
            attention_scope(run_sample)
        Dr["r_ya"] = r_ya
        P.barrier()


def make_gather(nc, P, Dr):
    r_gath = R("gath")
    Dr["r_gath"] = r_gath
    st = ExitStack()
    Dr["gather_stack"] = st
    nsb = OPTS["sb"]
    if nsb == 0 or "C" not in OPTS["phases"]:
        return iter(())
    stage = T(st.enter_context(nc.sbuf_tensor("g_stage", [64, 8192], F32)), "stage")
    pidx = T(st.enter_context(nc.sbuf_tensor("g_pidx", [64, SB], I32)), "pidx")
    pidf = T(st.enter_context(nc.sbuf_tensor("g_pidf", [64, SB], F32)), "pidf")
    idx4f = T(st.enter_context(nc.sbuf_tensor("g_idx4f", [64, SB, 4], F32)), "idx4f")
    idx4 = T(st.enter_context(nc.sbuf_tensor("g_idx4", [64, SB, 4], I32)), "idx4")
    P.op("pool", lambda e: e.memset(pidx[:], 0), writes=[pidx.r])
    for bb in range(nsb):
        P.dma("sp", pidx[:, bb:bb + 1], Dr["pt_col"][bb, :, :], writes=[pidx.r])
    P.op("dve", lambda e: e.tensor_copy(pidf[:], pidx[:]), reads=[pidx.r], writes=[pidf.r])
    for ch in range(4):
        P.op("dve", lambda e: e.tensor_scalar(idx4f[:, :, ch], pidf[:], 4.0, float(ch), ALU.mult, ALU.add), reads=[pidf.r], writes=[idx4f.r])
    P.op("dve", lambda e: e.tensor_copy(idx4[:], idx4f[:]), reads=[idx4f.r], writes=[idx4.r])

    def gen():
        for bb in range(nsb):
            for ci, cache in enumerate((Dr["cache_cmp_pg"], Dr["cache_sel_pg"])):
                for ch in range(4):
                    P._need("pool", P._deps([idx4.r], [stage.r]))
                    ins = nc.gpsimd.indirect_dma_start(out=stage[:, :], out_offset=None, in_=cache[:, :],
                                                       in_offset=bass.IndirectOffsetOnAxis(ap=idx4[:, bb, ch:ch + 1], axis=0),
                                                       bounds_check=2560 * 4 - 1, oob_is_err=False)
                    pool_, idx_ = P.dq["pool"]
                    key = pool_[idx_ % len(pool_)]
                    P.dq["pool"][1] = idx_ + 1
                    if P.cnt[key] > 0:
                        P._need("pool", [(key, P.cnt[key])])
                    P.cnt[key] += 16
                    ins.then_inc(P.sems[key], 16)
                    P._commit((key, P.cnt[key]), [idx4.r], [stage.r])
                    P.n_inst += 1
                    P.dma("pool", Dr["gath"][bb, ci, :, ch * 8192:(ch + 1) * 8192], stage[:, :], reads=[stage.r], writes=[r_gath])
                    yield
    return gen()

NTOK = 2048 + NS
NTL = 17
MG0 = 3096
DN_ALPHA = 2.0 ** 0.25
NVD = 4 * 1024 + 32


def _tile_rows(u):
    return NS if u == NTL - 1 else 128


def phase_D(nc, P, Dr, outs):
    with ExitStack() as st:
        def sb(name, shape, dt=F32):
            return T(st.enter_context(nc.sbuf_tensor("d_" + name, list(shape), dt)), name)

        def ps(name, shape, dt=F32):
            return PT(st.enter_context(nc.psum_tensor("d_" + name, list(shape), dt)), name)

        w_in = Dr["w_in"]
        identb = sb("identb", [128, 128], BF16)
        P.dma("pool", identb[:], Dr["masks"][:, 896:1024], writes=[identb.r])
        identf = sb("identf", [128, 128])
        P.dma("sp", identf[:], Dr["masks"][:, 896:1024], writes=[identf.r])
        vd = sb("vd", [128, NVD])
        P.dma("sp", vd[:], Dr["vecsD"][:, :], writes=[vd.r])
        sel4 = sb("sel4", [128, 4])
        P.dma("sp", sel4[:], Dr["sel4"][:, :], writes=[sel4.r])
        wmg = sb("wmg", [128, 8, 2048], BF16)
        wo = sb("wo", [128, 8, 1024], BF16)
        for kc in range(8):
            P.dma("pool", wmg[:, kc, :], w_in[kc * 128:(kc + 1) * 128, MG0:MG0 + 2048], writes=[wmg.r])
            P.dma("pool", wo[:, kc, :], Dr["w_o"][kc * 128:(kc + 1) * 128, :], writes=[wo.r])
        wpa = sb("wpa", [128, 4, 1024], BF16)
        wpb = sb("wpb", [128, 4, 1024], BF16)
        for kc in range(4):
            P.dma("pool", wpa[:, kc, :], Dr["w_pa"][kc * 128:(kc + 1) * 128, :], writes=[wpa.r])
            P.dma("pool", wpb[:, kc, :], Dr["w_pb"][kc * 128:(kc + 1) * 128, :], writes=[wpb.r])
        rw = sb("rw", [128, 8, 32])
        P.dma("sp", rw[:], Dr["router_w"].rearrange("(k p) e -> p k e", p=128), writes=[rw.r])

        xf = sb("xf", [128, 1024])
        xb = sb("xb", [128, 1024], BF16)
        xT = sb("xT", [128, 8, 128], BF16)
        sg = sb("sg", [128, 2048])
        yr4 = sb("yr4", [128, 4, 512], BF16)
        yr = sb("yr", [128, 512], BF16)
        ya = sb("ya", [128, 512], BF16)
        yT = sb("yT", [128, 8, 128], BF16)
        mm = sb("mm", [128, 1024])
        mb = sb("mb", [128, 1024], BF16)
        mT = sb("mT", [128, 8, 128], BF16)
        hp_ = sb("hpre", [128, 1024])
        hh = sb("hh", [128, 1024])
        hT = sb("hTf", [128, 8, 128])
        s1 = sb("s1", [128, 1])
        s2 = sb("s2", [128, 1])
        lg = sb("lg", [128, 32])
        m8 = sb("m8", [128, 8])
        msk = sb("msk", [128, 32])
        gt = sb("gt", [128, 32])
        ptr = ps("ptr", [128, 8, 128], BF16)
        ptf = [ps("ptf%d" % i, [128, 512]) for i in range(2)]
        pm = [ps("pm%d" % i, [128, 512]) for i in range(4)]
        pmi = [0]

        def nextpm():
            t = pm[pmi[0] % 4]
            pmi[0] += 1
            return t

        r_h = R("h_own")
        r_g = R("gates_own")
        for u in range(NTL):
            n = _tile_rows(u)
            if u < 16:
                P.dma("sp", xf[:], Dr["x_own"][u * 128:(u + 1) * 128, :], writes=[xf.r])
                P.dma("sp", yr4[:], Dr["y_r"][u * 512:(u + 1) * 512, :].rearrange("(k p) c -> p k c", p=128),
                      reads=[Dr["r_yr"]], writes=[yr4.r])
                P.dma("sp", ya[:], Dr["y_a_own"][u * 128:(u + 1) * 128, :], reads=[Dr["r_ya"]], writes=[ya.r])
                P.op("dve", lambda e: e.tensor_scalar(yr[:], yr4[:, 0, :], sel4[:, 0:1], None, ALU.mult), reads=[yr4.r, sel4.r], writes=[yr.r])
                for k in range(1, 4):
                    P.op("dve", lambda e: e.scalar_tensor_tensor(yr[:], yr4[:, k, :], sel4[:, k:k + 1], yr[:], ALU.mult, ALU.add),
                         reads=[yr4.r, sel4.r, yr.r], writes=[yr.r])
            else:
                P.dma("sp", xf[0:n, :], Dr["x_s"][:, :], writes=[xf.r])
                P.dma("sp", yr[0:n, :], Dr["y_r_s"][:, :], reads=[Dr["r_yr"]], writes=[yr.r])
                P.dma("sp", ya[0:n, :], Dr["y_a_own"][2048:2048 + n, :], reads=[Dr["r_ya"]], writes=[ya.r])
            P.op("act", lambda e: e.copy(xb[0:n, :], xf[0:n, :]), reads=[xf.r], writes=[xb.r])
            for kc in range(8):
                P.op("pe", lambda e: e.transpose(ptr[:, kc, 0:n], xb[0:n, kc * 128:(kc + 1) * 128], identb[0:n, 0:n]),
                     reads=[xb.r, identb.r], writes=[ptr.r])
            P.op("act", lambda e: e.copy(xT[:, :, 0:n], ptr[:, :, 0:n]), reads=[ptr.r], writes=[xT.r])
            for nch in range(4):
                p_ = nextpm()
                for kc in range(8):
                    P.op("pe", lambda e: e.matmul(p_[0:n, :], xT[:, kc, 0:n], wmg[:, kc, nch * 512:(nch + 1) * 512],
                                                  start=(kc == 0), stop=(kc == 7)), reads=[xT.r, wmg.r], writes=[p_.r])
                P.op("act", lambda e: e.activation(sg[0:n, nch * 512:(nch + 1) * 512], p_[0:n, :], AF.Sigmoid), reads=[p_.r], writes=[sg.r])
            for kc in range(4):
                P.op("pe", lambda e: e.transpose(ptr[:, kc, 0:n], yr[0:n, kc * 128:(kc + 1) * 128], identb[0:n, 0:n]),
                     reads=[yr.r, identb.r], writes=[ptr.r])
                P.op("pe", lambda e: e.transpose(ptr[:, 4 + kc, 0:n], ya[0:n, kc * 128:(kc + 1) * 128], identb[0:n, 0:n]),
                     reads=[ya.r, identb.r], writes=[ptr.r])
            P.op("act", lambda e: e.copy(yT[:, :, 0:n], ptr[:, :, 0:n]), reads=[ptr.r], writes=[yT.r])
            for nch in range(2):
                pa = nextpm()
                pb = nextpm()
                for kc in range(4):
                    P.op("pe", lambda e: e.matmul(pa[0:n, :], yT[:, kc, 0:n], wpa[:, kc, nch * 512:(nch + 1) * 512],
                                                  start=(kc == 0), stop=(kc == 3)), reads=[yT.r, wpa.r], writes=[pa.r])
                for kc in range(4):
                    P.op("pe", lambda e: e.matmul(pb[0:n, :], yT[:, 4 + kc, 0:n], wpb[:, kc, nch * 512:(nch + 1) * 512],
                                                  start=(kc == 0), stop=(kc == 3)), reads=[yT.r, wpb.r], writes=[pb.r])
                cs = slice(nch * 512, (nch + 1) * 512)
                P.op("dve", lambda e: e.tensor_tensor(mm[0:n, cs], pa[0:n, :], sg[0:n, nch * 512:(nch + 1) * 512], ALU.mult),
                     reads=[pa.r, sg.r], writes=[mm.r])
                P.op("dve", lambda e: e.tensor_tensor(hp_[0:n, cs], pb[0:n, :], sg[0:n, 1024 + nch * 512:1024 + (nch + 1) * 512], ALU.mult),
                     reads=[pb.r, sg.r], writes=[hp_.r])
                P.op("dve", lambda e: e.tensor_tensor(mb[0:n, cs], mm[0:n, cs], hp_[0:n, cs], ALU.add),
                     reads=[mm.r, hp_.r], writes=[mb.r])
            for kc in range(8):
                P.op("pe", lambda e: e.transpose(ptr[:, kc, 0:n], mb[0:n, kc * 128:(kc + 1) * 128], identb[0:n, 0:n]),
                     reads=[mb.r, identb.r], writes=[ptr.r])
            P.op("act", lambda e: e.copy(mT[:, :, 0:n], ptr[:, :, 0:n]), reads=[ptr.r], writes=[mT.r])
            for nch in range(2):
                p_ = nextpm()
                for kc in range(8):
                    P.op("pe", lambda e: e.matmul(p_[0:n, :], mT[:, kc, 0:n], wo[:, kc, nch * 512:(nch + 1) * 512],
                                                  start=(kc == 0), stop=(kc == 7)), reads=[mT.r, wo.r], writes=[p_.r])
                cs = slice(nch * 512, (nch + 1) * 512)
                P.op("dve", lambda e: e.scalar_tensor_tensor(hp_[0:n, cs], xf[0:n, cs], DN_ALPHA, p_[0:n, :], ALU.mult, ALU.add),
                     reads=[xf.r, p_.r], writes=[hp_.r])
            layer_norm(P, hp_, hh, mm, s1, s2, n, vd[0:n, 0:1024], vd[0:n, 1024:2048], vd.r)
            P.dma("pool", Dr["h_own"][u * 128:u * 128 + n, :], hh[0:n, :], reads=[hh.r], writes=[r_h])
            for kc in range(8):
                pt_ = ptf[kc // 4]
                P.op("pe", lambda e: e.transpose(pt_[:, (kc % 4) * 128:(kc % 4) * 128 + n], hh[0:n, kc * 128:(kc + 1) * 128], identf[0:n, 0:n]),
                     reads=[hh.r, identf.r], writes=[pt_.r])
            P.op("act", lambda e: e.copy(hT[:, 0:4, 0:n], ptf[0][:].rearrange("p (k t) -> p k t", k=4)[:, :, 0:n]), reads=[ptf[0].r], writes=[hT.r])
            P.op("dve", lambda e: e.tensor_copy(hT[:, 4:8, 0:n], ptf[1][:].rearrange("p (k t) -> p k t", k=4)[:, :, 0:n]), reads=[ptf[1].r], writes=[hT.r])
            p_ = nextpm()
            for kc in range(8):
                P.op("pe", lambda e: e.matmul(p_[0:n, 0:32], hT[:, kc, 0:n], rw[:, kc, :], start=(kc == 0), stop=(kc == 7)),
                     reads=[hT.r, rw.r], writes=[p_.r])
            P.op("dve", lambda e: e.tensor_tensor(lg[0:n, :], p_[0:n, 0:32], vd[0:n, 4096:4128], ALU.add), reads=[p_.r, vd.r], writes=[lg.r])
            P.op("dve", lambda e: e.max(out=m8[0:n, :], in_=lg[0:n, :]), reads=[lg.r], writes=[m8.r])
            P.op("dve", lambda e: e.tensor_scalar(msk[0:n, :], lg[0:n, :], m8[0:n, 3:4], None, ALU.is_ge), reads=[lg.r, m8.r], writes=[msk.r])
            P.op("dve", lambda e: e.tensor_scalar(s1[0:n, :], m8[0:n, 0:1], -1.0, None, ALU.mult), reads=[m8.r], writes=[s1.r])
            P.op("act", lambda e: e.activation(gt[0:n, :], lg[0:n, :], AF.Exp, bias=s1[0:n, 0:1]), reads=[lg.r, s1.r], writes=[gt.r])
            P.op("dve", lambda e: e.tensor_tensor(gt[0:n, :], gt[0:n, :], msk[0:n, :], ALU.mult), reads=[gt.r, msk.r], writes=[gt.r])
            P.op("dve", lambda e: e.tensor_reduce(s2[0:n, :], gt[0:n, :], AX.X, ALU.add), reads=[gt.r], writes=[s2.r])
            P.op("dve", lambda e: e.reciprocal(s2[0:n, :], s2[0:n, :]), reads=[s2.r], writes=[s2.r])
            P.op("dve", lambda e: e.tensor_scalar(gt[0:n, :], gt[0:n, :], s2[0:n, 0:1], None, ALU.mult), reads=[gt.r, s2.r], writes=[gt.r])
            P.dma("pool", Dr["gates_own"][u * 128:u * 128 + n, :], gt[0:n, :], reads=[gt.r], writes=[r_g])
        Dr["r_h"] = r_h
        Dr["r_g"] = r_g
        P.barrier()


def layer_norm(P, src, dst, tmp, s1, s2, n, g_ap, b_ap, vr):
    P.op("dve", lambda e: e.tensor_reduce(s1[0:n, :], src[0:n, :], AX.X, ALU.add), reads=[src.r], writes=[s1.r])
    P.op("dve", lambda e: e.tensor_scalar(s1[0:n, :], s1[0:n, :], -1.0 / 1024, None, ALU.mult), reads=[s1.r], writes=[s1.r])
    P.op("dve", lambda e: e.tensor_scalar(dst[0:n, :], src[0:n, :], s1[0:n, 0:1], None, ALU.add), reads=[src.r, s1.r], writes=[dst.r])
    P.op("dve", lambda e: e.tensor_tensor(tmp[0:n, :], dst[0:n, :], dst[0:n, :], ALU.mult), reads=[dst.r], writes=[tmp.r])
    P.op("dve", lambda e: e.tensor_reduce(s2[0:n, :], tmp[0:n, :], AX.X, ALU.add), reads=[tmp.r], writes=[s2.r])
    P.op("dve", lambda e: e.tensor_scalar(s2[0:n, :], s2[0:n, :], 1.0 / 1024, 1e-5, ALU.mult, ALU.add), reads=[s2.r], writes=[s2.r])
    P.op("act", lambda e: e.activation(s2[0:n, :], s2[0:n, :], AF.Sqrt), reads=[s2.r], writes=[s2.r])
    P.op("dve", lambda e: e.reciprocal(s2[0:n, :], s2[0:n, :]), reads=[s2.r], writes=[s2.r])
    P.op("dve", lambda e: e.tensor_scalar(dst[0:n, :], dst[0:n, :], s2[0:n, 0:1], None, ALU.mult), reads=[dst.r, s2.r], writes=[dst.r])
    P.op("dve", lambda e: e.tensor_tensor(dst[0:n, :], dst[0:n, :], g_ap, ALU.mult), reads=[dst.r, vr], writes=[dst.r])
    P.op("dve", lambda e: e.tensor_tensor(dst[0:n, :], dst[0:n, :], b_ap, ALU.add), reads=[dst.r, vr], writes=[dst.r])


def phase_E(nc, P, Dr, outs):
    with ExitStack() as st:
        def sb(name, shape, dt=F32):
            return T(st.enter_context(nc.sbuf_tensor("e_" + name, list(shape), dt)), name)

        def ps(name, shape, dt=F32):
            return PT(st.enter_context(nc.psum_tensor("e_" + name, list(shape), dt)), name)

        identb = sb("identb", [128, 128], BF16)
        P.dma("pool", identb[:], Dr["masks"][:, 896:1024], writes=[identb.r])
        identf = sb("identf", [128, 128])
        P.dma("sp", identf[:], Dr["masks"][:, 896:1024], writes=[identf.r])
        vd = sb("vd", [128, 2048])
        P.dma("sp", vd[:], Dr["vecsD"][:, 2048:4096], writes=[vd.r])
        b1a = sb("b1a", [128, 32, 16])
        P.dma("sp", b1a[:], Dr["mlp1_bT"].rearrange("e p c -> p e c"), writes=[b1a.r])
        b2 = sb("b2", [32, 1024])
        P.dma("sp", b2[:], Dr["mlp2_b"][:, :], writes=[b2.r])

        HT = 9
        hT = sb("hT", [128, 8, 1024 + NS], BF16)
        yacc = sb("yacc", [128, HT, 1024])
        gates = sb("gates", [128, HT, 32])
        gT = sb("gT", [32, HT, 128])
        s1 = sb("s1", [128, 1])
        s2 = sb("s2", [128, 1])
        ptr = ps("ptr", [128, 8, 128], BF16)
        pg_ = [ps("pgl%d" % i, [128, 512]) for i in range(4)]
        po = [ps("po%d" % i, [128, 512]) for i in range(3)]
        cnt = {"pg": 0, "po": 0, "w": 0, "a": 0}
        r_out = R("outE")
        outs.append(r_out)

        def scoped(names):
            stx = ExitStack()
            d = {}
            for nm_, shp, dt_ in names:
                d[nm_] = T(stx.enter_context(nc.sbuf_tensor("e_%s_%d" % (nm_, P.n_inst), list(shp), dt_)), nm_)
            return stx, d

        for half in range(2):
            tiles = list(range(half * 8, half * 8 + 8)) + ([16] if half == 1 else [])
            ntok = sum(_tile_rows(u) for u in tiles)
            groups = [(0, 512), (512, 512)] + ([(1024, NS)] if half == 1 else [])
            stx, dd = scoped([("hf", [128, 1024], F32), ("hb", [128, 1024], BF16)])
            hf, hb = dd["hf"], dd["hb"]
            for li, u in enumerate(tiles):
                n = _tile_rows(u)
                P.dma("sp", hf[0:n, :], Dr["h_own"][u * 128:u * 128 + n, :], reads=[Dr["r_h"]], writes=[hf.r])
                P.dma("sp", gates[0:n, li, :], Dr["gates_own"][u * 128:u * 128 + n, :], reads=[Dr["r_g"]], writes=[gates.r])
                P.op("act", lambda e: e.copy(hb[0:n, :], hf[0:n, :]), reads=[hf.r], writes=[hb.r])
                for kc in range(8):
                    P.op("pe", lambda e: e.transpose(ptr[:, kc, 0:n], hb[0:n, kc * 128:(kc + 1) * 128], identb[0:n, 0:n]),
                         reads=[hb.r, identb.r], writes=[ptr.r])
                P.op("dve", lambda e: e.tensor_copy(hT[:, :, li * 128:li * 128 + n], ptr[:, :, 0:n]), reads=[ptr.r], writes=[hT.r])
                pq = po[cnt["po"] % 3]
                cnt["po"] += 1
                P.op("pe", lambda e: e.transpose(pq[0:32, 0:n], gates[0:n, li, :], identf[0:n, 0:n]), reads=[gates.r, identf.r], writes=[pq.r])
                P.op("act", lambda e: e.copy(gT[:, li, 0:n], pq[0:32, 0:n]), reads=[pq.r], writes=[gT.r])
                for nch in range(2):
                    pq = po[cnt["po"] % 3]
                    cnt["po"] += 1
                    P.op("pe", lambda e: e.matmul(pq[0:n, :], gT[:, li, 0:n], b2[:, nch * 512:(nch + 1) * 512], start=True, stop=True),
                         reads=[gT.r, b2.r], writes=[pq.r])
                    P.op("act", lambda e: e.copy(yacc[0:n, li, nch * 512:(nch + 1) * 512], pq[0:n, :]), reads=[pq.r], writes=[yacc.r])
            P.barrier()
            stx.close()
            stx, dd = scoped([("w1_0", [128, 8, 2048], BF16), ("w1_1", [128, 8, 2048], BF16), ("w2_0", [128, 8, 1024], BF16),
                              ("w2_1", [128, 8, 1024], BF16), ("actT0", [128, 8, 512], BF16), ("actT1", [128, 8, 512], BF16),
                              ("tg0", [128, 512], F32), ("tg1", [128, 512], F32), ("tsg0", [128, 512], F32), ("tsg1", [128, 512], F32),
                              ("tl0", [128, 512], F32), ("tl1", [128, 512], F32)])
            w1 = [dd["w1_0"], dd["w1_1"]]
            w2 = [dd["w2_0"], dd["w2_1"]]
            actT = [dd["actT0"], dd["actT1"]]
            tg = [dd["tg0"], dd["tg1"]]
            tsg = [dd["tsg0"], dd["tsg1"]]
            tl = [dd["tl0"], dd["tl1"]]
            for ex in range(32):
                wb1 = w1[cnt["w"] % 2]
                wb2 = w2[cnt["w"] % 2]
                cnt["w"] += 1
                for kc in range(8):
                    P.dma("pool", wb1[:, kc, :], Dr["mlp1_w"][ex, kc * 128:(kc + 1) * 128, :], writes=[wb1.r])
                for kc in range(8):
                    P.dma("pool", wb2[:, kc, :], Dr["mlp2_w"][ex, kc * 128:(kc + 1) * 128, :], writes=[wb2.r])
                for (g0, gn) in groups:
                    aT = actT[cnt["a"] % 2]
                    cnt["a"] += 1
                    for fc in range(8):
                        pgl = pg_[cnt["pg"] % 4]
                        pll = pg_[(cnt["pg"] + 1) % 4]
                        cnt["pg"] += 2
                        for kc in range(8):
                            P.op("pe", lambda e: e.matmul(pgl[:, 0:gn], wb1[:, kc, fc * 128:(fc + 1) * 128], hT[:, kc, g0:g0 + gn],
                                                          start=(kc == 0), stop=(kc == 7)), reads=[wb1.r, hT.r], writes=[pgl.r])
                        for kc in range(8):
                            P.op("pe", lambda e: e.matmul(pll[:, 0:gn], wb1[:, kc, 1024 + fc * 128:1024 + (fc + 1) * 128], hT[:, kc, g0:g0 + gn],
                                                          start=(kc == 0), stop=(kc == 7)), reads=[wb1.r, hT.r], writes=[pll.r])
                        k2 = fc % 2
                        a_, s_, l_ = tg[k2], tsg[k2], tl[k2]
                        P.op("dve", lambda e: e.tensor_scalar(a_[:, 0:gn], pgl[:, 0:gn], b1a[:, ex, fc:fc + 1], 7.0, ALU.add, ALU.min),
                             reads=[pgl.r, b1a.r], writes=[a_.r])
                        P.op("act", lambda e: e.activation(s_[:, 0:gn], a_[:, 0:gn], AF.Sigmoid, scale=1.702), reads=[a_.r], writes=[s_.r])
                        P.op("dve", lambda e: e.tensor_scalar(l_[:, 0:gn], pll[:, 0:gn], b1a[:, ex, 8 + fc:9 + fc], 7.0, ALU.add, ALU.min),
                             reads=[pll.r, b1a.r], writes=[l_.r])
                        P.op("dve", lambda e: e.tensor_scalar(l_[:, 0:gn], l_[:, 0:gn], -7.0, 1.0, ALU.max, ALU.add), reads=[l_.r], writes=[l_.r])
                        P.op("dve", lambda e: e.tensor_tensor(a_[:, 0:gn], a_[:, 0:gn], s_[:, 0:gn], ALU.mult), reads=[a_.r, s_.r], writes=[a_.r])
                        P.op("dve", lambda e: e.tensor_tensor(aT[:, fc, 0:gn], a_[:, 0:gn], l_[:, 0:gn], ALU.mult), reads=[a_.r, l_.r], writes=[aT.r])
                    nt_in_g = (gn + 127) // 128
                    for tt in range(nt_in_g):
                        li = g0 // 128 + tt
                        n = min(128, gn - tt * 128)
                        for nch in range(2):
                            pq = po[cnt["po"] % 3]
                            cnt["po"] += 1
                            for fc in range(8):
                                P.op("pe", lambda e: e.matmul(pq[0:n, :], aT[:, fc, tt * 128:tt * 128 + n], wb2[:, fc, nch * 512:(nch + 1) * 512],
                                                              start=(fc == 0), stop=(fc == 7)), reads=[aT.r, wb2.r], writes=[pq.r])
                            ya = yacc[0:n, li, nch * 512:(nch + 1) * 512]
                            P.op("dve", lambda e: e.scalar_tensor_tensor(ya, pq[0:n, :], gates[0:n, li, ex:ex + 1], ya, ALU.mult, ALU.add),
                                 reads=[pq.r, gates.r, yacc.r], writes=[yacc.r])
            P.barrier()
            stx.close()
            stx, dd = scoped([("hf", [128, 1024], F32), ("t1", [128, 1024], F32), ("t2", [128, 1024], F32)])
            hf, t1, t2 = dd["hf"], dd["t1"], dd["t2"]
            for li, u in enumerate(tiles):
                n = _tile_rows(u)
                P.dma("sp", hf[0:n, :], Dr["h_own"][u * 128:u * 128 + n, :], reads=[Dr["r_h"]], writes=[hf.r])
                P.op("dve", lambda e: e.scalar_tensor_tensor(t1[0:n, :], hf[0:n, :], DN_ALPHA, yacc[0:n, li, :], ALU.mult, ALU.add),
                     reads=[hf.r, yacc.r], writes=[t1.r])
                layer_norm(P, t1, t2, hf, s1, s2, n, vd[0:n, 0:1024], vd[0:n, 1024:2048], vd.r)
                P.dma("sp", Dr["o_y"][u * 128:u * 128 + n, :], t2[0:n, :], reads=[t2.r], writes=[r_out])
            P.barrier()
            stx.close()
        P.barrier()


def build_program():
    nc = bass.Bass("TRN2", target_bir_lowering=False)
    Dr = {}

    def din(name, shape, dt=F32):
        Dr[name] = nc.dram_tensor(name, list(shape), dt, kind="ExternalInput").ap()
        _INPUT_NAMES.append(name)
    ph = OPTS["phases"]

    def dout(name, shape, dt=F32):
        Dr[name] = nc.dram_tensor(name, list(shape), dt, kind="ExternalOutput").ap()

    def dtmp(name, shape, dt=F32):
        Dr[name] = nc.dram_tensor(name, list(shape), dt).ap()

    din("x_full", [SEQ, D])
    din("x_own", [2048, D])
    din("x_s", [NS, D])
    din("w_in", [D, IN_COLS])
    din("rope_p", [SEQ, 16])
    din("rope_own", [2048, 16])
    din("rope_s", [NS, 16])
    din("cache_win", [SB, 512, 256])
    din("vecs", [128, NVEC])
    din("w_w2", [64, 512])
    din("w_a2", [64, 512])
    din("g_w2", [128, 512])
    din("masks", [128, NMASK])
    din("tmask", [128, 1])
    din("state_shift", [SB, R_COLS])
    din("state_wkv", [SB, 8, 64, 64])
    din("vecsD", [128, NVD])
    din("sel4", [128, 4])
    din("w_o", [D, D])
    din("w_pa", [512, D])
    din("w_pb", [512, D])
    din("router_w", [D, 32])
    if "E" in ph:
        din("mlp1_w", [32, D, 2048])
        din("mlp2_w", [32, D, D])
    din("mlp1_bT", [32, 128, 16])
    din("mlp2_b", [32, D])
    din("cmp_w1", [2, 2048, 256])
    din("cmp_w2", [2, 256, 64])
    din("cmp_pe", [2, 32, 64])
    din("cmp_b1T", [128, 2, 2])
    din("cmp_b2T", [64, 1])
    din("cmp_b2v", [128, 64])
    din("Gtab", [128, 8192])
    din("cover", [128, 4, 128])
    din("Ftab", [128, 17, 128])
    din("cbias", [128, 17, 2, 128])
    din("triS", [128, 4, 512])
    din("triW", [128, 8, 512])
    din("triS_s", [128, 512])
    din("triW_s", [128, 5, 512])
    din("pt_col", [SB, 64, 1], I32)
    if "C" in ph and OPTS["sb"] > 0:
        din("cache_cmp_pg", [2560 * 4, 8192])
        din("cache_sel_pg", [2560 * 4, 8192])

    dout("o_cmp_p", [SEQ, 256])
    dout("o_sel_p", [SEQ, 256])
    dout("o_win_p", [512, 256])
    dout("o_shift_p", [1, R_COLS])
    dout("o_cmp_s", [NS, 256])
    dout("o_sel_s", [NS, 256])
    dout("o_win_s", [SB, 512, 256])
    dout("o_shift_s", [SB, R_COLS])
    dout("o_wkv_p", [8, 64, 64])
    dout("o_wkv_s", [SB, 8, 64, 64])
    dout("o_y", [NTOK, D])

    dtmp("p_full", [SEQ, NA])
    dtmp("p_samp", [NS, IN_COLS])
    dtmp("y_r", [SEQ, 512], BF16)
    dtmp("y_r_s", [NS, 512], BF16)
    if DEBUG:
        dout("gath", [SB, 2, 64, 128 * 256])
    else:
        dtmp("gath", [SB, 2, 64, 128 * 256])
    if DEBUG:
        dout("y_a_own", [NTOK, 512], BF16)
        dout("h_own", [NTOK, D])
        dout("gates_own", [NTOK, 32])
    else:
        dtmp("y_a_own", [NTOK, 512], BF16)
        dtmp("h_own", [NTOK, D])
        dtmp("gates_own", [NTOK, 32])
    outs = []
    with ExitStack() as st:
        P = Prog(nc, st)
        Dr["gather_gen"] = make_gather(nc, P, Dr)
        phase_A(nc, P, Dr, outs)
        if "B" in ph:
            phase_B(nc, P, Dr, outs)
        if "C" in ph:
            phase_C(nc, P, Dr, outs)
        if "D" in ph:
            phase_D(nc, P, Dr, outs)
        if "E" in ph:
            phase_E(nc, P, Dr, outs)
        P.finish(outs + [Dr[k] for k in ("r_yr", "r_ya", "r_h", "r_g") if k in Dr])
        P._need("sp", [(k, v) for k, v in P.cnt.items() if v > 0])
        print("epochs", P.epoch, {k: v for k, v in P.cnt.items() if not k.startswith("d_")})
        print("program: n_inst=%d n_wait=%d" % (P.n_inst, P.n_wait))
    return nc


DEBUG = False
OPTS = {"phases": "ABCDE", "cq": 16, "sb": SB, "cores": 8, "cstop": 9}
_NC = None
_INPUT_NAMES = []


def _rope_table(pos):
    half = 8
    inv = (np.float32(500000.0) ** (-np.arange(half, dtype=np.float32) * np.float32(2.0) / np.float32(16))).astype(np.float32)
    ang = pos.astype(np.float32)[:, None] * inv[None, :]
    return np.concatenate([np.cos(ang), np.sin(ang)], axis=1).astype(np.float32)


def _const_masks():
    s = np.arange(128)[:, None]
    t = np.arange(128)[None, :]
    same = (s // 64) == (t // 64)
    Lblk = (same & (s <= t)).astype(np.float32)
    Oblk = same.astype(np.float32)
    strictU = (same & (s < t)).astype(np.float32)
    inclU = Lblk
    maskMA2 = np.concatenate([strictU, inclU, strictU, inclU], axis=1)
    maskNT = (same & (s > t)).astype(np.float32)
    ident = np.eye(128, dtype=np.float32)
    return np.ascontiguousarray(np.concatenate([Lblk, Oblk, maskMA2, maskNT, ident], axis=1))


def _nsa_tables(qq):
    f32 = np.float32
    NB = np.float32(-30000.0)
    kl = np.arange(128)[:, None]
    ql = np.arange(128)[None, :]
    Ftab = np.zeros((128, 17, 128), f32)
    cbias = np.zeros((128, 17, 2, 128), f32)
    sidx = np.arange(128)[None, :]
    for j in range(16):
        i = 4 * j + qq
        qpos = (128 * i + np.arange(128))[:, None]
        causal = (64 * sidx) <= qpos
        cur = qpos // 64
        forced = (sidx == 0) | (sidx == cur) | (sidx == cur - 1)
        F = np.where(forced, 1e6 + 16.0 * sidx, 0.0)
        F = np.where(causal, F, -1e30)
        Ftab[:, j, :] = F
        nbt = (32 * j + 32 + 127) // 128
        for slot, bt in ((1, nbt - 1), (0, nbt - 2)):
            if bt < 0:
                continue
            blk = 128 * bt + kl
            valid = (blk <= 510) & (16 * blk + 31 <= 128 * i + ql)
            cbias[:, j, slot, :] = np.where(valid, 0.0, NB)
    F = np.where((sidx == 0) | (sidx == 127), 1e6 + 16.0 * sidx, 0.0) * np.ones((128, 1))
    Ftab[:, 16, :] = F
    blk = 128 * 3 + kl
    cbias[:, 16, 1, :] = np.where(blk <= 510, 0.0, NB) * np.ones((1, 128))
    cbias[:, 16, 0, :] = 0.0
    rep4 = lambda m: np.tile(m, (1, 4))
    triS = np.zeros((128, 4, 512), f32)
    for rel in range(4):
        if rel < qq:
            m = np.zeros((128, 128), f32)
        elif rel == qq:
            m = np.where(kl <= ql, 0.0, NB)
        else:
            m = np.full((128, 128), NB)
        triS[:, rel, :] = rep4(m)
    triW = np.zeros((128, 8, 512), f32)
    for rel in range(8):
        dlt = qq + 4 - rel
        if dlt < 0 or dlt > 4:
            m = np.full((128, 128), NB)
        elif dlt == 0:
            m = np.where(kl <= ql, 0.0, NB)
        elif dlt == 4:
            m = np.where(kl >= ql, 0.0, NB)
        else:
            m = np.zeros((128, 128), f32)
        triW[:, rel, :] = rep4(m)
    qv = ql < DS
    triS_s = rep4(np.where((kl < DS) & (kl <= ql), 0.0, NB))
    triW_s = np.zeros((128, 5, 512), f32)
    for c in range(5):
        kidx = 128 * c + kl
        ok = (kidx < 512 + DS) & (kidx <= 512 + ql) & (kidx >= ql)
        triW_s[:, c, :] = rep4(np.where(ok, 0.0, NB))
    return (Ftab.astype(f32), cbias.astype(f32), triS.astype(f32), triW.astype(f32), triS_s.astype(f32), triW_s.astype(f32))


def _shared_tables():
    f32 = np.float32
    s = np.arange(128)[:, None]
    x = np.arange(8192)[None, :]
    G = ((x // 64) == s).astype(f32)
    cover = np.zeros((128, 4, 128), f32)
    for bt in range(4):
        blk = 128 * bt + np.arange(128)[:, None]
        ss = np.arange(128)[None, :]
        cover[:, bt, :] = ((blk >= 4 * ss - 1) & (blk <= 4 * ss + 3) & (blk <= 510)).astype(f32)
    return G, cover


def kernel(**inputs):
    global _NC
    if _NC is None:
        _NC = build_program()
    nc = _NC
    g = lambda k: np.asarray(inputs[k])
    f32 = np.float32
    C = np.ascontiguousarray
    x_prompt = g("x_prompt")
    x_sample = g("x_sample")
    w_in = C(g("w_in")[0])
    cache_win = g("cache_win_kv")[0].reshape(32, 512, 256)
    rope_p = _rope_table(np.arange(SEQ))
    rope_s = np.tile(_rope_table(PAST + np.arange(DS)), (SB, 1))
    vec = np.concatenate([g("mu_shift")[0], g("w0")[0], g("a0")[0], g("k_k")[0], g("k_a")[0], g("gn_g")[0],
                          g("gn_b")[0], g("r_k")[0].reshape(-1)]).astype(f32)
    vecs = C(np.broadcast_to(vec[None, :], (128, NVEC)))
    vecD = np.concatenate([g("ln1_g")[0], g("ln1_b")[0], g("ln2_g")[0], g("ln2_b")[0], g("router_b")[0]]).astype(f32)
    vecsD = C(np.broadcast_to(vecD[None, :], (128, NVD)))
    masks = _const_masks()
    tmask = np.zeros((128, 1), f32)
    tmask[0:DS] = 1.0
    tmask[64:64 + DS] = 1.0
    state_shift = g("state_shift")[0]
    state_wkv = g("state_wkv")[0]
    page_table = g("page_table").astype(np.int32)
    Gtab, cover = _shared_tables()
    shared = {
        "w_in": w_in, "rope_p": rope_p, "rope_s": rope_s, "vecs": vecs, "masks": masks, "tmask": tmask,
        "w_w2": C(g("w_w2")[0]), "w_a2": C(g("w_a2")[0]), "g_w2": C(g("g_w2")[0]),
        "vecsD": vecsD, "w_o": C(g("w_o")[0]), "w_pa": C(g("w_pa")[0]), "w_pb": C(g("w_pb")[0]),
        "router_w": C(g("router_w")[0]), "mlp1_w": C(g("mlp1_w")[0]), "mlp2_w": C(g("mlp2_w")[0]),
        "mlp1_bT": C(g("mlp1_b")[0].reshape(32, 16, 128).transpose(0, 2, 1)), "mlp2_b": C(g("mlp2_b")[0]),
        "cmp_w1": C(g("cmp_w1")[0]), "cmp_w2": C(g("cmp_w2")[0]), "cmp_pe": C(g("cmp_pe")[0]),
        "cmp_b1T": C(g("cmp_b1")[0].reshape(2, 2, 128).transpose(2, 0, 1)),
        "cmp_b2T": C(g("cmp_b2")[0][0].reshape(64, 1)),
        "cmp_b2v": C(np.broadcast_to(g("cmp_b2")[0][1][None, :], (128, 64))),
        "Gtab": Gtab, "cover": cover,
        "cache_cmp_pg": g("cache_cmp_kv")[0].reshape(2560 * 4, 8192),
        "cache_sel_pg": g("cache_sel_kv")[0].reshape(2560 * 4, 8192),
    }
    tabs = [_nsa_tables(qq) for qq in range(4)]
    in_maps = []
    for c in range(8):
        b, qq = c // 4, c % 4
        own = np.concatenate([np.arange(128 * (4 * j + qq), 128 * (4 * j + qq) + 128) for j in range(16)])
        Ftab, cbias, triS, triW, triS_s, triW_s = tabs[qq]
        sel4 = np.zeros((128, 4), f32)
        sel4[:, qq] = 1.0
        m = dict(shared)
        m.update({
            "x_full": C(x_prompt[b]),
            "x_own": C(x_prompt[b][own]),
            "rope_own": C(rope_p[own]),
            "x_s": C(x_sample[SB * c:SB * c + SB].reshape(NS, D)),
            "cache_win": C(cache_win[SB * c:SB * c + SB]),
            "state_shift": C(state_shift[SB * c:SB * c + SB]),
            "state_wkv": C(state_wkv[SB * c:SB * c + SB]),
            "sel4": sel4, "Ftab": Ftab, "cbias": cbias, "triS": triS, "triW": triW, "triS_s": triS_s, "triW_s": triW_s,
            "pt_col": C(page_table[SB * c:SB * c + SB].reshape(SB, 64, 1)),
        })
        in_maps.append(m)
    ncores = OPTS["cores"]
    in_maps = [{k: v for k, v in mp.items() if k in _INPUT_NAMES} for mp in in_maps[:ncores]]
    res = run_bass_kernel_spmd(nc, in_maps, core_ids=list(range(ncores)))
    rs = list(res.results)
    global _LAST
    _LAST = rs
    if ncores < 8:
        return None
    y_prompt = np.zeros((2, SEQ, D), f32)
    y_sample = np.zeros((32, DS, D), f32)
    for c in range(8):
        b, qq = c // 4, c % 4
        oy = rs[c]["o_y"]
        for j in range(16):
            i = 4 * j + qq
            y_prompt[b, 128 * i:128 * i + 128] = oy[j * 128:(j + 1) * 128]
        y_sample[SB * c:SB * c + SB] = oy[2048:2048 + NS].reshape(SB, DS, D)
    cmp_p = np.stack([rs[0]["o_cmp_p"], rs[4]["o_cmp_p"]]).reshape(1, 2, SEQ, 2, 2, 64)
    sel_p = np.stack([rs[0]["o_sel_p"], rs[4]["o_sel_p"]]).reshape(1, 2, SEQ, 2, 2, 64)
    win_p = np.stack([rs[0]["o_win_p"], rs[4]["o_win_p"]]).reshape(1, 2, 512, 2, 2, 64)
    wkv_p = np.stack([rs[0]["o_wkv_p"], rs[4]["o_wkv_p"]]).reshape(1, 2, 8, 64, 64)
    shift_p = np.stack([rs[0]["o_shift_p"], rs[4]["o_shift_p"]]).reshape(1, 2, R_COLS)
    cmp_s = np.concatenate([rs[c]["o_cmp_s"] for c in range(8)]).reshape(1, 32, DS, 2, 2, 64)
    sel_s = np.concatenate([rs[c]["o_sel_s"] for c in range(8)]).reshape(1, 32, DS, 2, 2, 64)
    win_s = np.concatenate([rs[c]["o_win_s"] for c in range(8)]).reshape(1, 32, 512, 2, 2, 64)
    wkv_s = np.concatenate([rs[c]["o_wkv_s"] for c in range(8)]).reshape(1, 32, 8, 64, 64)
    shift_s = np.concatenate([rs[c]["o_shift_s"] for c in range(8)]).reshape(1, 32, R_COLS)
    return (y_prompt, y_sample, cmp_p.astype(f32), sel_p.astype(f32), win_p.astype(f32), wkv_p.astype(f32),
            shift_p.astype(f32), cmp_s.astype(f32), sel_s.astype(f32), win_s.astype(f32), wkv_s.astype(f32),
            shift_s.astype(f32))


_LAST = None
```

```python
import numpy as np
from contextlib import ExitStack
import concourse.bass as bass
import concourse.mybir as mybir
from concourse.bass_utils import run_bass_kernel_spmd

F32 = mybir.dt.float32
BF16 = mybir.dt.bfloat16
I32 = mybir.dt.int32
ALU = mybir.AluOpType
AF = mybir.ActivationFunctionType
AX = mybir.AxisListType

D = 1024
SEQ = 8192
NT = SEQ // 128
R_COLS = 1792
KV0 = 1792 + 512
NKV = 768
NA = R_COLS + NKV
IN_COLS = 5144
DS = 4
SB = 4
NS = SB * DS
PAST = 8192


class R:
    __slots__ = ("name", "w", "rs", "excl")

    def __init__(self, name=""):
        self.name = name
        self.w = None
        self.rs = []
        self.excl = False


class Prog:
    NDMA = 24

    def __init__(self, nc, stack):
        self.nc = nc
        self.stack = stack
        self.eng = {"pe": nc.tensor, "dve": nc.vector, "act": nc.scalar,
                    "pool": nc.gpsimd, "sp": nc.sync}
        self.sems = {}
        self.cnt = {}
        self.cur = {}
        self.dead = set()
        self.epoch = 0
        for k in self.eng:
            key = k + "#0"
            self.sems[key] = stack.enter_context(nc.semaphore("prog_" + k + "_0"))
            self.cnt[key] = 0
            self.cur[k] = key
        self.dq = {}
        for q in ("sp", "pool", "act"):
            pool = []
            for i in range(self.NDMA):
                key = "d_%s_%d" % (q, i)
                self.sems[key] = stack.enter_context(nc.semaphore(key))
                self.cnt[key] = 0
                pool.append(key)
            self.dq[q] = [pool, 0]
        self.waited = {k: {} for k in self.eng}
        self.n_inst = 0
        self.n_wait = 0
        self.pe_selfsync = False

    def _need(self, e, deps):
        best = {}
        for d in deps:
            if d is None:
                continue
            k, v = d
            if k in self.dead:
                continue
            if e == "pe" and k.startswith("pe#") and not self.pe_selfsync:
                continue
            if v > best.get(k, 0):
                best[k] = v
        for k, v in best.items():
            if self.waited[e].get(k, 0) >= v:
                continue
            self.eng[e].wait_ge(self.sems[k], v)
            self.waited[e][k] = v
            self.n_wait += 1

    def _deps(self, reads, writes):
        deps = []
        for r in reads:
            deps.append(r.w)
            if r.excl:
                deps.extend(r.rs)
        for w in writes:
            deps.append(w.w)
            deps.extend(w.rs)
        return deps

    def _commit(self, tok, reads, writes):
        for r in reads:
            if r.excl:
                r.w = tok
                r.rs = []
                continue
            r.rs.append(tok)
            if len(r.rs) > 48:
                best = {}
                for k, v in r.rs:
                    if v > best.get(k, 0):
                        best[k] = v
                r.rs = list(best.items())
        for w in writes:
            w.w = tok
            w.rs = []

    def op(self, e, fn, reads=(), writes=()):
        self._need(e, self._deps(reads, writes))
        ins = fn(self.eng[e])
        key = self.cur[e]
        self.cnt[key] += 1
        ins.then_inc(self.sems[key], 1)
        tok = (key, self.cnt[key])
        self._commit(tok, reads, writes)
        self.n_inst += 1
        return tok

    def dma(self, q, out, in_, reads=(), writes=(), **kw):
        pool, idx = self.dq[q]
        key = pool[idx % len(pool)]
        self.dq[q][1] = idx + 1
        deps = self._deps(reads, writes)
        if self.cnt[key] > 0:
            deps.append((key, self.cnt[key]))
        self._need(q, deps)
        ins = self.eng[q].dma_start(out=out, in_=in_, **kw)
        self.cnt[key] += 16
        ins.then_inc(self.sems[key], 16)
        tok = (key, self.cnt[key])
        self._commit(tok, reads, writes)
        self.n_inst += 1
        return tok

    def finish(self, regions):
        self._need("sp", [r.w for r in regions])

    def barrier(self):
        allc = [(k, v) for k, v in self.cnt.items() if v > 0 and k not in self.dead]
        for e in self.eng:
            self._need(e, allc)
        for e in self.eng:
            key = self.cur[e]
            if self.cnt[key] > 12000:
                self.dead.add(key)
                self.epoch += 1
                nk = "%s#%d" % (e, self.epoch)
                self.sems[nk] = self.stack.enter_context(self.nc.semaphore("prog_%s_%d" % (e, self.epoch)))
                self.cnt[nk] = 0
                self.cur[e] = nk


def PT(t, name):
    x = T(t, name)
    x.r.excl = True
    return x


class T:
    def __init__(self, t, name):
        self.t = t
        self.r = R(name)

    def __getitem__(self, k):
        return self.t[k]


def phase_A(nc, P, Dr, outs):
    x_full, x_s, w_in = Dr["x_full"], Dr["x_s"], Dr["w_in"]
    p_full, p_samp = Dr["p_full"], Dr["p_samp"]
    with ExitStack() as st:
        def sb(name, shape, dt=F32):
            return T(st.enter_context(nc.sbuf_tensor("a_" + name, list(shape), dt)), name)

        def ps(name, shape, dt=F32):
            return PT(st.enter_context(nc.psum_tensor("a_" + name, list(shape), dt)), name)

        ident = sb("ident", [128, 128], BF16)
        P.op("pool", lambda e: e.memset(ident[:], 0.0), writes=[ident.r])
        P.op("pool", lambda e: e.affine_select(ident[:], ident[:], [[-1, 128]], ALU.not_equal, 1.0,
                                               base=0, channel_multiplier=1),
             reads=[ident.r], writes=[ident.r])
        ropeT = sb("ropeT", [128, NT, 16])
        P.dma("sp", ropeT[:], Dr["rope_p"].rearrange("(n p) d -> p n d", p=128), writes=[ropeT.r])
        ropeS = sb("ropeS", [NS, 16])
        P.dma("sp", ropeS[:], Dr["rope_s"][:, :], writes=[ropeS.r])

        wA = sb("wA", [128, 8, NA], BF16)
        for kc in range(8):
            P.dma("pool", wA[:, kc, 0:R_COLS], w_in[kc * 128:(kc + 1) * 128, 0:R_COLS], writes=[wA.r])
            P.dma("pool", wA[:, kc, R_COLS:NA], w_in[kc * 128:(kc + 1) * 128, KV0:KV0 + NKV], writes=[wA.r])

        xb = [sb("xb%d" % i, [128, D], BF16) for i in range(2)]
        xT = [sb("xT%d" % i, [128, 8, 128], BF16) for i in range(2)]
        pt = [sb("ptile%d" % i, [128, NA]) for i in range(2)]
        rtmp = [sb("rtmp%d" % i, [128, 4, 6, 8]) for i in range(2)]
        ptr = [ps("ptr%d" % i, [128, 8, 128], BF16) for i in range(2)]
        pmm = [ps("pmm%d" % i, [128, 512]) for i in range(5)]
        pmm_i = [0]

        def rope_apply(tile_t, c0, cs_ap, tmp, n):
            v = tile_t.t[0:n, c0:c0 + 768].rearrange("p (a k g d) -> p a k g d", a=3, k=2, g=2)
            x1 = v[:, :, 0, :, 0:8]
            x2 = v[:, :, 0, :, 8:16]
            cos = cs_ap[:, 0:8].unsqueeze(1).unsqueeze(1).to_broadcast([n, 3, 2, 8])
            sin = cs_ap[:, 8:16].unsqueeze(1).unsqueeze(1).to_broadcast([n, 3, 2, 8])
            t = tmp.t[0:n]
            a1, a2, a3, a4 = [t[:, i, :, :].rearrange("p (a g) d -> p a g d", a=3) for i in range(4)]
            rd = [tile_t.r, tmp.r]
            P.op("dve", lambda e: e.tensor_tensor(a1, x1, cos, ALU.mult), reads=rd, writes=[tmp.r])
            P.op("dve", lambda e: e.tensor_tensor(a2, x2, sin, ALU.mult), reads=rd, writes=[tmp.r])
            P.op("dve", lambda e: e.tensor_tensor(a3, x2, cos, ALU.mult), reads=rd, writes=[tmp.r])
            P.op("dve", lambda e: e.tensor_tensor(a4, x1, sin, ALU.mult), reads=rd, writes=[tmp.r])
            P.op("dve", lambda e: e.tensor_tensor(x1, a1, a2, ALU.subtract), reads=rd, writes=[tile_t.r])
            P.op("dve", lambda e: e.tensor_tensor(x2, a3, a4, ALU.add), reads=rd, writes=[tile_t.r])

        r_pfull = R("p_full")
        r_out = R("outsA")
        outs.append(r_out)
        for ti in range(NT):
            b = ti % 2
            t0 = ti * 128
            P.dma("pool", xb[b][:], x_full[t0:t0 + 128, :], writes=[xb[b].r])
            for kc in range(8):
                P.op("pe", lambda e: e.transpose(ptr[b][:, kc, :], xb[b][:, kc * 128:(kc + 1) * 128], ident[:]),
                     reads=[xb[b].r, ident.r], writes=[ptr[b].r])
            P.op("act", lambda e: e.copy(xT[b][:], ptr[b][:]), reads=[ptr[b].r], writes=[xT[b].r])
            for nch in range(5):
                pm = pmm[pmm_i[0] % 5]
                pmm_i[0] += 1
                for kc in range(8):
                    P.op("pe", lambda e: e.matmul(pm[:], xT[b][:, kc, :], wA[:, kc, nch * 512:(nch + 1) * 512],
                                                  start=(kc == 0), stop=(kc == 7)),
                         reads=[xT[b].r, wA.r], writes=[pm.r])
                dst = pt[b][:, nch * 512:(nch + 1) * 512]
                if nch % 2 == 0:
                    P.op("dve", lambda e: e.tensor_copy(dst, pm[:]), reads=[pm.r], writes=[pt[b].r])
                else:
                    P.op("act", lambda e: e.copy(dst, pm[:]), reads=[pm.r], writes=[pt[b].r])
            rope_apply(pt[b], R_COLS, ropeT[:, ti, :], rtmp[b], 128)
            P.dma("sp", p_full[t0:t0 + 128, :], pt[b][:], reads=[pt[b].r], writes=[r_pfull])
            P.dma("sp", Dr["o_cmp_p"][t0:t0 + 128, :], pt[b][:, R_COLS:R_COLS + 256], reads=[pt[b].r], writes=[r_out])
            P.dma("sp", Dr["o_sel_p"][t0:t0 + 128, :], pt[b][:, R_COLS + 256:R_COLS + 512], reads=[pt[b].r], writes=[r_out])
            if ti >= NT - 4:
                w0 = (ti - (NT - 4)) * 128
                P.dma("sp", Dr["o_win_p"][w0:w0 + 128, :], pt[b][:, R_COLS + 512:R_COLS + 768], reads=[pt[b].r], writes=[r_out])
            if ti == NT - 1:
                P.dma("sp", Dr["o_shift_p"][0:1, :], pt[b][127:128, 0:R_COLS], reads=[pt[b].r], writes=[r_out])

        xsb = sb("xsb", [NS, D], BF16)
        xsT = sb("xsT", [128, 8, NS], BF16)
        psT = ptr[0]
        P.dma("pool", xsb[:], x_s[:, :], writes=[xsb.r])
        for kc in range(8):
            P.op("pe", lambda e: e.transpose(psT[:, kc, 0:NS], xsb[:, kc * 128:(kc + 1) * 128], ident[0:NS, 0:NS]),
                 reads=[xsb.r, ident.r], writes=[psT.r])
        P.op("act", lambda e: e.copy(xsT[:], psT[:, :, 0:NS]), reads=[psT.r], writes=[xsT.r])
        psamp = sb("psamp", [NS, IN_COLS])
        wS = [sb("wS%d" % i, [128, 8, 512], BF16) for i in range(2)]
        ncht = (IN_COLS + 511) // 512
        for nch in range(ncht):
            c0 = nch * 512
            cw = min(512, IN_COLS - c0)
            wb = wS[nch % 2]
            for kc in range(8):
                P.dma("pool", wb[:, kc, 0:cw], w_in[kc * 128:(kc + 1) * 128, c0:c0 + cw], writes=[wb.r])
            pm = pmm[pmm_i[0] % 5]
            pmm_i[0] += 1
            for kc in range(8):
                P.op("pe", lambda e: e.matmul(pm[0:NS, 0:cw], xsT[:, kc, :], wb[:, kc, 0:cw],
                                              start=(kc == 0), stop=(kc == 7)),
                     reads=[xsT.r, wb.r], writes=[pm.r])
            P.op("dve", lambda e: e.tensor_copy(psamp[:, c0:c0 + cw], pm[0:NS, 0:cw]), reads=[pm.r], writes=[psamp.r])
        rope_apply(psamp, KV0, ropeS[:, :], rtmp[0], NS)
        r_psamp = R("p_samp")
        P.dma("sp", p_samp[:, :], psamp[:], reads=[psamp.r], writes=[r_psamp])
        P.dma("sp", Dr["o_cmp_s"][:, :], psamp[:, KV0:KV0 + 256], reads=[psamp.r], writes=[r_out])
        P.dma("sp", Dr["o_sel_s"][:, :], psamp[:, KV0 + 256:KV0 + 512], reads=[psamp.r], writes=[r_out])
        for bb in range(SB):
            P.dma("sp", Dr["o_win_s"][bb, 508:512, :], psamp[bb * DS:(bb + 1) * DS, KV0 + 512:KV0 + 768],
                  reads=[psamp.r], writes=[r_out])
            P.dma("sp", Dr["o_win_s"][bb, 0:508, :], Dr["cache_win"][bb, 4:512, :], writes=[r_out])
            P.dma("sp", Dr["o_shift_s"][bb:bb + 1, :], psamp[bb * DS + DS - 1:bb * DS + DS, 0:R_COLS],
                  reads=[psamp.r], writes=[r_out])
        Dr["r_pfull"] = r_pfull
        Dr["r_psamp"] = r_psamp
        P.barrier()

VEC_OFF = {"mu": (0, 1792), "w0": (1792, 512), "a0": (2304, 512), "kk": (2816, 512), "ka": (3328, 512),
           "gng": (3840, 512), "gnb": (4352, 512), "rk": (4864, 512)}
NVEC = 5376
NMASK = 128 + 128 + 512 + 128 + 128


def phase_B(nc, P, Dr, outs):
    P.pe_selfsync = False
    with ExitStack() as st:
        def sb(name, shape, dt=F32):
            return T(st.enter_context(nc.sbuf_tensor("b_" + name, list(shape), dt)), name)

        def ps(name, shape, dt=F32):
            return PT(st.enter_context(nc.psum_tensor("b_" + name, list(shape), dt)), name)

        vecs = sb("vecs", [128, NVEC])
        P.dma("sp", vecs[:], Dr["vecs"][:, :], writes=[vecs.r])
        V = lambda k: vecs[:, VEC_OFF[k][0]:VEC_OFF[k][0] + VEC_OFF[k][1]]
        wlora = sb("wlora", [128, 512])
        P.dma("sp", wlora[0:64, :], Dr["w_w2"][:, :], writes=[wlora.r])
        P.dma("sp", wlora[64:128, :], Dr["w_a2"][:, :], writes=[wlora.r])
        gw2 = sb("gw2", [128, 512])
        P.dma("sp", gw2[:], Dr["g_w2"][:, :], writes=[gw2.r])
        masks = sb("masks", [128, NMASK])
        P.dma("sp", masks[:], Dr["masks"][:, :], writes=[masks.r])
        Lblk = masks[:, 0:128]
        Oblk = masks[:, 128:256]
        maskMA2 = masks[:, 256:768]
        maskNT = masks[:, 768:896]
        identF = masks[:, 896:1024]
        tmask = sb("tmask", [128, 1])
        P.dma("sp", tmask[:], Dr["tmask"][:, :], writes=[tmask.r])
        ones = sb("onesc", [128, 1])
        P.op("pool", lambda e: e.memset(ones[:], 1.0), writes=[ones.r])

        pr = sb("pr", [128, R_COLS])
        prev = sb("prev", [128, R_COLS])
        xm = sb("xm", [128, R_COLS])
        la = sb("la", [128, 256])
        laT = sb("laT", [128, 256])
        tA = sb("tA", [128, 512])
        tB = sb("tB", [128, 512])
        lw = sb("lw", [128, 512])
        aicl = sb("aicl", [128, 512])
        g_sb = sb("g_sb", [128, 512])
        kk = sb("kk", [128, 512])
        kkn = sb("kkn", [128, 512])
        kh = sb("kh", [128, 512])
        a_s = sb("a_s", [128, 512])
        b_s = sb("b_s", [128, 512])
        ss = sb("ss", [128, 8])
        rinv = sb("rinv", [128, 8])
        cum_sb = sb("cum_sb", [128, 512])
        e_sb = sb("e_sb", [128, 512])
        einv = sb("einv", [128, 512])
        ea = sb("ea", [128, 512])
        ec = sb("ec", [128, 512])
        at = sb("at", [128, 512])
        rt = sb("rt", [128, 512])
        bt = sb("bt", [128, 512])
        kt = sb("kt", [128, 512])
        bh = sb("bh", [128, 512])
        kh2 = sb("kh2", [128, 512])
        wc = sb("wc", [128, 8])
        FM_ar = sb("FM_ar", [128, 4, 256])
        FM_b = sb("FM_b", [128, 4, 128])
        FM_k = sb("FM_k", [128, 4, 128])
        MM = [sb("MM%d" % h, [128, 512]) for h in range(8)]
        TmA = [sb("TmA%d" % g, [128, 4, 128]) for g in range(2)]
        XAs = [[sb("XA%d_%d" % (g, i), [128, 4, 128]) for i in range(2)] for g in range(2)]
        XTAs = [[sb("XTA%d_%d" % (g, i), [128, 4, 128]) for i in range(2)] for g in range(2)]
        PMAs = [sb("PMA%d" % g, [128, 4, 128]) for g in range(2)]
        ZT_sb = sb("ZT_sb", [128, 512])
        UT_sb = sb("UT_sb", [128, 512])
        y_sb = sb("y_sb", [128, 512])
        yc = sb("yc", [128, 512])
        st1 = sb("st1", [128, 8])
        st2 = sb("st2", [128, 8])
        yo = sb("yo", [128, 512], BF16)
        ST = sb("ST", [128, 256])
        Sio = sb("Sio", [64, 512])

        pg = [ps("pg%d" % i, [128, 512]) for i in range(2)]
        pi = ps("pi", [128, 512])
        pv = [ps("pv%d" % i, [128, 512]) for i in range(2)]
        ZT_ps = ps("ZT_ps", [128, 512])
        UT_ps = ps("UT_ps", [128, 512])
        SN_ps = ps("SN_ps", [128, 512])

        def TT(eng, out, in0, in1, op, rd, wr):
            P.op(eng, lambda e: e.tensor_tensor(out, in0, in1, op), reads=rd, writes=wr)

        def ACT(out, in_, func, rd, wr, **kw):
            P.op("act", lambda e: e.activation(out, in_, func, **kw), reads=rd, writes=wr)

        last_rt = {}

        def MMUL(out, lhsT, rhs, rd, wr, start=True, stop=True, sync=False, rt=None):
            bank = id(wr[0])
            if rt is not None:
                prev = last_rt.get(bank)
                sync = sync or (prev is not None and prev != rt)
            last_rt[bank] = rt
            P.pe_selfsync = sync
            P.op("pe", lambda e: e.matmul(out, lhsT, rhs, start=start, stop=stop), reads=rd, writes=wr)
            P.pe_selfsync = False

        def TR(out, in_, idn, rd, wr):
            P.op("pe", lambda e: e.transpose(out, in_, idn), reads=rd + [masks.r], writes=wr)

        def load_state(src_ap):
            P.dma("sp", Sio[:].rearrange("i (h j) -> i h j", h=8), src_ap.rearrange("h i j -> i h j"), writes=[Sio.r])
            for hp in range(4):
                TR(pg[0][:, hp * 64:(hp + 1) * 64], Sio[:, hp * 128:(hp + 1) * 128], identF[0:64, 0:64], [Sio.r], [pg[0].r])
            P.op("dve", lambda e: e.tensor_copy(ST[:], pg[0][:, 0:256]), reads=[pg[0].r], writes=[ST.r])

        def store_state(dst_ap, rout):
            for hp in range(4):
                TR(pg[0][0:64, hp * 128:(hp + 1) * 128], ST[:, hp * 64:(hp + 1) * 64], identF, [ST.r], [pg[0].r])
            P.op("dve", lambda e: e.tensor_copy(Sio[:], pg[0][0:64, :]), reads=[pg[0].r], writes=[Sio.r])
            P.dma("sp", dst_ap.rearrange("h i j -> i h j"), Sio[:].rearrange("i (h j) -> i h j", h=8), reads=[Sio.r], writes=[rout])

        def rwkv_tile(load_fn, sample, pre_chunk, post_chunk, y_store):
            load_fn(pr, prev)
            TT("dve", prev[:], prev[:], pr[:], ALU.subtract, [prev.r, pr.r], [prev.r])
            TT("dve", prev[:], prev[:], V("mu"), ALU.mult, [prev.r, vecs.r], [prev.r])
            TT("dve", xm[:], prev[:], pr[:], ALU.add, [prev.r, pr.r], [xm.r])
            r_ = xm[:, 0:512]
            k_ = xm[:, 512:1024]
            v_ = xm[:, 1024:1536]
            ACT(la[:, 0:64], xm[:, 1536:1600], AF.Tanh, [xm.r], [la.r])
            ACT(la[:, 64:128], xm[:, 1600:1664], AF.Copy, [xm.r], [la.r])
            ACT(la[:, 128:256], xm[:, 1664:1792], AF.Sigmoid, [xm.r], [la.r])
            TR(pg[0][:, 0:128], la[:, 0:128], identF, [la.r], [pg[0].r])
            TR(pg[0][:, 128:256], la[:, 128:256], identF, [la.r], [pg[0].r])
            ACT(laT[:], pg[0][:, 0:256], AF.Copy, [pg[0].r], [laT.r])
            MMUL(pg[1][:], laT[0:64, 0:128], wlora[0:64, :], [laT.r, wlora.r], [pg[1].r])
            TT("dve", tA[:], pg[1][:], V("w0"), ALU.add, [pg[1].r, vecs.r], [tA.r])
            ACT(tA[:], tA[:], AF.Sigmoid, [tA.r], [tA.r])
            if sample:
                P.op("dve", lambda e: e.tensor_scalar(lw[:], tA[:], -0.6065306597126334, tmask[:, 0:1], ALU.mult, ALU.mult),
                     reads=[tA.r, tmask.r], writes=[lw.r])
            else:
                P.op("dve", lambda e: e.tensor_scalar(lw[:], tA[:], -0.6065306597126334, None, ALU.mult),
                     reads=[tA.r], writes=[lw.r])
            MMUL(pg[0][:], laT[64:128, 0:128], wlora[64:128, :], [laT.r, wlora.r], [pg[0].r])
            TT("dve", aicl[:], pg[0][:], V("a0"), ALU.add, [pg[0].r, vecs.r], [aicl.r])
            ACT(aicl[:], aicl[:], AF.Sigmoid, [aicl.r], [aicl.r])
            MMUL(pg[1][:], laT[:, 128:256], gw2[:], [laT.r, gw2.r], [pg[1].r])
            ACT(g_sb[:], pg[1][:], AF.Copy, [pg[1].r], [g_sb.r])
            TT("dve", kk[:], k_, V("kk"), ALU.mult, [xm.r, vecs.r], [kk.r])
            TT("dve", kkn[:], kk[:], kk[:], ALU.mult, [kk.r], [kkn.r])
            P.op("dve", lambda e: e.tensor_reduce(ss[:], kkn[:].rearrange("p (h j) -> p h j", h=8), AX.X, ALU.add),
                 reads=[kkn.r], writes=[ss.r])
            P.op("dve", lambda e: e.tensor_scalar(ss[:], ss[:], 1e-24, None, ALU.max), reads=[ss.r], writes=[ss.r])
            ACT(rinv[:], ss[:], AF.Sqrt, [ss.r], [rinv.r])
            P.op("dve", lambda e: e.reciprocal(rinv[:], rinv[:]), reads=[rinv.r], writes=[rinv.r])
            TT("dve", kkn[:].rearrange("p (h j) -> p h j", h=8), kk[:].rearrange("p (h j) -> p h j", h=8),
               rinv[:].unsqueeze(2).to_broadcast([128, 8, 64]), ALU.mult, [kk.r, rinv.r], [kkn.r])
            P.op("dve", lambda e: e.scalar_tensor_tensor(kh[:], aicl[:], -1.0, V("ka"), ALU.add, ALU.mult),
                 reads=[aicl.r, vecs.r], writes=[kh.r])
            P.op("dve", lambda e: e.scalar_tensor_tensor(kh[:], kh[:], 1.0, k_, ALU.add, ALU.mult),
                 reads=[kh.r, xm.r], writes=[kh.r])
            P.op("dve", lambda e: e.tensor_scalar(a_s[:], kkn[:], -1.0, None, ALU.mult), reads=[kkn.r], writes=[a_s.r])
            TT("dve", b_s[:], kkn[:], aicl[:], ALU.mult, [kkn.r, aicl.r], [b_s.r])
            if sample:
                P.op("dve", lambda e: e.tensor_scalar(kh[:], kh[:], tmask[:, 0:1], None, ALU.mult), reads=[kh.r, tmask.r], writes=[kh.r])
                P.op("dve", lambda e: e.tensor_scalar(b_s[:], b_s[:], tmask[:, 0:1], None, ALU.mult), reads=[b_s.r, tmask.r], writes=[b_s.r])
            MMUL(pg[0][:], Lblk, lw[:], [masks.r, lw.r], [pg[0].r])
            MMUL(pg[1][:], Oblk, lw[:], [masks.r, lw.r], [pg[1].r])
            ACT(cum_sb[:], pg[0][:], AF.Copy, [pg[0].r], [cum_sb.r])
            ACT(e_sb[:], pg[0][:], AF.Exp, [pg[0].r], [e_sb.r])
            ACT(einv[:], pg[0][:], AF.Exp, [pg[0].r], [einv.r], scale=-1.0)
            TT("dve", tA[:], pg[0][:], lw[:], ALU.subtract, [pg[0].r, lw.r], [tA.r])
            ACT(ea[:], tA[:], AF.Exp, [tA.r], [ea.r])
            TT("dve", tB[:], pg[1][:], cum_sb[:], ALU.subtract, [pg[1].r, cum_sb.r], [tB.r])
            ACT(ec[:], tB[:], AF.Exp, [tB.r], [ec.r])
            TT("dve", at[:], a_s[:], ea[:], ALU.mult, [a_s.r, ea.r], [at.r])
            TT("dve", rt[:], r_, e_sb[:], ALU.mult, [xm.r, e_sb.r], [rt.r])
            TT("dve", bt[:], b_s[:], einv[:], ALU.mult, [b_s.r, einv.r], [bt.r])
            TT("dve", kt[:], kh[:], einv[:], ALU.mult, [kh.r, einv.r], [kt.r])
            TT("dve", bh[:], b_s[:], ec[:], ALU.mult, [b_s.r, ec.r], [bh.r])
            TT("dve", kh2[:], kh[:], ec[:], ALU.mult, [kh.r, ec.r], [kh2.r])
            for c2 in range(2):
                rows = slice(c2 * 64, c2 * 64 + 64)
                for hp in range(4):
                    MMUL(SN_ps[:, 256 + c2 * 4 + hp:256 + c2 * 4 + hp + 1], lw[rows, hp * 128:(hp + 1) * 128], ones[rows, 0:1],
                         [lw.r, ones.r], [SN_ps.r], rt=c2)
            ACT(wc[:], SN_ps[:, 256:264], AF.Exp, [SN_ps.r], [wc.r])
            for qi, q in enumerate((at, rt)):
                for hp in range(4):
                    TR(pg[qi][:, hp * 128:(hp + 1) * 128], q[:, hp * 128:(hp + 1) * 128], identF, [q.r], [pg[qi].r])
                P.op("act" if qi == 0 else "dve",
                     (lambda e: e.copy(FM_ar[:, :, 0:128], pg[0][:].rearrange("p (h t) -> p h t", h=4))) if qi == 0 else
                     (lambda e: e.tensor_copy(FM_ar[:, :, 128:256], pg[1][:].rearrange("p (h t) -> p h t", h=4))),
                     reads=[pg[qi].r], writes=[FM_ar.r])
            for qi, (q, dst) in enumerate(((bt, FM_b), (kt, FM_k))):
                for hp in range(4):
                    TR(pg[qi][:, hp * 128:(hp + 1) * 128], q[:, hp * 128:(hp + 1) * 128], identF, [q.r], [pg[qi].r])
                if qi == 0:
                    P.op("act", lambda e: e.copy(dst[:].rearrange("p h t -> p (h t)"), pg[0][:]), reads=[pg[0].r], writes=[dst.r])
                else:
                    P.op("dve", lambda e: e.tensor_copy(dst[:].rearrange("p h t -> p (h t)"), pg[1][:]), reads=[pg[1].r], writes=[dst.r])
            for h in range(8):
                hp, h2 = h // 2, h % 2
                rows = slice(h2 * 64, h2 * 64 + 64)
                MMUL(pi[:, 0:256], FM_b[rows, hp, :], FM_ar[rows, hp, :], [FM_b.r, FM_ar.r], [pi.r])
                MMUL(pi[:, 256:512], FM_k[rows, hp, :], FM_ar[rows, hp, :], [FM_k.r, FM_ar.r], [pi.r])
                TT("dve", MM[h][:], pi[:], maskMA2, ALU.mult, [pi.r, masks.r], [MM[h].r])
            banks = [(pv[0], pv[1], pi), (ZT_ps, UT_ps, SN_ps)]
            for grp in range(2):
                bX, bXT, bP = banks[grp]
                XA, XTA, PMA = XAs[grp], XTAs[grp], PMAs[grp]
                for q4 in range(4):
                    h = grp * 4 + q4
                    hp, h2 = h // 2, h % 2
                    rows = slice(h2 * 64, h2 * 64 + 64)
                    MMUL(bXT[:, q4 * 128:(q4 + 1) * 128], FM_ar[rows, hp, 0:128], FM_b[rows, hp, :], [FM_ar.r, FM_b.r], [bXT.r], rt=h2)
                    P.op("dve", lambda e: e.tensor_copy(XA[0][:, q4, :], MM[h][:, 0:128]), reads=[MM[h].r], writes=[XA[0].r])
                    TT("dve", PMA[:, q4, :], MM[h][:, 0:128], identF, ALU.add, [MM[h].r, masks.r], [PMA.r])
                TT("dve", XTA[0][:], bXT[:].rearrange("p (h t) -> p h t", h=4), maskNT.unsqueeze(1).to_broadcast([128, 4, 128]), ALU.mult,
                   [bXT.r, masks.r], [XTA[0].r])
            for k in range(1, 6):
                for grp in range(2):
                    bX, bXT, bP = banks[grp]
                    XA, XTA, PMA = XAs[grp], XTAs[grp], PMAs[grp]
                    Xo, XTo = XA[(k - 1) % 2], XTA[(k - 1) % 2]
                    Xn, XTn = XA[k % 2], XTA[k % 2]
                    for q4 in range(4):
                        MMUL(bXT[:, q4 * 128:(q4 + 1) * 128], Xo[:, q4, :], XTo[:, q4, :], [Xo.r, XTo.r], [bXT.r])
                    if k < 5:
                        for q4 in range(4):
                            MMUL(bX[:, q4 * 128:(q4 + 1) * 128], XTo[:, q4, :], Xo[:, q4, :], [Xo.r, XTo.r], [bX.r])
                    P.op("act", lambda e: e.copy(XTn[:].rearrange("p h t -> p (h t)"), bXT[:]), reads=[bXT.r], writes=[XTn.r])
                    if k < 5:
                        P.op("dve", lambda e: e.tensor_copy(Xn[:].rearrange("p h t -> p (h t)"), bX[:]), reads=[bX.r], writes=[Xn.r])
                    for q4 in range(4):
                        MMUL(bP[:, q4 * 128:(q4 + 1) * 128], XTn[:, q4, :], PMA[:, q4, :], [XTn.r, PMA.r], [bP.r])
                    dstP = TmA[grp] if k == 5 else PMA
                    TT("dve", dstP[:].rearrange("p h t -> p (h t)"), bP[:], PMA[:].rearrange("p h t -> p (h t)"), ALU.add,
                       [bP.r, PMA.r], [dstP.r])
            for c2 in range(2):
                cs = slice(c2 * 64, c2 * 64 + 64)
                cc = slice(c2 * 64, c2 * 64 + 64)
                if pre_chunk is not None:
                    pre_chunk(c2)
                for h in range(8):
                    hp, h2 = h // 2, h % 2
                    rows = slice(h2 * 64, h2 * 64 + 64)
                    hc = slice(h * 64, h * 64 + 64)
                    Sh = ST[rows, hp * 64:(hp + 1) * 64]
                    MMUL(ZT_ps[cs, hc], FM_ar[rows, hp, cc], Sh, [FM_ar.r, ST.r], [ZT_ps.r], start=True, stop=False, rt=h2)
                    MMUL(ZT_ps[cs, hc], MM[h][cs, 256 + c2 * 64:256 + c2 * 64 + 64], xm[cs, 1024 + h * 64:1024 + h * 64 + 64],
                         [MM[h].r, xm.r], [ZT_ps.r], start=False, stop=True, rt=c2)
                P.op("act", lambda e: e.copy(ZT_sb[cs, :], ZT_ps[cs, :]), reads=[ZT_ps.r], writes=[ZT_sb.r])
                for h in range(8):
                    hc = slice(h * 64, h * 64 + 64)
                    MMUL(UT_ps[cs, hc], TmA[h // 4][cs, h % 4, cc], ZT_sb[cs, hc], [TmA[h // 4].r, ZT_sb.r], [UT_ps.r], rt=c2)
                P.op("dve", lambda e: e.tensor_copy(UT_sb[cs, :], UT_ps[cs, :]), reads=[UT_ps.r], writes=[UT_sb.r])
                yps = pg[c2]
                for h in range(8):
                    hp, h2 = h // 2, h % 2
                    rows = slice(h2 * 64, h2 * 64 + 64)
                    hc = slice(h * 64, h * 64 + 64)
                    Sh = ST[rows, hp * 64:(hp + 1) * 64]
                    vh = xm[cs, 1024 + h * 64:1024 + h * 64 + 64]
                    MMUL(yps[cs, hc], FM_ar[rows, hp, 128 + c2 * 64:128 + c2 * 64 + 64], Sh, [FM_ar.r, ST.r], [yps.r], start=True, stop=False, rt=h2)
                    MMUL(yps[cs, hc], MM[h][cs, 128 + c2 * 64:128 + c2 * 64 + 64], UT_sb[cs, hc], [MM[h].r, UT_sb.r], [yps.r], start=False, stop=False, rt=c2)
                    MMUL(yps[cs, hc], MM[h][cs, 384 + c2 * 64:384 + c2 * 64 + 64], vh, [MM[h].r, xm.r], [yps.r], start=False, stop=True, rt=c2)
                    MMUL(SN_ps[rows, hp * 64:(hp + 1) * 64], bh[cs, hc], UT_sb[cs, hc], [bh.r, UT_sb.r], [SN_ps.r], start=True, stop=False, rt=c2)
                    MMUL(SN_ps[rows, hp * 64:(hp + 1) * 64], kh2[cs, hc], vh, [kh2.r, xm.r], [SN_ps.r], start=False, stop=True, rt=c2)
                P.op("act", lambda e: e.copy(y_sb[cs, :], yps[cs, :]), reads=[yps.r], writes=[y_sb.r])
                TT("dve", ST[:].rearrange("p (h i) -> p h i", h=4), ST[:].rearrange("p (h i) -> p h i", h=4),
                   wc[:, c2 * 4:c2 * 4 + 4].unsqueeze(2).to_broadcast([128, 4, 64]), ALU.mult, [ST.r, wc.r], [ST.r])
                TT("dve", ST[:], ST[:], SN_ps[:, 0:256], ALU.add, [ST.r, SN_ps.r], [ST.r])
                if post_chunk is not None:
                    post_chunk(c2)
            y3 = y_sb[:].rearrange("p (h j) -> p h j", h=8)
            yc3 = yc[:].rearrange("p (h j) -> p h j", h=8)
            P.op("dve", lambda e: e.tensor_reduce(st1[:], y3, AX.X, ALU.add), reads=[y_sb.r], writes=[st1.r])
            P.op("dve", lambda e: e.tensor_scalar(st1[:], st1[:], 1.0 / 64, None, ALU.mult), reads=[st1.r], writes=[st1.r])
            TT("dve", yc3, y3, st1[:].unsqueeze(2).to_broadcast([128, 8, 64]), ALU.subtract, [y_sb.r, st1.r], [yc.r])
            TT("dve", tA[:], yc[:], yc[:], ALU.mult, [yc.r], [tA.r])
            P.op("dve", lambda e: e.tensor_reduce(st2[:], tA[:].rearrange("p (h j) -> p h j", h=8), AX.X, ALU.add), reads=[tA.r], writes=[st2.r])
            P.op("dve", lambda e: e.tensor_scalar(st2[:], st2[:], 1.0 / 64, 64e-5, ALU.mult, ALU.add), reads=[st2.r], writes=[st2.r])
            ACT(st2[:], st2[:], AF.Sqrt, [st2.r], [st2.r])
            P.op("dve", lambda e: e.reciprocal(st2[:], st2[:]), reads=[st2.r], writes=[st2.r])
            TT("dve", yc3, yc3, st2[:].unsqueeze(2).to_broadcast([128, 8, 64]), ALU.mult, [yc.r, st2.r], [yc.r])
            TT("dve", yc[:], yc[:], V("gng"), ALU.mult, [yc.r, vecs.r], [yc.r])
            TT("dve", yc[:], yc[:], V("gnb"), ALU.add, [yc.r, vecs.r], [yc.r])
            TT("dve", tB[:], r_, kh[:], ALU.mult, [xm.r, kh.r], [tB.r])
            TT("dve", tB[:], tB[:], V("rk"), ALU.mult, [tB.r, vecs.r], [tB.r])
            P.op("dve", lambda e: e.tensor_reduce(st1[:], tB[:].rearrange("p (h j) -> p h j", h=8), AX.X, ALU.add), reads=[tB.r], writes=[st1.r])
            TT("dve", tB[:].rearrange("p (h j) -> p h j", h=8), v_.rearrange("p (h j) -> p h j", h=8),
               st1[:].unsqueeze(2).to_broadcast([128, 8, 64]), ALU.mult, [xm.r, st1.r], [tB.r])
            TT("dve", yc[:], yc[:], tB[:], ALU.add, [yc.r, tB.r], [yc.r])
            TT("dve", yo[:], yc[:], g_sb[:], ALU.mult, [yc.r, g_sb.r], [yo.r])
            y_store(yo)

        P.op("dve", lambda e: e.memset(ST[:], 0.0), writes=[ST.r])
        r_yr = R("y_r")
        r_out = R("outB")
        outs.append(r_out)
        p_full = Dr["p_full"]
        for ti in range(NT):
            t0 = ti * 128

            def load_fn(pr_t, prev_t, t0=t0, ti=ti):
                P.dma("sp", pr_t[:], p_full[t0:t0 + 128, 0:R_COLS], reads=[Dr["r_pfull"]], writes=[pr_t.r])
                if ti == 0:
                    P.op("dve", lambda e: e.memset(prev_t[0:1, :], 0.0), writes=[prev_t.r])
                    P.dma("sp", prev_t[1:128, :], p_full[0:127, 0:R_COLS], reads=[Dr["r_pfull"]], writes=[prev_t.r])
                else:
                    P.dma("sp", prev_t[:], p_full[t0 - 1:t0 + 127, 0:R_COLS], reads=[Dr["r_pfull"]], writes=[prev_t.r])

            def y_store(yo_t, t0=t0):
                P.dma("pool", Dr["y_r"][t0:t0 + 128, :], yo_t[:], reads=[yo_t.r], writes=[r_yr])

            post = None
            if ti == NT - 1:
                def post(c2):
                    if c2 == 1:
                        store_state(Dr["o_wkv_p"], r_out)
            rwkv_tile(load_fn, False, None, post, y_store)
            if ti % 2 == 1:
                next(Dr["gather_gen"], None)

        p_samp = Dr["p_samp"]
        for tp in range(SB // 2):
            def load_fn(pr_t, prev_t, tp=tp):
                P.op("dve", lambda e: e.memset(pr_t[:], 0.0), writes=[pr_t.r])
                P.op("dve", lambda e: e.memset(prev_t[:], 0.0), writes=[prev_t.r])
                for c2 in range(2):
                    bb = tp * 2 + c2
                    P.dma("sp", pr_t[c2 * 64:c2 * 64 + DS, :], p_samp[bb * DS:(bb + 1) * DS, 0:R_COLS],
                          reads=[Dr["r_psamp"]], writes=[pr_t.r])
                    P.dma("sp", prev_t[c2 * 64:c2 * 64 + 1, :], Dr["state_shift"][bb:bb + 1, :], writes=[prev_t.r])
                    P.dma("sp", prev_t[c2 * 64 + 1:c2 * 64 + DS, :], p_samp[bb * DS:(bb + 1) * DS - 1, 0:R_COLS],
                          reads=[Dr["r_psamp"]], writes=[prev_t.r])

            def pre(c2, tp=tp):
                load_state(Dr["state_wkv"][tp * 2 + c2])

            def post(c2, tp=tp):
                store_state(Dr["o_wkv_s"][tp * 2 + c2], r_out)

            def y_store(yo_t, tp=tp):
                for c2 in range(2):
                    bb = tp * 2 + c2
                    P.dma("pool", Dr["y_r_s"][bb * DS:(bb + 1) * DS, :], yo_t[c2 * 64:c2 * 64 + DS, :], reads=[yo_t.r], writes=[r_yr])

            rwkv_tile(load_fn, True, pre, post, y_store)
        Dr["r_yr"] = r_yr
        P.barrier()
    P.pe_selfsync = False

QG0 = 1792
GATE0 = 3072
NEGB = -30000.0


def phase_C(nc, P, Dr, outs):
    for _ in Dr["gather_gen"]:
        pass
    P.barrier()
    Dr["gather_stack"].close()
    with ExitStack() as st:
        def sb(name, shape, dt=F32):
            return T(st.enter_context(nc.sbuf_tensor("c_" + name, list(shape), dt)), name)

        def ps(name, shape, dt=F32):
            return PT(st.enter_context(nc.psum_tensor("c_" + name, list(shape), dt)), name)

        def TT(eng, out, in0, in1, op, rd, wr):
            P.op(eng, lambda e: e.tensor_tensor(out, in0, in1, op), reads=rd, writes=wr)

        def MMUL(out, lhsT, rhs, rd, wr, start=True, stop=True):
            P.op("pe", lambda e: e.matmul(out, lhsT, rhs, start=start, stop=stop, skip_group_check=True), reads=rd, writes=wr)

        identb = sb("identb", [128, 128], BF16)
        P.dma("pool", identb[:], Dr["masks"][:, 896:1024], writes=[identb.r])
        identf = sb("identf", [128, 128])
        P.dma("sp", identf[:], Dr["masks"][:, 896:1024], writes=[identf.r])
        onesf = sb("onesf", [128, 128])
        P.op("pool", lambda e: e.memset(onesf[:], 1.0), writes=[onesf.r])
        NKT = 65
        ksT = [sb("ksT%d" % g, [65, NKT * 128], BF16) for g in range(2)]
        kwT = [sb("kwT%d" % g, [65, NKT * 128], BF16) for g in range(2)]
        vsw = sb("vsw", [128, NKT, 4, 65], BF16)
        kcmpT = [sb("kcmpT%d" % g, [65, 512], BF16) for g in range(2)]
        vcmp = sb("vcmp", [128, 4, 2, 65], BF16)
        nm = sb("nm", [128, 12])
        kmaxb = sb("kmaxb", [128, 1])
        for g in range(2):
            P.op("pool", lambda e: e.memset(ksT[g][64:65, :], 1.0), writes=[ksT[g].r])
            P.op("pool", lambda e: e.memset(kwT[g][64:65, :], 1.0), writes=[kwT[g].r])
            P.op("pool", lambda e: e.memset(kcmpT[g][64:65, :], 1.0), writes=[kcmpT[g].r])
        P.op("pool", lambda e: e.memset(vsw[:, :, :, 64:65], 1.0), writes=[vsw.r])
        P.op("pool", lambda e: e.memset(vcmp[:, :, :, 64:65], 1.0), writes=[vcmp.r])
        kvb = [sb("kvb%d" % i, [128, 768], BF16) for i in range(2)]
        sqt = sb("sqt", [128, 256])
        nt4 = sb("nt4", [128, 4])
        ptr = ps("ptr", [128, 8, 128], BF16)

        r_ya = R("y_a_own")

        def ingest_tile(kb, ti, do_cmp, do_sel, do_win, kcT, vcT, first):
            c0 = ti * 128
            rd = [kb.r, identb.r]
            ing = OPTS.get("ing", 15)
            do_cmp = do_cmp and bool(ing & 1)
            do_sel = do_sel and bool(ing & 2)
            do_win = do_win and bool(ing & 4)
            if do_cmp:
                P.op("pe", lambda e: e.transpose(ptr[:, 0, :], kb[:, 0:128], identb[:]), reads=rd, writes=[ptr.r])
                P.op("pe", lambda e: e.transpose(ptr[:, 1, :], kb[:, 128:256], identb[:]), reads=rd, writes=[ptr.r])
            if do_sel:
                P.op("pe", lambda e: e.transpose(ptr[0:64, 2, :], kb[:, 256:320], identb[:]), reads=rd, writes=[ptr.r])
                P.op("pe", lambda e: e.transpose(ptr[0:64, 3, :], kb[:, 320:384], identb[:]), reads=rd, writes=[ptr.r])
            if do_win:
                P.op("pe", lambda e: e.transpose(ptr[0:64, 4, :], kb[:, 512:576], identb[:]), reads=rd, writes=[ptr.r])
                P.op("pe", lambda e: e.transpose(ptr[0:64, 5, :], kb[:, 576:640], identb[:]), reads=rd, writes=[ptr.r])
            if do_cmp:
                P.op("act", lambda e: e.copy(kcT[:, c0:c0 + 128], ptr[:, 0, :]), reads=[ptr.r], writes=[kcT.r])
                P.op("act", lambda e: e.copy(vcT[:, c0:c0 + 128], ptr[:, 1, :]), reads=[ptr.r], writes=[vcT.r])
            if do_sel:
                P.op("dve", lambda e: e.tensor_copy(ksT[0][0:64, c0:c0 + 128], ptr[0:64, 2, :]), reads=[ptr.r], writes=[ksT[0].r])
                P.op("dve", lambda e: e.tensor_copy(ksT[1][0:64, c0:c0 + 128], ptr[0:64, 3, :]), reads=[ptr.r], writes=[ksT[1].r])
                P.op("act", lambda e: e.copy(vsw[:, ti, 0:2, 0:64], kb[:, 384:512].rearrange("p (g d) -> p g d", g=2)),
                     reads=[kb.r], writes=[vsw.r])
            if do_win:
                P.op("dve", lambda e: e.tensor_copy(kwT[0][0:64, c0:c0 + 128], ptr[0:64, 4, :]), reads=[ptr.r], writes=[kwT[0].r])
                P.op("dve", lambda e: e.tensor_copy(kwT[1][0:64, c0:c0 + 128], ptr[0:64, 5, :]), reads=[ptr.r], writes=[kwT[1].r])
                P.op("act", lambda e: e.copy(vsw[:, ti, 2:4, 0:64], kb[:, 640:768].rearrange("p (g d) -> p g d", g=2)),
                     reads=[kb.r], writes=[vsw.r])
            if not (ing & 8):
                return
            kk = kb[:, 256:768].rearrange("p (a r) -> p a r", a=2)[:, :, 0:128]
            P.op("dve", lambda e: e.tensor_tensor(sqt[:].rearrange("p (a r) -> p a r", a=2), kk, kk, ALU.mult), reads=[kb.r], writes=[sqt.r])
            P.op("dve", lambda e: e.tensor_reduce(nt4[:], sqt[:].rearrange("p (a d) -> p a d", a=4), AX.X, ALU.add), reads=[sqt.r], writes=[nt4.r])
            if first:
                P.op("dve", lambda e: e.tensor_copy(nm[:, 0:4], nt4[:]), reads=[nt4.r], writes=[nm.r])
            else:
                TT("dve", nm[:, 0:4], nm[:, 0:4], nt4[:], ALU.max, [nm.r, nt4.r], [nm.r])

        def compress_all(kcT, vcT):
            with ExitStack() as st2:
                def sb2(name, shape, dt=F32):
                    return T(st2.enter_context(nc.sbuf_tensor("c2_" + name + "_%d" % P.n_inst, list(shape), dt)), name)

                def ps2(name, shape, dt=F32):
                    return PT(st2.enter_context(nc.psum_tensor("c2_" + name + "_%d" % P.n_inst, list(shape), dt)), name)
                w1c = sb2("w1c", [128, 2, 32, 256], BF16)
                for kv in range(2):
                    src = Dr["cmp_w1"][kv].rearrange("(j d) h -> d j h", d=64)
                    P.dma("pool", w1c[0:64, kv, :, :], src, writes=[w1c.r])
                    P.dma("pool", w1c[64:128, kv, :, :], src, writes=[w1c.r])
                w2c = sb2("w2c", [128, 2, 2, 64], BF16)
                for kv in range(2):
                    P.dma("pool", w2c[:, kv, :, :], Dr["cmp_w2"][kv].rearrange("(c p) d -> p c d", p=128), writes=[w2c.r])
                pef = sb2("pef", [32, 2, 64])
                P.dma("sp", pef[:], Dr["cmp_pe"].rearrange("k j d -> j k d"), writes=[pef.r])
                peT = sb2("peT", [64, 2, 32], BF16)
                b1c = sb2("b1c", [128, 2, 2])
                P.dma("sp", b1c[:], Dr["cmp_b1T"][:, :, :], writes=[b1c.r])
                b2k = sb2("b2k", [64, 1])
                P.dma("sp", b2k[:], Dr["cmp_b2T"][:, :], writes=[b2k.r])
                b2v = sb2("b2v", [128, 64])
                P.dma("sp", b2v[:], Dr["cmp_b2v"][:, :], writes=[b2v.r])
                cb = sb2("cb", [128, 2, 2])
                hx = sb2("hx", [128, 512])
                hu = sb2("hu", [128, 512])
                hT = sb2("hT", [128, 2, 512], BF16)
                kcf = sb2("kcf", [64, 512])
                P.op("pool", lambda e: e.memset(hT[:], 0.0), writes=[hT.r])
                pc = [ps2("pc%d" % i, [128, 512]) for i in range(2)]
                pk = ps2("pk", [128, 512])
                pcm = ps2("pcm", [128, 512])
                for kv in range(2):
                    P.op("pe", lambda e: e.transpose(pcm[0:64, kv * 32:(kv + 1) * 32], pef[:, kv, :], identf[0:32, 0:32]),
                         reads=[pef.r, identf.r], writes=[pcm.r])
                P.op("dve", lambda e: e.tensor_copy(peT[:].rearrange("p k j -> p (k j)"), pcm[0:64, 0:64]), reads=[pcm.r], writes=[peT.r])
                for kv in range(2):
                    for hc in range(2):
                        col = kv * 2 + hc
                        for j in range(32):
                            MMUL(pcm[:, 64 + col:65 + col], w1c[0:64, kv, j, hc * 128:(hc + 1) * 128], peT[:, kv, j:j + 1],
                                 [w1c.r, peT.r], [pcm.r], start=(j == 0), stop=(j == 31))
                TT("dve", cb[:].rearrange("p k c -> p (k c)"), pcm[:, 64:68], b1c[:].rearrange("p k c -> p (k c)"), ALU.add,
                   [pcm.r, b1c.r], [cb.r])
                for kv, srcT in ((0, kcT), (1, vcT)):
                    for g in range(2):
                        rows = slice(g * 64, g * 64 + 64)
                        for hc in range(2):
                            pp = pc[hc]
                            for j in range(32):
                                MMUL(pp[:, 0:511], w1c[rows, kv, j, hc * 128:(hc + 1) * 128],
                                     srcT[rows, j:j + 16 * 510 + 1:16], [w1c.r, srcT.r], [pp.r], start=(j == 0), stop=(j == 31))
                            P.op("act", lambda e: e.activation(hx[:, 0:511], pp[:, 0:511], AF.Identity, bias=cb[:, kv, hc:hc + 1]),
                                 reads=[pp.r, cb.r], writes=[hx.r])
                            TT("dve", hu[:, 0:511], hx[:, 0:511], hx[:, 0:511], ALU.mult, [hx.r], [hu.r])
                            P.op("dve", lambda e: e.tensor_scalar(hu[:, 0:511], hu[:, 0:511], 0.044715, 1.0, ALU.mult, ALU.add), reads=[hu.r], writes=[hu.r])
                            TT("dve", hu[:, 0:511], hu[:, 0:511], hx[:, 0:511], ALU.mult, [hu.r, hx.r], [hu.r])
                            P.op("act", lambda e: e.activation(hu[:, 0:511], hu[:, 0:511], AF.Sigmoid, scale=1.5957691216057308), reads=[hu.r], writes=[hu.r])
                            TT("dve", hT[:, hc, 0:511], hx[:, 0:511], hu[:, 0:511], ALU.mult, [hx.r, hu.r], [hT.r])
                        if kv == 0:
                            for hc in range(2):
                                MMUL(pk[0:64, 0:511], w2c[:, 0, hc, :], hT[:, hc, 0:511], [w2c.r, hT.r], [pk.r], start=(hc == 0), stop=(hc == 1))
                            P.op("act", lambda e: e.activation(kcf[:, 0:511], pk[0:64, 0:511], AF.Identity, bias=b2k[:, 0:1]),
                                 reads=[pk.r, b2k.r], writes=[kcf.r])
                            P.op("pool", lambda e: e.memset(kcf[:, 511:512], 0.0), writes=[kcf.r])
                            P.op("dve", lambda e: e.tensor_copy(kcmpT[g][0:64, :], kcf[:, :]), reads=[kcf.r], writes=[kcmpT[g].r])
                            TT("dve", kcf[:, :], kcf[:, :], kcf[:, :], ALU.mult, [kcf.r], [kcf.r])
                            for bt in range(4):
                                MMUL(pcm[:, 80 + g * 4 + bt:81 + g * 4 + bt], kcf[:, bt * 128:(bt + 1) * 128], onesf[0:64, 0:1],
                                     [kcf.r, onesf.r], [pcm.r])
                            P.op("dve", lambda e: e.tensor_copy(nm[:, 4 + g * 4:8 + g * 4], pcm[:, 80 + g * 4:84 + g * 4]), reads=[pcm.r], writes=[nm.r])
                        else:
                            for bt in range(4):
                                nb = 128
                                for hc in range(2):
                                    MMUL(pk[0:nb, bt * 64:(bt + 1) * 64], hT[:, hc, bt * 128:bt * 128 + nb], w2c[:, 1, hc, :],
                                         [hT.r, w2c.r], [pk.r], start=(hc == 0), stop=(hc == 1))
                                TT("dve", vcmp[0:nb, bt, g, 0:64], pk[0:nb, bt * 64:(bt + 1) * 64], b2v[0:nb, :], ALU.add, [pk.r, b2v.r], [vcmp.r])
                P.op("pe", lambda e: e.transpose(pcm[0:12, 128:256], nm[:, 0:12], identf[:]), reads=[nm.r, identf.r], writes=[pcm.r])
                P.op("dve", lambda e: e.tensor_reduce(hx[0:12, 0:1], pcm[0:12, 128:256], AX.X, ALU.max), reads=[pcm.r], writes=[hx.r])
                P.op("pe", lambda e: e.transpose(pcm[0:1, 256:268], hx[0:12, 0:1], identf[0:12, 0:12]), reads=[hx.r, identf.r], writes=[pcm.r])
                P.op("dve", lambda e: e.tensor_reduce(hx[0:1, 1:2], pcm[0:1, 256:268], AX.X, ALU.max), reads=[pcm.r], writes=[hx.r])
                P.op("act", lambda e: e.activation(hx[0:1, 2:3], hx[0:1, 1:2], AF.Sqrt), reads=[hx.r], writes=[hx.r])
                MMUL(pcm[:, 300:301], onesf[0:1, :], hx[0:1, 2:3], [onesf.r, hx.r], [pcm.r])
                P.op("dve", lambda e: e.tensor_copy(kmaxb[:], pcm[:, 300:301]), reads=[pcm.r], writes=[kmaxb.r])
                P.barrier()

        def attention_scope(run):
            with ExitStack() as st3:
                def sb3(name, shape, dt=F32):
                    return T(st3.enter_context(nc.sbuf_tensor("c3_" + name + "_%d" % P.n_inst, list(shape), dt)), name)

                def ps3(name, shape, dt=F32):
                    return PT(st3.enter_context(nc.psum_tensor("c3_" + name + "_%d" % P.n_inst, list(shape), dt)), name)
                A = {}
                A["G"] = sb3("G", [128, 8192], BF16)
                P.dma("pool", A["G"][:], Dr["Gtab"][:, :], writes=[A["G"].r])
                A["cover"] = sb3("cover", [128, 4, 128], BF16)
                P.dma("pool", A["cover"][:], Dr["cover"][:, :, :], writes=[A["cover"].r])
                A["wq"] = sb3("wq", [128, 8, 536], BF16)
                for kc in range(8):
                    P.dma("pool", A["wq"][:, kc, 0:512], Dr["w_in"][kc * 128:(kc + 1) * 128, QG0:QG0 + 512], writes=[A["wq"].r])
                    P.dma("pool", A["wq"][:, kc, 512:536], Dr["w_in"][kc * 128:(kc + 1) * 128, GATE0:GATE0 + 24], writes=[A["wq"].r])
                A["Ftab"] = sb3("Ftab", [128, 128])
                A["cb2"] = sb3("cb2", [128, 2, 128], BF16)
                A["triS"] = sb3("triS", [128, 4, 512], BF16)
                A["triW"] = sb3("triW", [128, 8, 512], BF16)
                A["xb"] = sb3("xb", [128, 1024], BF16)
                A["xT"] = sb3("xT", [128, 8, 128], BF16)
                A["qf"] = sb3("qf", [128, 512])
                A["rt"] = sb3("rt", [128, 4, 8, 8])
                A["rope"] = sb3("rope", [128, 16])
                A["gts"] = sb3("gts", [128, 24])
                A["qn"] = sb3("qn", [128, 8])
                A["qa"] = sb3("qa", [128, 8, 65], BF16)
                A["qT"] = sb3("qT", [65, 8, 128], BF16)
                A["pT"] = [sb3("pT%d" % i, [128, 512], BF16) for i in range(2)]
                A["ov"] = sb3("ov", [128, 4, 65])
                A["rl"] = sb3("rl", [128, 4])
                A["cf"] = sb3("cf", [128, 4])
                A["imp"] = sb3("imp", [128, 128])
                A["sc"] = sb3("sc", [128, 128])
                A["sc2"] = sb3("sc2", [128, 128])
                A["m8a"] = sb3("m8a", [128, 8])
                A["m8b"] = sb3("m8b", [128, 8])
                A["mb4"] = sb3("mb4", [128, 4, 128], BF16)
                A["ya"] = sb3("ya", [128, 8, 64])
                A["yab"] = sb3("yab", [128, 512], BF16)
                A["tmp"] = sb3("tmp", [128, 4, 64])
                A["sT"] = [ps3("sT%d" % i, [128, 512]) for i in range(2)]
                A["po"] = [ps3("po%d" % i, [128, 512]) for i in range(3)]
                A["ir"] = ps3("ir", [128, 512])
                A["pq"] = ps3("pq", [128, 512])
                A["cnt"] = {"sT": 0, "po": 0, "pT": 0}
                run(A)
                P.barrier()

        def q_prepare(A, n, load_x, load_q, rope_src):
            qf, gts, qa, qT, pq = A["qf"], A["gts"], A["qa"], A["qT"], A["pq"]
            if n < 128:
                P.op("pool", lambda e: e.memset(qa[:], 0.0), writes=[qa.r])
            if load_x is not None:
                load_x(A["xb"])
                for kc in range(8):
                    P.op("pe", lambda e: e.transpose(ptr[:, kc, :], A["xb"][:, kc * 128:(kc + 1) * 128], identb[:]),
                         reads=[A["xb"].r, identb.r], writes=[ptr.r])
                P.op("act", lambda e: e.copy(A["xT"][:], ptr[:]), reads=[ptr.r], writes=[A["xT"].r])
                for kc in range(8):
                    MMUL(pq[:, :], A["xT"][:, kc, :], A["wq"][:, kc, 0:512], [A["xT"].r, A["wq"].r], [pq.r], start=(kc == 0), stop=(kc == 7))
                P.op("act", lambda e: e.activation(qf[:], pq[:], AF.Copy, scale=0.125), reads=[pq.r], writes=[qf.r])
                for kc in range(8):
                    MMUL(pq[:, 0:24], A["xT"][:, kc, :], A["wq"][:, kc, 512:536], [A["xT"].r, A["wq"].r], [pq.r], start=(kc == 0), stop=(kc == 7))
                P.op("act", lambda e: e.activation(gts[:], pq[:, 0:24], AF.Sigmoid), reads=[pq.r], writes=[gts.r])
            else:
                load_q(qf, gts)
                P.op("act", lambda e: e.activation(qf[0:n, :], qf[0:n, :], AF.Copy, scale=0.125), reads=[qf.r], writes=[qf.r])
                P.op("act", lambda e: e.activation(gts[0:n, :], gts[0:n, :], AF.Sigmoid), reads=[gts.r], writes=[gts.r])
            P.dma("sp", A["rope"][0:n, :], rope_src, writes=[A["rope"].r])
            q3 = qf[0:n, :].rearrange("p (h d) -> p h d", h=8)
            x1 = q3[:, :, 0:8]
            x2 = q3[:, :, 8:16]
            cos = A["rope"][0:n, 0:8].unsqueeze(1).to_broadcast([n, 8, 8])
            sin = A["rope"][0:n, 8:16].unsqueeze(1).to_broadcast([n, 8, 8])
            rt = A["rt"]
            rd = [qf.r, rt.r, A["rope"].r]
            TT("dve", rt[0:n, 0], x1, cos, ALU.mult, rd, [rt.r])
            TT("dve", rt[0:n, 1], x2, sin, ALU.mult, rd, [rt.r])
            TT("dve", rt[0:n, 2], x2, cos, ALU.mult, rd, [rt.r])
            TT("dve", rt[0:n, 3], x1, sin, ALU.mult, rd, [rt.r])
            TT("dve", x1, rt[0:n, 0], rt[0:n, 1], ALU.subtract, rd, [qf.r])
            TT("dve", x2, rt[0:n, 2], rt[0:n, 3], ALU.add, rd, [qf.r])
            TT("dve", A["ya"][0:n].rearrange("p h d -> p (h d)"), qf[0:n, :], qf[0:n, :], ALU.mult, [qf.r], [A["ya"].r])
            P.op("dve", lambda e: e.tensor_reduce(A["qn"][0:n, :], A["ya"][0:n], AX.X, ALU.add), reads=[A["ya"].r], writes=[A["qn"].r])
            P.op("act", lambda e: e.activation(A["qn"][0:n, :], A["qn"][0:n, :], AF.Sqrt), reads=[A["qn"].r], writes=[A["qn"].r])
            P.op("dve", lambda e: e.tensor_scalar(A["qn"][0:n, :], A["qn"][0:n, :], kmaxb[0:n, 0:1], -1.0, ALU.mult, ALU.mult),
                 reads=[A["qn"].r, kmaxb.r], writes=[A["qn"].r])
            P.op("dve", lambda e: e.tensor_copy(qa[0:n, :, 0:64], q3), reads=[qf.r], writes=[qa.r])
            P.op("dve", lambda e: e.tensor_copy(qa[0:n, :, 64:65], A["qn"][0:n, :].unsqueeze(2)), reads=[A["qn"].r], writes=[qa.r])
            for h in range(8):
                P.op("pe", lambda e: e.transpose(ptr[0:65, h, :], qa[:, h, :], identb[:]), reads=[qa.r, identb.r], writes=[ptr.r])
            P.op("act", lambda e: e.copy(qT[:], ptr[0:65, :, :]), reads=[ptr.r], writes=[qT.r])

        def nsa_qtile(A, n, cfg):
            qT, gts, ya = A["qT"], A["gts"], A["ya"]
            cnt = A["cnt"]

            def next_sT():
                t = A["sT"][cnt["sT"] % 2]
                cnt["sT"] += 1
                return t

            def next_pT():
                t = A["pT"][cnt["pT"] % 2]
                cnt["pT"] += 1
                return t

            def next_po():
                t = A["po"][cnt["po"] % 3]
                cnt["po"] += 1
                return t

            def finish_branch(po_t, g, br, first):
                ov, rl, cf = A["ov"], A["rl"], A["cf"]
                P.op("act", lambda e: e.copy(ov[:].rearrange("p h d -> p (h d)"), po_t[:, 0:260]), reads=[po_t.r], writes=[ov.r])
                P.op("dve", lambda e: e.tensor_scalar(rl[:], ov[:, :, 64], 1e-30, None, ALU.max), reads=[ov.r], writes=[rl.r])
                P.op("dve", lambda e: e.reciprocal(rl[:], rl[:]), reads=[rl.r], writes=[rl.r])
                g3 = gts[:, :].rearrange("p (h b) -> p h b", b=3)
                TT("dve", cf[:], rl[:], g3[:, 4 * g:4 * g + 4, br], ALU.mult, [rl.r, gts.r], [cf.r])
                dst = ya[:, 4 * g:4 * g + 4, :]
                cfb = cf[:].unsqueeze(2).to_broadcast([128, 4, 64])
                if first:
                    TT("dve", dst, ov[:, :, 0:64], cfb, ALU.mult, [ov.r, cf.r], [ya.r])
                else:
                    TT("dve", A["tmp"][:], ov[:, :, 0:64], cfb, ALU.mult, [ov.r, cf.r], [A["tmp"].r])
                    TT("dve", dst, dst, A["tmp"][:], ALU.add, [ya.r, A["tmp"].r], [ya.r])

            for g in range(2):
                qTg = qT[0:65, 4 * g:4 * g + 4, :].rearrange("p h q -> p (h q)")
                po_t = next_po()
                ir = A["ir"]
                nbt = cfg["nbt"]
                for bt in range(nbt):
                    sT = next_sT()
                    slots = [s for (b_, s) in cfg["cb_tiles"] if b_ == bt]
                    MMUL(sT[:, :], kcmpT[g][0:65, bt * 128:(bt + 1) * 128], qTg, [kcmpT[g].r, qT.r], [sT.r], start=True, stop=(not slots))
                    for s in slots:
                        for h4 in range(4):
                            MMUL(sT[:, h4 * 128:(h4 + 1) * 128], identb[:], A["cb2"][:, s, :], [identb.r, A["cb2"].r], [sT.r],
                                 start=False, stop=(h4 == 3))
                    pT = next_pT()
                    P.op("act", lambda e: e.activation(pT[:], sT[:], AF.Exp), reads=[sT.r], writes=[pT.r])
                    for h4 in range(4):
                        MMUL(po_t[:, h4 * 65:(h4 + 1) * 65], pT[:, h4 * 128:(h4 + 1) * 128], vcmp[:, bt, g, :], [pT.r, vcmp.r], [po_t.r],
                             start=(bt == 0 and h4 == 0), stop=(bt == nbt - 1))
                        MMUL(ir[:, h4 * 128:(h4 + 1) * 128], pT[:, h4 * 128:(h4 + 1) * 128], A["cover"][:, bt, :], [pT.r, A["cover"].r], [ir.r],
                             start=(bt == 0 and h4 == 0), stop=(bt == nbt - 1))
                finish_branch(po_t, g, 0, True)
                rl = A["rl"]
                imp = A["imp"]
                P.op("dve", lambda e: e.tensor_scalar(imp[:], ir[:, 0:128], rl[:, 0:1], None, ALU.mult), reads=[ir.r, rl.r], writes=[imp.r])
                for h4 in range(1, 4):
                    P.op("dve", lambda e: e.scalar_tensor_tensor(imp[:], ir[:, h4 * 128:(h4 + 1) * 128], rl[:, h4:h4 + 1], imp[:], ALU.mult, ALU.add),
                         reads=[ir.r, rl.r, imp.r], writes=[imp.r])
                sc, sc2, m8a, m8b = A["sc"], A["sc2"], A["m8a"], A["m8b"]
                TT("dve", sc[:], imp[:], A["Ftab"][:], ALU.add, [imp.r, A["Ftab"].r], [sc.r])
                P.op("dve", lambda e: e.max(out=m8a[:], in_=sc[:]), reads=[sc.r], writes=[m8a.r])
                P.op("dve", lambda e: e.match_replace(out=sc2[:], in_to_replace=m8a[:], in_values=sc[:], imm_value=-3.0e38),
                     reads=[sc.r, m8a.r], writes=[sc2.r])
                P.op("dve", lambda e: e.max(out=m8b[:], in_=sc2[:]), reads=[sc2.r], writes=[m8b.r])
                tc_ = cfg["topk_col"]
                P.op("dve", lambda e: e.tensor_scalar(sc2[:], sc[:], m8b[:, tc_:tc_ + 1], None, ALU.is_ge), reads=[sc.r, m8b.r], writes=[sc2.r])
                P.op("dve", lambda e: e.tensor_scalar(sc[:], sc[:], -1.0e29, None, ALU.is_gt), reads=[sc.r], writes=[sc.r])
                TT("dve", sc[:], sc[:], sc2[:], ALU.mult, [sc.r, sc2.r], [sc.r])
                P.op("dve", lambda e: e.tensor_scalar(sc[:], sc[:], -NEGB, NEGB, ALU.mult, ALU.add), reads=[sc.r], writes=[sc.r])
                pq = A["pq"]
                P.op("pe", lambda e: e.transpose(pq[:, 0:128], sc[:], identf[:]), reads=[sc.r, identf.r], writes=[pq.r])
                mb4 = A["mb4"]
                P.op("dve", lambda e: e.tensor_copy(mb4[:], pq[:, 0:128].unsqueeze(1).to_broadcast([128, 4, 128])), reads=[pq.r], writes=[mb4.r])
                po_t = next_po()
                tiles = cfg["sel_tiles"]
                for ii, (c, use_G, tri) in enumerate(tiles):
                    sT = next_sT()
                    last = (not use_G) and (tri is None)
                    MMUL(sT[:, :], ksT[g][0:65, c * 128:(c + 1) * 128], qTg, [ksT[g].r, qT.r], [sT.r], start=True, stop=last)
                    if use_G:
                        MMUL(sT[:, :], A["G"][:, c * 128:(c + 1) * 128], mb4[:].rearrange("p h q -> p (h q)"), [A["G"].r, mb4.r], [sT.r],
                             start=False, stop=(tri is None))
                    if tri is not None:
                        MMUL(sT[:, :], identb[:], A["triS"][:, tri, :], [identb.r, A["triS"].r], [sT.r], start=False, stop=True)
                    pT = next_pT()
                    P.op("act", lambda e: e.activation(pT[:], sT[:], AF.Exp), reads=[sT.r], writes=[pT.r])
                    for h4 in range(4):
                        MMUL(po_t[:, h4 * 65:(h4 + 1) * 65], pT[:, h4 * 128:(h4 + 1) * 128], vsw[:, c, g, :], [pT.r, vsw.r], [po_t.r],
                             start=(ii == 0 and h4 == 0), stop=(ii == len(tiles) - 1))
                finish_branch(po_t, g, 1, False)
                po_t = next_po()
                tiles = cfg["win_tiles"]
                for ii, (c, slot) in enumerate(tiles):
                    sT = next_sT()
                    MMUL(sT[:, :], kwT[g][0:65, c * 128:(c + 1) * 128], qTg, [kwT[g].r, qT.r], [sT.r], start=True, stop=False)
                    MMUL(sT[:, :], identb[:], A["triW"][:, slot, :], [identb.r, A["triW"].r], [sT.r], start=False, stop=True)
                    pT = next_pT()
                    P.op("act", lambda e: e.activation(pT[:], sT[:], AF.Exp), reads=[sT.r], writes=[pT.r])
                    for h4 in range(4):
                        MMUL(po_t[:, h4 * 65:(h4 + 1) * 65], pT[:, h4 * 128:(h4 + 1) * 128], vsw[:, c, 2 + g, :], [pT.r, vsw.r], [po_t.r],
                             start=(ii == 0 and h4 == 0), stop=(ii == len(tiles) - 1))
                finish_branch(po_t, g, 2, False)
            P.op("act", lambda e: e.copy(A["yab"][:], ya[:].rearrange("p h d -> p (h d)")), reads=[ya.r], writes=[A["yab"].r])

        def bail():
            Dr["r_ya"] = r_ya
            P.barrier()
        if OPTS.get("cstop", 9) <= 1:
            return bail()
        with ExitStack() as stp:
            kcT = T(stp.enter_context(nc.sbuf_tensor("c_kcT_p", [128, SEQ], BF16)), "kcT")
            vcT = T(stp.enter_context(nc.sbuf_tensor("c_vcT_p", [128, SEQ], BF16)), "vcT")
            for ti in range(NT):
                kb = kvb[ti % 2]
                P.dma("pool", kb[:], Dr["p_full"][ti * 128:(ti + 1) * 128, R_COLS:NA], reads=[Dr["r_pfull"]], writes=[kb.r])
                ingest_tile(kb, ti, True, True, True, kcT, vcT, ti == 0)
            if OPTS.get("cstop", 9) > 2:
                compress_all(kcT, vcT)
        if OPTS.get("cstop", 9) <= 3:
            return bail()

        def run_prompt(A):
            for j in range(OPTS["cq"]):
                P.dma("sp", A["Ftab"][:], Dr["Ftab"][:, j, :], writes=[A["Ftab"].r])
                P.dma("pool", A["cb2"][:], Dr["cbias"][:, j, :, :], writes=[A["cb2"].r])
                if j == 0:
                    P.dma("pool", A["triS"][:], Dr["triS"][:, :, :], writes=[A["triS"].r])
                    P.dma("pool", A["triW"][:], Dr["triW"][:, :, :], writes=[A["triW"].r])

                def load_x(xb, j=j):
                    P.dma("pool", xb[:], Dr["x_own"][j * 128:(j + 1) * 128, :], writes=[xb.r])
                q_prepare(A, 128, load_x, None, Dr["rope_own"][j * 128:(j + 1) * 128, :])
                nbt = (32 * j + 32 + 127) // 128
                cb_tiles = [(nbt - 1, 1)] + ([(nbt - 2, 0)] if nbt >= 2 else [])
                cfg = {"nbt": nbt, "cb_tiles": cb_tiles, "topk_col": 7,
                       "sel_tiles": [(c, True, (c - 4 * j) if c >= 4 * j else None) for c in range(4 * j + 4)],
                       "win_tiles": [(c, c - (4 * j - 4)) for c in range(max(4 * j - 4, 0), 4 * j + 4)]}
                nsa_qtile(A, 128, cfg)
                P.dma("sp", Dr["y_a_own"][j * 128:(j + 1) * 128, :], A["yab"][:], reads=[A["yab"].r], writes=[r_ya])
        attention_scope(run_prompt)

        r_gath = Dr["r_gath"]
        for bb in range(OPTS["sb"]):
            with ExitStack() as stp:
                kcT = T(stp.enter_context(nc.sbuf_tensor("c_kcT_s%d" % bb, [128, SEQ], BF16)), "kcT")
                vcT = T(stp.enter_context(nc.sbuf_tensor("c_vcT_s%d" % bb, [128, SEQ], BF16)), "vcT")
                for ti in range(64):
                    kb = kvb[ti % 2]
                    P.dma("pool", kb[:, 0:256], Dr["gath"][bb, 0, ti, :].rearrange("(p c) -> p c", p=128), reads=[r_gath], writes=[kb.r])
                    P.dma("pool", kb[:, 256:512], Dr["gath"][bb, 1, ti, :].rearrange("(p c) -> p c", p=128), reads=[r_gath], writes=[kb.r])
                    ingest_tile(kb, ti, True, True, False, kcT, vcT, ti == 0)
                kb = kvb[0]
                P.op("pool", lambda e: e.memset(kb[:], 0.0), writes=[kb.r])
                P.dma("pool", kb[0:DS, 256:512], Dr["p_samp"][bb * DS:(bb + 1) * DS, KV0 + 256:KV0 + 512], reads=[Dr["r_psamp"]], writes=[kb.r])
                ingest_tile(kb, 64, False, True, False, kcT, vcT, False)
                for c in range(5):
                    kb = kvb[(c + 1) % 2]
                    if c < 4:
                        P.dma("pool", kb[:, 512:768], Dr["cache_win"][bb, c * 128:(c + 1) * 128, :], writes=[kb.r])
                    else:
                        P.op("pool", lambda e: e.memset(kb[:], 0.0), writes=[kb.r])
                        P.dma("pool", kb[0:DS, 512:768], Dr["p_samp"][bb * DS:(bb + 1) * DS, KV0 + 512:KV0 + 768], reads=[Dr["r_psamp"]], writes=[kb.r])
                    ingest_tile(kb, c, False, False, True, kcT, vcT, False)
                compress_all(kcT, vcT)

            def run_sample(A, bb=bb):
                P.dma("sp", A["Ftab"][:], Dr["Ftab"][:, 16, :], writes=[A["Ftab"].r])
                P.dma("pool", A["cb2"][:], Dr["cbias"][:, 16, :, :], writes=[A["cb2"].r])
                P.dma("pool", A["triS"][:, 0, :], Dr["triS_s"][:, :], writes=[A["triS"].r])
                P.dma("pool", A["triW"][:, 0:5, :], Dr["triW_s"][:, :, :], writes=[A["triW"].r])

                def load_q(qf, gts):
                    P.dma("sp", qf[0:DS, :], Dr["p_samp"][bb * DS:(bb + 1) * DS, QG0:QG0 + 512], reads=[Dr["r_psamp"]], writes=[qf.r])
                    P.dma("sp", gts[0:DS, :], Dr["p_samp"][bb * DS:(bb + 1) * DS, GATE0:GATE0 + 24], reads=[Dr["r_psamp"]], writes=[gts.r])
                P.op("pool", lambda e: e.memset(A["gts"][:], 0.0), writes=[A["gts"].r])
                q_prepare(A, DS, None, load_q, Dr["rope_s"][0:DS, :])
                cfg = {"nbt": 4, "cb_tiles": [(3, 1), (2, 0)], "topk_col": 6,
                       "sel_tiles": [(c, True, None) for c in range(64)] + [(64, False, 0)],
                       "win_tiles": [(c, c) for c in range(5)]}
                nsa_qtile(A, DS, cfg)
                P.dma("sp", Dr["y_a_own"][2048 + bb * DS:2048 + (bb + 1) * DS, :], A["yab"][0:DS, :], reads=[A["yab"].r], writes=[r_ya])
            attention_scope(run_sample)
        Dr["r_ya"] = r_ya
        P.barrier()


def make_gather(nc, P, Dr):
    r_gath = R("gath")
    Dr["r_gath"] = r_gath
    st = ExitStack()
    Dr["gather_stack"] = st
    nsb = OPTS["sb"]
    if nsb == 0 or "C" not in OPTS["phases"]:
        return iter(())
    stage = T(st.enter_context(nc.sbuf_tensor("g_stage", [64, 8192], F32)), "stage")
    pidx = T(st.enter_context(nc.sbuf_tensor("g_pidx", [64, SB], I32)), "pidx")
    pidf = T(st.enter_context(nc.sbuf_tensor("g_pidf", [64, SB], F32)), "pidf")
    idx4f = T(st.enter_context(nc.sbuf_tensor("g_idx4f", [64, SB, 4], F32)), "idx4f")
    idx4 = T(st.enter_context(nc.sbuf_tensor("g_idx4", [64, SB, 4], I32)), "idx4")
    P.op("pool", lambda e: e.memset(pidx[:], 0), writes=[pidx.r])
    for bb in range(nsb):
        P.dma("sp", pidx[:, bb:bb + 1], Dr["pt_col"][bb, :, :], writes=[pidx.r])
    P.op("dve", lambda e: e.tensor_copy(pidf[:], pidx[:]), reads=[pidx.r], writes=[pidf.r])
    for ch in range(4):
        P.op("dve", lambda e: e.tensor_scalar(idx4f[:, :, ch], pidf[:], 4.0, float(ch), ALU.mult, ALU.add), reads=[pidf.r], writes=[idx4f.r])
    P.op("dve", lambda e: e.tensor_copy(idx4[:], idx4f[:]), reads=[idx4f.r], writes=[idx4.r])

    def gen():
        for bb in range(nsb):
            for ci, cache in enumerate((Dr["cache_cmp_pg"], Dr["cache_sel_pg"])):
                for ch in range(4):
                    P._need("pool", P._deps([idx4.r], [stage.r]))
                    ins = nc.gpsimd.indirect_dma_start(out=stage[:, :], out_offset=None, in_=cache[:, :],
                                                       in_offset=bass.IndirectOffsetOnAxis(ap=idx4[:, bb, ch:ch + 1], axis=0),
                                                       bounds_check=2560 * 4 - 1, oob_is_err=False)
                    pool_, idx_ = P.dq["pool"]
                    key = pool_[idx_ % len(pool_)]
                    P.dq["pool"][1] = idx_ + 1
                    if P.cnt[key] > 0:
                        P._need("pool", [(key, P.cnt[key])])
                    P.cnt[key] += 16
                    ins.then_inc(P.sems[key], 16)
                    P._commit((key, P.cnt[key]), [idx4.r], [stage.r])
                    P.n_inst += 1
                    P.dma("pool", Dr["gath"][bb, ci, :, ch * 8192:(ch + 1) * 8192], stage[:, :], reads=[stage.r], writes=[r_gath])
                    yield
    return gen()

NTOK = 2048 + NS
NTL = 17
MG0 = 3096
DN_ALPHA = 2.0 ** 0.25
NVD = 4 * 1024 + 32


def _tile_rows(u):
    return NS if u == NTL - 1 else 128


def phase_D(nc, P, Dr, outs):
    with ExitStack() as st:
        def sb(name, shape, dt=F32):
            return T(st.enter_context(nc.sbuf_tensor("d_" + name, list(shape), dt)), name)

        def ps(name, shape, dt=F32):
            return PT(st.enter_context(nc.psum_tensor("d_" + name, list(shape), dt)), name)

        w_in = Dr["w_in"]
        identb = sb("identb", [128, 128], BF16)
        P.dma("pool", identb[:], Dr["masks"][:, 896:1024], writes=[identb.r])
        identf = sb("identf", [128, 128])
        P.dma("sp", identf[:], Dr["masks"][:, 896:1024], writes=[identf.r])
        vd = sb("vd", [128, NVD])
        P.dma("sp", vd[:], Dr["vecsD"][:, :], writes=[vd.r])
        sel4 = sb("sel4", [128, 4])
        P.dma("sp", sel4[:], Dr["sel4"][:, :], writes=[sel4.r])
        wmg = sb("wmg", [128, 8, 2048], BF16)
        wo = sb("wo", [128, 8, 1024], BF16)
        for kc in range(8):
            P.dma("pool", wmg[:, kc, :], w_in[kc * 128:(kc + 1) * 128, MG0:MG0 + 2048], writes=[wmg.r])
            P.dma("pool", wo[:, kc, :], Dr["w_o"][kc * 128:(kc + 1) * 128, :], writes=[wo.r])
        wpa = sb("wpa", [128, 4, 1024], BF16)
        wpb = sb("wpb", [128, 4, 1024], BF16)
        for kc in range(4):
            P.dma("pool", wpa[:, kc, :], Dr["w_pa"][kc * 128:(kc + 1) * 128, :], writes=[wpa.r])
            P.dma("pool", wpb[:, kc, :], Dr["w_pb"][kc * 128:(kc + 1) * 128, :], writes=[wpb.r])
        rw = sb("rw", [128, 8, 32])
        P.dma("sp", rw[:], Dr["router_w"].rearrange("(k p) e -> p k e", p=128), writes=[rw.r])

        xf = sb("xf", [128, 1024])
        xb = sb("xb", [128, 1024], BF16)
        xT = sb("xT", [128, 8, 128], BF16)
        sg = sb("sg", [128, 2048])
        yr4 = sb("yr4", [128, 4, 512], BF16)
        yr = sb("yr", [128, 512], BF16)
        ya = sb("ya", [128, 512], BF16)
        yT = sb("yT", [128, 8, 128], BF16)
        mm = sb("mm", [128, 1024])
        mb = sb("mb", [128, 1024], BF16)
        mT = sb("mT", [128, 8, 128], BF16)
        hp_ = sb("hpre", [128, 1024])
        hh = sb("hh", [128, 1024])
        hT = sb("hTf", [128, 8, 128])
        s1 = sb("s1", [128, 1])
        s2 = sb("s2", [128, 1])
        lg = sb("lg", [128, 32])
        m8 = sb("m8", [128, 8])
        msk = sb("msk", [128, 32])
        gt = sb("gt", [128, 32])
        ptr = ps("ptr", [128, 8, 128], BF16)
        ptf = [ps("ptf%d" % i, [128, 512]) for i in range(2)]
        pm = [ps("pm%d" % i, [128, 512]) for i in range(4)]
        pmi = [0]

        def nextpm():
            t = pm[pmi[0] % 4]
            pmi[0] += 1
            return t

        r_h = R("h_own")
        r_g = R("gates_own")
        for u in range(NTL):
            n = _tile_rows(u)
            if u < 16:
                P.dma("sp", xf[:], Dr["x_own"][u * 128:(u + 1) * 128, :], writes=[xf.r])
                P.dma("sp", yr4[:], Dr["y_r"][u * 512:(u + 1) * 512, :].rearrange("(k p) c -> p k c", p=128),
                      reads=[Dr["r_yr"]], writes=[yr4.r])
                P.dma("sp", ya[:], Dr["y_a_own"][u * 128:(u + 1) * 128, :], reads=[Dr["r_ya"]], writes=[ya.r])
                P.op("dve", lambda e: e.tensor_scalar(yr[:], yr4[:, 0, :], sel4[:, 0:1], None, ALU.mult), reads=[yr4.r, sel4.r], writes=[yr.r])
                for k in range(1, 4):
                    P.op("dve", lambda e: e.scalar_tensor_tensor(yr[:], yr4[:, k, :], sel4[:, k:k + 1], yr[:], ALU.mult, ALU.add),
                         reads=[yr4.r, sel4.r, yr.r], writes=[yr.r])
            else:
                P.dma("sp", xf[0:n, :], Dr["x_s"][:, :], writes=[xf.r])
                P.dma("sp", yr[0:n, :], Dr["y_r_s"][:, :], reads=[Dr["r_yr"]], writes=[yr.r])
                P.dma("sp", ya[0:n, :], Dr["y_a_own"][2048:2048 + n, :], reads=[Dr["r_ya"]], writes=[ya.r])
            P.op("act", lambda e: e.copy(xb[0:n, :], xf[0:n, :]), reads=[xf.r], writes=[xb.r])
            for kc in range(8):
                P.op("pe", lambda e: e.transpose(ptr[:, kc, 0:n], xb[0:n, kc * 128:(kc + 1) * 128], identb[0:n, 0:n]),
                     reads=[xb.r, identb.r], writes=[ptr.r])
            P.op("act", lambda e: e.copy(xT[:, :, 0:n], ptr[:, :, 0:n]), reads=[ptr.r], writes=[xT.r])
            for nch in range(4):
                p_ = nextpm()
                for kc in range(8):
                    P.op("pe", lambda e: e.matmul(p_[0:n, :], xT[:, kc, 0:n], wmg[:, kc, nch * 512:(nch + 1) * 512],
                                                  start=(kc == 0), stop=(kc == 7)), reads=[xT.r, wmg.r], writes=[p_.r])
                P.op("act", lambda e: e.activation(sg[0:n, nch * 512:(nch + 1) * 512], p_[0:n, :], AF.Sigmoid), reads=[p_.r], writes=[sg.r])
            for kc in range(4):
                P.op("pe", lambda e: e.transpose(ptr[:, kc, 0:n], yr[0:n, kc * 128:(kc + 1) * 128], identb[0:n, 0:n]),
                     reads=[yr.r, identb.r], writes=[ptr.r])
                P.op("pe", lambda e: e.transpose(ptr[:, 4 + kc, 0:n], ya[0:n, kc * 128:(kc + 1) * 128], identb[0:n, 0:n]),
                     reads=[ya.r, identb.r], writes=[ptr.r])
            P.op("act", lambda e: e.copy(yT[:, :, 0:n], ptr[:, :, 0:n]), reads=[ptr.r], writes=[yT.r])
            for nch in range(2):
                pa = nextpm()
                pb = nextpm()
                for kc in range(4):
                    P.op("pe", lambda e: e.matmul(pa[0:n, :], yT[:, kc, 0:n], wpa[:, kc, nch * 512:(nch + 1) * 512],
                                                  start=(kc == 0), stop=(kc == 3)), reads=[yT.r, wpa.r], writes=[pa.r])
                for kc in range(4):
                    P.op("pe", lambda e: e.matmul(pb[0:n, :], yT[:, 4 + kc, 0:n], wpb[:, kc, nch * 512:(nch + 1) * 512],
                                                  start=(kc == 0), stop=(kc == 3)), reads=[yT.r, wpb.r], writes=[pb.r])
                cs = slice(nch * 512, (nch + 1) * 512)
                P.op("dve", lambda e: e.tensor_tensor(mm[0:n, cs], pa[0:n, :], sg[0:n, nch * 512:(nch + 1) * 512], ALU.mult),
                     reads=[pa.r, sg.r], writes=[mm.r])
                P.op("dve", lambda e: e.tensor_tensor(hp_[0:n, cs], pb[0:n, :], sg[0:n, 1024 + nch * 512:1024 + (nch + 1) * 512], ALU.mult),
                     reads=[pb.r, sg.r], writes=[hp_.r])
                P.op("dve", lambda e: e.tensor_tensor(mb[0:n, cs], mm[0:n, cs], hp_[0:n, cs], ALU.add),
                     reads=[mm.r, hp_.r], writes=[mb.r])
            for kc in range(8):
                P.op("pe", lambda e: e.transpose(ptr[:, kc, 0:n], mb[0:n, kc * 128:(kc + 1) * 128], identb[0:n, 0:n]),
                     reads=[mb.r, identb.r], writes=[ptr.r])
            P.op("act", lambda e: e.copy(mT[:, :, 0:n], ptr[:, :, 0:n]), reads=[ptr.r], writes=[mT.r])
            for nch in range(2):
                p_ = nextpm()
                for kc in range(8):
                    P.op("pe", lambda e: e.matmul(p_[0:n, :], mT[:, kc, 0:n], wo[:, kc, nch * 512:(nch + 1) * 512],
                                                  start=(kc == 0), stop=(kc == 7)), reads=[mT.r, wo.r], writes=[p_.r])
                cs = slice(nch * 512, (nch + 1) * 512)
                P.op("dve", lambda e: e.scalar_tensor_tensor(hp_[0:n, cs], xf[0:n, cs], DN_ALPHA, p_[0:n, :], ALU.mult, ALU.add),
                     reads=[xf.r, p_.r], writes=[hp_.r])
            layer_norm(P, hp_, hh, mm, s1, s2, n, vd[0:n, 0:1024], vd[0:n, 1024:2048], vd.r)
            P.dma("pool", Dr["h_own"][u * 128:u * 128 + n, :], hh[0:n, :], reads=[hh.r], writes=[r_h])
            for kc in range(8):
                pt_ = ptf[kc // 4]
                P.op("pe", lambda e: e.transpose(pt_[:, (kc % 4) * 128:(kc % 4) * 128 + n], hh[0:n, kc * 128:(kc + 1) * 128], identf[0:n, 0:n]),
                     reads=[hh.r, identf.r], writes=[pt_.r])
            P.op("act", lambda e: e.copy(hT[:, 0:4, 0:n], ptf[0][:].rearrange("p (k t) -> p k t", k=4)[:, :, 0:n]), reads=[ptf[0].r], writes=[hT.r])
            P.op("dve", lambda e: e.tensor_copy(hT[:, 4:8, 0:n], ptf[1][:].rearrange("p (k t) -> p k t", k=4)[:, :, 0:n]), reads=[ptf[1].r], writes=[hT.r])
            p_ = nextpm()
            for kc in range(8):
                P.op("pe", lambda e: e.matmul(p_[0:n, 0:32], hT[:, kc, 0:n], rw[:, kc, :], start=(kc == 0), stop=(kc == 7)),
                     reads=[hT.r, rw.r], writes=[p_.r])
            P.op("dve", lambda e: e.tensor_tensor(lg[0:n, :], p_[0:n, 0:32], vd[0:n, 4096:4128], ALU.add), reads=[p_.r, vd.r], writes=[lg.r])
            P.op("dve", lambda e: e.max(out=m8[0:n, :], in_=lg[0:n, :]), reads=[lg.r], writes=[m8.r])
            P.op("dve", lambda e: e.tensor_scalar(msk[0:n, :], lg[0:n, :], m8[0:n, 3:4], None, ALU.is_ge), reads=[lg.r, m8.r], writes=[msk.r])
            P.op("dve", lambda e: e.tensor_scalar(s1[0:n, :], m8[0:n, 0:1], -1.0, None, ALU.mult), reads=[m8.r], writes=[s1.r])
            P.op("act", lambda e: e.activation(gt[0:n, :], lg[0:n, :], AF.Exp, bias=s1[0:n, 0:1]), reads=[lg.r, s1.r], writes=[gt.r])
            P.op("dve", lambda e: e.tensor_tensor(gt[0:n, :], gt[0:n, :], msk[0:n, :], ALU.mult), reads=[gt.r, msk.r], writes=[gt.r])
            P.op("dve", lambda e: e.tensor_reduce(s2[0:n, :], gt[0:n, :], AX.X, ALU.add), reads=[gt.r], writes=[s2.r])
            P.op("dve", lambda e: e.reciprocal(s2[0:n, :], s2[0:n, :]), reads=[s2.r], writes=[s2.r])
            P.op("dve", lambda e: e.tensor_scalar(gt[0:n, :], gt[0:n, :], s2[0:n, 0:1], None, ALU.mult), reads=[gt.r, s2.r], writes=[gt.r])
            P.dma("pool", Dr["gates_own"][u * 128:u * 128 + n, :], gt[0:n, :], reads=[gt.r], writes=[r_g])
        Dr["r_h"] = r_h
        Dr["r_g"] = r_g
        P.barrier()


def layer_norm(P, src, dst, tmp, s1, s2, n, g_ap, b_ap, vr):
    P.op("dve", lambda e: e.tensor_reduce(s1[0:n, :], src[0:n, :], AX.X, ALU.add), reads=[src.r], writes=[s1.r])
    P.op("dve", lambda e: e.tensor_scalar(s1[0:n, :], s1[0:n, :], -1.0 / 1024, None, ALU.mult), reads=[s1.r], writes=[s1.r])
    P.op("dve", lambda e: e.tensor_scalar(dst[0:n, :], src[0:n, :], s1[0:n, 0:1], None, ALU.add), reads=[src.r, s1.r], writes=[dst.r])
    P.op("dve", lambda e: e.tensor_tensor(tmp[0:n, :], dst[0:n, :], dst[0:n, :], ALU.mult), reads=[dst.r], writes=[tmp.r])
    P.op("dve", lambda e: e.tensor_reduce(s2[0:n, :], tmp[0:n, :], AX.X, ALU.add), reads=[tmp.r], writes=[s2.r])
    P.op("dve", lambda e: e.tensor_scalar(s2[0:n, :], s2[0:n, :], 1.0 / 1024, 1e-5, ALU.mult, ALU.add), reads=[s2.r], writes=[s2.r])
    P.op("act", lambda e: e.activation(s2[0:n, :], s2[0:n, :], AF.Sqrt), reads=[s2.r], writes=[s2.r])
    P.op("dve", lambda e: e.reciprocal(s2[0:n, :], s2[0:n, :]), reads=[s2.r], writes=[s2.r])
    P.op("dve", lambda e: e.tensor_scalar(dst[0:n, :], dst[0:n, :], s2[0:n, 0:1], None, ALU.mult), reads=[dst.r, s2.r], writes=[dst.r])
    P.op("dve", lambda e: e.tensor_tensor(dst[0:n, :], dst[0:n, :], g_ap, ALU.mult), reads=[dst.r, vr], writes=[dst.r])
    P.op("dve", lambda e: e.tensor_tensor(dst[0:n, :], dst[0:n, :], b_ap, ALU.add), reads=[dst.r, vr], writes=[dst.r])


def phase_E(nc, P, Dr, outs):
    with ExitStack() as st:
        def sb(name, shape, dt=F32):
            return T(st.enter_context(nc.sbuf_tensor("e_" + name, list(shape), dt)), name)

        def ps(name, shape, dt=F32):
            return PT(st.enter_context(nc.psum_tensor("e_" + name, list(shape), dt)), name)

        identb = sb("identb", [128, 128], BF16)
        P.dma("pool", identb[:], Dr["masks"][:, 896:1024], writes=[identb.r])
        identf = sb("identf", [128, 128])
        P.dma("sp", identf[:], Dr["masks"][:, 896:1024], writes=[identf.r])
        vd = sb("vd", [128, 2048])
        P.dma("sp", vd[:], Dr["vecsD"][:, 2048:4096], writes=[vd.r])
        b1a = sb("b1a", [128, 32, 16])
        P.dma("sp", b1a[:], Dr["mlp1_bT"].rearrange("e p c -> p e c"), writes=[b1a.r])
        b2 = sb("b2", [32, 1024])
        P.dma("sp", b2[:], Dr["mlp2_b"][:, :], writes=[b2.r])

        HT = 9
        hT = sb("hT", [128, 8, 1024 + NS], BF16)
        yacc = sb("yacc", [128, HT, 1024])
        gates = sb("gates", [128, HT, 32])
        gT = sb("gT", [32, HT, 128])
        s1 = sb("s1", [128, 1])
        s2 = sb("s2", [128, 1])
        ptr = ps("ptr", [128, 8, 128], BF16)
        pg_ = [ps("pgl%d" % i, [128, 512]) for i in range(4)]
        po = [ps("po%d" % i, [128, 512]) for i in range(3)]
        cnt = {"pg": 0, "po": 0, "w": 0, "a": 0}
        r_out = R("outE")
        outs.append(r_out)

        def scoped(names):
            stx = ExitStack()
            d = {}
            for nm_, shp, dt_ in names:
                d[nm_] = T(stx.enter_context(nc.sbuf_tensor("e_%s_%d" % (nm_, P.n_inst), list(shp), dt_)), nm_)
            return stx, d

        for half in range(2):
            tiles = list(range(half * 8, half * 8 + 8)) + ([16] if half == 1 else [])
            ntok = sum(_tile_rows(u) for u in tiles)
            groups = [(0, 512), (512, 512)] + ([(1024, NS)] if half == 1 else [])
            stx, dd = scoped([("hf", [128, 1024], F32), ("hb", [128, 1024], BF16)])
            hf, hb = dd["hf"], dd["hb"]
            for li, u in enumerate(tiles):
                n = _tile_rows(u)
                P.dma("sp", hf[0:n, :], Dr["h_own"][u * 128:u * 128 + n, :], reads=[Dr["r_h"]], writes=[hf.r])
                P.dma("sp", gates[0:n, li, :], Dr["gates_own"][u * 128:u * 128 + n, :], reads=[Dr["r_g"]], writes=[gates.r])
                P.op("act", lambda e: e.copy(hb[0:n, :], hf[0:n, :]), reads=[hf.r], writes=[hb.r])
                for kc in range(8):
                    P.op("pe", lambda e: e.transpose(ptr[:, kc, 0:n], hb[0:n, kc * 128:(kc + 1) * 128], identb[0:n, 0:n]),
                         reads=[hb.r, identb.r], writes=[ptr.r])
                P.op("dve", lambda e: e.tensor_copy(hT[:, :, li * 128:li * 128 + n], ptr[:, :, 0:n]), reads=[ptr.r], writes=[hT.r])
                pq = po[cnt["po"] % 3]
                cnt["po"] += 1
                P.op("pe", lambda e: e.transpose(pq[0:32, 0:n], gates[0:n, li, :], identf[0:n, 0:n]), reads=[gates.r, identf.r], writes=[pq.r])
                P.op("act", lambda e: e.copy(gT[:, li, 0:n], pq[0:32, 0:n]), reads=[pq.r], writes=[gT.r])
                for nch in range(2):
                    pq = po[cnt["po"] % 3]
                    cnt["po"] += 1
                    P.op("pe", lambda e: e.matmul(pq[0:n, :], gT[:, li, 0:n], b2[:, nch * 512:(nch + 1) * 512], start=True, stop=True),
                         reads=[gT.r, b2.r], writes=[pq.r])
                    P.op("act", lambda e: e.copy(yacc[0:n, li, nch * 512:(nch + 1) * 512], pq[0:n, :]), reads=[pq.r], writes=[yacc.r])
            P.barrier()
            stx.close()
            stx, dd = scoped([("w1_0", [128, 8, 2048], BF16), ("w1_1", [128, 8, 2048], BF16), ("w2_0", [128, 8, 1024], BF16),
                              ("w2_1", [128, 8, 1024], BF16), ("actT0", [128, 8, 512], BF16), ("actT1", [128, 8, 512], BF16),
                              ("tg0", [128, 512], F32), ("tg1", [128, 512], F32), ("tsg0", [128, 512], F32), ("tsg1", [128, 512], F32),
                              ("tl0", [128, 512], F32), ("tl1", [128, 512], F32)])
            w1 = [dd["w1_0"], dd["w1_1"]]
            w2 = [dd["w2_0"], dd["w2_1"]]
            actT = [dd["actT0"], dd["actT1"]]
            tg = [dd["tg0"], dd["tg1"]]
            tsg = [dd["tsg0"], dd["tsg1"]]
            tl = [dd["tl0"], dd["tl1"]]
            for ex in range(32):
                wb1 = w1[cnt["w"] % 2]
                wb2 = w2[cnt["w"] % 2]
                cnt["w"] += 1
                for kc in range(8):
                    P.dma("pool", wb1[:, kc, :], Dr["mlp1_w"][ex, kc * 128:(kc + 1) * 128, :], writes=[wb1.r])
                for kc in range(8):
                    P.dma("pool", wb2[:, kc, :], Dr["mlp2_w"][ex, kc * 128:(kc + 1) * 128, :], writes=[wb2.r])
                for (g0, gn) in groups:
                    aT = actT[cnt["a"] % 2]
                    cnt["a"] += 1
                    for fc in range(8):
                        pgl = pg_[cnt["pg"] % 4]
                        pll = pg_[(cnt["pg"] + 1) % 4]
                        cnt["pg"] += 2
                        for kc in range(8):
                            P.op("pe", lambda e: e.matmul(pgl[:, 0:gn], wb1[:, kc, fc * 128:(fc + 1) * 128], hT[:, kc, g0:g0 + gn],
                                                          start=(kc == 0), stop=(kc == 7)), reads=[wb1.r, hT.r], writes=[pgl.r])
                        for kc in range(8):
                            P.op("pe", lambda e: e.matmul(pll[:, 0:gn], wb1[:, kc, 1024 + fc * 128:1024 + (fc + 1) * 128], hT[:, kc, g0:g0 + gn],
                                                          start=(kc == 0), stop=(kc == 7)), reads=[wb1.r, hT.r], writes=[pll.r])
                        k2 = fc % 2
                        a_, s_, l_ = tg[k2], tsg[k2], tl[k2]
                        P.op("dve", lambda e: e.tensor_scalar(a_[:, 0:gn], pgl[:, 0:gn], b1a[:, ex, fc:fc + 1], 7.0, ALU.add, ALU.min),
                             reads=[pgl.r, b1a.r], writes=[a_.r])
                        P.op("act", lambda e: e.activation(s_[:, 0:gn], a_[:, 0:gn], AF.Sigmoid, scale=1.702), reads=[a_.r], writes=[s_.r])
                        P.op("dve", lambda e: e.tensor_scalar(l_[:, 0:gn], pll[:, 0:gn], b1a[:, ex, 8 + fc:9 + fc], 7.0, ALU.add, ALU.min),
                             reads=[pll.r, b1a.r], writes=[l_.r])
                        P.op("dve", lambda e: e.tensor_scalar(l_[:, 0:gn], l_[:, 0:gn], -7.0, 1.0, ALU.max, ALU.add), reads=[l_.r], writes=[l_.r])
                        P.op("dve", lambda e: e.tensor_tensor(a_[:, 0:gn], a_[:, 0:gn], s_[:, 0:gn], ALU.mult), reads=[a_.r, s_.r], writes=[a_.r])
                        P.op("dve", lambda e: e.tensor_tensor(aT[:, fc, 0:gn], a_[:, 0:gn], l_[:, 0:gn], ALU.mult), reads=[a_.r, l_.r], writes=[aT.r])
                    nt_in_g = (gn + 127) // 128
                    for tt in range(nt_in_g):
                        li = g0 // 128 + tt
                        n = min(128, gn - tt * 128)
                        for nch in range(2):
                            pq = po[cnt["po"] % 3]
                            cnt["po"] += 1
                            for fc in range(8):
                                P.op("pe", lambda e: e.matmul(pq[0:n, :], aT[:, fc, tt * 128:tt * 128 + n], wb2[:, fc, nch * 512:(nch + 1) * 512],
                                                              start=(fc == 0), stop=(fc == 7)), reads=[aT.r, wb2.r], writes=[pq.r])
                            ya = yacc[0:n, li, nch * 512:(nch + 1) * 512]
                            P.op("dve", lambda e: e.scalar_tensor_tensor(ya, pq[0:n, :], gates[0:n, li, ex:ex + 1], ya, ALU.mult, ALU.add),
                                 reads=[pq.r, gates.r, yacc.r], writes=[yacc.r])
            P.barrier()
            stx.close()
            stx, dd = scoped([("hf", [128, 1024], F32), ("t1", [128, 1024], F32), ("t2", [128, 1024], F32)])
            hf, t1, t2 = dd["hf"], dd["t1"], dd["t2"]
            for li, u in enumerate(tiles):
                n = _tile_rows(u)
                P.dma("sp", hf[0:n, :], Dr["h_own"][u * 128:u * 128 + n, :], reads=[Dr["r_h"]], writes=[hf.r])
                P.op("dve", lambda e: e.scalar_tensor_tensor(t1[0:n, :], hf[0:n, :], DN_ALPHA, yacc[0:n, li, :], ALU.mult, ALU.add),
                     reads=[hf.r, yacc.r], writes=[t1.r])
                layer_norm(P, t1, t2, hf, s1, s2, n, vd[0:n, 0:1024], vd[0:n, 1024:2048], vd.r)
                P.dma("sp", Dr["o_y"][u * 128:u * 128 + n, :], t2[0:n, :], reads=[t2.r], writes=[r_out])
            P.barrier()
            stx.close()
        P.barrier()


def build_program():
    nc = bass.Bass("TRN2", target_bir_lowering=False)
    Dr = {}

    def din(name, shape, dt=F32):
        Dr[name] = nc.dram_tensor(name, list(shape), dt, kind="ExternalInput").ap()
        _INPUT_NAMES.append(name)
    ph = OPTS["phases"]

    def dout(name, shape, dt=F32):
        Dr[name] = nc.dram_tensor(name, list(shape), dt, kind="ExternalOutput").ap()

    def dtmp(name, shape, dt=F32):
        Dr[name] = nc.dram_tensor(name, list(shape), dt).ap()

    din("x_full", [SEQ, D])
    din("x_own", [2048, D])
    din("x_s", [NS, D])
    din("w_in", [D, IN_COLS])
    din("rope_p", [SEQ, 16])
    din("rope_own", [2048, 16])
    din("rope_s", [NS, 16])
    din("cache_win", [SB, 512, 256])
    din("vecs", [128, NVEC])
    din("w_w2", [64, 512])
    din("w_a2", [64, 512])
    din("g_w2", [128, 512])
    din("masks", [128, NMASK])
    din("tmask", [128, 1])
    din("state_shift", [SB, R_COLS])
    din("state_wkv", [SB, 8, 64, 64])
    din("vecsD", [128, NVD])
    din("sel4", [128, 4])
    din("w_o", [D, D])
    din("w_pa", [512, D])
    din("w_pb", [512, D])
    din("router_w", [D, 32])
    if "E" in ph:
        din("mlp1_w", [32, D, 2048])
        din("mlp2_w", [32, D, D])
    din("mlp1_bT", [32, 128, 16])
    din("mlp2_b", [32, D])
    din("cmp_w1", [2, 2048, 256])
    din("cmp_w2", [2, 256, 64])
    din("cmp_pe", [2, 32, 64])
    din("cmp_b1T", [128, 2, 2])
    din("cmp_b2T", [64, 1])
    din("cmp_b2v", [128, 64])
    din("Gtab", [128, 8192])
    din("cover", [128, 4, 128])
    din("Ftab", [128, 17, 128])
    din("cbias", [128, 17, 2, 128])
    din("triS", [128, 4, 512])
    din("triW", [128, 8, 512])
    din("triS_s", [128, 512])
    din("triW_s", [128, 5, 512])
    din("pt_col", [SB, 64, 1], I32)
    if "C" in ph and OPTS["sb"] > 0:
        din("cache_cmp_pg", [2560 * 4, 8192])
        din("cache_sel_pg", [2560 * 4, 8192])

    dout("o_cmp_p", [SEQ, 256])
    dout("o_sel_p", [SEQ, 256])
    dout("o_win_p", [512, 256])
    dout("o_shift_p", [1, R_COLS])
    dout("o_cmp_s", [NS, 256])
    dout("o_sel_s", [NS, 256])
    dout("o_win_s", [SB, 512, 256])
    dout("o_shift_s", [SB, R_COLS])
    dout("o_wkv_p", [8, 64, 64])
    dout("o_wkv_s", [SB, 8, 64, 64])
    dout("o_y", [NTOK, D])

    dtmp("p_full", [SEQ, NA])
    dtmp("p_samp", [NS, IN_COLS])
    dtmp("y_r", [SEQ, 512], BF16)
    dtmp("y_r_s", [NS, 512], BF16)
    if DEBUG:
        dout("gath", [SB, 2, 64, 128 * 256])
    else:
        dtmp("gath", [SB, 2, 64, 128 * 256])
    if DEBUG:
        dout("y_a_own", [NTOK, 512], BF16)
        dout("h_own", [NTOK, D])
        dout("gates_own", [NTOK, 32])
    else:
        dtmp("y_a_own", [NTOK, 512], BF16)
        dtmp("h_own", [NTOK, D])
        dtmp("gates_own", [NTOK, 32])
    outs = []
    with ExitStack() as st:
        P = Prog(nc, st)
        Dr["gather_gen"] = make_gather(nc, P, Dr)
        phase_A(nc, P, Dr, outs)
        if "B" in ph:
            phase_B(nc, P, Dr, outs)
        if "C" in ph:
            phase_C(nc, P, Dr, outs)
        if "D" in ph:
            phase_D(nc, P, Dr, outs)
        if "E" in ph:
            phase_E(nc, P, Dr, outs)
        P.finish(outs + [Dr[k] for k in ("r_yr", "r_ya", "r_h", "r_g") if k in Dr])
        P._need("sp", [(k, v) for k, v in P.cnt.items() if v > 0])
        print("epochs", P.epoch, {k: v for k, v in P.cnt.items() if not k.startswith("d_")})
        print("program: n_inst=%d n_wait=%d" % (P.n_inst, P.n_wait))
    return nc


DEBUG = False
OPTS = {"phases": "ABCDE", "cq": 16, "sb": SB, "cores": 8, "cstop": 9}
_NC = None
_INPUT_NAMES = []


def _rope_table(pos):
    half = 8
    inv = (np.float32(500000.0) ** (-np.arange(half, dtype=np.float32) * np.float32(2.0) / np.float32(16))).astype(np.float32)
    ang = pos.astype(np.float32)[:, None] * inv[None, :]
    return np.concatenate([np.cos(ang), np.sin(ang)], axis=1).astype(np.float32)


def _const_masks():
    s = np.arange(128)[:, None]
    t = np.arange(128)[None, :]
    same = (s // 64) == (t // 64)
    Lblk = (same & (s <= t)).astype(np.float32)
    Oblk = same.astype(np.float32)
    strictU = (same & (s < t)).astype(np.float32)
    inclU = Lblk
    maskMA2 = np.concatenate([strictU, inclU, strictU, inclU], axis=1)
    maskNT = (same & (s > t)).astype(np.float32)
    ident = np.eye(128, dtype=np.float32)
    return np.ascontiguousarray(np.concatenate([Lblk, Oblk, maskMA2, maskNT, ident], axis=1))


def _nsa_tables(qq):
    f32 = np.float32
    NB = np.float32(-30000.0)
    kl = np.arange(128)[:, None]
    ql = np.arange(128)[None, :]
    Ftab = np.zeros((128, 17, 128), f32)
    cbias = np.zeros((128, 17, 2, 128), f32)
    sidx = np.arange(128)[None, :]
    for j in range(16):
        i = 4 * j + qq
        qpos = (128 * i + np.arange(128))[:, None]
        causal = (64 * sidx) <= qpos
        cur = qpos // 64
        forced = (sidx == 0) | (sidx == cur) | (sidx == cur - 1)
        F = np.where(forced, 1e6 + 16.0 * sidx, 0.0)
        F = np.where(causal, F, -1e30)
        Ftab[:, j, :] = F
        nbt = (32 * j + 32 + 127) // 128
        for slot, bt in ((1, nbt - 1), (0, nbt - 2)):
            if bt < 0:
                continue
            blk = 128 * bt + kl
            valid = (blk <= 510) & (16 * blk + 31 <= 128 * i + ql)
            cbias[:, j, slot, :] = np.where(valid, 0.0, NB)
    F = np.where((sidx == 0) | (sidx == 127), 1e6 + 16.0 * sidx, 0.0) * np.ones((128, 1))
    Ftab[:, 16, :] = F
    blk = 128 * 3 + kl
    cbias[:, 16, 1, :] = np.where(blk <= 510, 0.0, NB) * np.ones((1, 128))
    cbias[:, 16, 0, :] = 0.0
    rep4 = lambda m: np.tile(m, (1, 4))
    triS = np.zeros((128, 4, 512), f32)
    for rel in range(4):
        if rel < qq:
            m = np.zeros((128, 128), f32)
        elif rel == qq:
            m = np.where(kl <= ql, 0.0, NB)
        else:
            m = np.full((128, 128), NB)
        triS[:, rel, :] = rep4(m)
    triW = np.zeros((128, 8, 512), f32)
    for rel in range(8):
        dlt = qq + 4 - rel
        if dlt < 0 or dlt > 4:
            m = np.full((128, 128), NB)
        elif dlt == 0:
            m = np.where(kl <= ql, 0.0, NB)
        elif dlt == 4:
            m = np.where(kl >= ql, 0.0, NB)
        else:
            m = np.zeros((128, 128), f32)
        triW[:, rel, :] = rep4(m)
    qv = ql < DS
    triS_s = rep4(np.where((kl < DS) & (kl <= ql), 0.0, NB))
    triW_s = np.zeros((128, 5, 512), f32)
    for c in range(5):
        kidx = 128 * c + kl
        ok = (kidx < 512 + DS) & (kidx <= 512 + ql) & (kidx >= ql)
        triW_s[:, c, :] = rep4(np.where(ok, 0.0, NB))
    return (Ftab.astype(f32), cbias.astype(f32), triS.astype(f32), triW.astype(f32), triS_s.astype(f32), triW_s.astype(f32))


def _shared_tables():
    f32 = np.float32
    s = np.arange(128)[:, None]
    x = np.arange(8192)[None, :]
    G = ((x // 64) == s).astype(f32)
    cover = np.zeros((128, 4, 128), f32)
    for bt in range(4):
        blk = 128 * bt + np.arange(128)[:, None]
        ss = np.arange(128)[None, :]
        cover[:, bt, :] = ((blk >= 4 * ss - 1) & (blk <= 4 * ss + 3) & (blk <= 510)).astype(f32)
    return G, cover


def kernel(**inputs):
    global _NC
    if _NC is None:
        _NC = build_program()
    nc = _NC
    g = lambda k: np.asarray(inputs[k])
    f32 = np.float32
    C = np.ascontiguousarray
    x_prompt = g("x_prompt")
    x_sample = g("x_sample")
    w_in = C(g("w_in")[0])
    cache_win = g("cache_win_kv")[0].reshape(32, 512, 256)
    rope_p = _rope_table(np.arange(SEQ))
    rope_s = np.tile(_rope_table(PAST + np.arange(DS)), (SB, 1))
    vec = np.concatenate([g("mu_shift")[0], g("w0")[0], g("a0")[0], g("k_k")[0], g("k_a")[0], g("gn_g")[0],
                          g("gn_b")[0], g("r_k")[0].reshape(-1)]).astype(f32)
    vecs = C(np.broadcast_to(vec[None, :], (128, NVEC)))
    vecD = np.concatenate([g("ln1_g")[0], g("ln1_b")[0], g("ln2_g")[0], g("ln2_b")[0], g("router_b")[0]]).astype(f32)
    vecsD = C(np.broadcast_to(vecD[None, :], (128, NVD)))
    masks = _const_masks()
    tmask = np.zeros((128, 1), f32)
    tmask[0:DS] = 1.0
    tmask[64:64 + DS] = 1.0
    state_shift = g("state_shift")[0]
    state_wkv = g("state_wkv")[0]
    page_table = g("page_table").astype(np.int32)
    Gtab, cover = _shared_tables()
    shared = {
        "w_in": w_in, "rope_p": rope_p, "rope_s": rope_s, "vecs": vecs, "masks": masks, "tmask": tmask,
        "w_w2": C(g("w_w2")[0]), "w_a2": C(g("w_a2")[0]), "g_w2": C(g("g_w2")[0]),
        "vecsD": vecsD, "w_o": C(g("w_o")[0]), "w_pa": C(g("w_pa")[0]), "w_pb": C(g("w_pb")[0]),
        "router_w": C(g("router_w")[0]), "mlp1_w": C(g("mlp1_w")[0]), "mlp2_w": C(g("mlp2_w")[0]),
        "mlp1_bT": C(g("mlp1_b")[0].reshape(32, 16, 128).transpose(0, 2, 1)), "mlp2_b": C(g("mlp2_b")[0]),
        "cmp_w1": C(g("cmp_w1")[0]), "cmp_w2": C(g("cmp_w2")[0]), "cmp_pe": C(g("cmp_pe")[0]),
        "cmp_b1T": C(g("cmp_b1")[0].reshape(2, 2, 128).transpose(2, 0, 1)),
        "cmp_b2T": C(g("cmp_b2")[0][0].reshape(64, 1)),
        "cmp_b2v": C(np.broadcast_to(g("cmp_b2")[0][1][None, :], (128, 64))),
        "Gtab": Gtab, "cover": cover,
        "cache_cmp_pg": g("cache_cmp_kv")[0].reshape(2560 * 4, 8192),
        "cache_sel_pg": g("cache_sel_kv")[0].reshape(2560 * 4, 8192),
    }
    tabs = [_nsa_tables(qq) for qq in range(4)]
    in_maps = []
    for c in range(8):
        b, qq = c // 4, c % 4
        own = np.concatenate([np.arange(128 * (4 * j + qq), 128 * (4 * j + qq) + 128) for j in range(16)])
        Ftab, cbias, triS, triW, triS_s, triW_s = tabs[qq]
        sel4 = np.zeros((128, 4), f32)
        sel4[:, qq] = 1.0
        m = dict(shared)
        m.update({
            "x_full": C(x_prompt[b]),
            "x_own": C(x_prompt[b][own]),
            "rope_own": C(rope_p[own]),
            "x_s": C(x_sample[SB * c:SB * c + SB].reshape(NS, D)),
            "cache_win": C(cache_win[SB * c:SB * c + SB]),
            "state_shift": C(state_shift[SB * c:SB * c + SB]),
            "state_wkv": C(state_wkv[SB * c:SB * c + SB]),
            "sel4": sel4, "Ftab": Ftab, "cbias": cbias, "triS": triS, "triW": triW, "triS_s": triS_s, "triW_s": triW_s,
            "pt_col": C(page_table[SB * c:SB * c + SB].reshape(SB, 64, 1)),
        })
        in_maps.append(m)
    ncores = OPTS["cores"]
    in_maps = [{k: v for k, v in mp.items() if k in _INPUT_NAMES} for mp in in_maps[:ncores]]
    res = run_bass_kernel_spmd(nc, in_maps, core_ids=list(range(ncores)))
    rs = list(res.results)
    global _LAST
    _LAST = rs
    if ncores < 8:
        return None
    y_prompt = np.zeros((2, SEQ, D), f32)
    y_sample = np.zeros((32, DS, D), f32)
    for c in range(8):
        b, qq = c // 4, c % 4
        oy = rs[c]["o_y"]
        for j in range(16):
            i = 4 * j + qq
            y_prompt[b, 128 * i:128 * i + 128] = oy[j * 128:(j + 1) * 128]
        y_sample[SB * c:SB * c + SB] = oy[2048:2048 + NS].reshape(SB, DS, D)
    cmp_p = np.stack([rs[0]["o_cmp_p"], rs[4]["o_cmp_p"]]).reshape(1, 2, SEQ, 2, 2, 64)
    sel_p = np.stack([rs[0]["o_sel_p"], rs[4]["o_sel_p"]]).reshape(1, 2, SEQ, 2, 2, 64)
    win_p = np.stack([rs[0]["o_win_p"], rs[4]["o_win_p"]]).reshape(1, 2, 512, 2, 2, 64)
    wkv_p = np.stack([rs[0]["o_wkv_p"], rs[4]["o_wkv_p"]]).reshape(1, 2, 8, 64, 64)
    shift_p = np.stack([rs[0]["o_shift_p"], rs[4]["o_shift_p"]]).reshape(1, 2, R_COLS)
    cmp_s = np.concatenate([rs[c]["o_cmp_s"] for c in range(8)]).reshape(1, 32, DS, 2, 2, 64)
    sel_s = np.concatenate([rs[c]["o_sel_s"] for c in range(8)]).reshape(1, 32, DS, 2, 2, 64)
    win_s = np.concatenate([rs[c]["o_win_s"] for c in range(8)]).reshape(1, 32, 512, 2, 2, 64)
    wkv_s = np.concatenate([rs[c]["o_wkv_s"] for c in range(8)]).reshape(1, 32, 8, 64, 64)
    shift_s = np.concatenate([rs[c]["o_shift_s"] for c in range(8)]).reshape(1, 32, R_COLS)
    return (y_prompt, y_sample, cmp_p.astype(f32), sel_p.astype(f32), win_p.astype(f32), wkv_p.astype(f32),
            shift_p.astype(f32), cmp_s.astype(f32), sel_s.astype(f32), win_s.astype(f32), wkv_s.astype(f32),
            shift_s.astype(f32))


_LAST = None
```
